# Optimizing a Trainium2 kernel written in Bass

```python
import jax, jax.numpy as jnp
from jax import lax
import numpy as np

D_MODEL = 1024
BATCH = 2
SEQ = 8192
DEPTH = 1

N_HEADS = 8
HEAD_DIM = 64
ATTN_WIDTH = N_HEADS * HEAD_DIM
IDX_HEADS = 8
IDX_DIM = 64
TOPK_MAX = 256
Q_BLOCK = 128
CONV_CH = 512
CONV_WIDTH = 31
D_FF = -(-8 * D_MODEL // (3 * 256)) * 256
N_MOD = 6
EPS = 1e-6

kernel_name = "hybrid_dsa_conformer_gated_block"


def _in_split_sizes():
    return [ATTN_WIDTH, ATTN_WIDTH, ATTN_WIDTH,
            IDX_HEADS * IDX_DIM, IDX_DIM, IDX_HEADS,
            2 * CONV_CH, D_MODEL, D_MODEL]


def rmsnorm(x, g):
    xf = x.astype(jnp.float32)
    y = xf * lax.rsqrt(jnp.mean(xf * xf, axis=-1, keepdims=True) + EPS) * g.astype(jnp.float32)
    return y.astype(x.dtype)


def layernorm(x, g, b):
    xf = x.astype(jnp.float32)
    mu = jnp.mean(xf, axis=-1, keepdims=True)
    var = jnp.mean(jnp.square(xf - mu), axis=-1, keepdims=True)
    y = (xf - mu) * lax.rsqrt(var + EPS) * g.astype(jnp.float32) + b.astype(jnp.float32)
    return y.astype(x.dtype)


def modulate(h, shift, scale):
    return h * (1.0 + scale[:, None, :]) + shift[:, None, :]


def dsa_attention(q, k, v, q_idx, k_idx, w_idx):
    B, S = q.shape[0], q.shape[1]
    topk = min(TOPK_MAX, S // 4)
    n_blocks = S // Q_BLOCK
    f32 = jnp.float32
    slopes = jnp.exp2(-8.0 * jnp.arange(1, N_HEADS + 1, dtype=f32) / N_HEADS)
    key_pos = jnp.arange(S)
    k_idx32 = k_idx.astype(f32)
    gather = jax.vmap(lambda arr, ix: arr[ix])

    def one_block(i):
        start = i * Q_BLOCK
        qb = lax.dynamic_slice_in_dim(q, start, Q_BLOCK, axis=1).astype(f32)
        qib = lax.dynamic_slice_in_dim(q_idx, start, Q_BLOCK, axis=1).astype(f32)
        wb = lax.dynamic_slice_in_dim(w_idx, start, Q_BLOCK, axis=1).astype(f32)
        q_pos = start + jnp.arange(Q_BLOCK)
        idx_logits = jnp.einsum('btjd,bsd->btjs', qib, k_idx32) * (IDX_DIM ** -0.5)
        score = jnp.einsum('btj,btjs->bts', wb, jax.nn.relu(idx_logits))
        causal = key_pos[None, :] <= q_pos[:, None]
        score = jnp.where(causal[None], score, -jnp.inf)
        _, sel = lax.top_k(score, topk)
        k_sel = gather(k, sel).astype(f32)
        v_sel = gather(v, sel).astype(f32)
        s = jnp.einsum('bthd,btkhd->bhtk', qb, k_sel) * (HEAD_DIM ** -0.5)
        dist = (q_pos[None, :, None] - sel).astype(f32)
        s = s - slopes[None, :, None, None] * dist[:, None]
        valid = sel <= q_pos[None, :, None]
        s = jnp.where(valid[:, None], s, -jnp.inf)
        p = jax.nn.softmax(s, axis=-1)
        out = jnp.einsum('bhtk,btkhd->bthd', p, v_sel)
        return out.astype(q.dtype)

    outs = lax.map(one_block, jnp.arange(n_blocks))
    return jnp.transpose(outs, (1, 0, 2, 3, 4)).reshape(B, S, N_HEADS * HEAD_DIM)


def conformer_conv(u, w_dw, b_dw, ln_g, ln_b):
    a, g = jnp.split(u, 2, axis=-1)
    z = a * jax.nn.sigmoid(g)
    z = lax.conv_general_dilated(z, w_dw, window_strides=(1,), padding=[(CONV_WIDTH - 1, 0)],
                                 dimension_numbers=('NWC', 'WIO', 'NWC'),
                                 feature_group_count=CONV_CH) + b_dw
    z = layernorm(z, ln_g, ln_b)
    return jax.nn.silu(z)


def setup_inputs(seed: int = 0) -> dict:
    key = jax.random.key(seed)
    ks = jax.random.split(key, 20)
    n_in = sum(_in_split_sizes())

    def nrm(k, shape, fan_in, gain=1.0):
        return jax.random.normal(k, shape, jnp.float32) * (gain * fan_in ** -0.5)

    def gain_vec(k, shape):
        return 1.0 + 0.02 * jax.random.normal(k, shape, jnp.float32)

    def small(k, shape):
        return 0.02 * jax.random.normal(k, shape, jnp.float32)

    return {
        "x": jax.random.normal(ks[0], (BATCH, SEQ, D_MODEL), jnp.float32),
        "c": jax.random.normal(ks[1], (BATCH, D_MODEL), jnp.float32),
        "norm_mix_g": gain_vec(ks[2], (DEPTH, D_MODEL)),
        "w_in": nrm(ks[3], (DEPTH, D_MODEL, n_in), D_MODEL),
        "w_dw": nrm(ks[4], (DEPTH, CONV_WIDTH, 1, CONV_CH), CONV_WIDTH),
        "b_dw": small(ks[5], (DEPTH, CONV_CH)),
        "conv_ln_g": gain_vec(ks[6], (DEPTH, CONV_CH)),
        "conv_ln_b": small(ks[7], (DEPTH, CONV_CH)),
        "w_attn_proj": nrm(ks[8], (DEPTH, ATTN_WIDTH, D_MODEL), ATTN_WIDTH),
        "w_conv_proj": nrm(ks[9], (DEPTH, CONV_CH, D_MODEL), CONV_CH),
        "w_out": nrm(ks[10], (DEPTH, D_MODEL, D_MODEL), D_MODEL),
        "norm_ffn_g": gain_vec(ks[11], (DEPTH, D_MODEL)),
        "w_ffn_in": nrm(ks[12], (DEPTH, D_MODEL, 2 * D_FF), D_MODEL),
        "w_ffn_out": nrm(ks[13], (DEPTH, D_FF, D_MODEL), D_FF),
        "w_ada": nrm(ks[14], (DEPTH, D_MODEL, N_MOD * D_MODEL), D_MODEL, 0.5),
        "b_ada": small(ks[15], (DEPTH, N_MOD * D_MODEL)),
        "norm_final_g": gain_vec(ks[16], (D_MODEL,)),
    }


def reference(x, c, norm_mix_g, w_in, w_dw, b_dw, conv_ln_g, conv_ln_b, w_attn_proj,
              w_conv_proj, w_out, norm_ffn_g, w_ffn_in, w_ffn_out, w_ada, b_ada, norm_final_g):
    B, S, _ = x.shape
    split_at = list(np.cumsum(_in_split_sizes())[:-1])
    c_act = jax.nn.silu(c)
    for l in range(DEPTH):
        mod = c_act @ w_ada[l] + b_ada[l]
        sh_m, sc_m, g_m, sh_f, sc_f, g_f = jnp.split(mod, N_MOD, axis=-1)

        h = modulate(rmsnorm(x, norm_mix_g[l]), sh_m, sc_m)
        proj = h @ w_in[l]
        q, k, v, qi, ki, wi, u, ga, gb = jnp.split(proj, split_at, axis=-1)
        q = q.reshape(B, S, N_HEADS, HEAD_DIM)
        k = k.reshape(B, S, N_HEADS, HEAD_DIM)
        v = v.reshape(B, S, N_HEADS, HEAD_DIM)
        qi = qi.reshape(B, S, IDX_HEADS, IDX_DIM)
        wi = wi * (IDX_HEADS ** -0.5)
        y_attn = dsa_attention(q, k, v, qi, ki, wi) @ w_attn_proj[l]
        y_conv = conformer_conv(u, w_dw[l], b_dw[l], conv_ln_g[l], conv_ln_b[l]) @ w_conv_proj[l]
        merged = jax.nn.sigmoid(ga) * y_attn + jax.nn.sigmoid(gb) * y_conv
        x = x + g_m[:, None, :] * (merged @ w_out[l])

        h = modulate(rmsnorm(x, norm_ffn_g[l]), sh_f, sc_f)
        a, b = jnp.split(h @ w_ffn_in[l], 2, axis=-1)
        x = x + g_f[:, None, :] * ((jax.nn.silu(a) * b) @ w_ffn_out[l])
    return rmsnorm(x, norm_final_g)
```

```python
import contextlib
import numpy as np
import ml_dtypes
import concourse.bass as bass
import concourse.mybir as mybir
from concourse.bass_utils import run_bass_kernel_spmd

F32 = mybir.dt.float32
BF16 = mybir.dt.bfloat16
FP8 = mybir.dt.float8e4
AF = mybir.ActivationFunctionType
ALU = mybir.AluOpType
AX = mybir.AxisListType

PE, ACT, DVE, POOL, SP = "tensor", "scalar", "vector", "gpsimd", "sync"
ENGS = [PE, ACT, DVE, POOL, SP]
NDMASEM = 32
NPOOLSEM = 6

S = 8192
D = 1024
NT = 16
EPS = 1e-6
NEG = -30000.0
NITER = 14
EXPB = -16.0
MASKB = 240000.0
ACT_SHARE = 0.0


class Res:
    __slots__ = ("w", "rs")

    def __init__(self):
        self.w = None
        self.rs = []


class Op:
    __slots__ = ("eng", "fn", "deps", "signal", "dma", "dsem", "dval", "prev_dma")

    def __init__(self, eng, fn, dma):
        self.eng = eng; self.fn = fn; self.deps = []
        self.signal = False; self.dma = dma; self.dsem = None; self.dval = 0; self.prev_dma = None


class Prog:
    def __init__(self, nc):
        self.nc = nc
        self.ops = {e: [] for e in ENGS}
        self.ndma = 0
        self.npool = 0
        self.dma_last = [None] * (NDMASEM + NPOOLSEM)
        self.bar = {e: [] for e in ENGS}

    def barrier(self):
        lasts = []
        for e in ENGS:
            for o in reversed(self.ops[e]):
                if not o.dma:
                    lasts.append(o)
                    break
        lasts += [p for p in self.dma_last if p is not None]
        for e in ENGS:
            self.bar[e] = list(lasts)

    def op(self, eng, fn, reads=(), writes=(), dma=False):
        o = Op(eng, fn, dma)
        deps = list(self.bar[eng])
        self.bar[eng] = []
        for r in reads:
            if r.w is not None:
                deps.append(r.w)
        for w in writes:
            if w.w is not None:
                deps.append(w.w)
            deps.extend(w.rs)
        seen = set()
        for d in deps:
            if d is o or id(d) in seen:
                continue
            seen.add(id(d))
            if d.eng == eng and not d.dma and (eng == PE or eng == SP):
                continue
            o.deps.append(d)
            if not d.dma:
                d.signal = True
        if dma:
            if eng == POOL:
                slot = NDMASEM + (self.npool % NPOOLSEM)
                o.dval = 16 * (self.npool // NPOOLSEM + 1)
                self.npool += 1
            else:
                slot = self.ndma % NDMASEM
                o.dval = 16 * (self.ndma // NDMASEM + 1)
                self.ndma += 1
            o.dsem = slot
            o.prev_dma = self.dma_last[slot]
            self.dma_last[slot] = o
        self.ops[eng].append(o)
        for r in reads:
            r.rs.append(o)
        for w in writes:
            w.w = o
            w.rs = []
        return o

    def emit(self):
        nc = self.nc
        sigval = {}
        for e in ENGS:
            c = 0
            for o in self.ops[e]:
                if o.signal and not o.dma:
                    c += 1
                    sigval[id(o)] = c
        with contextlib.ExitStack() as st:
            esem = {e: st.enter_context(nc.semaphore("s_" + e)) for e in ENGS}
            dsem = [st.enter_context(nc.semaphore("d%d" % i)) for i in range(NDMASEM + NPOOLSEM)]
            block = st.enter_context(nc.Block())
            prog = self

            def run(engname, eng):
                waited = {}

                def wait(key, sem, val):
                    if waited.get(key, 0) >= val:
                        return
                    eng.wait_ge(sem, val)
                    waited[key] = val

                for o in prog.ops[engname]:
                    for d in o.deps:
                        if d.dma:
                            wait(("d", d.dsem), dsem[d.dsem], d.dval)
                        else:
                            wait(("e", d.eng), esem[d.eng], sigval[id(d)])
                    if o.dma and o.prev_dma is not None:
                        p = o.prev_dma
                        wait(("d", p.dsem), dsem[p.dsem], p.dval)
                    ins = o.fn(eng)
                    if o.dma:
                        ins.then_inc(dsem[o.dsem], 16)
                    elif o.signal:
                        ins.then_inc(esem[engname], 1)
                if engname == SP:
                    for p in prog.dma_last:
                        if p is not None:
                            wait(("d", p.dsem), dsem[p.dsem], p.dval)

            @block.tensor
            def _(eng):
                run(PE, eng)

            @block.scalar
            def _(eng):
                run(ACT, eng)

            @block.vector
            def _(eng):
                run(DVE, eng)

            @block.gpsimd
            def _(eng):
                run(POOL, eng)

            @block.sync
            def _(eng):
                run(SP, eng)


class Arena:
    def __init__(self, nc, kb):
        self.words = kb * 256
        self.t = nc.alloc_sbuf_tensor("arena", [128, self.words], F32)
        self.off = 0
        self.marks = []

    def mark(self):
        self.marks.append(self.off)

    def release(self):
        self.off = self.marks.pop()

    def tile(self, shape, dt):
        n = 1
        for s in shape:
            n *= s
        esz = 4 if dt == F32 else (1 if dt == FP8 else 2)
        words = (n * esz + 3) // 4
        words = (words + 7) // 8 * 8
        assert self.off + words <= self.words, ("arena overflow", self.off, words, self.words)
        ap = self.t[:, self.off:self.off + words]
        self.off += words
        if dt != F32:
            ap = ap.bitcast(dt)
        ap = ap[:, 0:n]
        if len(shape) == 2:
            ap = ap.rearrange("p (a b) -> p a b", b=shape[1])
        elif len(shape) == 3:
            ap = ap.rearrange("p (a b c) -> p a b c", b=shape[1], c=shape[2])
        return ap


def build(debug=False):
    nc = bass.Bass("TRN2", target_bir_lowering=False)

    def din(name, shape, dt=F32):
        return nc.dram_tensor(name, shape, dt, kind="ExternalInput").ap()

    def dscr(name, shape, dt=BF16):
        return nc.dram_tensor(name, shape, dt, kind="ExternalOutput" if debug else "Internal").ap()

    xk = din("xk", [S, D])
    cT = din("cT", [128, 8])
    w_ada = din("w_ada", [D, 6144])
    b_adaT = din("b_adaT", [128, 48])
    gmixT = din("gmixT", [128, 8]); gffnT = din("gffnT", [128, 8]); gfinT = din("gfinT", [128, 8])
    w_in = din("w_in", [D, 5192])
    w_dwT = din("w_dwT", [128, 4, 31])
    b_dwT = din("b_dwT", [128, 4]); lngT = din("lngT", [128, 4]); lnbT = din("lnbT", [128, 4])
    w_ap = din("w_ap", [512, D]); w_cp = din("w_cp", [512, D]); w_out = din("w_out", [D, D])
    w_fi = din("w_fi", [D, 5632]); w_fo = din("w_fo", [2816, D])
    ident_d = din("ident", [128, 128], BF16)
    identf_d = din("identf", [128, 128])
    kaug_d = din("kaug", [3, 8, S], BF16)
    kbias_d = din("kbias", [1, S], BF16)
    cb_d = din("cb", [128, 4, 512], BF16)
    hvalid_d = din("hvalid", [128, 128])
    kcp1_d = din("kcp1", [128, 64])
    out_d = nc.dram_tensor("out", [2048, D], F32, kind="ExternalOutput").ap()

    w_in_b = dscr("w_in_b", [D, 5192]); w_ap_b = dscr("w_ap_b", [512, D]); w_cp_b = dscr("w_cp_b", [512, D])
    w_out_b = dscr("w_out_b", [D, D]); w_fi_b = dscr("w_fi_b", [D, 5632]); w_fo_b = dscr("w_fo_b", [2816, D])
    kT_s = dscr("kT_s", [8, 64, S]); v_s = dscr("v_s", [S, 520])
    qT_s = dscr("qT_s", [8, 64, 2048]); qiT_s = dscr("qiT_s", [4, 128, 2048])
    zT_s = dscr("zT_s", [4, 128, 4, 544]); gaT_s = dscr("gaT_s", [8, 128, 2048]); gbT_s = dscr("gbT_s", [8, 128, 2048])
    OT_s = dscr("OT_s", [4, 128, 2048])
    if debug:
        dbg_sc = nc.dram_tensor("dbg_sc", [16, 128, S], F32, kind="ExternalOutput").ap()
        dbg_thr = nc.dram_tensor("dbg_thr", [128, 16 * 4], F32, kind="ExternalOutput").ap()

    P = Prog(nc)
    A = Arena(nc, 204)
    ps_cm = nc.psum_tensor("ps", [128, 8, 512], F32)
    ps = ps_cm.__enter__()
    psb = [ps[:, b, :].bitcast(BF16) for b in range(8)]
    rb = [Res() for _ in range(8)]

    def dma(out, in_, reads=(), writes=(), eng=SP, **kw):
        return P.op(eng, lambda e: e.dma_start(out=out, in_=in_, **kw), reads=reads, writes=writes, dma=True)

    ident = A.tile([128], BF16); r_c = Res()
    identf = A.tile([128], F32)
    onesf = A.tile([128], F32)
    modT = A.tile([48], F32); r_mod = Res()
    ab = A.tile([4, 8], F32)
    gfin = A.tile([8], F32)
    wiall = A.tile([16, 8], F32); r_wi = Res()
    hval = A.tile([128], F32)
    kcp1 = A.tile([64], F32)
    dma(ident, ident_d, writes=[r_c]); dma(identf, identf_d, writes=[r_c]); dma(hval, hvalid_d, writes=[r_c])
    dma(kcp1, kcp1_d, writes=[r_c]); dma(gfin, gfinT, writes=[r_c])
    P.op(DVE, lambda e: e.memset(onesf, 1.0), writes=[r_c])

    r_cast = {}
    for name, src, dst in [("w_in", w_in, w_in_b), ("w_ap", w_ap, w_ap_b), ("w_cp", w_cp, w_cp_b), ("w_out", w_out, w_out_b),
                           ("w_fi", w_fi, w_fi_b), ("w_fo", w_fo, w_fo_b)]:
        r_cast[name] = Res()
        dma(dst, src, writes=[r_cast[name]], eng=POOL, max_dma_last_dim=4096)

    A.mark()
    kiT = A.tile([S], BF16); r_ki = Res()
    A.mark()
    cts = A.tile([8], F32); cact = A.tile([8], F32); r_ca = Res()
    bada = A.tile([48], F32); gm = A.tile([8], F32); gf = A.tile([8], F32)
    dma(cts, cT, writes=[r_ca]); dma(bada, b_adaT, writes=[r_ca]); dma(gm, gmixT, writes=[r_ca]); dma(gf, gffnT, writes=[r_ca])
    P.op(ACT, lambda e: e.activation(out=cact, in_=cts, func=AF.Silu), reads=[r_ca], writes=[r_ca])
    wa = [A.tile([8, 768], F32) for _ in range(2)]; r_wa = [Res(), Res()]
    w_ada_v = w_ada.rearrange("(k p) n -> p k n", p=128)
    r_mod2 = Res()

    def ada_piece(j, bank=0, r_dst=None):
        b = j % 2
        dma(wa[b], w_ada_v[:, :, j * 768:(j + 1) * 768], writes=[r_wa[b]])
        for m in range(6):
            col = j * 6 + m
            for kc in range(8):
                P.op(PE, lambda e, b=b, m=m, kc=kc, col=col: e.matmul(ps[:, bank, col:col + 1], lhsT=wa[b][:, kc, m * 128:(m + 1) * 128],
                                                                  rhs=cact[:, kc:kc + 1], start=(kc == 0), stop=(kc == 7)),
                     reads=[r_wa[b], r_ca], writes=[rb[bank]])
        if r_dst is not None:
            c0_, c1_ = j * 6, j * 6 + 6
            P.op(DVE, lambda e: e.tensor_tensor(out=modT[:, c0_:c1_], in0=ps[:, bank, c0_:c1_], in1=bada[:, c0_:c1_], op=ALU.add), reads=[rb[bank], r_ca], writes=[r_dst])

    for j in range(3):
        ada_piece(j)
    P.op(DVE, lambda e: e.tensor_tensor(out=modT[:, 0:18], in0=ps[:, 0, 0:18], in1=bada[:, 0:18], op=ALU.add), reads=[rb[0], r_ca], writes=[r_mod])
    P.op(DVE, lambda e: e.tensor_scalar(out=ab[:, 1, :], in0=modT[:, 8:16], scalar1=1.0, scalar2=None, op0=ALU.add), writes=[r_mod])
    P.op(DVE, lambda e: e.tensor_tensor(out=ab[:, 0, :], in0=ab[:, 1, :], in1=gm, op=ALU.mult), reads=[r_ca], writes=[r_mod])

    def ada_finish():
        P.op(DVE, lambda e: e.tensor_scalar(out=ab[:, 3, :], in0=modT[:, 32:40], scalar1=1.0, scalar2=None, op0=ALU.add), writes=[r_mod2])
        P.op(DVE, lambda e: e.tensor_tensor(out=ab[:, 2, :], in0=ab[:, 3, :], in1=gf, op=ALU.mult), reads=[r_ca], writes=[r_mod2])
    a1 = ab[:, 0, :]; b1 = modT[:, 0:8]; a2 = ab[:, 2, :]; b2 = modT[:, 24:32]; g_m = modT[:, 16:24]; g_f = modT[:, 40:48]

    def prep_front(xrows, nblk, np_, xt, r_xt, xn, r_xn, st, r_st):
        dma(xt[0:np_, 0:nblk, :], xrows.rearrange("(b p) d -> p b d", p=np_), writes=[r_xt])
        for bl in range(nblk):
            P.op(ACT, lambda e, bl=bl: e.activation(out=xn[0:np_, bl, :], in_=xt[0:np_, bl, :], func=AF.Square, accum_out=st[0:np_, bl:bl + 1]),
                 reads=[r_xt], writes=[r_xn, r_st])
        P.op(ACT, lambda e: e.activation(out=st[0:np_, 4:4 + nblk], in_=st[0:np_, 0:nblk], func=AF.Sqrt, bias=EPS, scale=1.0 / D), writes=[r_st])
        P.op(DVE, lambda e: e.reciprocal(out=st[0:np_, 8:8 + nblk], in_=st[0:np_, 4:4 + nblk]), reads=[r_st], writes=[r_st])
        for bl in range(nblk):
            P.op(DVE, lambda e, bl=bl: e.tensor_scalar(out=xn[0:np_, bl, :], in0=xt[0:np_, bl, :], scalar1=st[0:np_, 8 + bl:9 + bl], scalar2=None, op0=ALU.mult),
                 reads=[r_xt, r_st], writes=[r_xn])

    def prep_back(nblk, np_, xn, r_xn, hT, r_hTk, banks, avec, bvec):
        ntok = nblk * np_
        for kc in range(8):
            bk = banks[kc // 2]
            for bl in range(nblk):
                o = (kc % 2) * 512 + bl * np_
                P.op(PE, lambda e, kc=kc, bl=bl, bk=bk, o=o: e.transpose(out=psb[bk][:, o:o + np_], in_=xn[0:np_, bl, kc * 128:(kc + 1) * 128],
                                                                     identity=ident[0:np_, 0:np_]),
                     reads=[r_xn, r_c], writes=[rb[bk]])
            if kc % 2 == 1:
                for k2 in (kc - 1, kc):
                    o = (k2 % 2) * 512
                    eng_ = DVE
                    if eng_ == DVE:
                        P.op(DVE, lambda e, k2=k2, bk=bk, o=o: e.tensor_scalar(out=hT[:, k2, 0:ntok], in0=psb[bk][:, o:o + ntok], scalar1=avec[:, k2:k2 + 1],
                                                                           scalar2=bvec[:, k2:k2 + 1], op0=ALU.mult, op1=ALU.add),
                             reads=[rb[bk], r_mod], writes=[r_hTk[k2]])
                    else:
                        P.op(ACT, lambda e, k2=k2, bk=bk, o=o: e.activation(out=hT[:, k2, 0:ntok], in_=psb[bk][:, o:o + ntok], func=AF.Identity, scale=avec[:, k2:k2 + 1],
                                                                        bias=bvec[:, k2:k2 + 1]),
                             reads=[rb[bk], r_mod], writes=[r_hTk[k2]])

    def prep(xrows, nblk, np_, xt, r_xt, xn, r_xn, hT, r_hT, st, r_st, banks, avec, bvec):
        prep_front(xrows, nblk, np_, xt, r_xt, xn, r_xn, st, r_st)
        prep_back(nblk, np_, xn, r_xn, hT, [r_hT] * 8, banks, avec, bvec)

    def load_w(dst, src_b, rows, c0, c1, r_w, cname="w_in"):
        dma(dst, src_b.rearrange("(k p) n -> p k n", p=128)[:, :, c0:c1], reads=[r_cast[cname]], writes=[r_w])

    evac_rr = [0]

    def evac(out, in_, reads, writes, func=None, scale=1.0):
        evac_rr[0] += 1
        if func is not None or evac_rr[0] % 2 == 0:
            f = func if func is not None else AF.Copy
            return P.op(ACT, lambda e: e.activation(out=out, in_=in_, func=f, scale=scale), reads=reads, writes=writes)
        if scale != 1.0:
            return P.op(DVE, lambda e: e.tensor_scalar(out=out, in0=in_, scalar1=scale, scalar2=None, op0=ALU.mult), reads=reads, writes=writes)
        return P.op(DVE, lambda e: e.tensor_copy(out=out, in_=in_), reads=reads, writes=writes)

    A.mark()
    wk = A.tile([8, 512], BF16); wv = A.tile([8, 512], BF16); wki = A.tile([8, 128], BF16); r_w1 = Res()
    wst = [A.tile([8, 512], F32) for _ in range(2)]; r_wst = [Res(), Res()]
    w_in_v = w_in.rearrange("(k p) n -> p k n", p=128)
    dma(wst[0], w_in_v[:, :, 512:1024], writes=[r_wst[0]])
    dma(wst[1], w_in_v[:, :, 1024:1536], writes=[r_wst[1]])
    P.op(POOL, lambda e: e.tensor_copy(out=wk, in_=wst[0]), reads=[r_wst[0]], writes=[r_w1])
    P.op(DVE, lambda e: e.tensor_copy(out=wv, in_=wst[1]), reads=[r_wst[1]], writes=[r_w1])
    dma(wst[0][:, :, 0:64], w_in_v[:, :, 2048:2112], writes=[r_wst[0]])
    P.op(ACT, lambda e: e.activation(out=wki[:, :, 0:64], in_=wst[0][:, :, 0:64], func=AF.Copy), reads=[r_wst[0]], writes=[r_w1])
    P.op(ACT, lambda e: e.activation(out=wki[:, :, 64:128], in_=wst[0][:, :, 0:64], func=AF.Copy), reads=[r_wst[0]], writes=[r_w1])
    xts = [A.tile([4, D], F32) for _ in range(2)]; r_xts = [Res(), Res()]
    xns = [A.tile([4, D], BF16) for _ in range(2)]; r_xns = [Res(), Res()]
    hTs = [A.tile([8, 512], BF16) for _ in range(2)]; r_hTs = [[Res() for _ in range(8)] for _ in range(2)]
    sts = [A.tile([12], F32) for _ in range(2)]; r_sts = [Res(), Res()]
    ksts = [A.tile([4, 512], BF16) for _ in range(2)]; r_ksts = [Res(), Res()]
    vsts = [A.tile([4, 8, 65], BF16) for _ in range(2)]; r_vsts = [Res(), Res()]
    for b in range(2):
        P.op(DVE, lambda e, b=b: e.memset(vsts[b], 1.0), writes=[r_vsts[b]])
    kT_v = kT_s.rearrange("(m hh) d s -> (hh d) m s", hh=2)
    v_v = v_s.rearrange("(n p) c -> p n c", p=128)

    def front1a(T):
        b = T % 2
        prep_front(xk[T * 512:(T + 1) * 512, :], 4, 128, xts[b], r_xts[b], xns[b], r_xns[b], sts[b], r_sts[b])

    def back1a(T):
        b = T % 2
        prep_back(4, 128, xns[b], r_xns[b], hTs[b], r_hTs[b], [0, 1, 2, 3], a1, b1)

    front1a(0)
    back1a(0)
    for T in range(NT):
        b = T % 2
        hT = hTs[b]
        if T + 1 < NT:
            front1a(T + 1)
        for m in range(4):
            bk = 4 + (m % 2)
            for kc in range(8):
                P.op(PE, lambda e, m=m, kc=kc, bk=bk, hT=hT: e.matmul(ps[:, bk, :], lhsT=wk[:, kc, m * 128:(m + 1) * 128], rhs=hT[:, kc, :],
                                                                  start=(kc == 0), stop=(kc == 7)), reads=[r_w1, r_hTs[b][kc]], writes=[rb[bk]])
            evac(ksts[b][:, m, :], ps[:, bk, :], [rb[bk]], [r_ksts[b]])
        dma(kT_v[:, :, T * 512:(T + 1) * 512], ksts[b], reads=[r_ksts[b]])
        for bl in range(4):
            bk = 6 + (bl % 2)
            for kc in range(8):
                P.op(PE, lambda e, bl=bl, kc=kc, bk=bk, hT=hT: e.matmul(ps[:, bk, :], lhsT=hT[:, kc, bl * 128:(bl + 1) * 128], rhs=wv[:, kc, :],
                                                                    start=(kc == 0), stop=(kc == 7)), reads=[r_w1, r_hTs[b][kc]], writes=[rb[bk]])
            evac(vsts[b][:, bl, :, 0:64], ps[:, bk, :].rearrange("p (h d) -> p h d", d=64), [rb[bk]], [r_vsts[b]])
        dma(v_v[:, T * 4:(T + 1) * 4, :], vsts[b].rearrange("p n h c -> p n (h c)"), reads=[r_vsts[b]])
        for kc in range(8):
            P.op(PE, lambda e, kc=kc, hT=hT: e.matmul(ps[:, 4, :], lhsT=wki[:, kc, :], rhs=hT[:, kc, :], start=(kc == 0), stop=(kc == 7)),
                 reads=[r_w1, r_hTs[b][kc]], writes=[rb[4]])
        evac(kiT[:, T * 512:(T + 1) * 512], ps[:, 4, :], [rb[4]], [r_ki])
        if T + 1 < NT:
            back1a(T + 1)
        if T < 5:
            ada_piece(3 + T, bank=4, r_dst=r_mod2)
        if T == 5:
            ada_finish()
    P.barrier()
    A.release()
    A.release()

    A.mark()
    wq = A.tile([8, 512], BF16); wqi = A.tile([8, 512], BF16); wwi = A.tile([8, 8], BF16)
    wu = A.tile([8, 1024], BF16); wga = A.tile([8, 1024], BF16); wgb = A.tile([8, 1024], BF16); r_w2 = Res()
    load_w(wq, w_in_b, D, 0, 512, r_w2); load_w(wqi, w_in_b, D, 1536, 2048, r_w2); load_w(wwi, w_in_b, D, 2112, 2120, r_w2)
    load_w(wu, w_in_b, D, 2120, 3144, r_w2); load_w(wga, w_in_b, D, 3144, 4168, r_w2); load_w(wgb, w_in_b, D, 4168, 5192, r_w2)
    xt = A.tile([4, D], F32); r_xt = Res(); xn = A.tile([4, D], BF16); r_xn = Res()
    hT = A.tile([8, 512], BF16); r_hT = Res(); st = A.tile([12], F32); r_st = Res()
    xth = A.tile([1, D], F32); r_xth = Res(); xnh = A.tile([1, D], BF16); r_xnh = Res()
    hTh = A.tile([8, 32], BF16); r_hTh = Res(); sth = A.tile([12], F32); r_sth = Res()
    stg = [A.tile([512], BF16) for _ in range(3)]; r_stg = [Res() for _ in range(3)]
    sgm = [A.tile([512], F32) for _ in range(2)]; r_sgm = [Res(), Res()]
    stg_i = [0]
    qT_v = qT_s.rearrange("(m hh) d s -> (hh d) m s", hh=2)

    def stage_out(dst, src_ps, bk, func=None):
        i = stg_i[0] % 3
        stg_i[0] += 1
        evac(stg[i], src_ps, [rb[bk]], [r_stg[i]], func=func)
        dma(dst, stg[i], reads=[r_stg[i]])

    def proj_fm(w, m, hT_, r_h, bk, n):
        for kc in range(8):
            P.op(PE, lambda e, kc=kc: e.matmul(ps[:, bk, 0:n], lhsT=w[:, kc, m * 128:(m + 1) * 128], rhs=hT_[:, kc, 0:n], start=(kc == 0), stop=(kc == 7)),
                 reads=[r_w2, r_h], writes=[rb[bk]])

    for g in range(4):
        T = 4 * g + 3
        prep(xk[T * 512:(T + 1) * 512, :], 4, 128, xt, r_xt, xn, r_xn, hT, r_hT, st, r_st, [0, 1, 2, 3], a1, b1)
        cs = slice(g * 512, (g + 1) * 512)
        bkc = [0]

        def nb():
            bkc[0] += 1
            return 4 + bkc[0] % 4
        for m in range(4):
            bk = nb(); proj_fm(wq, m, hT, r_hT, bk, 512); stage_out(qT_v[:, m, cs], ps[:, bk, :], bk)
        for m in range(4):
            bk = nb(); proj_fm(wqi, m, hT, r_hT, bk, 512); stage_out(qiT_s[m, :, cs], ps[:, bk, :], bk)
        for m in range(8):
            bk = nb(); proj_fm(wga, m, hT, r_hT, bk, 512); stage_out(gaT_s[m, :, cs], ps[:, bk, :], bk, func=AF.Sigmoid)
        for m in range(8):
            bk = nb(); proj_fm(wgb, m, hT, r_hT, bk, 512); stage_out(gbT_s[m, :, cs], ps[:, bk, :], bk, func=AF.Sigmoid)
        for bl in range(4):
            bk = nb()
            for kc in range(8):
                P.op(PE, lambda e, kc=kc, bl=bl, bk=bk: e.matmul(ps[:, bk, 0:8], lhsT=hT[:, kc, bl * 128:(bl + 1) * 128], rhs=wwi[:, kc, :], start=(kc == 0), stop=(kc == 7)),
                     reads=[r_w2, r_hT], writes=[rb[bk]])
            P.op(DVE, lambda e, bl=bl, bk=bk, g=g: e.tensor_scalar(out=wiall[:, g * 4 + bl, :], in0=ps[:, bk, 0:8], scalar1=float(8 ** -0.5 * 64 ** -0.5), scalar2=None, op0=ALU.mult),
                 reads=[rb[bk]], writes=[r_wi])
        for i in range(4):
            bka = nb(); proj_fm(wu, i, hT, r_hT, bka, 512)
            bkg = nb(); proj_fm(wu, 4 + i, hT, r_hT, bkg, 512)
            si = i % 2
            P.op(ACT, lambda e, si=si, bkg=bkg: e.activation(out=sgm[si], in_=ps[:, bkg, :], func=AF.Sigmoid), reads=[rb[bkg]], writes=[r_sgm[si]])
            j = stg_i[0] % 3; stg_i[0] += 1
            P.op(DVE, lambda e, si=si, bka=bka, j=j: e.tensor_tensor(out=stg[j], in0=ps[:, bka, :], in1=sgm[si], op=ALU.mult), reads=[rb[bka], r_sgm[si]], writes=[r_stg[j]])
            dma(zT_s[i, :, g, 32:544], stg[j], reads=[r_stg[j]])
        prep(xk[T * 512 - 32:T * 512, :], 1, 32, xth, r_xth, xnh, r_xnh, hTh, r_hTh, sth, r_sth, [0, 1, 2, 3], a1, b1)
        for i in range(4):
            bka = nb(); proj_fm(wu, i, hTh, r_hTh, bka, 32)
            bkg = nb(); proj_fm(wu, 4 + i, hTh, r_hTh, bkg, 32)
            si = i % 2
            P.op(ACT, lambda e, si=si, bkg=bkg: e.activation(out=sgm[si][:, 0:32], in_=ps[:, bkg, 0:32], func=AF.Sigmoid), reads=[rb[bkg]], writes=[r_sgm[si]])
            j = stg_i[0] % 3; stg_i[0] += 1
            P.op(DVE, lambda e, si=si, bka=bka, j=j: e.tensor_tensor(out=sgm[si][:, 32:64], in0=ps[:, bka, 0:32], in1=sgm[si][:, 0:32], op=ALU.mult), reads=[rb[bka]], writes=[r_sgm[si]])
            P.op(DVE, lambda e, si=si, j=j, g=g: e.tensor_tensor(out=stg[j][:, 0:32], in0=sgm[si][:, 32:64], in1=hval[:, g * 32:(g + 1) * 32], op=ALU.mult), reads=[r_c], writes=[r_stg[j], r_sgm[si]])
            dma(zT_s[i, :, g, 0:32], stg[j][:, 0:32], reads=[r_stg[j]])
    P.barrier()
    A.release()

    A.mark()
    cbt = A.tile([4, 512], BF16); kbt = A.tile([1536], BF16); r_c2 = Res()
    dma(cbt, cb_d, writes=[r_c2]); dma(kbt[0:1, :], kbias_d[:, 0:1536], writes=[r_c2])
    onesb = A.tile([128], BF16)
    nidm = A.tile([128], BF16)
    P.op(DVE, lambda e: e.memset(onesb, 1.0), writes=[r_c2])
    P.op(DVE, lambda e: e.tensor_scalar(out=nidm, in0=ident, scalar1=-MASKB, scalar2=None, op0=ALU.mult), reads=[r_c], writes=[r_c2])
    scoress = [A.tile([S], F32) for _ in range(2)]; r_scs = [Res(), Res()]
    maskq = A.tile([S], BF16); r_mq = Res()
    junk = maskq; r_junk = r_mq
    maskTs = [A.tile([64, 256], FP8) for _ in range(2)]; r_mTs = [Res(), Res()]
    qit = A.tile([4, 256], BF16); r_qit = Res()
    qaugs = [A.tile([8, 256], BF16) for _ in range(2)]; r_qas = [Res(), Res()]
    wdg = A.tile([8, 128], BF16); r_wdg = Res()
    Rt = [A.tile([2, 512], BF16) for _ in range(3)]; r_Rt = [Res() for _ in range(3)]
    bs = A.tile([16], F32); r_bs = Res()
    bsa = A.tile([4], F32); r_bsa = Res(); r_junkA = Res()
    cntk = A.tile([64], F32); r_ck = Res()
    U = A.tile([68], F32); r_U = Res()
    kt = [A.tile([8, 512], BF16) for _ in range(2)]; r_kt = [Res(), Res()]
    vt = [A.tile([4, 520], BF16) for _ in range(2)]; r_vt = [Res(), Res()]
    Pt = [A.tile([2, 256], BF16) for _ in range(3)]; r_Pt = [Res() for _ in range(3)]
    den = A.tile([16], F32); r_den = Res()
    Ob = A.tile([2, 512], BF16); r_Ob = Res()
    OTst = A.tile([4, 256], BF16); r_OT = Res()
    P.op(DVE, lambda e: e.memset(U, 0.0), writes=[r_U])
    P.op(DVE, lambda e: e.memset(U[:, 64:66], 1.0), writes=[r_U])
    for i in range(2):
        P.op(DVE, lambda e, i=i: e.memset(qaugs[i], 0.0), writes=[r_qas[i]])
    kT_hv = kT_s.rearrange("h d s -> d h s")
    kaug_v = kaug_d
    v_v2 = v_s.rearrange("(n p) c -> p n c", p=128)
    LB = 2
    SB = 7

    def idx_gen(qb):
        qp, q2 = qb // 2, qb % 2
        g = qp // 2
        E = 2048 * (g + 1)
        nch = E // 512
        c0 = g * 512 + (qp % 2) * 256
        j4 = qb % 4
        scores = scoress[qb % 2]; r_sc = r_scs[qb % 2]
        if q2 == 0:
            dma(qit, qiT_s.rearrange("m p s -> p m s")[:, :, c0:c0 + 256], writes=[r_qit])
        for j in range(8):
            P.op(POOL, lambda e, j=j: e.tensor_scalar(out=wdg[:, j, :], in0=ident, scalar1=wiall[:, qb, j:j + 1], scalar2=None, op0=ALU.mult),
                 reads=[r_wi, r_c], writes=[r_wdg])
        yield 0.5
        units = [(c, m) for c in range(nch) for m in range(4)]

        def emit_diag(u):
            c, m = units[u]
            ks = slice(c * 512, (c + 1) * 512)
            has_kb = (c < 3)
            has_cb = (c == nch - 1)
            lastdiag = not (has_kb or has_cb)
            ri = u % 3
            for hh in range(2):
                imm = 2 * m + hh
                P.op(PE, lambda e, m=m, hh=hh, ri=ri, imm=imm, lastdiag=lastdiag: e.matmul(ps[:, SB, :], lhsT=wdg[:, 2 * m + hh, :], rhs=Rt[ri][:, hh, :], start=(imm == 0), stop=(lastdiag and imm == 7)),
                     reads=[r_wdg, r_Rt[ri]], writes=[rb[SB]])
            if m == 3:
                if has_kb:
                    P.op(PE, lambda e, ks=ks, has_cb=has_cb: e.matmul(ps[:, SB, :], lhsT=onesb[0:1, :], rhs=kbt[0:1, ks], start=False, stop=(not has_cb)),
                         reads=[r_c2], writes=[rb[SB]])
                if has_cb:
                    P.op(PE, lambda e: e.matmul(ps[:, SB, :], lhsT=ident, rhs=cbt[:, j4, :], start=False, stop=True), reads=[r_c2, r_c], writes=[rb[SB]])
                P.op(ACT, lambda e, ks=ks: e.activation(out=scores[:, ks], in_=ps[:, SB, :], func=AF.Copy), reads=[rb[SB]], writes=[r_sc])

        for u, (c, m) in enumerate(units):
            ks = slice(c * 512, (c + 1) * 512)
            for hh in range(2):
                pr = slice(hh * 64, hh * 64 + 64)
                P.op(PE, lambda e, m=m, hh=hh, pr=pr, ks=ks: e.matmul(ps[:, LB + hh, :], lhsT=qit[pr, m, q2 * 128:(q2 + 1) * 128], rhs=kiT[pr, ks],
                                                                start=True, stop=True), reads=[r_qit, r_ki], writes=[rb[LB + hh]])
            ri = u % 3
            P.op(ACT, lambda e, ri=ri: e.activation(out=Rt[ri], in_=ps[:, LB:LB + 2, :], func=AF.Relu), reads=[rb[LB], rb[LB + 1]], writes=[r_Rt[ri]])
            if u > 0:
                emit_diag(u - 1)
            yield 0.8
        emit_diag(len(units) - 1)
        if debug:
            dma(dbg_sc[qb, :, 0:E], scores[:, 0:E], reads=[r_sc])
        yield 0.5

    def bis_gen(qb):
        qp, q2 = qb // 2, qb % 2
        g = qp // 2
        E = 2048 * (g + 1)
        nkc = E // 128
        c0 = g * 512 + (qp % 2) * 256
        scores = scoress[qb % 2]; r_sc = r_scs[qb % 2]
        maskT = maskTs[qp % 2]; r_mT = r_mTs[qp % 2]
        qaug = qaugs[qp % 2]; r_qa = r_qas[qp % 2]
        if q2 == 0:
            dma(qaug[0:64, :, :], qT_s.rearrange("h d s -> d h s")[:, :, c0:c0 + 256], writes=[r_qa])
        sc = scores[:, 0:E]
        tpass = E / 960.0
        P.op(DVE, lambda e: e.tensor_reduce(out=bs[:, 0:1], in_=sc, axis=AX.X, op=ALU.max), reads=[r_sc], writes=[r_bs])
        P.op(DVE, lambda e: e.tensor_scalar(out=bs[:, 9:10], in0=bs[:, 0:1], scalar1=-1.0, scalar2=None, op0=ALU.mult), writes=[r_bs])
        P.op(DVE, lambda e: e.tensor_tensor(out=bs[:, 9:10], in0=bs[:, 9:10], in1=bs[:, 0:1], op=ALU.max), writes=[r_bs])
        P.op(DVE, lambda e: e.tensor_scalar(out=bs[:, 9:10], in0=bs[:, 9:10], scalar1=3.0, scalar2=3.0, op0=ALU.mult, op1=ALU.add), writes=[r_bs])
        P.op(DVE, lambda e: e.tensor_tensor(out=bs[:, 3:4], in0=bs[:, 0:1], in1=bs[:, 9:10], op=ALU.subtract), writes=[r_bs])
        P.op(DVE, lambda e: e.tensor_scalar(out=bs[:, 6:7], in0=bs[:, 3:4], scalar1=20000.0, scalar2=None, op0=ALU.add), writes=[r_bs])
        P.op(DVE, lambda e: e.tensor_tensor(out=bs[:, 7:8], in0=bs[:, 9:10], in1=bs[:, 6:7], op=ALU.subtract), writes=[r_bs])
        yield tpass + 1.0
        for it in range(NITER + 1):
            P.op(DVE, lambda e: e.tensor_scalar(out=junk[:, 0:E], in0=sc, scalar1=bs[:, 3:4], scalar2=None, op0=ALU.is_ge, op1=ALU.add, accum_out=bs[:, 4:5]),
                 reads=[r_sc], writes=[r_junk, r_bs])
            P.op(DVE, lambda e: e.tensor_scalar(out=bs[:, 5:6], in0=bs[:, 4:5], scalar1=255.5, scalar2=None, op0=ALU.is_ge), writes=[r_bs])
            if it == 0:
                P.op(DVE, lambda e: e.tensor_scalar(out=bs[:, 1:2], in0=bs[:, 5:6], scalar1=bs[:, 6:7], scalar2=-20000.0, op0=ALU.mult, op1=ALU.add), writes=[r_bs])
                P.op(DVE, lambda e: e.scalar_tensor_tensor(out=bs[:, 2:3], in0=bs[:, 5:6], scalar=bs[:, 7:8], in1=bs[:, 6:7], op0=ALU.mult, op1=ALU.add), writes=[r_bs])
            else:
                P.op(DVE, lambda e: e.tensor_scalar(out=bs[:, 2:3], in0=bs[:, 2:3], scalar1=0.5, scalar2=None, op0=ALU.mult), writes=[r_bs])
                P.op(DVE, lambda e: e.scalar_tensor_tensor(out=bs[:, 1:2], in0=bs[:, 5:6], scalar=bs[:, 2:3], in1=bs[:, 1:2], op0=ALU.mult, op1=ALU.add), writes=[r_bs])
            P.op(DVE, lambda e: e.scalar_tensor_tensor(out=bs[:, 3:4], in0=bs[:, 2:3], scalar=0.5, in1=bs[:, 1:2], op0=ALU.mult, op1=ALU.add), writes=[r_bs])
            yield tpass + 0.8
        if debug:
            dma(dbg_thr[:, qb * 4:qb * 4 + 4], bs[:, 0:4], reads=[r_bs])
        P.op(DVE, lambda e: e.tensor_scalar(out=maskq[:, 0:E], in0=sc, scalar1=bs[:, 1:2], scalar2=None, op0=ALU.is_ge), reads=[r_sc, r_junkA], writes=[r_mq, r_bs])
        P.op(DVE, lambda e: e.tensor_reduce(out=cntk[:, 0:nkc], in_=maskq[:, 0:E].rearrange("p (a b) -> p a b", b=128), axis=AX.X, op=ALU.add), writes=[r_ck, r_mq])
        P.op(DVE, lambda e: e.tensor_scalar(out=cntk[:, 0:nkc], in0=cntk[:, 0:nkc], scalar1=0.5, scalar2=None, op0=ALU.is_ge), writes=[r_ck])
        P.op(DVE, lambda e: e.tensor_tensor(out=cntk[:, 0:nkc], in0=cntk[:, 0:nkc], in1=kcp1[:, 0:nkc], op=ALU.mult), reads=[r_c], writes=[r_ck])
        P.op(DVE, lambda e: e.tensor_reduce(out=bs[:, 8:9], in_=cntk[:, 0:nkc], axis=AX.X, op=ALU.max), writes=[r_ck, r_bs])
        P.op(DVE, lambda e: e.tensor_scalar(out=U[:, 66:67], in0=bs[:, 8:9], scalar1=-1.0, scalar2=None, op0=ALU.add), writes=[r_U, r_bs])
        yield 2 * tpass
        P.op(PE, lambda e: e.matmul(ps[0:67, LB, 0:128], lhsT=U[:, 0:67], rhs=identf, start=True, stop=True), reads=[r_U, r_c], writes=[rb[LB]])
        for h in range(8):
            P.op(ACT, lambda e, h=h: e.activation(out=qaug[64:67, h, q2 * 128:(q2 + 1) * 128], in_=ps[64:67, LB, 0:128], func=AF.Copy), reads=[rb[LB]], writes=[r_qa])
        yield 1.0
        for k8 in range(nkc // 8):
            for kk in range(8):
                kc = k8 * 8 + kk
                P.op(PE, lambda e, kc=kc, kk=kk: e.transpose(out=psb[LB + 1][:, kk * 128:(kk + 1) * 128], in_=maskq[:, kc * 128:(kc + 1) * 128], identity=ident),
                     reads=[r_mq, r_c], writes=[rb[LB + 1]])
            mo = maskT[:, k8 * 8:(k8 + 1) * 8, q2 * 128:(q2 + 1) * 128]
            mi = psb[LB + 1].rearrange("p (a b) -> p a b", b=128)
            P.op(ACT, lambda e, mo=mo, mi=mi: e.activation(out=mo, in_=mi, func=AF.Copy, scale=-1.0, bias=1.0, saturate=False), reads=[rb[LB + 1]], writes=[r_mT])
            yield 1.0

    accb = [4, 5, 6]

    def att_gen(qp):
        g = qp // 2
        E = 2048 * (g + 1)
        nch = E // 512
        nkc = E // 128
        c0 = g * 512 + (qp % 2) * 256
        maskT = maskTs[qp % 2]; r_mT = r_mTs[qp % 2]
        qaug = qaugs[qp % 2]; r_qa = r_qas[qp % 2]
        first_in_bank = {}
        steps = [(c, kl, hp) for c in range(nch) for kl in range(4) for hp in range(4)]

        def emit_loads(c):
            kb_ = c % 2
            ks = slice(c * 512, (c + 1) * 512)
            dma(kt[kb_][0:64, :, :], kT_hv[:, :, ks], writes=[r_kt[kb_]])
            dma(kt[kb_][64:67, :, :], kaug_v[:, :, ks], writes=[r_kt[kb_]])
            dma(vt[kb_], v_v2[:, c * 4:(c + 1) * 4, :], writes=[r_vt[kb_]])

        def emit_ST(i):
            c, kl, hp = steps[i]
            kb_ = c % 2
            kc = c * 4 + kl
            sbk = i % 2
            for hh in range(2):
                h = hp * 2 + hh
                o = hh * 256
                P.op(PE, lambda e, h=h, o=o, sbk=sbk, kb_=kb_, kl=kl: e.matmul(ps[:, sbk, o:o + 256], lhsT=kt[kb_][0:67, h, kl * 128:(kl + 1) * 128],
                                                                    rhs=qaug[0:67, h, :], start=True, stop=False),
                     reads=[r_kt[kb_], r_qa], writes=[rb[sbk]])
                P.op(PE, lambda e, o=o, sbk=sbk, kc=kc: e.matmul(ps[:, sbk, o:o + 256], lhsT=nidm, rhs=maskT[:, kc, :], start=False, stop=True),
                     reads=[r_mT, r_c2], writes=[rb[sbk]])

        emit_loads(0)
        if nch > 1:
            emit_loads(1)
        emit_ST(0)
        for i, (c, kl, hp) in enumerate(steps):
            kb_ = c % 2
            kc = c * 4 + kl
            sbk = i % 2
            if i + 1 < len(steps):
                emit_ST(i + 1)
            pi = i % 3
            P.op(ACT, lambda e, pi=pi, sbk=sbk: e.activation(out=Pt[pi], in_=ps[:, sbk, :].rearrange("p (a b) -> p a b", b=256), func=AF.Exp, bias=EXPB, scale=0.125),
                 reads=[rb[sbk]], writes=[r_Pt[pi]])
            for q2 in range(2):
                for hh in range(2):
                    h = hp * 2 + hh
                    a = q2 * 8 + h
                    ab_ = accb[a // 6]
                    col = (a % 6) * 65
                    st_ = (kc == 0) and (ab_ not in first_in_bank)
                    if kc == 0:
                        first_in_bank[ab_] = True
                    P.op(PE, lambda e, pi=pi, hh=hh, q2=q2, h=h, ab_=ab_, col=col, st_=st_, kb_=kb_, kl=kl, kc=kc: e.matmul(
                        ps[:, ab_, col:col + 65], lhsT=Pt[pi][:, hh, q2 * 128:(q2 + 1) * 128], rhs=vt[kb_][:, kl, h * 65:(h + 1) * 65],
                        start=st_, stop=(kc == nkc - 1), skip_group_check=True),
                        reads=[r_Pt[pi], r_vt[kb_]], writes=[rb[ab_]])
            if kl == 3 and hp == 3 and c + 2 < nch:
                emit_loads(c + 2)
            yield 0.8
        for a in range(16):
            ab_ = accb[a // 6]; col = (a % 6) * 65
            P.op(DVE, lambda e, a=a, ab_=ab_, col=col: e.tensor_copy(out=den[:, a:a + 1], in_=ps[:, ab_, col + 64:col + 65]), reads=[rb[ab_]], writes=[r_den])
        P.op(DVE, lambda e: e.reciprocal(out=den, in_=den), writes=[r_den])
        for a in range(16):
            ab_ = accb[a // 6]; col = (a % 6) * 65
            q2 = a // 8; h = a % 8
            P.op(DVE, lambda e, a=a, ab_=ab_, col=col, q2=q2, h=h: e.tensor_scalar(out=Ob[:, q2, h * 64:(h + 1) * 64], in0=ps[:, ab_, col:col + 64], scalar1=den[:, a:a + 1], scalar2=None, op0=ALU.mult),
                 reads=[rb[ab_]], writes=[r_Ob, r_den])
        yield 3.0
        for q2 in range(2):
            for m in range(4):
                P.op(PE, lambda e, q2=q2, m=m: e.transpose(out=psb[0][:, (q2 * 4 + m) * 128:(q2 * 4 + m + 1) * 128], in_=Ob[:, q2, m * 128:(m + 1) * 128], identity=ident),
                     reads=[r_Ob, r_c], writes=[rb[0]])
        for q2 in range(2):
            P.op(ACT, lambda e, q2=q2: e.activation(out=OTst[:, :, q2 * 128:(q2 + 1) * 128], in_=psb[0][:, q2 * 512:(q2 + 1) * 512].rearrange("p (a b) -> p a b", b=128), func=AF.Copy),
                 reads=[rb[0]], writes=[r_OT])
        dma(OT_s.rearrange("m p s -> p m s")[:, :, c0:c0 + 256], OTst, reads=[r_OT])
        yield 1.0

    NQB, NPR = 16, 8
    done = {"IDX": 0, "BIS": 0, "ATT": 0}
    nitems = {"IDX": NQB, "BIS": NQB, "ATT": NPR}
    mk = {"IDX": idx_gen, "BIS": bis_gen, "ATT": att_gen}
    cur = {"IDX": None, "BIS": None, "ATT": None}
    clk = {"IDX": 0.0, "BIS": 0.0, "ATT": 0.0}
    now = [0.0]

    def can_start(s):
        i = done[s]
        if i >= nitems[s]:
            return False
        if s == "IDX":
            return done["BIS"] >= i - 1
        if s == "BIS":
            return done["IDX"] > i and done["ATT"] >= i // 2 - 1
        return done["BIS"] > 2 * i + 1

    while any(done[s] < nitems[s] for s in done):
        cands = []
        for s in ("ATT", "IDX", "BIS"):
            if cur[s] is None and can_start(s):
                cur[s] = mk[s](done[s])
                clk[s] = max(clk[s], now[0])
            if cur[s] is not None:
                cands.append(s)
        assert cands, ("scheduler deadlock", done)
        s = min(cands, key=lambda k: clk[k])
        now[0] = clk[s]
        cost = next(cur[s], None)
        if cost is None:
            cur[s] = None
            done[s] += 1
        else:
            clk[s] += cost
    P.barrier()
    A.release()
    A.release()

    A.mark()
    wap = A.tile([4, D], BF16); wcp = A.tile([4, D], BF16); wo = A.tile([8, D], BF16); r_w3 = Res()
    load_w(wap, w_ap_b, 512, 0, D, r_w3, "w_ap"); load_w(wcp, w_cp_b, 512, 0, D, r_w3, "w_cp"); load_w(wo, w_out_b, D, 0, D, r_w3, "w_out")
    wfo_t = [A.tile([22, 128], BF16) for _ in range(2)]; r_wfo = [Res(), Res()]
    wdw = A.tile([4, 31], F32); cvp = A.tile([3, 4], F32); r_cv = Res()
    dma(wdw, w_dwT, writes=[r_cv]); dma(cvp[:, 0, :], b_dwT, writes=[r_cv]); dma(cvp[:, 1, :], lngT, writes=[r_cv]); dma(cvp[:, 2, :], lnbT, writes=[r_cv])
    wdd = A.tile([31, 128], BF16); r_wdd = Res()
    xtb = [A.tile([D], F32) for _ in range(2)]; r_xtb = [Res(), Res()]
    xTs = [A.tile([8, 512], F32) for _ in range(2)]; r_xTs = [Res(), Res()]
    OT = A.tile([4, 512], BF16); zT = A.tile([4, 544], BF16); r_in3 = Res()
    gab = [A.tile([2, 512], BF16) for _ in range(2)]; r_gab = [Res(), Res()]
    cv = A.tile([4, 512], F32); r_cvo = Res()
    sq = A.tile([512], F32); r_sq = Res()
    stt = A.tile([3, 512], F32); r_stt = Res()
    zc = A.tile([4, 512], BF16); r_zc = Res()
    t1 = A.tile([512], F32); r_t1 = Res()
    mg = A.tile([8, 512], BF16); r_mg = Res()
    h2s = [A.tile([8, 512], BF16) for _ in range(2)]; r_h2s = [Res(), Res()]
    gT = A.tile([22, 512], BF16); r_gT = Res()
    wfi_t = [A.tile([8, 256], BF16) for _ in range(2)]; r_wfi = [Res() for _ in range(2)]
    sl = [A.tile([512], F32) for _ in range(2)]; r_sl = [Res(), Res()]
    sq2 = A.tile([512], F32); r_sq2 = Res()
    rs2 = A.tile([512], F32); r_rs2 = Res()
    yo = A.tile([D], F32); r_yo = Res()
    w_fo_v = w_fo_b.rearrange("(k p) n -> p k n", p=128)
    w_fi_v = w_fi_b.rearrange("(k p) n -> p k n", p=128)

    def rstd_sumsq(n, src_fn, nchunks, sqt, r_sqt, bank, dst, r_dst, rd):
        for m in range(nchunks):
            P.op(ACT, lambda e, m=m: e.activation(out=sqt, in_=src_fn(m), func=AF.Square), reads=rd, writes=[r_sqt])
            P.op(PE, lambda e, m=m: e.matmul(ps[:, bank, :], lhsT=onesf, rhs=sqt, start=(m == 0), stop=(m == nchunks - 1)), reads=[r_sqt, r_c], writes=[rb[bank]])
        P.op(ACT, lambda e: e.activation(out=dst, in_=ps[:, bank, :], func=AF.Sqrt, bias=EPS, scale=1.0 / n), reads=[rb[bank]], writes=[r_dst])
        P.op(DVE, lambda e: e.reciprocal(out=dst, in_=dst), writes=[r_dst])

    def front_gen(g):
        T = 4 * g + 3
        cs = slice(g * 512, (g + 1) * 512)
        xT = xTs[g % 2]; r_xT = r_xTs[g % 2]
        h2 = h2s[g % 2]; r_h2 = r_h2s[g % 2]
        dma(OT, OT_s.rearrange("m p s -> p m s")[:, :, cs], writes=[r_in3])
        dma(zT, zT_s[:, :, g, :].rearrange("m p s -> p m s"), writes=[r_in3])
        for bl in range(4):
            xb_ = xtb[bl % 2]; rxb = r_xtb[bl % 2]
            dma(xb_, xk[T * 512 + bl * 128:T * 512 + (bl + 1) * 128, :], writes=[rxb])
            for half in range(2):
                bk = half
                for k4 in range(4):
                    kc = half * 4 + k4
                    P.op(PE, lambda e, kc=kc, k4=k4, bk=bk, xb_=xb_: e.transpose(out=ps[:, bk, k4 * 128:(k4 + 1) * 128], in_=xb_[:, kc * 128:(kc + 1) * 128], identity=identf),
                         reads=[rxb, r_c], writes=[rb[bk]])
                evac(xT[:, half * 4:half * 4 + 4, bl * 128:(bl + 1) * 128], ps[:, bk, :].rearrange("p (a b) -> p a b", b=128), [rb[bk]], [r_xT])
            yield 3.0
        for i in range(4):
            bk = i % 2
            for k in range(31):
                P.op(POOL, lambda e, i=i, k=k: e.tensor_scalar(out=wdd[:, k, :], in0=ident, scalar1=wdw[:, i, k:k + 1], scalar2=None, op0=ALU.mult),
                     reads=[r_cv, r_c], writes=[r_wdd])
            for k in range(31):
                P.op(PE, lambda e, i=i, k=k, bk=bk: e.matmul(ps[:, bk, :], lhsT=wdd[:, k, :], rhs=zT[:, i, k + 2:k + 514], start=(k == 0), stop=(k == 30)),
                     reads=[r_wdd, r_in3], writes=[rb[bk]])
            P.op(DVE, lambda e, i=i, bk=bk: e.tensor_scalar(out=cv[:, i, :], in0=ps[:, bk, :], scalar1=cvp[:, 0, i:i + 1], scalar2=None, op0=ALU.add),
                 reads=[rb[bk], r_cv], writes=[r_cvo])
            yield 7.0
        for i in range(4):
            P.op(PE, lambda e, i=i: e.matmul(ps[:, 2, :], lhsT=onesf, rhs=cv[:, i, :], start=(i == 0), stop=(i == 3)), reads=[r_cvo, r_c], writes=[rb[2]])
        P.op(ACT, lambda e: e.activation(out=stt[:, 0, :], in_=ps[:, 2, :], func=AF.Copy, scale=1.0 / 512), reads=[rb[2]], writes=[r_stt])
        for i in range(4):
            P.op(DVE, lambda e, i=i: e.tensor_tensor(out=cv[:, i, :], in0=cv[:, i, :], in1=stt[:, 0, :], op=ALU.subtract), reads=[r_stt], writes=[r_cvo])
        yield 4.0
        rstd_sumsq(512, lambda m: cv[:, m, :], 4, sq, r_sq, 3, stt[:, 2, :], r_stt, [r_cvo])
        yield 4.0
        for i in range(4):
            P.op(DVE, lambda e, i=i: e.tensor_tensor(out=cv[:, i, :], in0=cv[:, i, :], in1=stt[:, 2, :], op=ALU.mult), reads=[r_stt], writes=[r_cvo])
            P.op(DVE, lambda e, i=i: e.tensor_scalar(out=cv[:, i, :], in0=cv[:, i, :], scalar1=cvp[:, 1, i:i + 1], scalar2=cvp[:, 2, i:i + 1], op0=ALU.mult, op1=ALU.add),
                 reads=[r_cv], writes=[r_cvo])
            P.op(ACT, lambda e, i=i: e.activation(out=zc[:, i, :], in_=cv[:, i, :], func=AF.Silu), reads=[r_cvo], writes=[r_zc])
        yield 4.0
        for m in range(8):
            b0 = (2 * m) % 4; b1_ = (2 * m + 1) % 4
            gb_ = gab[m % 2]; rg = r_gab[m % 2]
            dma(gb_[:, 0, :], gaT_s[m, :, cs], writes=[rg])
            dma(gb_[:, 1, :], gbT_s[m, :, cs], writes=[rg])
            for k in range(4):
                P.op(PE, lambda e, m=m, k=k, b0=b0: e.matmul(ps[:, b0, :], lhsT=wap[:, k, m * 128:(m + 1) * 128], rhs=OT[:, k, :], start=(k == 0), stop=(k == 3)),
                     reads=[r_w3, r_in3], writes=[rb[b0]])
            for k in range(4):
                P.op(PE, lambda e, m=m, k=k, b1_=b1_: e.matmul(ps[:, b1_, :], lhsT=wcp[:, k, m * 128:(m + 1) * 128], rhs=zc[:, k, :], start=(k == 0), stop=(k == 3)),
                     reads=[r_w3, r_zc], writes=[rb[b1_]])
            P.op(DVE, lambda e, b0=b0, gb_=gb_: e.tensor_tensor(out=t1, in0=ps[:, b0, :], in1=gb_[:, 0, :], op=ALU.mult), reads=[rb[b0], rg], writes=[r_t1])
            P.op(DVE, lambda e, b1_=b1_, gb_=gb_: e.tensor_tensor(out=sq, in0=ps[:, b1_, :], in1=gb_[:, 1, :], op=ALU.mult), reads=[rb[b1_], rg], writes=[r_sq])
            P.op(DVE, lambda e, m=m: e.tensor_tensor(out=mg[:, m, :], in0=t1, in1=sq, op=ALU.add), reads=[r_t1, r_sq], writes=[r_mg])
            yield 2.0
        for m in range(8):
            bk = m % 4
            for k in range(8):
                P.op(PE, lambda e, m=m, k=k, bk=bk: e.matmul(ps[:, bk, :], lhsT=wo[:, k, m * 128:(m + 1) * 128], rhs=mg[:, k, :], start=(k == 0), stop=(k == 7)),
                     reads=[r_w3, r_mg], writes=[rb[bk]])
            P.op(DVE, lambda e, m=m, bk=bk: e.scalar_tensor_tensor(out=xT[:, m, :], in0=ps[:, bk, :], scalar=g_m[:, m:m + 1], in1=xT[:, m, :], op0=ALU.mult, op1=ALU.add),
                 reads=[rb[bk], r_mod2], writes=[r_xT])
            yield 2.0
        rstd_sumsq(D, lambda m: xT[:, m, :], 8, sq, r_sq, 3, stt[:, 2, :], r_stt, [r_xT])
        yield 6.0
        for m in range(8):
            P.op(DVE, lambda e, m=m: e.tensor_tensor(out=t1, in0=xT[:, m, :], in1=stt[:, 2, :], op=ALU.mult), reads=[r_xT, r_stt], writes=[r_t1])
            P.op(DVE, lambda e, m=m: e.tensor_scalar(out=h2[:, m, :], in0=t1, scalar1=a2[:, m:m + 1], scalar2=b2[:, m:m + 1], op0=ALU.mult, op1=ALU.add),
                 reads=[r_mod2], writes=[r_h2, r_t1])
        yield 6.0

    def ffn_gen(g):
        xT = xTs[g % 2]; r_xT = r_xTs[g % 2]
        h2 = h2s[g % 2]; r_h2 = r_h2s[g % 2]
        for i in range(22):
            wb_ = i % 2
            dma(wfi_t[wb_][:, :, 0:128], w_fi_v[:, :, i * 128:(i + 1) * 128], reads=[r_cast["w_fi"]], writes=[r_wfi[wb_]])
            dma(wfi_t[wb_][:, :, 128:256], w_fi_v[:, :, 2816 + i * 128:2816 + (i + 1) * 128], reads=[r_cast["w_fi"]], writes=[r_wfi[wb_]])
            b0 = 4 + (2 * i) % 4; b1_ = 4 + (2 * i + 1) % 4
            for k in range(8):
                P.op(PE, lambda e, k=k, wb_=wb_, b0=b0: e.matmul(ps[:, b0, :], lhsT=wfi_t[wb_][:, k, 0:128], rhs=h2[:, k, :], start=(k == 0), stop=(k == 7)),
                     reads=[r_wfi[wb_], r_h2], writes=[rb[b0]])
            for k in range(8):
                P.op(PE, lambda e, k=k, wb_=wb_, b1_=b1_: e.matmul(ps[:, b1_, :], lhsT=wfi_t[wb_][:, k, 128:256], rhs=h2[:, k, :], start=(k == 0), stop=(k == 7)),
                     reads=[r_wfi[wb_], r_h2], writes=[rb[b1_]])
            si = i % 2
            P.op(ACT, lambda e, si=si, b0=b0: e.activation(out=sl[si], in_=ps[:, b0, :], func=AF.Silu), reads=[rb[b0]], writes=[r_sl[si]])
            P.op(DVE, lambda e, si=si, b1_=b1_, i=i: e.tensor_tensor(out=gT[:, i, :], in0=ps[:, b1_, :], in1=sl[si], op=ALU.mult), reads=[rb[b1_], r_sl[si]], writes=[r_gT])
            yield 3.5
        for m in range(8):
            bk = 4 + m % 4
            wf = wfo_t[m % 2]; rwf = r_wfo[m % 2]
            dma(wf, w_fo_v[:, :, m * 128:(m + 1) * 128], reads=[r_cast["w_fo"]], writes=[rwf])
            for i in range(22):
                P.op(PE, lambda e, i=i, bk=bk, wf=wf: e.matmul(ps[:, bk, :], lhsT=wf[:, i, :], rhs=gT[:, i, :], start=(i == 0), stop=(i == 21)),
                     reads=[rwf, r_gT], writes=[rb[bk]])
            P.op(DVE, lambda e, m=m, bk=bk: e.scalar_tensor_tensor(out=xT[:, m, :], in0=ps[:, bk, :], scalar=g_f[:, m:m + 1], in1=xT[:, m, :], op0=ALU.mult, op1=ALU.add),
                 reads=[rb[bk], r_mod2], writes=[r_xT])
            yield 4.8
        rstd_sumsq(D, lambda m: xT[:, m, :], 8, sq2, r_sq2, 7, rs2, r_rs2, [r_xT])
        yield 6.0
        for m in range(8):
            P.op(DVE, lambda e, m=m: e.scalar_tensor_tensor(out=xT[:, m, :], in0=xT[:, m, :], scalar=gfin[:, m:m + 1], in1=rs2, op0=ALU.mult, op1=ALU.mult),
                 reads=[r_rs2, r_c], writes=[r_xT])
        yield 4.0
        for bl in range(4):
            for m in range(8):
                bk = 4 + (m // 4 + 2 * bl) % 4
                P.op(PE, lambda e, m=m, bl=bl, bk=bk: e.transpose(out=ps[:, bk, (m % 4) * 128:(m % 4 + 1) * 128], in_=xT[:, m, bl * 128:(bl + 1) * 128], identity=identf),
                     reads=[r_xT, r_c], writes=[rb[bk]])
                if m % 4 == 3:
                    evac(yo[:, (m // 4) * 512:(m // 4 + 1) * 512], ps[:, bk, :], [rb[bk]], [r_yo])
            dma(out_d[g * 512 + bl * 128:g * 512 + (bl + 1) * 128, :], yo, reads=[r_yo])
            yield 2.0

    done3 = {"F": 0, "N": 0}
    mk3 = {"F": front_gen, "N": ffn_gen}
    cur3 = {"F": None, "N": None}
    clk3 = {"F": 0.0, "N": 0.0}
    now3 = [0.0]

    def can_start3(s):
        i = done3[s]
        if i >= 4:
            return False
        if s == "F":
            return done3["N"] >= i - 1
        return done3["F"] > i

    while done3["F"] < 4 or done3["N"] < 4:
        cands = []
        for s in ("N", "F"):
            if cur3[s] is None and can_start3(s):
                cur3[s] = mk3[s](done3[s])
                clk3[s] = max(clk3[s], now3[0])
            if cur3[s] is not None:
                cands.append(s)
        assert cands, ("phase-3 scheduler deadlock", done3)
        s = min(cands, key=lambda k: clk3[k])
        now3[0] = clk3[s]
        cost = next(cur3[s], None)
        if cost is None:
            cur3[s] = None
            done3[s] += 1
        else:
            clk3[s] += cost
    A.release()
    P.emit()
    return nc


_NC_CACHE = {}


def _consts():
    ident = np.eye(128, dtype=np.float32)
    slopes = np.exp2(-np.arange(1, 9, dtype=np.float64))
    s = np.arange(S)
    kaug = np.zeros((3, 8, S), np.float32)
    for h in range(8):
        kaug[0, h] = 8.0 * slopes[h] * (s % 128)
        kaug[1, h] = 8.0 * slopes[h] * 128.0 * (s // 128)
        kaug[2, h] = -8.0 * 128.0 * slopes[h]
    cb = np.zeros((128, 4, 512), np.float32)
    p = np.arange(128)[:, None]
    sp = np.arange(512)[None, :]
    for j4 in range(4):
        cb[:, j4, :] = np.where(sp <= 128 * j4 + p, 0.0, NEG)
    kcp1 = np.tile(np.arange(1, 65, dtype=np.float32)[None, :], (128, 1))
    return ident, kaug, cb, kcp1


def _in_maps(inputs):
    x = np.asarray(inputs["x"], np.float32)
    c = np.asarray(inputs["c"], np.float32)
    bf = ml_dtypes.bfloat16
    ident, kaug, cb, kcp1 = _consts()

    def fm(v, n):
        return np.ascontiguousarray(np.asarray(v, np.float32).reshape(n, 128).T)

    shared = {
        "w_ada": np.ascontiguousarray(inputs["w_ada"][0], np.float32),
        "b_adaT": fm(inputs["b_ada"][0], 48),
        "gmixT": fm(inputs["norm_mix_g"][0], 8), "gffnT": fm(inputs["norm_ffn_g"][0], 8), "gfinT": fm(inputs["norm_final_g"], 8),
        "w_in": np.ascontiguousarray(inputs["w_in"][0], np.float32),
        "w_dwT": np.ascontiguousarray(np.asarray(inputs["w_dw"][0][:, 0, :], np.float32).T.reshape(4, 128, 31).transpose(1, 0, 2)),
        "b_dwT": fm(inputs["b_dw"][0], 4), "lngT": fm(inputs["conv_ln_g"][0], 4), "lnbT": fm(inputs["conv_ln_b"][0], 4),
        "w_ap": np.ascontiguousarray(inputs["w_attn_proj"][0], np.float32), "w_cp": np.ascontiguousarray(inputs["w_conv_proj"][0], np.float32),
        "w_out": np.ascontiguousarray(inputs["w_out"][0], np.float32),
        "w_fi": np.ascontiguousarray(inputs["w_ffn_in"][0], np.float32), "w_fo": np.ascontiguousarray(inputs["w_ffn_out"][0], np.float32),
        "ident": ident.astype(bf), "identf": ident, "kaug": kaug.astype(bf), "cb": cb.astype(bf), "kcp1": kcp1,
    }
    maps = []
    for core in range(8):
        b, r = core // 4, core % 4
        pad = 1536 - 512 * r
        xk = np.zeros((S, D), np.float32)
        xk[pad:] = x[b, :S - pad]
        kbias = np.zeros((1, S), np.float32)
        kbias[0, :pad] = NEG
        hv = np.zeros((128, 128), np.float32)
        for g in range(4):
            pos = 2048 * g + 1504 + np.arange(32)
            hv[:, g * 32:(g + 1) * 32] = (pos >= pad).astype(np.float32)[None, :]
        m = dict(shared)
        m.update({"xk": xk, "cT": fm(c[b], 8), "kbias": kbias.astype(bf), "hvalid": hv})
        maps.append(m)
    return maps


def kernel(**inputs):
    if "nc" not in _NC_CACHE:
        _NC_CACHE["nc"] = build(False)
    nc = _NC_CACHE["nc"]
    maps = _in_maps(inputs)
    res = run_bass_kernel_spmd(nc, maps, core_ids=list(range(8)))
    out = np.zeros((2, S, D), np.float32)
    for core in range(8):
        b, r = core // 4, core % 4
        o = np.asarray(res.results[core]["out"], np.float32)
        for g in range(4):
            p0 = 2048 * g + 512 * r
            out[b, p0:p0 + 512] = o[g * 512:(g + 1) * 512]
    return out
```

```python
import contextlib
import numpy as np
import ml_dtypes
import concourse.bass as bass
import concourse.mybir as mybir
from concourse.bass_utils import run_bass_kernel_spmd

F32 = mybir.dt.float32
BF16 = mybir.dt.bfloat16
FP8 = mybir.dt.float8e4
AF = mybir.ActivationFunctionType
ALU = mybir.AluOpType
AX = mybir.AxisListType

PE, ACT, DVE, POOL, SP = "tensor", "scalar", "vector", "gpsimd", "sync"
ENGS = [PE, ACT, DVE, POOL, SP]
NDMASEM = 32
NPOOLSEM = 6

S = 8192
D = 1024
NT = 16
EPS = 1e-6
NEG = -30000.0
NITER = 14
EXPB = -16.0
MASKB = 240000.0
ACT_SHARE = 0.0


class Res:
    __slots__ = ("w", "rs")

    def __init__(self):
        self.w = None
        self.rs = []


class Op:
    __slots__ = ("eng", "fn", "deps", "signal", "dma", "dsem", "dval", "prev_dma")

    def __init__(self, eng, fn, dma):
        self.eng = eng; self.fn = fn; self.deps = []
        self.signal = False; self.dma = dma; self.dsem = None; self.dval = 0; self.prev_dma = None


class Prog:
    def __init__(self, nc):
        self.nc = nc
        self.ops = {e: [] for e in ENGS}
        self.ndma = 0
        self.npool = 0
        self.dma_last = [None] * (NDMASEM + NPOOLSEM)
        self.bar = {e: [] for e in ENGS}

    def barrier(self):
        lasts = []
        for e in ENGS:
            for o in reversed(self.ops[e]):
                if not o.dma:
                    lasts.append(o)
                    break
        lasts += [p for p in self.dma_last if p is not None]
        for e in ENGS:
            self.bar[e] = list(lasts)

    def op(self, eng, fn, reads=(), writes=(), dma=False):
        o = Op(eng, fn, dma)
        deps = list(self.bar[eng])
        self.bar[eng] = []
        for r in reads:
            if r.w is not None:
                deps.append(r.w)
        for w in writes:
            if w.w is not None:
                deps.append(w.w)
            deps.extend(w.rs)
        seen = set()
        for d in deps:
            if d is o or id(d) in seen:
                continue
            seen.add(id(d))
            if d.eng == eng and not d.dma and (eng == PE or eng == SP):
                continue
            o.deps.append(d)
            if not d.dma:
                d.signal = True
        if dma:
            if eng == POOL:
                slot = NDMASEM + (self.npool % NPOOLSEM)
                o.dval = 16 * (self.npool // NPOOLSEM + 1)
                self.npool += 1
            else:
                slot = self.ndma % NDMASEM
                o.dval = 16 * (self.ndma // NDMASEM + 1)
                self.ndma += 1
            o.dsem = slot
            o.prev_dma = self.dma_last[slot]
            self.dma_last[slot] = o
        self.ops[eng].append(o)
        for r in reads:
            r.rs.append(o)
        for w in writes:
            w.w = o
            w.rs = []
        return o

    def emit(self):
        nc = self.nc
        sigval = {}
        for e in ENGS:
            c = 0
            for o in self.ops[e]:
                if o.signal and not o.dma:
                    c += 1
                    sigval[id(o)] = c
        with contextlib.ExitStack() as st:
            esem = {e: st.enter_context(nc.semaphore("s_" + e)) for e in ENGS}
            dsem = [st.enter_context(nc.semaphore("d%d" % i)) for i in range(NDMASEM + NPOOLSEM)]
            block = st.enter_context(nc.Block())
            prog = self

            def run(engname, eng):
                waited = {}

                def wait(key, sem, val):
                    if waited.get(key, 0) >= val:
                        return
                    eng.wait_ge(sem, val)
                    waited[key] = val

                for o in prog.ops[engname]:
                    for d in o.deps:
                        if d.dma:
                            wait(("d", d.dsem), dsem[d.dsem], d.dval)
                        else:
                            wait(("e", d.eng), esem[d.eng], sigval[id(d)])
                    if o.dma and o.prev_dma is not None:
                        p = o.prev_dma
                        wait(("d", p.dsem), dsem[p.dsem], p.dval)
                    ins = o.fn(eng)
                    if o.dma:
                        ins.then_inc(dsem[o.dsem], 16)
                    elif o.signal:
                        ins.then_inc(esem[engname], 1)
                if engname == SP:
                    for p in prog.dma_last:
                        if p is not None:
                            wait(("d", p.dsem), dsem[p.dsem], p.dval)

            @block.tensor
            def _(eng):
                run(PE, eng)

            @block.scalar
            def _(eng):
                run(ACT, eng)

            @block.vector
            def _(eng):
                run(DVE, eng)

            @block.gpsimd
            def _(eng):
                run(POOL, eng)

            @block.sync
            def _(eng):
                run(SP, eng)


class Arena:
    def __init__(self, nc, kb):
        self.words = kb * 256
        self.t = nc.alloc_sbuf_tensor("arena", [128, self.words], F32)
        self.off = 0
        self.marks = []

    def mark(self):
        self.marks.append(self.off)

    def release(self):
        self.off = self.marks.pop()

    def tile(self, shape, dt):
        n = 1
        for s in shape:
            n *= s
        esz = 4 if dt == F32 else (1 if dt == FP8 else 2)
        words = (n * esz + 3) // 4
        words = (words + 7) // 8 * 8
        assert self.off + words <= self.words, ("arena overflow", self.off, words, self.words)
        ap = self.t[:, self.off:self.off + words]
        self.off += words
        if dt != F32:
            ap = ap.bitcast(dt)
        ap = ap[:, 0:n]
        if len(shape) == 2:
            ap = ap.rearrange("p (a b) -> p a b", b=shape[1])
        elif len(shape) == 3:
            ap = ap.rearrange("p (a b c) -> p a b c", b=shape[1], c=shape[2])
        return ap


def build(debug=False):
    nc = bass.Bass("TRN2", target_bir_lowering=False)

    def din(name, shape, dt=F32):
        return nc.dram_tensor(name, shape, dt, kind="ExternalInput").ap()

    def dscr(name, shape, dt=BF16):
        return nc.dram_tensor(name, shape, dt, kind="ExternalOutput" if debug else "Internal").ap()

    xk = din("xk", [S, D])
    cT = din("cT", [128, 8])
    w_ada = din("w_ada", [D, 6144])
    b_adaT = din("b_adaT", [128, 48])
    gmixT = din("gmixT", [128, 8]); gffnT = din("gffnT", [128, 8]); gfinT = din("gfinT", [128, 8])
    w_in = din("w_in", [D, 5192])
    w_dwT = din("w_dwT", [128, 4, 31])
    b_dwT = din("b_dwT", [128, 4]); lngT = din("lngT", [128, 4]); lnbT = din("lnbT", [128, 4])
    w_ap = din("w_ap", [512, D]); w_cp = din("w_cp", [512, D]); w_out = din("w_out", [D, D])
    w_fi = din("w_fi", [D, 5632]); w_fo = din("w_fo", [2816, D])
    ident_d = din("ident", [128, 128], BF16)
    identf_d = din("identf", [128, 128])
    kaug_d = din("kaug", [3, 8, S], BF16)
    kbias_d = din("kbias", [1, S], BF16)
    cb_d = din("cb", [128, 4, 512], BF16)
    hvalid_d = din("hvalid", [128, 128])
    kcp1_d = din("kcp1", [128, 64])
    out_d = nc.dram_tensor("out", [2048, D], F32, kind="ExternalOutput").ap()

    w_in_b = dscr("w_in_b", [D, 5192]); w_ap_b = dscr("w_ap_b", [512, D]); w_cp_b = dscr("w_cp_b", [512, D])
    w_out_b = dscr("w_out_b", [D, D]); w_fi_b = dscr("w_fi_b", [D, 5632]); w_fo_b = dscr("w_fo_b", [2816, D])
    kT_s = dscr("kT_s", [8, 64, S]); v_s = dscr("v_s", [S, 520])
    qT_s = dscr("qT_s", [8, 64, 2048]); qiT_s = dscr("qiT_s", [4, 128, 2048])
    zT_s = dscr("zT_s", [4, 128, 4, 544]); gaT_s = dscr("gaT_s", [8, 128, 2048]); gbT_s = dscr("gbT_s", [8, 128, 2048])
    OT_s = dscr("OT_s", [4, 128, 2048])
    if debug:
        dbg_sc = nc.dram_tensor("dbg_sc", [16, 128, S], F32, kind="ExternalOutput").ap()
        dbg_thr = nc.dram_tensor("dbg_thr", [128, 16 * 4], F32, kind="ExternalOutput").ap()

    P = Prog(nc)
    A = Arena(nc, 204)
    ps_cm = nc.psum_tensor("ps", [128, 8, 512], F32)
    ps = ps_cm.__enter__()
    psb = [ps[:, b, :].bitcast(BF16) for b in range(8)]
    rb = [Res() for _ in range(8)]

    def dma(out, in_, reads=(), writes=(), eng=SP, **kw):
        return P.op(eng, lambda e: e.dma_start(out=out, in_=in_, **kw), reads=reads, writes=writes, dma=True)

    ident = A.tile([128], BF16); r_c = Res()
    identf = A.tile([128], F32)
    onesf = A.tile([128], F32)
    modT = A.tile([48], F32); r_mod = Res()
    ab = A.tile([4, 8], F32)
    gfin = A.tile([8], F32)
    wiall = A.tile([16, 8], F32); r_wi = Res()
    hval = A.tile([128], F32)
    kcp1 = A.tile([64], F32)
    dma(ident, ident_d, writes=[r_c]); dma(identf, identf_d, writes=[r_c]); dma(hval, hvalid_d, writes=[r_c])
    dma(kcp1, kcp1_d, writes=[r_c]); dma(gfin, gfinT, writes=[r_c])
    P.op(DVE, lambda e: e.memset(onesf, 1.0), writes=[r_c])

    r_cast = {}
    for name, src, dst in [("w_in", w_in, w_in_b), ("w_ap", w_ap, w_ap_b), ("w_cp", w_cp, w_cp_b), ("w_out", w_out, w_out_b),
                           ("w_fi", w_fi, w_fi_b), ("w_fo", w_fo, w_fo_b)]:
        r_cast[name] = Res()
        dma(dst, src, writes=[r_cast[name]], eng=POOL, max_dma_last_dim=4096)

    A.mark()
    kiT = A.tile([S], BF16); r_ki = Res()
    A.mark()
    cts = A.tile([8], F32); cact = A.tile([8], F32); r_ca = Res()
    bada = A.tile([48], F32); gm = A.tile([8], F32); gf = A.tile([8], F32)
    dma(cts, cT, writes=[r_ca]); dma(bada, b_adaT, writes=[r_ca]); dma(gm, gmixT, writes=[r_ca]); dma(gf, gffnT, writes=[r_ca])
    P.op(ACT, lambda e: e.activation(out=cact, in_=cts, func=AF.Silu), reads=[r_ca], writes=[r_ca])
    wa = [A.tile([8, 768], F32) for _ in range(2)]; r_wa = [Res(), Res()]
    w_ada_v = w_ada.rearrange("(k p) n -> p k n", p=128)
    r_mod2 = Res()

    def ada_piece(j, bank=0, r_dst=None):
        b = j % 2
        dma(wa[b], w_ada_v[:, :, j * 768:(j + 1) * 768], writes=[r_wa[b]])
        for m in range(6):
            col = j * 6 + m
            for kc in range(8):
                P.op(PE, lambda e, b=b, m=m, kc=kc, col=col: e.matmul(ps[:, bank, col:col + 1], lhsT=wa[b][:, kc, m * 128:(m + 1) * 128],
                                                                  rhs=cact[:, kc:kc + 1], start=(kc == 0), stop=(kc == 7)),
                     reads=[r_wa[b], r_ca], writes=[rb[bank]])
        if r_dst is not None:
            c0_, c1_ = j * 6, j * 6 + 6
            P.op(DVE, lambda e: e.tensor_tensor(out=modT[:, c0_:c1_], in0=ps[:, bank, c0_:c1_], in1=bada[:, c0_:c1_], op=ALU.add), reads=[rb[bank], r_ca], writes=[r_dst])

    for j in range(3):
        ada_piece(j)
    P.op(DVE, lambda e: e.tensor_tensor(out=modT[:, 0:18], in0=ps[:, 0, 0:18], in1=bada[:, 0:18], op=ALU.add), reads=[rb[0], r_ca], writes=[r_mod])
    P.op(DVE, lambda e: e.tensor_scalar(out=ab[:, 1, :], in0=modT[:, 8:16], scalar1=1.0, scalar2=None, op0=ALU.add), writes=[r_mod])
    P.op(DVE, lambda e: e.tensor_tensor(out=ab[:, 0, :], in0=ab[:, 1, :], in1=gm, op=ALU.mult), reads=[r_ca], writes=[r_mod])

    def ada_finish():
        P.op(DVE, lambda e: e.tensor_scalar(out=ab[:, 3, :], in0=modT[:, 32:40], scalar1=1.0, scalar2=None, op0=ALU.add), writes=[r_mod2])
        P.op(DVE, lambda e: e.tensor_tensor(out=ab[:, 2, :], in0=ab[:, 3, :], in1=gf, op=ALU.mult), reads=[r_ca], writes=[r_mod2])
    a1 = ab[:, 0, :]; b1 = modT[:, 0:8]; a2 = ab[:, 2, :]; b2 = modT[:, 24:32]; g_m = modT[:, 16:24]; g_f = modT[:, 40:48]

    def prep_front(xrows, nblk, np_, xt, r_xt, xn, r_xn, st, r_st):
        dma(xt[0:np_, 0:nblk, :], xrows.rearrange("(b p) d -> p b d", p=np_), writes=[r_xt])
        for bl in range(nblk):
            P.op(ACT, lambda e, bl=bl: e.activation(out=xn[0:np_, bl, :], in_=xt[0:np_, bl, :], func=AF.Square, accum_out=st[0:np_, bl:bl + 1]),
                 reads=[r_xt], writes=[r_xn, r_st])
        P.op(ACT, lambda e: e.activation(out=st[0:np_, 4:4 + nblk], in_=st[0:np_, 0:nblk], func=AF.Sqrt, bias=EPS, scale=1.0 / D), writes=[r_st])
        P.op(DVE, lambda e: e.reciprocal(out=st[0:np_, 8:8 + nblk], in_=st[0:np_, 4:4 + nblk]), reads=[r_st], writes=[r_st])
        for bl in range(nblk):
            P.op(DVE, lambda e, bl=bl: e.tensor_scalar(out=xn[0:np_, bl, :], in0=xt[0:np_, bl, :], scalar1=st[0:np_, 8 + bl:9 + bl], scalar2=None, op0=ALU.mult),
                 reads=[r_xt, r_st], writes=[r_xn])

    def prep_back(nblk, np_, xn, r_xn, hT, r_hTk, banks, avec, bvec):
        ntok = nblk * np_
        for kc in range(8):
            bk = banks[kc // 2]
            for bl in range(nblk):
                o = (kc % 2) * 512 + bl * np_
                P.op(PE, lambda e, kc=kc, bl=bl, bk=bk, o=o: e.transpose(out=psb[bk][:, o:o + np_], in_=xn[0:np_, bl, kc * 128:(kc + 1) * 128],
                                                                     identity=ident[0:np_, 0:np_]),
                     reads=[r_xn, r_c], writes=[rb[bk]])
            if kc % 2 == 1:
                for k2 in (kc - 1, kc):
                    o = (k2 % 2) * 512
                    eng_ = DVE
                    if eng_ == DVE:
                        P.op(DVE, lambda e, k2=k2, bk=bk, o=o: e.tensor_scalar(out=hT[:, k2, 0:ntok], in0=psb[bk][:, o:o + ntok], scalar1=avec[:, k2:k2 + 1],
                                                                           scalar2=bvec[:, k2:k2 + 1], op0=ALU.mult, op1=ALU.add),
                             reads=[rb[bk], r_mod], writes=[r_hTk[k2]])
                    else:
                        P.op(ACT, lambda e, k2=k2, bk=bk, o=o: e.activation(out=hT[:, k2, 0:ntok], in_=psb[bk][:, o:o + ntok], func=AF.Identity, scale=avec[:, k2:k2 + 1],
                                                                        bias=bvec[:, k2:k2 + 1]),
                             reads=[rb[bk], r_mod], writes=[r_hTk[k2]])

    def prep(xrows, nblk, np_, xt, r_xt, xn, r_xn, hT, r_hT, st, r_st, banks, avec, bvec):
        prep_front(xrows, nblk, np_, xt, r_xt, xn, r_xn, st, r_st)
        prep_back(nblk, np_, xn, r_xn, hT, [r_hT] * 8, banks, avec, bvec)

    def load_w(dst, src_b, rows, c0, c1, r_w, cname="w_in"):
        dma(dst, src_b.rearrange("(k p) n -> p k n", p=128)[:, :, c0:c1], reads=[r_cast[cname]], writes=[r_w])

    evac_rr = [0]

    def evac(out, in_, reads, writes, func=None, scale=1.0):
        evac_rr[0] += 1
        if func is not None or evac_rr[0] % 2 == 0:
            f = func if func is not None else AF.Copy
            return P.op(ACT, lambda e: e.activation(out=out, in_=in_, func=f, scale=scale), reads=reads, writes=writes)
        if scale != 1.0:
            return P.op(DVE, lambda e: e.tensor_scalar(out=out, in0=in_, scalar1=scale, scalar2=None, op0=ALU.mult), reads=reads, writes=writes)
        return P.op(DVE, lambda e: e.tensor_copy(out=out, in_=in_), reads=reads, writes=writes)

    A.mark()
    wk = A.tile([8, 512], BF16); wv = A.tile([8, 512], BF16); wki = A.tile([8, 128], BF16); r_w1 = Res()
    wst = [A.tile([8, 512], F32) for _ in range(2)]; r_wst = [Res(), Res()]
    w_in_v = w_in.rearrange("(k p) n -> p k n", p=128)
    dma(wst[0], w_in_v[:, :, 512:1024], writes=[r_wst[0]])
    dma(wst[1], w_in_v[:, :, 1024:1536], writes=[r_wst[1]])
    P.op(POOL, lambda e: e.tensor_copy(out=wk, in_=wst[0]), reads=[r_wst[0]], writes=[r_w1])
    P.op(DVE, lambda e: e.tensor_copy(out=wv, in_=wst[1]), reads=[r_wst[1]], writes=[r_w1])
    dma(wst[0][:, :, 0:64], w_in_v[:, :, 2048:2112], writes=[r_wst[0]])
    P.op(ACT, lambda e: e.activation(out=wki[:, :, 0:64], in_=wst[0][:, :, 0:64], func=AF.Copy), reads=[r_wst[0]], writes=[r_w1])
    P.op(ACT, lambda e: e.activation(out=wki[:, :, 64:128], in_=wst[0][:, :, 0:64], func=AF.Copy), reads=[r_wst[0]], writes=[r_w1])
    xts = [A.tile([4, D], F32) for _ in range(2)]; r_xts = [Res(), Res()]
    xns = [A.tile([4, D], BF16) for _ in range(2)]; r_xns = [Res(), Res()]
    hTs = [A.tile([8, 512], BF16) for _ in range(2)]; r_hTs = [[Res() for _ in range(8)] for _ in range(2)]
    sts = [A.tile([12], F32) for _ in range(2)]; r_sts = [Res(), Res()]
    ksts = [A.tile([4, 512], BF16) for _ in range(2)]; r_ksts = [Res(), Res()]
    vsts = [A.tile([4, 8, 65], BF16) for _ in range(2)]; r_vsts = [Res(), Res()]
    for b in range(2):
        P.op(DVE, lambda e, b=b: e.memset(vsts[b], 1.0), writes=[r_vsts[b]])
    kT_v = kT_s.rearrange("(m hh) d s -> (hh d) m s", hh=2)
    v_v = v_s.rearrange("(n p) c -> p n c", p=128)

    def front1a(T):
        b = T % 2
        prep_front(xk[T * 512:(T + 1) * 512, :], 4, 128, xts[b], r_xts[b], xns[b], r_xns[b], sts[b], r_sts[b])

    def back1a(T):
        b = T % 2
        prep_back(4, 128, xns[b], r_xns[b], hTs[b], r_hTs[b], [0, 1, 2, 3], a1, b1)

    front1a(0)
    back1a(0)
    for T in range(NT):
        b = T % 2
        hT = hTs[b]
        if T + 1 < NT:
            front1a(T + 1)
        for m in range(4):
            bk = 4 + (m % 2)
            for kc in range(8):
                P.op(PE, lambda e, m=m, kc=kc, bk=bk, hT=hT: e.matmul(ps[:, bk, :], lhsT=wk[:, kc, m * 128:(m + 1) * 128], rhs=hT[:, kc, :],
                                                                  start=(kc == 0), stop=(kc == 7)), reads=[r_w1, r_hTs[b][kc]], writes=[rb[bk]])
            evac(ksts[b][:, m, :], ps[:, bk, :], [rb[bk]], [r_ksts[b]])
        dma(kT_v[:, :, T * 512:(T + 1) * 512], ksts[b], reads=[r_ksts[b]])
        for bl in range(4):
            bk = 6 + (bl % 2)
            for kc in range(8):
                P.op(PE, lambda e, bl=bl, kc=kc, bk=bk, hT=hT: e.matmul(ps[:, bk, :], lhsT=hT[:, kc, bl * 128:(bl + 1) * 128], rhs=wv[:, kc, :],
                                                                    start=(kc == 0), stop=(kc == 7)), reads=[r_w1, r_hTs[b][kc]], writes=[rb[bk]])
            evac(vsts[b][:, bl, :, 0:64], ps[:, bk, :].rearrange("p (h d) -> p h d", d=64), [rb[bk]], [r_vsts[b]])
        dma(v_v[:, T * 4:(T + 1) * 4, :], vsts[b].rearrange("p n h c -> p n (h c)"), reads=[r_vsts[b]])
        for kc in range(8):
            P.op(PE, lambda e, kc=kc, hT=hT: e.matmul(ps[:, 4, :], lhsT=wki[:, kc, :], rhs=hT[:, kc, :], start=(kc == 0), stop=(kc == 7)),
                 reads=[r_w1, r_hTs[b][kc]], writes=[rb[4]])
        evac(kiT[:, T * 512:(T + 1) * 512], ps[:, 4, :], [rb[4]], [r_ki])
        if T + 1 < NT:
            back1a(T + 1)
        if T < 5:
            ada_piece(3 + T, bank=4, r_dst=r_mod2)
        if T == 5:
            ada_finish()
    P.barrier()
    A.release()
    A.release()

    A.mark()
    wq = A.tile([8, 512], BF16); wqi = A.tile([8, 512], BF16); wwi = A.tile([8, 8], BF16)
    wu = A.tile([8, 1024], BF16); wga = A.tile([8, 1024], BF16); wgb = A.tile([8, 1024], BF16); r_w2 = Res()
    load_w(wq, w_in_b, D, 0, 512, r_w2); load_w(wqi, w_in_b, D, 1536, 2048, r_w2); load_w(wwi, w_in_b, D, 2112, 2120, r_w2)
    load_w(wu, w_in_b, D, 2120, 3144, r_w2); load_w(wga, w_in_b, D, 3144, 4168, r_w2); load_w(wgb, w_in_b, D, 4168, 5192, r_w2)
    xt = A.tile([4, D], F32); r_xt = Res(); xn = A.tile([4, D], BF16); r_xn = Res()
    hT = A.tile([8, 512], BF16); r_hT = Res(); st = A.tile([12], F32); r_st = Res()
    xth = A.tile([1, D], F32); r_xth = Res(); xnh = A.tile([1, D], BF16); r_xnh = Res()
    hTh = A.tile([8, 32], BF16); r_hTh = Res(); sth = A.tile([12], F32); r_sth = Res()
    stg = [A.tile([512], BF16) for _ in range(3)]; r_stg = [Res() for _ in range(3)]
    sgm = [A.tile([512], F32) for _ in range(2)]; r_sgm = [Res(), Res()]
    stg_i = [0]
    qT_v = qT_s.rearrange("(m hh) d s -> (hh d) m s", hh=2)

    def stage_out(dst, src_ps, bk, func=None):
        i = stg_i[0] % 3
        stg_i[0] += 1
        evac(stg[i], src_ps, [rb[bk]], [r_stg[i]], func=func)
        dma(dst, stg[i], reads=[r_stg[i]])

    def proj_fm(w, m, hT_, r_h, bk, n):
        for kc in range(8):
            P.op(PE, lambda e, kc=kc: e.matmul(ps[:, bk, 0:n], lhsT=w[:, kc, m * 128:(m + 1) * 128], rhs=hT_[:, kc, 0:n], start=(kc == 0), stop=(kc == 7)),
                 reads=[r_w2, r_h], writes=[rb[bk]])

    for g in range(4):
        T = 4 * g + 3
        prep(xk[T * 512:(T + 1) * 512, :], 4, 128, xt, r_xt, xn, r_xn, hT, r_hT, st, r_st, [0, 1, 2, 3], a1, b1)
        cs = slice(g * 512, (g + 1) * 512)
        bkc = [0]

        def nb():
            bkc[0] += 1
            return 4 + bkc[0] % 4
        for m in range(4):
            bk = nb(); proj_fm(wq, m, hT, r_hT, bk, 512); stage_out(qT_v[:, m, cs], ps[:, bk, :], bk)
        for m in range(4):
            bk = nb(); proj_fm(wqi, m, hT, r_hT, bk, 512); stage_out(qiT_s[m, :, cs], ps[:, bk, :], bk)
        for m in range(8):
            bk = nb(); proj_fm(wga, m, hT, r_hT, bk, 512); stage_out(gaT_s[m, :, cs], ps[:, bk, :], bk, func=AF.Sigmoid)
        for m in range(8):
            bk = nb(); proj_fm(wgb, m, hT, r_hT, bk, 512); stage_out(gbT_s[m, :, cs], ps[:, bk, :], bk, func=AF.Sigmoid)
        for bl in range(4):
            bk = nb()
            for kc in range(8):
                P.op(PE, lambda e, kc=kc, bl=bl, bk=bk: e.matmul(ps[:, bk, 0:8], lhsT=hT[:, kc, bl * 128:(bl + 1) * 128], rhs=wwi[:, kc, :], start=(kc == 0), stop=(kc == 7)),
                     reads=[r_w2, r_hT], writes=[rb[bk]])
            P.op(DVE, lambda e, bl=bl, bk=bk, g=g: e.tensor_scalar(out=wiall[:, g * 4 + bl, :], in0=ps[:, bk, 0:8], scalar1=float(8 ** -0.5 * 64 ** -0.5), scalar2=None, op0=ALU.mult),
                 reads=[rb[bk]], writes=[r_wi])
        for i in range(4):
            bka = nb(); proj_fm(wu, i, hT, r_hT, bka, 512)
            bkg = nb(); proj_fm(wu, 4 + i, hT, r_hT, bkg, 512)
            si = i % 2
            P.op(ACT, lambda e, si=si, bkg=bkg: e.activation(out=sgm[si], in_=ps[:, bkg, :], func=AF.Sigmoid), reads=[rb[bkg]], writes=[r_sgm[si]])
            j = stg_i[0] % 3; stg_i[0] += 1
            P.op(DVE, lambda e, si=si, bka=bka, j=j: e.tensor_tensor(out=stg[j], in0=ps[:, bka, :], in1=sgm[si], op=ALU.mult), reads=[rb[bka], r_sgm[si]], writes=[r_stg[j]])
            dma(zT_s[i, :, g, 32:544], stg[j], reads=[r_stg[j]])
        prep(xk[T * 512 - 32:T * 512, :], 1, 32, xth, r_xth, xnh, r_xnh, hTh, r_hTh, sth, r_sth, [0, 1, 2, 3], a1, b1)
        for i in range(4):
            bka = nb(); proj_fm(wu, i, hTh, r_hTh, bka, 32)
            bkg = nb(); proj_fm(wu, 4 + i, hTh, r_hTh, bkg, 32)
            si = i % 2
            P.op(ACT, lambda e, si=si, bkg=bkg: e.activation(out=sgm[si][:, 0:32], in_=ps[:, bkg, 0:32], func=AF.Sigmoid), reads=[rb[bkg]], writes=[r_sgm[si]])
            j = stg_i[0] % 3; stg_i[0] += 1
            P.op(DVE, lambda e, si=si, bka=bka, j=j: e.tensor_tensor(out=sgm[si][:, 32:64], in0=ps[:, bka, 0:32], in1=sgm[si][:, 0:32], op=ALU.mult), reads=[rb[bka]], writes=[r_sgm[si]])
            P.op(DVE, lambda e, si=si, j=j, g=g: e.tensor_tensor(out=stg[j][:, 0:32], in0=sgm[si][:, 32:64], in1=hval[:, g * 32:(g + 1) * 32], op=ALU.mult), reads=[r_c], writes=[r_stg[j], r_sgm[si]])
            dma(zT_s[i, :, g, 0:32], stg[j][:, 0:32], reads=[r_stg[j]])
    P.barrier()
    A.release()

    A.mark()
    cbt = A.tile([4, 512], BF16); kbt = A.tile([1536], BF16); r_c2 = Res()
    dma(cbt, cb_d, writes=[r_c2]); dma(kbt[0:1, :], kbias_d[:, 0:1536], writes=[r_c2])
    onesb = A.tile([128], BF16)
    nidm = A.tile([128], BF16)
    P.op(DVE, lambda e: e.memset(onesb, 1.0), writes=[r_c2])
    P.op(DVE, lambda e: e.tensor_scalar(out=nidm, in0=ident, scalar1=-MASKB, scalar2=None, op0=ALU.mult), reads=[r_c], writes=[r_c2])
    scoress = [A.tile([S], F32) for _ in range(2)]; r_scs = [Res(), Res()]
    maskq = A.tile([S], BF16); r_mq = Res()
    junk = maskq; r_junk = r_mq
    maskTs = [A.tile([64, 256], FP8) for _ in range(2)]; r_mTs = [Res(), Res()]
    qit = A.tile([4, 256], BF16); r_qit = Res()
    qaugs = [A.tile([8, 256], BF16) for _ in range(2)]; r_qas = [Res(), Res()]
    wdgs = [A.tile([8, 128], BF16) for _ in range(2)]; r_wdgs = [Res(), Res()]
    Rt = [A.tile([2, 512], BF16) for _ in range(3)]; r_Rt = [Res() for _ in range(3)]
    bs = A.tile([16], F32); r_bs = Res()
    bsa = A.tile([4], F32); r_bsa = Res(); r_junkA = Res()
    cntk = A.tile([64], F32); r_ck = Res()
    U = A.tile([68], F32); r_U = Res()
    kt = [A.tile([8, 512], BF16) for _ in range(2)]; r_kt = [Res(), Res()]
    vt = [A.tile([4, 520], BF16) for _ in range(2)]; r_vt = [Res(), Res()]
    Pt = [A.tile([2, 256], BF16) for _ in range(3)]; r_Pt = [Res() for _ in range(3)]
    den = A.tile([16], F32); r_den = Res()
    Ob = A.tile([2, 512], BF16); r_Ob = Res()
    OTst = A.tile([4, 256], BF16); r_OT = Res()
    P.op(DVE, lambda e: e.memset(U, 0.0), writes=[r_U])
    P.op(DVE, lambda e: e.memset(U[:, 64:66], 1.0), writes=[r_U])
    for i in range(2):
        P.op(DVE, lambda e, i=i: e.memset(qaugs[i], 0.0), writes=[r_qas[i]])
    kT_hv = kT_s.rearrange("h d s -> d h s")
    kaug_v = kaug_d
    v_v2 = v_s.rearrange("(n p) c -> p n c", p=128)
    LB = 2
    SB = 7

    def idx_gen(qb):
        qp, q2 = qb // 2, qb % 2
        g = qp // 2
        E = 2048 * (g + 1)
        nch = E // 512
        c0 = g * 512 + (qp % 2) * 256
        j4 = qb % 4
        scores = scoress[qb % 2]; r_sc = r_scs[qb % 2]
        wdg = wdgs[qb % 2]; r_wdg = r_wdgs[qb % 2]
        if q2 == 0:
            dma(qit, qiT_s.rearrange("m p s -> p m s")[:, :, c0:c0 + 256], writes=[r_qit])
        for j in range(8):
            P.op(DVE, lambda e, j=j: e.tensor_scalar(out=wdg[:, j, :], in0=ident, scalar1=wiall[:, qb, j:j + 1], scalar2=None, op0=ALU.mult),
                 reads=[r_wi, r_c], writes=[r_wdg])
        yield 0.5
        units = [(c, m) for c in range(nch) for m in range(4)]

        def emit_diag(u):
            c, m = units[u]
            ks = slice(c * 512, (c + 1) * 512)
            has_kb = (c < 3)
            has_cb = (c == nch - 1)
            lastdiag = not (has_kb or has_cb)
            ri = u % 3
            for hh in range(2):
                imm = 2 * m + hh
                P.op(PE, lambda e, m=m, hh=hh, ri=ri, imm=imm, lastdiag=lastdiag: e.matmul(ps[:, SB, :], lhsT=wdg[:, 2 * m + hh, :], rhs=Rt[ri][:, hh, :], start=(imm == 0), stop=(lastdiag and imm == 7)),
                     reads=[r_wdg, r_Rt[ri]], writes=[rb[SB]])
            if m == 3:
                if has_kb:
                    P.op(PE, lambda e, ks=ks, has_cb=has_cb: e.matmul(ps[:, SB, :], lhsT=onesb[0:1, :], rhs=kbt[0:1, ks], start=False, stop=(not has_cb)),
                         reads=[r_c2], writes=[rb[SB]])
                if has_cb:
                    P.op(PE, lambda e: e.matmul(ps[:, SB, :], lhsT=ident, rhs=cbt[:, j4, :], start=False, stop=True), reads=[r_c2, r_c], writes=[rb[SB]])
                P.op(ACT, lambda e, ks=ks: e.activation(out=scores[:, ks], in_=ps[:, SB, :], func=AF.Copy), reads=[rb[SB]], writes=[r_sc])

        for u, (c, m) in enumerate(units):
            ks = slice(c * 512, (c + 1) * 512)
            for hh in range(2):
                pr = slice(hh * 64, hh * 64 + 64)
                P.op(PE, lambda e, m=m, hh=hh, pr=pr, ks=ks: e.matmul(ps[:, LB + hh, :], lhsT=qit[pr, m, q2 * 128:(q2 + 1) * 128], rhs=kiT[pr, ks],
                                                                start=True, stop=True), reads=[r_qit, r_ki], writes=[rb[LB + hh]])
            ri = u % 3
            P.op(ACT, lambda e, ri=ri: e.activation(out=Rt[ri], in_=ps[:, LB:LB + 2, :], func=AF.Relu), reads=[rb[LB], rb[LB + 1]], writes=[r_Rt[ri]])
            if u > 0:
                emit_diag(u - 1)
            yield 0.8
        emit_diag(len(units) - 1)
        if debug:
            dma(dbg_sc[qb, :, 0:E], scores[:, 0:E], reads=[r_sc])
        yield 0.5

    def bis_gen(qb):
        qp, q2 = qb // 2, qb % 2
        g = qp // 2
        E = 2048 * (g + 1)
        nkc = E // 128
        c0 = g * 512 + (qp % 2) * 256
        scores = scoress[qb % 2]; r_sc = r_scs[qb % 2]
        maskT = maskTs[qp % 2]; r_mT = r_mTs[qp % 2]
        qaug = qaugs[qp % 2]; r_qa = r_qas[qp % 2]
        if q2 == 0:
            dma(qaug[0:64, :, :], qT_s.rearrange("h d s -> d h s")[:, :, c0:c0 + 256], writes=[r_qa])
        sc = scores[:, 0:E]
        tpass = E / 960.0
        P.op(DVE, lambda e: e.tensor_reduce(out=bs[:, 0:1], in_=sc, axis=AX.X, op=ALU.max), reads=[r_sc], writes=[r_bs])
        P.op(DVE, lambda e: e.tensor_scalar(out=bs[:, 9:10], in0=bs[:, 0:1], scalar1=-1.0, scalar2=None, op0=ALU.mult), writes=[r_bs])
        P.op(DVE, lambda e: e.tensor_tensor(out=bs[:, 9:10], in0=bs[:, 9:10], in1=bs[:, 0:1], op=ALU.max), writes=[r_bs])
        P.op(DVE, lambda e: e.tensor_scalar(out=bs[:, 9:10], in0=bs[:, 9:10], scalar1=3.0, scalar2=3.0, op0=ALU.mult, op1=ALU.add), writes=[r_bs])
        P.op(DVE, lambda e: e.tensor_tensor(out=bs[:, 3:4], in0=bs[:, 0:1], in1=bs[:, 9:10], op=ALU.subtract), writes=[r_bs])
        P.op(DVE, lambda e: e.tensor_scalar(out=bs[:, 6:7], in0=bs[:, 3:4], scalar1=20000.0, scalar2=None, op0=ALU.add), writes=[r_bs])
        P.op(DVE, lambda e: e.tensor_tensor(out=bs[:, 7:8], in0=bs[:, 9:10], in1=bs[:, 6:7], op=ALU.subtract), writes=[r_bs])
        yield tpass + 1.0
        for it in range(NITER + 1):
            P.op(DVE, lambda e: e.tensor_scalar(out=junk[:, 0:E], in0=sc, scalar1=bs[:, 3:4], scalar2=None, op0=ALU.is_ge, op1=ALU.add, accum_out=bs[:, 4:5]),
                 reads=[r_sc], writes=[r_junk, r_bs])
            P.op(DVE, lambda e: e.tensor_scalar(out=bs[:, 5:6], in0=bs[:, 4:5], scalar1=255.5, scalar2=None, op0=ALU.is_ge), writes=[r_bs])
            if it == 0:
                P.op(DVE, lambda e: e.tensor_scalar(out=bs[:, 1:2], in0=bs[:, 5:6], scalar1=bs[:, 6:7], scalar2=-20000.0, op0=ALU.mult, op1=ALU.add), writes=[r_bs])
                P.op(DVE, lambda e: e.scalar_tensor_tensor(out=bs[:, 2:3], in0=bs[:, 5:6], scalar=bs[:, 7:8], in1=bs[:, 6:7], op0=ALU.mult, op1=ALU.add), writes=[r_bs])
            else:
                P.op(DVE, lambda e: e.tensor_scalar(out=bs[:, 2:3], in0=bs[:, 2:3], scalar1=0.5, scalar2=None, op0=ALU.mult), writes=[r_bs])
                P.op(DVE, lambda e: e.scalar_tensor_tensor(out=bs[:, 1:2], in0=bs[:, 5:6], scalar=bs[:, 2:3], in1=bs[:, 1:2], op0=ALU.mult, op1=ALU.add), writes=[r_bs])
            P.op(DVE, lambda e: e.scalar_tensor_tensor(out=bs[:, 3:4], in0=bs[:, 2:3], scalar=0.5, in1=bs[:, 1:2], op0=ALU.mult, op1=ALU.add), writes=[r_bs])
            yield tpass + 0.8
        if debug:
            dma(dbg_thr[:, qb * 4:qb * 4 + 4], bs[:, 0:4], reads=[r_bs])
        P.op(DVE, lambda e: e.tensor_scalar(out=maskq[:, 0:E], in0=sc, scalar1=bs[:, 1:2], scalar2=None, op0=ALU.is_ge), reads=[r_sc, r_junkA], writes=[r_mq, r_bs])
        P.op(DVE, lambda e: e.tensor_reduce(out=cntk[:, 0:nkc], in_=maskq[:, 0:E].rearrange("p (a b) -> p a b", b=128), axis=AX.X, op=ALU.add), writes=[r_ck, r_mq])
        P.op(DVE, lambda e: e.tensor_scalar(out=cntk[:, 0:nkc], in0=cntk[:, 0:nkc], scalar1=0.5, scalar2=None, op0=ALU.is_ge), writes=[r_ck])
        P.op(DVE, lambda e: e.tensor_tensor(out=cntk[:, 0:nkc], in0=cntk[:, 0:nkc], in1=kcp1[:, 0:nkc], op=ALU.mult), reads=[r_c], writes=[r_ck])
        P.op(DVE, lambda e: e.tensor_reduce(out=bs[:, 8:9], in_=cntk[:, 0:nkc], axis=AX.X, op=ALU.max), writes=[r_ck, r_bs])
        P.op(DVE, lambda e: e.tensor_scalar(out=U[:, 66:67], in0=bs[:, 8:9], scalar1=-1.0, scalar2=None, op0=ALU.add), writes=[r_U, r_bs])
        yield 2 * tpass
        P.op(PE, lambda e: e.matmul(ps[0:67, LB, 0:128], lhsT=U[:, 0:67], rhs=identf, start=True, stop=True), reads=[r_U, r_c], writes=[rb[LB]])
        for h in range(8):
            P.op(ACT, lambda e, h=h: e.activation(out=qaug[64:67, h, q2 * 128:(q2 + 1) * 128], in_=ps[64:67, LB, 0:128], func=AF.Copy), reads=[rb[LB]], writes=[r_qa])
        yield 1.0
        for k8 in range(nkc // 8):
            for kk in range(8):
                kc = k8 * 8 + kk
                P.op(PE, lambda e, kc=kc, kk=kk: e.transpose(out=psb[LB + 1][:, kk * 128:(kk + 1) * 128], in_=maskq[:, kc * 128:(kc + 1) * 128], identity=ident),
                     reads=[r_mq, r_c], writes=[rb[LB + 1]])
            mo = maskT[:, k8 * 8:(k8 + 1) * 8, q2 * 128:(q2 + 1) * 128]
            mi = psb[LB + 1].rearrange("p (a b) -> p a b", b=128)
            P.op(ACT, lambda e, mo=mo, mi=mi: e.activation(out=mo, in_=mi, func=AF.Copy, scale=-1.0, bias=1.0, saturate=False), reads=[rb[LB + 1]], writes=[r_mT])
            yield 1.0

    accb = [4, 5, 6]

    def att_gen(qp):
        g = qp // 2
        E = 2048 * (g + 1)
        nch = E // 512
        nkc = E // 128
        c0 = g * 512 + (qp % 2) * 256
        maskT = maskTs[qp % 2]; r_mT = r_mTs[qp % 2]
        qaug = qaugs[qp % 2]; r_qa = r_qas[qp % 2]
        first_in_bank = {}
        steps = [(c, kl, hp) for c in range(nch) for kl in range(4) for hp in range(4)]

        def emit_loads(c):
            kb_ = c % 2
            ks = slice(c * 512, (c + 1) * 512)
            dma(kt[kb_][0:64, :, :], kT_hv[:, :, ks], writes=[r_kt[kb_]])
            dma(kt[kb_][64:67, :, :], kaug_v[:, :, ks], writes=[r_kt[kb_]])
            dma(vt[kb_], v_v2[:, c * 4:(c + 1) * 4, :], writes=[r_vt[kb_]])

        def emit_ST(i):
            c, kl, hp = steps[i]
            kb_ = c % 2
            kc = c * 4 + kl
            sbk = i % 2
            for hh in range(2):
                h = hp * 2 + hh
                o = hh * 256
                P.op(PE, lambda e, h=h, o=o, sbk=sbk, kb_=kb_, kl=kl: e.matmul(ps[:, sbk, o:o + 256], lhsT=kt[kb_][0:67, h, kl * 128:(kl + 1) * 128],
                                                                    rhs=qaug[0:67, h, :], start=True, stop=False),
                     reads=[r_kt[kb_], r_qa], writes=[rb[sbk]])
                P.op(PE, lambda e, o=o, sbk=sbk, kc=kc: e.matmul(ps[:, sbk, o:o + 256], lhsT=nidm, rhs=maskT[:, kc, :], start=False, stop=True),
                     reads=[r_mT, r_c2], writes=[rb[sbk]])

        emit_loads(0)
        if nch > 1:
            emit_loads(1)
        emit_ST(0)
        for i, (c, kl, hp) in enumerate(steps):
            kb_ = c % 2
            kc = c * 4 + kl
            sbk = i % 2
            if i + 1 < len(steps):
                emit_ST(i + 1)
            pi = i % 3
            P.op(ACT, lambda e, pi=pi, sbk=sbk: e.activation(out=Pt[pi], in_=ps[:, sbk, :].rearrange("p (a b) -> p a b", b=256), func=AF.Exp, bias=EXPB, scale=0.125),
                 reads=[rb[sbk]], writes=[r_Pt[pi]])
            for q2 in range(2):
                for hh in range(2):
                    h = hp * 2 + hh
                    a = q2 * 8 + h
                    ab_ = accb[a // 6]
                    col = (a % 6) * 65
                    st_ = (kc == 0) and (ab_ not in first_in_bank)
                    if kc == 0:
                        first_in_bank[ab_] = True
                    P.op(PE, lambda e, pi=pi, hh=hh, q2=q2, h=h, ab_=ab_, col=col, st_=st_, kb_=kb_, kl=kl, kc=kc: e.matmul(
                        ps[:, ab_, col:col + 65], lhsT=Pt[pi][:, hh, q2 * 128:(q2 + 1) * 128], rhs=vt[kb_][:, kl, h * 65:(h + 1) * 65],
                        start=st_, stop=(kc == nkc - 1), skip_group_check=True),
                        reads=[r_Pt[pi], r_vt[kb_]], writes=[rb[ab_]])
            if kl == 3 and hp == 3 and c + 2 < nch:
                emit_loads(c + 2)
            yield 0.8
        for a in range(16):
            ab_ = accb[a // 6]; col = (a % 6) * 65
            P.op(DVE, lambda e, a=a, ab_=ab_, col=col: e.tensor_copy(out=den[:, a:a + 1], in_=ps[:, ab_, col + 64:col + 65]), reads=[rb[ab_]], writes=[r_den])
        P.op(DVE, lambda e: e.reciprocal(out=den, in_=den), writes=[r_den])
        for a in range(16):
            ab_ = accb[a // 6]; col = (a % 6) * 65
            q2 = a // 8; h = a % 8
            P.op(DVE, lambda e, a=a, ab_=ab_, col=col, q2=q2, h=h: e.tensor_scalar(out=Ob[:, q2, h * 64:(h + 1) * 64], in0=ps[:, ab_, col:col + 64], scalar1=den[:, a:a + 1], scalar2=None, op0=ALU.mult),
                 reads=[rb[ab_]], writes=[r_Ob, r_den])
        yield 3.0
        for q2 in range(2):
            for m in range(4):
                P.op(PE, lambda e, q2=q2, m=m: e.transpose(out=psb[0][:, (q2 * 4 + m) * 128:(q2 * 4 + m + 1) * 128], in_=Ob[:, q2, m * 128:(m + 1) * 128], identity=ident),
                     reads=[r_Ob, r_c], writes=[rb[0]])
        for q2 in range(2):
            P.op(ACT, lambda e, q2=q2: e.activation(out=OTst[:, :, q2 * 128:(q2 + 1) * 128], in_=psb[0][:, q2 * 512:(q2 + 1) * 512].rearrange("p (a b) -> p a b", b=128), func=AF.Copy),
                 reads=[rb[0]], writes=[r_OT])
        dma(OT_s.rearrange("m p s -> p m s")[:, :, c0:c0 + 256], OTst, reads=[r_OT])
        yield 1.0

    NQB, NPR = 16, 8
    done = {"IDX": 0, "BIS": 0, "ATT": 0}
    nitems = {"IDX": NQB, "BIS": NQB, "ATT": NPR}
    mk = {"IDX": idx_gen, "BIS": bis_gen, "ATT": att_gen}
    cur = {"IDX": None, "BIS": None, "ATT": None}
    clk = {"IDX": 0.0, "BIS": 0.0, "ATT": 0.0}
    now = [0.0]

    def can_start(s):
        i = done[s]
        if i >= nitems[s]:
            return False
        if s == "IDX":
            return done["BIS"] >= i - 1
        if s == "BIS":
            return done["IDX"] > i and done["ATT"] >= i // 2 - 1
        return done["BIS"] > 2 * i + 1

    while any(done[s] < nitems[s] for s in done):
        cands = []
        for s in ("ATT", "IDX", "BIS"):
            if cur[s] is None and can_start(s):
                cur[s] = mk[s](done[s])
                clk[s] = max(clk[s], now[0])
            if cur[s] is not None:
                cands.append(s)
        assert cands, ("scheduler deadlock", done)
        s = min(cands, key=lambda k: clk[k])
        now[0] = clk[s]
        cost = next(cur[s], None)
        if cost is None:
            cur[s] = None
            done[s] += 1
        else:
            clk[s] += cost
    P.barrier()
    A.release()
    A.release()

    A.mark()
    wap = A.tile([4, D], BF16); wcp = A.tile([4, D], BF16); wo = A.tile([8, D], BF16); r_w3 = Res()
    load_w(wap, w_ap_b, 512, 0, D, r_w3, "w_ap"); load_w(wcp, w_cp_b, 512, 0, D, r_w3, "w_cp"); load_w(wo, w_out_b, D, 0, D, r_w3, "w_out")
    wfo_t = [A.tile([22, 128], BF16) for _ in range(2)]; r_wfo = [Res(), Res()]
    wdw = A.tile([4, 31], F32); cvp = A.tile([3, 4], F32); r_cv = Res()
    dma(wdw, w_dwT, writes=[r_cv]); dma(cvp[:, 0, :], b_dwT, writes=[r_cv]); dma(cvp[:, 1, :], lngT, writes=[r_cv]); dma(cvp[:, 2, :], lnbT, writes=[r_cv])
    wdd = A.tile([31, 128], BF16); r_wdd = Res()
    xtb = [A.tile([D], F32) for _ in range(2)]; r_xtb = [Res(), Res()]
    xTs = [A.tile([8, 512], F32) for _ in range(2)]; r_xTs = [Res(), Res()]
    OT = A.tile([4, 512], BF16); zT = A.tile([4, 544], BF16); r_in3 = Res()
    gab = [A.tile([2, 512], BF16) for _ in range(2)]; r_gab = [Res(), Res()]
    cv = A.tile([4, 512], F32); r_cvo = Res()
    sq = A.tile([512], F32); r_sq = Res()
    stt = A.tile([3, 512], F32); r_stt = Res()
    zc = A.tile([4, 512], BF16); r_zc = Res()
    t1 = A.tile([512], F32); r_t1 = Res()
    mg = A.tile([8, 512], BF16); r_mg = Res()
    h2s = [A.tile([8, 512], BF16) for _ in range(2)]; r_h2s = [Res(), Res()]
    gT = A.tile([22, 512], BF16); r_gT = Res()
    wfi_t = [A.tile([8, 256], BF16) for _ in range(2)]; r_wfi = [Res() for _ in range(2)]
    sl = [A.tile([512], F32) for _ in range(2)]; r_sl = [Res(), Res()]
    sq2 = A.tile([512], F32); r_sq2 = Res()
    rs2 = A.tile([512], F32); r_rs2 = Res()
    yo = A.tile([D], F32); r_yo = Res()
    w_fo_v = w_fo_b.rearrange("(k p) n -> p k n", p=128)
    w_fi_v = w_fi_b.rearrange("(k p) n -> p k n", p=128)

    def rstd_sumsq(n, src_fn, nchunks, sqt, r_sqt, bank, dst, r_dst, rd):
        for m in range(nchunks):
            P.op(ACT, lambda e, m=m: e.activation(out=sqt, in_=src_fn(m), func=AF.Square), reads=rd, writes=[r_sqt])
            P.op(PE, lambda e, m=m: e.matmul(ps[:, bank, :], lhsT=onesf, rhs=sqt, start=(m == 0), stop=(m == nchunks - 1)), reads=[r_sqt, r_c], writes=[rb[bank]])
        P.op(ACT, lambda e: e.activation(out=dst, in_=ps[:, bank, :], func=AF.Sqrt, bias=EPS, scale=1.0 / n), reads=[rb[bank]], writes=[r_dst])
        P.op(DVE, lambda e: e.reciprocal(out=dst, in_=dst), writes=[r_dst])

    def front_gen(g):
        T = 4 * g + 3
        cs = slice(g * 512, (g + 1) * 512)
        xT = xTs[g % 2]; r_xT = r_xTs[g % 2]
        h2 = h2s[g % 2]; r_h2 = r_h2s[g % 2]
        dma(OT, OT_s.rearrange("m p s -> p m s")[:, :, cs], writes=[r_in3])
        dma(zT, zT_s[:, :, g, :].rearrange("m p s -> p m s"), writes=[r_in3])
        for bl in range(4):
            xb_ = xtb[bl % 2]; rxb = r_xtb[bl % 2]
            dma(xb_, xk[T * 512 + bl * 128:T * 512 + (bl + 1) * 128, :], writes=[rxb])
            for half in range(2):
                bk = half
                for k4 in range(4):
                    kc = half * 4 + k4
                    P.op(PE, lambda e, kc=kc, k4=k4, bk=bk, xb_=xb_: e.transpose(out=ps[:, bk, k4 * 128:(k4 + 1) * 128], in_=xb_[:, kc * 128:(kc + 1) * 128], identity=identf),
                         reads=[rxb, r_c], writes=[rb[bk]])
                evac(xT[:, half * 4:half * 4 + 4, bl * 128:(bl + 1) * 128], ps[:, bk, :].rearrange("p (a b) -> p a b", b=128), [rb[bk]], [r_xT])
            yield 3.0
        for i in range(4):
            bk = i % 2
            for k in range(31):
                P.op(DVE, lambda e, i=i, k=k: e.tensor_scalar(out=wdd[:, k, :], in0=ident, scalar1=wdw[:, i, k:k + 1], scalar2=None, op0=ALU.mult),
                     reads=[r_cv, r_c], writes=[r_wdd])
            for k in range(31):
                P.op(PE, lambda e, i=i, k=k, bk=bk: e.matmul(ps[:, bk, :], lhsT=wdd[:, k, :], rhs=zT[:, i, k + 2:k + 514], start=(k == 0), stop=(k == 30)),
                     reads=[r_wdd, r_in3], writes=[rb[bk]])
            P.op(DVE, lambda e, i=i, bk=bk: e.tensor_scalar(out=cv[:, i, :], in0=ps[:, bk, :], scalar1=cvp[:, 0, i:i + 1], scalar2=None, op0=ALU.add),
                 reads=[rb[bk], r_cv], writes=[r_cvo])
            yield 7.0
        for i in range(4):
            P.op(PE, lambda e, i=i: e.matmul(ps[:, 2, :], lhsT=onesf, rhs=cv[:, i, :], start=(i == 0), stop=(i == 3)), reads=[r_cvo, r_c], writes=[rb[2]])
        P.op(ACT, lambda e: e.activation(out=stt[:, 0, :], in_=ps[:, 2, :], func=AF.Copy, scale=1.0 / 512), reads=[rb[2]], writes=[r_stt])
        for i in range(4):
            P.op(DVE, lambda e, i=i: e.tensor_tensor(out=cv[:, i, :], in0=cv[:, i, :], in1=stt[:, 0, :], op=ALU.subtract), reads=[r_stt], writes=[r_cvo])
        yield 4.0
        rstd_sumsq(512, lambda m: cv[:, m, :], 4, sq, r_sq, 3, stt[:, 2, :], r_stt, [r_cvo])
        yield 4.0
        for i in range(4):
            P.op(DVE, lambda e, i=i: e.tensor_tensor(out=cv[:, i, :], in0=cv[:, i, :], in1=stt[:, 2, :], op=ALU.mult), reads=[r_stt], writes=[r_cvo])
            P.op(DVE, lambda e, i=i: e.tensor_scalar(out=cv[:, i, :], in0=cv[:, i, :], scalar1=cvp[:, 1, i:i + 1], scalar2=cvp[:, 2, i:i + 1], op0=ALU.mult, op1=ALU.add),
                 reads=[r_cv], writes=[r_cvo])
            P.op(ACT, lambda e, i=i: e.activation(out=zc[:, i, :], in_=cv[:, i, :], func=AF.Silu), reads=[r_cvo], writes=[r_zc])
        yield 4.0
        for m in range(8):
            b0 = (2 * m) % 4; b1_ = (2 * m + 1) % 4
            gb_ = gab[m % 2]; rg = r_gab[m % 2]
            dma(gb_[:, 0, :], gaT_s[m, :, cs], writes=[rg])
            dma(gb_[:, 1, :], gbT_s[m, :, cs], writes=[rg])
            for k in range(4):
                P.op(PE, lambda e, m=m, k=k, b0=b0: e.matmul(ps[:, b0, :], lhsT=wap[:, k, m * 128:(m + 1) * 128], rhs=OT[:, k, :], start=(k == 0), stop=(k == 3)),
                     reads=[r_w3, r_in3], writes=[rb[b0]])
            for k in range(4):
                P.op(PE, lambda e, m=m, k=k, b1_=b1_: e.matmul(ps[:, b1_, :], lhsT=wcp[:, k, m * 128:(m + 1) * 128], rhs=zc[:, k, :], start=(k == 0), stop=(k == 3)),
                     reads=[r_w3, r_zc], writes=[rb[b1_]])
            P.op(DVE, lambda e, b0=b0, gb_=gb_: e.tensor_tensor(out=t1, in0=ps[:, b0, :], in1=gb_[:, 0, :], op=ALU.mult), reads=[rb[b0], rg], writes=[r_t1])
            P.op(DVE, lambda e, b1_=b1_, gb_=gb_: e.tensor_tensor(out=sq, in0=ps[:, b1_, :], in1=gb_[:, 1, :], op=ALU.mult), reads=[rb[b1_], rg], writes=[r_sq])
            P.op(DVE, lambda e, m=m: e.tensor_tensor(out=mg[:, m, :], in0=t1, in1=sq, op=ALU.add), reads=[r_t1, r_sq], writes=[r_mg])
            yield 2.0
        for m in range(8):
            bk = m % 4
            for k in range(8):
                P.op(PE, lambda e, m=m, k=k, bk=bk: e.matmul(ps[:, bk, :], lhsT=wo[:, k, m * 128:(m + 1) * 128], rhs=mg[:, k, :], start=(k == 0), stop=(k == 7)),
                     reads=[r_w3, r_mg], writes=[rb[bk]])
            P.op(DVE, lambda e, m=m, bk=bk: e.scalar_tensor_tensor(out=xT[:, m, :], in0=ps[:, bk, :], scalar=g_m[:, m:m + 1], in1=xT[:, m, :], op0=ALU.mult, op1=ALU.add),
                 reads=[rb[bk], r_mod2], writes=[r_xT])
            yield 2.0
        rstd_sumsq(D, lambda m: xT[:, m, :], 8, sq, r_sq, 3, stt[:, 2, :], r_stt, [r_xT])
        yield 6.0
        for m in range(8):
            P.op(DVE, lambda e, m=m: e.tensor_tensor(out=t1, in0=xT[:, m, :], in1=stt[:, 2, :], op=ALU.mult), reads=[r_xT, r_stt], writes=[r_t1])
            P.op(DVE, lambda e, m=m: e.tensor_scalar(out=h2[:, m, :], in0=t1, scalar1=a2[:, m:m + 1], scalar2=b2[:, m:m + 1], op0=ALU.mult, op1=ALU.add),
                 reads=[r_mod2], writes=[r_h2, r_t1])
        yield 6.0

    def ffn_gen(g):
        xT = xTs[g % 2]; r_xT = r_xTs[g % 2]
        h2 = h2s[g % 2]; r_h2 = r_h2s[g % 2]
        for i in range(22):
            wb_ = i % 2
            dma(wfi_t[wb_][:, :, 0:128], w_fi_v[:, :, i * 128:(i + 1) * 128], reads=[r_cast["w_fi"]], writes=[r_wfi[wb_]])
            dma(wfi_t[wb_][:, :, 128:256], w_fi_v[:, :, 2816 + i * 128:2816 + (i + 1) * 128], reads=[r_cast["w_fi"]], writes=[r_wfi[wb_]])
            b0 = 4 + (2 * i) % 4; b1_ = 4 + (2 * i + 1) % 4
            for k in range(8):
                P.op(PE, lambda e, k=k, wb_=wb_, b0=b0: e.matmul(ps[:, b0, :], lhsT=wfi_t[wb_][:, k, 0:128], rhs=h2[:, k, :], start=(k == 0), stop=(k == 7)),
                     reads=[r_wfi[wb_], r_h2], writes=[rb[b0]])
            for k in range(8):
                P.op(PE, lambda e, k=k, wb_=wb_, b1_=b1_: e.matmul(ps[:, b1_, :], lhsT=wfi_t[wb_][:, k, 128:256], rhs=h2[:, k, :], start=(k == 0), stop=(k == 7)),
                     reads=[r_wfi[wb_], r_h2], writes=[rb[b1_]])
            si = i % 2
            P.op(ACT, lambda e, si=si, b0=b0: e.activation(out=sl[si], in_=ps[:, b0, :], func=AF.Silu), reads=[rb[b0]], writes=[r_sl[si]])
            P.op(DVE, lambda e, si=si, b1_=b1_, i=i: e.tensor_tensor(out=gT[:, i, :], in0=ps[:, b1_, :], in1=sl[si], op=ALU.mult), reads=[rb[b1_], r_sl[si]], writes=[r_gT])
            yield 3.5
        for m in range(8):
            bk = 4 + m % 4
            wf = wfo_t[m % 2]; rwf = r_wfo[m % 2]
            dma(wf, w_fo_v[:, :, m * 128:(m + 1) * 128], reads=[r_cast["w_fo"]], writes=[rwf])
            for i in range(22):
                P.op(PE, lambda e, i=i, bk=bk, wf=wf: e.matmul(ps[:, bk, :], lhsT=wf[:, i, :], rhs=gT[:, i, :], start=(i == 0), stop=(i == 21)),
                     reads=[rwf, r_gT], writes=[rb[bk]])
            P.op(DVE, lambda e, m=m, bk=bk: e.scalar_tensor_tensor(out=xT[:, m, :], in0=ps[:, bk, :], scalar=g_f[:, m:m + 1], in1=xT[:, m, :], op0=ALU.mult, op1=ALU.add),
                 reads=[rb[bk], r_mod2], writes=[r_xT])
            yield 4.8
        rstd_sumsq(D, lambda m: xT[:, m, :], 8, sq2, r_sq2, 7, rs2, r_rs2, [r_xT])
        yield 6.0
        for m in range(8):
            P.op(DVE, lambda e, m=m: e.scalar_tensor_tensor(out=xT[:, m, :], in0=xT[:, m, :], scalar=gfin[:, m:m + 1], in1=rs2, op0=ALU.mult, op1=ALU.mult),
                 reads=[r_rs2, r_c], writes=[r_xT])
        yield 4.0
        for bl in range(4):
            for m in range(8):
                bk = 4 + (m // 4 + 2 * bl) % 4
                P.op(PE, lambda e, m=m, bl=bl, bk=bk: e.transpose(out=ps[:, bk, (m % 4) * 128:(m % 4 + 1) * 128], in_=xT[:, m, bl * 128:(bl + 1) * 128], identity=identf),
                     reads=[r_xT, r_c], writes=[rb[bk]])
                if m % 4 == 3:
                    evac(yo[:, (m // 4) * 512:(m // 4 + 1) * 512], ps[:, bk, :], [rb[bk]], [r_yo])
            dma(out_d[g * 512 + bl * 128:g * 512 + (bl + 1) * 128, :], yo, reads=[r_yo])
            yield 2.0

    done3 = {"F": 0, "N": 0}
    mk3 = {"F": front_gen, "N": ffn_gen}
    cur3 = {"F": None, "N": None}
    clk3 = {"F": 0.0, "N": 0.0}
    now3 = [0.0]

    def can_start3(s):
        i = done3[s]
        if i >= 4:
            return False
        if s == "F":
            return done3["N"] >= i - 1
        return done3["F"] > i

    while done3["F"] < 4 or done3["N"] < 4:
        cands = []
        for s in ("N", "F"):
            if cur3[s] is None and can_start3(s):
                cur3[s] = mk3[s](done3[s])
                clk3[s] = max(clk3[s], now3[0])
            if cur3[s] is not None:
                cands.append(s)
        assert cands, ("phase-3 scheduler deadlock", done3)
        s = min(cands, key=lambda k: clk3[k])
        now3[0] = clk3[s]
        cost = next(cur3[s], None)
        if cost is None:
            cur3[s] = None
            done3[s] += 1
        else:
            clk3[s] += cost
    A.release()
    P.emit()
    return nc


_NC_CACHE = {}


def _consts():
    ident = np.eye(128, dtype=np.float32)
    slopes = np.exp2(-np.arange(1, 9, dtype=np.float64))
    s = np.arange(S)
    kaug = np.zeros((3, 8, S), np.float32)
    for h in range(8):
        kaug[0, h] = 8.0 * slopes[h] * (s % 128)
        kaug[1, h] = 8.0 * slopes[h] * 128.0 * (s // 128)
        kaug[2, h] = -8.0 * 128.0 * slopes[h]
    cb = np.zeros((128, 4, 512), np.float32)
    p = np.arange(128)[:, None]
    sp = np.arange(512)[None, :]
    for j4 in range(4):
        cb[:, j4, :] = np.where(sp <= 128 * j4 + p, 0.0, NEG)
    kcp1 = np.tile(np.arange(1, 65, dtype=np.float32)[None, :], (128, 1))
    return ident, kaug, cb, kcp1


def _in_maps(inputs):
    x = np.asarray(inputs["x"], np.float32)
    c = np.asarray(inputs["c"], np.float32)
    bf = ml_dtypes.bfloat16
    ident, kaug, cb, kcp1 = _consts()

    def fm(v, n):
        return np.ascontiguousarray(np.asarray(v, np.float32).reshape(n, 128).T)

    shared = {
        "w_ada": np.ascontiguousarray(inputs["w_ada"][0], np.float32),
        "b_adaT": fm(inputs["b_ada"][0], 48),
        "gmixT": fm(inputs["norm_mix_g"][0], 8), "gffnT": fm(inputs["norm_ffn_g"][0], 8), "gfinT": fm(inputs["norm_final_g"], 8),
        "w_in": np.ascontiguousarray(inputs["w_in"][0], np.float32),
        "w_dwT": np.ascontiguousarray(np.asarray(inputs["w_dw"][0][:, 0, :], np.float32).T.reshape(4, 128, 31).transpose(1, 0, 2)),
        "b_dwT": fm(inputs["b_dw"][0], 4), "lngT": fm(inputs["conv_ln_g"][0], 4), "lnbT": fm(inputs["conv_ln_b"][0], 4),
        "w_ap": np.ascontiguousarray(inputs["w_attn_proj"][0], np.float32), "w_cp": np.ascontiguousarray(inputs["w_conv_proj"][0], np.float32),
        "w_out": np.ascontiguousarray(inputs["w_out"][0], np.float32),
        "w_fi": np.ascontiguousarray(inputs["w_ffn_in"][0], np.float32), "w_fo": np.ascontiguousarray(inputs["w_ffn_out"][0], np.float32),
        "ident": ident.astype(bf), "identf": ident, "kaug": kaug.astype(bf), "cb": cb.astype(bf), "kcp1": kcp1,
    }
    maps = []
    for core in range(8):
        b, r = core // 4, core % 4
        pad = 1536 - 512 * r
        xk = np.zeros((S, D), np.float32)
        xk[pad:] = x[b, :S - pad]
        kbias = np.zeros((1, S), np.float32)
        kbias[0, :pad] = NEG
        hv = np.zeros((128, 128), np.float32)
        for g in range(4):
            pos = 2048 * g + 1504 + np.arange(32)
            hv[:, g * 32:(g + 1) * 32] = (pos >= pad).astype(np.float32)[None, :]
        m = dict(shared)
        m.update({"xk": xk, "cT": fm(c[b], 8), "kbias": kbias.astype(bf), "hvalid": hv})
        maps.append(m)
    return maps


def kernel(**inputs):
    if "nc" not in _NC_CACHE:
        _NC_CACHE["nc"] = build(False)
    nc = _NC_CACHE["nc"]
    maps = _in_maps(inputs)
    res = run_bass_kernel_spmd(nc, maps, core_ids=list(range(8)))
    out = np.zeros((2, S, D), np.float32)
    for core in range(8):
        b, r = core // 4, core % 4
        o = np.asarray(res.results[core]["out"], np.float32)
        for g in range(4):
            p0 = 2048 * g + 512 * r
            out[b, p0:p0 + 512] = o[g * 512:(g + 1) * 512]
    return out
```

```python
import contextlib
import numpy as np
import ml_dtypes
import concourse.bass as bass
import concourse.mybir as mybir
from concourse.bass_utils import run_bass_kernel_spmd

F32 = mybir.dt.float32
BF16 = mybir.dt.bfloat16
FP8 = mybir.dt.float8e4
AF = mybir.ActivationFunctionType
ALU = mybir.AluOpType
AX = mybir.AxisListType

PE, ACT, DVE, POOL, SP = "tensor", "scalar", "vector", "gpsimd", "sync"
ENGS = [PE, ACT, DVE, POOL, SP]
NDMASEM = 32
NPOOLSEM = 6

S = 8192
D = 1024
NT = 16
EPS = 1e-6
NEG = -30000.0
NITER = 14
EXPB = -16.0
MASKB = 240000.0
ACT_SHARE = 0.0


class Res:
    __slots__ = ("w", "rs")

    def __init__(self):
        self.w = None
        self.rs = []


class Op:
    __slots__ = ("eng", "fn", "deps", "signal", "dma", "dsem", "dval", "prev_dma")

    def __init__(self, eng, fn, dma):
        self.eng = eng; self.fn = fn; self.deps = []
        self.signal = False; self.dma = dma; self.dsem = None; self.dval = 0; self.prev_dma = None


class Prog:
    def __init__(self, nc):
        self.nc = nc
        self.ops = {e: [] for e in ENGS}
        self.ndma = 0
        self.npool = 0
        self.dma_last = [None] * (NDMASEM + NPOOLSEM)
        self.bar = {e: [] for e in ENGS}

    def barrier(self):
        lasts = []
        for e in ENGS:
            for o in reversed(self.ops[e]):
                if not o.dma:
                    lasts.append(o)
                    break
        lasts += [p for p in self.dma_last if p is not None]
        for e in ENGS:
            self.bar[e] = list(lasts)

    def op(self, eng, fn, reads=(), writes=(), dma=False):
        o = Op(eng, fn, dma)
        deps = list(self.bar[eng])
        self.bar[eng] = []
        for r in reads:
            if r.w is not None:
                deps.append(r.w)
        for w in writes:
            if w.w is not None:
                deps.append(w.w)
            deps.extend(w.rs)
        seen = set()
        for d in deps:
            if d is o or id(d) in seen:
                continue
            seen.add(id(d))
            if d.eng == eng and not d.dma and (eng == PE or eng == SP):
                continue
            o.deps.append(d)
            if not d.dma:
                d.signal = True
        if dma:
            if eng == POOL:
                slot = NDMASEM + (self.npool % NPOOLSEM)
                o.dval = 16 * (self.npool // NPOOLSEM + 1)
                self.npool += 1
            else:
                slot = self.ndma % NDMASEM
                o.dval = 16 * (self.ndma // NDMASEM + 1)
                self.ndma += 1
            o.dsem = slot
            o.prev_dma = self.dma_last[slot]
            self.dma_last[slot] = o
        self.ops[eng].append(o)
        for r in reads:
            r.rs.append(o)
        for w in writes:
            w.w = o
            w.rs = []
        return o

    def emit(self):
        nc = self.nc
        sigval = {}
        for e in ENGS:
            c = 0
            for o in self.ops[e]:
                if o.signal and not o.dma:
                    c += 1
                    sigval[id(o)] = c
        with contextlib.ExitStack() as st:
            esem = {e: st.enter_context(nc.semaphore("s_" + e)) for e in ENGS}
            dsem = [st.enter_context(nc.semaphore("d%d" % i)) for i in range(NDMASEM + NPOOLSEM)]
            block = st.enter_context(nc.Block())
            prog = self

            def run(engname, eng):
                waited = {}

                def wait(key, sem, val):
                    if waited.get(key, 0) >= val:
                        return
                    eng.wait_ge(sem, val)
                    waited[key] = val

                for o in prog.ops[engname]:
                    for d in o.deps:
                        if d.dma:
                            wait(("d", d.dsem), dsem[d.dsem], d.dval)
                        else:
                            wait(("e", d.eng), esem[d.eng], sigval[id(d)])
                    if o.dma and o.prev_dma is not None:
                        p = o.prev_dma
                        wait(("d", p.dsem), dsem[p.dsem], p.dval)
                    ins = o.fn(eng)
                    if o.dma:
                        ins.then_inc(dsem[o.dsem], 16)
                    elif o.signal:
                        ins.then_inc(esem[engname], 1)
                if engname == SP:
                    for p in prog.dma_last:
                        if p is not None:
                            wait(("d", p.dsem), dsem[p.dsem], p.dval)

            @block.tensor
            def _(eng):
                run(PE, eng)

            @block.scalar
            def _(eng):
                run(ACT, eng)

            @block.vector
            def _(eng):
                run(DVE, eng)

            @block.gpsimd
            def _(eng):
                run(POOL, eng)

            @block.sync
            def _(eng):
                run(SP, eng)


class Arena:
    def __init__(self, nc, kb):
        self.words = kb * 256
        self.t = nc.alloc_sbuf_tensor("arena", [128, self.words], F32)
        self.off = 0
        self.marks = []

    def mark(self):
        self.marks.append(self.off)

    def release(self):
        self.off = self.marks.pop()

    def tile(self, shape, dt):
        n = 1
        for s in shape:
            n *= s
        esz = 4 if dt == F32 else (1 if dt == FP8 else 2)
        words = (n * esz + 3) // 4
        words = (words + 7) // 8 * 8
        assert self.off + words <= self.words, ("arena overflow", self.off, words, self.words)
        ap = self.t[:, self.off:self.off + words]
        self.off += words
        if dt != F32:
            ap = ap.bitcast(dt)
        ap = ap[:, 0:n]
        if len(shape) == 2:
            ap = ap.rearrange("p (a b) -> p a b", b=shape[1])
        elif len(shape) == 3:
            ap = ap.rearrange("p (a b c) -> p a b c", b=shape[1], c=shape[2])
        return ap


def build(debug=False):
    nc = bass.Bass("TRN2", target_bir_lowering=False)

    def din(name, shape, dt=F32):
        return nc.dram_tensor(name, shape, dt, kind="ExternalInput").ap()

    def dscr(name, shape, dt=BF16):
        return nc.dram_tensor(name, shape, dt, kind="ExternalOutput" if debug else "Internal").ap()

    xk = din("xk", [S, D])
    cT = din("cT", [128, 8])
    w_ada = din("w_ada", [D, 6144])
    b_adaT = din("b_adaT", [128, 48])
    gmixT = din("gmixT", [128, 8]); gffnT = din("gffnT", [128, 8]); gfinT = din("gfinT", [128, 8])
    w_in = din("w_in", [D, 5192])
    w_dwT = din("w_dwT", [128, 4, 31])
    b_dwT = din("b_dwT", [128, 4]); lngT = din("lngT", [128, 4]); lnbT = din("lnbT", [128, 4])
    w_ap = din("w_ap", [512, D]); w_cp = din("w_cp", [512, D]); w_out = din("w_out", [D, D])
    w_fi = din("w_fi", [D, 5632]); w_fo = din("w_fo", [2816, D])
    ident_d = din("ident", [128, 128], BF16)
    identf_d = din("identf", [128, 128])
    kaug_d = din("kaug", [3, 8, S], BF16)
    kbias_d = din("kbias", [1, S], BF16)
    cb_d = din("cb", [128, 4, 512], BF16)
    hvalid_d = din("hvalid", [128, 128])
    kcp1_d = din("kcp1", [128, 64])
    out_d = nc.dram_tensor("out", [2048, D], F32, kind="ExternalOutput").ap()

    w_in_b = dscr("w_in_b", [D, 5192]); w_ap_b = dscr("w_ap_b", [512, D]); w_cp_b = dscr("w_cp_b", [512, D])
    w_out_b = dscr("w_out_b", [D, D]); w_fi_b = dscr("w_fi_b", [D, 5632]); w_fo_b = dscr("w_fo_b", [2816, D])
    kT_s = dscr("kT_s", [8, 64, S]); v_s = dscr("v_s", [S, 520])
    qT_s = dscr("qT_s", [8, 64, 2048]); qiT_s = dscr("qiT_s", [4, 128, 2048])
    zT_s = dscr("zT_s", [4, 128, 4, 544]); gaT_s = dscr("gaT_s", [8, 128, 2048]); gbT_s = dscr("gbT_s", [8, 128, 2048])
    OT_s = dscr("OT_s", [4, 128, 2048])
    if debug:
        dbg_sc = nc.dram_tensor("dbg_sc", [16, 128, S], F32, kind="ExternalOutput").ap()
        dbg_thr = nc.dram_tensor("dbg_thr", [128, 16 * 4], F32, kind="ExternalOutput").ap()

    P = Prog(nc)
    A = Arena(nc, 204)
    ps_cm = nc.psum_tensor("ps", [128, 8, 512], F32)
    ps = ps_cm.__enter__()
    psb = [ps[:, b, :].bitcast(BF16) for b in range(8)]
    rb = [Res() for _ in range(8)]

    def dma(out, in_, reads=(), writes=(), eng=SP, **kw):
        return P.op(eng, lambda e: e.dma_start(out=out, in_=in_, **kw), reads=reads, writes=writes, dma=True)

    ident = A.tile([128], BF16); r_c = Res()
    identf = A.tile([128], F32)
    onesf = A.tile([128], F32)
    modT = A.tile([48], F32); r_mod = Res()
    ab = A.tile([4, 8], F32)
    gfin = A.tile([8], F32)
    wiall = A.tile([16, 8], F32); r_wi = Res()
    hval = A.tile([128], F32)
    kcp1 = A.tile([64], F32)
    dma(ident, ident_d, writes=[r_c]); dma(identf, identf_d, writes=[r_c]); dma(hval, hvalid_d, writes=[r_c])
    dma(kcp1, kcp1_d, writes=[r_c]); dma(gfin, gfinT, writes=[r_c])
    P.op(DVE, lambda e: e.memset(onesf, 1.0), writes=[r_c])

    r_cast = {}
    for name, src, dst in [("w_in", w_in, w_in_b), ("w_ap", w_ap, w_ap_b), ("w_cp", w_cp, w_cp_b), ("w_out", w_out, w_out_b),
                           ("w_fi", w_fi, w_fi_b), ("w_fo", w_fo, w_fo_b)]:
        r_cast[name] = Res()
        dma(dst, src, writes=[r_cast[name]], eng=POOL, max_dma_last_dim=4096)

    A.mark()
    kiT = A.tile([S], BF16); r_ki = Res()
    A.mark()
    cts = A.tile([8], F32); cact = A.tile([8], F32); r_ca = Res()
    bada = A.tile([48], F32); gm = A.tile([8], F32); gf = A.tile([8], F32)
    dma(cts, cT, writes=[r_ca]); dma(bada, b_adaT, writes=[r_ca]); dma(gm, gmixT, writes=[r_ca]); dma(gf, gffnT, writes=[r_ca])
    P.op(ACT, lambda e: e.activation(out=cact, in_=cts, func=AF.Silu), reads=[r_ca], writes=[r_ca])
    wa = [A.tile([8, 768], F32) for _ in range(2)]; r_wa = [Res(), Res()]
    w_ada_v = w_ada.rearrange("(k p) n -> p k n", p=128)
    r_mod2 = Res()

    def ada_piece(j, bank=0, r_dst=None):
        b = j % 2
        dma(wa[b], w_ada_v[:, :, j * 768:(j + 1) * 768], writes=[r_wa[b]])
        for m in range(6):
            col = j * 6 + m
            for kc in range(8):
                P.op(PE, lambda e, b=b, m=m, kc=kc, col=col: e.matmul(ps[:, bank, col:col + 1], lhsT=wa[b][:, kc, m * 128:(m + 1) * 128],
                                                                  rhs=cact[:, kc:kc + 1], start=(kc == 0), stop=(kc == 7)),
                     reads=[r_wa[b], r_ca], writes=[rb[bank]])
        if r_dst is not None:
            c0_, c1_ = j * 6, j * 6 + 6
            P.op(DVE, lambda e: e.tensor_tensor(out=modT[:, c0_:c1_], in0=ps[:, bank, c0_:c1_], in1=bada[:, c0_:c1_], op=ALU.add), reads=[rb[bank], r_ca], writes=[r_dst])

    for j in range(3):
        ada_piece(j)
    P.op(DVE, lambda e: e.tensor_tensor(out=modT[:, 0:18], in0=ps[:, 0, 0:18], in1=bada[:, 0:18], op=ALU.add), reads=[rb[0], r_ca], writes=[r_mod])
    P.op(DVE, lambda e: e.tensor_scalar(out=ab[:, 1, :], in0=modT[:, 8:16], scalar1=1.0, scalar2=None, op0=ALU.add), writes=[r_mod])
    P.op(DVE, lambda e: e.tensor_tensor(out=ab[:, 0, :], in0=ab[:, 1, :], in1=gm, op=ALU.mult), reads=[r_ca], writes=[r_mod])

    def ada_finish():
        P.op(DVE, lambda e: e.tensor_scalar(out=ab[:, 3, :], in0=modT[:, 32:40], scalar1=1.0, scalar2=None, op0=ALU.add), writes=[r_mod2])
        P.op(DVE, lambda e: e.tensor_tensor(out=ab[:, 2, :], in0=ab[:, 3, :], in1=gf, op=ALU.mult), reads=[r_ca], writes=[r_mod2])
    a1 = ab[:, 0, :]; b1 = modT[:, 0:8]; a2 = ab[:, 2, :]; b2 = modT[:, 24:32]; g_m = modT[:, 16:24]; g_f = modT[:, 40:48]

    def prep_front(xrows, nblk, np_, xt, r_xt, xn, r_xn, st, r_st):
        dma(xt[0:np_, 0:nblk, :], xrows.rearrange("(b p) d -> p b d", p=np_), writes=[r_xt])
        for bl in range(nblk):
            P.op(ACT, lambda e, bl=bl: e.activation(out=xn[0:np_, bl, :], in_=xt[0:np_, bl, :], func=AF.Square, accum_out=st[0:np_, bl:bl + 1]),
                 reads=[r_xt], writes=[r_xn, r_st])
        P.op(ACT, lambda e: e.activation(out=st[0:np_, 4:4 + nblk], in_=st[0:np_, 0:nblk], func=AF.Sqrt, bias=EPS, scale=1.0 / D), writes=[r_st])
        P.op(DVE, lambda e: e.reciprocal(out=st[0:np_, 8:8 + nblk], in_=st[0:np_, 4:4 + nblk]), reads=[r_st], writes=[r_st])
        for bl in range(nblk):
            P.op(DVE, lambda e, bl=bl: e.tensor_scalar(out=xn[0:np_, bl, :], in0=xt[0:np_, bl, :], scalar1=st[0:np_, 8 + bl:9 + bl], scalar2=None, op0=ALU.mult),
                 reads=[r_xt, r_st], writes=[r_xn])

    def prep_back(nblk, np_, xn, r_xn, hT, r_hTk, banks, avec, bvec):
        ntok = nblk * np_
        for kc in range(8):
            bk = banks[kc // 2]
            for bl in range(nblk):
                o = (kc % 2) * 512 + bl * np_
                P.op(PE, lambda e, kc=kc, bl=bl, bk=bk, o=o: e.transpose(out=psb[bk][:, o:o + np_], in_=xn[0:np_, bl, kc * 128:(kc + 1) * 128],
                                                                     identity=ident[0:np_, 0:np_]),
                     reads=[r_xn, r_c], writes=[rb[bk]])
            if kc % 2 == 1:
                for k2 in (kc - 1, kc):
                    o = (k2 % 2) * 512
                    eng_ = DVE
                    if eng_ == DVE:
                        P.op(DVE, lambda e, k2=k2, bk=bk, o=o: e.tensor_scalar(out=hT[:, k2, 0:ntok], in0=psb[bk][:, o:o + ntok], scalar1=avec[:, k2:k2 + 1],
                                                                           scalar2=bvec[:, k2:k2 + 1], op0=ALU.mult, op1=ALU.add),
                             reads=[rb[bk], r_mod], writes=[r_hTk[k2]])
                    else:
                        P.op(ACT, lambda e, k2=k2, bk=bk, o=o: e.activation(out=hT[:, k2, 0:ntok], in_=psb[bk][:, o:o + ntok], func=AF.Identity, scale=avec[:, k2:k2 + 1],
                                                                        bias=bvec[:, k2:k2 + 1]),
                             reads=[rb[bk], r_mod], writes=[r_hTk[k2]])

    def prep(xrows, nblk, np_, xt, r_xt, xn, r_xn, hT, r_hT, st, r_st, banks, avec, bvec):
        prep_front(xrows, nblk, np_, xt, r_xt, xn, r_xn, st, r_st)
        prep_back(nblk, np_, xn, r_xn, hT, [r_hT] * 8, banks, avec, bvec)

    def load_w(dst, src_b, rows, c0, c1, r_w, cname="w_in"):
        dma(dst, src_b.rearrange("(k p) n -> p k n", p=128)[:, :, c0:c1], reads=[r_cast[cname]], writes=[r_w])

    evac_rr = [0]

    def evac(out, in_, reads, writes, func=None, scale=1.0):
        evac_rr[0] += 1
        if func is not None or evac_rr[0] % 2 == 0:
            f = func if func is not None else AF.Copy
            return P.op(ACT, lambda e: e.activation(out=out, in_=in_, func=f, scale=scale), reads=reads, writes=writes)
        if scale != 1.0:
            return P.op(DVE, lambda e: e.tensor_scalar(out=out, in0=in_, scalar1=scale, scalar2=None, op0=ALU.mult), reads=reads, writes=writes)
        return P.op(DVE, lambda e: e.tensor_copy(out=out, in_=in_), reads=reads, writes=writes)

    A.mark()
    wk = A.tile([8, 512], BF16); wv = A.tile([8, 512], BF16); wki = A.tile([8, 128], BF16); r_w1 = Res()
    wst = [A.tile([8, 512], F32) for _ in range(2)]; r_wst = [Res(), Res()]
    w_in_v = w_in.rearrange("(k p) n -> p k n", p=128)
    dma(wst[0], w_in_v[:, :, 512:1024], writes=[r_wst[0]])
    dma(wst[1], w_in_v[:, :, 1024:1536], writes=[r_wst[1]])
    P.op(ACT, lambda e: e.activation(out=wk, in_=wst[0], func=AF.Copy), reads=[r_wst[0]], writes=[r_w1])
    P.op(DVE, lambda e: e.tensor_copy(out=wv, in_=wst[1]), reads=[r_wst[1]], writes=[r_w1])
    dma(wst[0][:, :, 0:64], w_in_v[:, :, 2048:2112], writes=[r_wst[0]])
    P.op(ACT, lambda e: e.activation(out=wki[:, :, 0:64], in_=wst[0][:, :, 0:64], func=AF.Copy), reads=[r_wst[0]], writes=[r_w1])
    P.op(ACT, lambda e: e.activation(out=wki[:, :, 64:128], in_=wst[0][:, :, 0:64], func=AF.Copy), reads=[r_wst[0]], writes=[r_w1])
    xts = [A.tile([4, D], F32) for _ in range(2)]; r_xts = [Res(), Res()]
    xns = [A.tile([4, D], BF16) for _ in range(2)]; r_xns = [Res(), Res()]
    hTs = [A.tile([8, 512], BF16) for _ in range(2)]; r_hTs = [[Res() for _ in range(8)] for _ in range(2)]
    sts = [A.tile([12], F32) for _ in range(2)]; r_sts = [Res(), Res()]
    ksts = [A.tile([4, 512], BF16) for _ in range(2)]; r_ksts = [Res(), Res()]
    vsts = [A.tile([4, 8, 65], BF16) for _ in range(2)]; r_vsts = [Res(), Res()]
    for b in range(2):
        P.op(DVE, lambda e, b=b: e.memset(vsts[b], 1.0), writes=[r_vsts[b]])
    kT_v = kT_s.rearrange("(m hh) d s -> (hh d) m s", hh=2)
    v_v = v_s.rearrange("(n p) c -> p n c", p=128)

    def front1a(T):
        b = T % 2
        prep_front(xk[T * 512:(T + 1) * 512, :], 4, 128, xts[b], r_xts[b], xns[b], r_xns[b], sts[b], r_sts[b])

    def back1a(T):
        b = T % 2
        prep_back(4, 128, xns[b], r_xns[b], hTs[b], r_hTs[b], [0, 1, 2, 3], a1, b1)

    front1a(0)
    back1a(0)
    for T in range(NT):
        b = T % 2
        hT = hTs[b]
        if T + 1 < NT:
            front1a(T + 1)
        for m in range(4):
            bk = 4 + (m % 2)
            for kc in range(8):
                P.op(PE, lambda e, m=m, kc=kc, bk=bk, hT=hT: e.matmul(ps[:, bk, :], lhsT=wk[:, kc, m * 128:(m + 1) * 128], rhs=hT[:, kc, :],
                                                                  start=(kc == 0), stop=(kc == 7)), reads=[r_w1, r_hTs[b][kc]], writes=[rb[bk]])
            evac(ksts[b][:, m, :], ps[:, bk, :], [rb[bk]], [r_ksts[b]])
        dma(kT_v[:, :, T * 512:(T + 1) * 512], ksts[b], reads=[r_ksts[b]])
        for bl in range(4):
            bk = 6 + (bl % 2)
            for kc in range(8):
                P.op(PE, lambda e, bl=bl, kc=kc, bk=bk, hT=hT: e.matmul(ps[:, bk, :], lhsT=hT[:, kc, bl * 128:(bl + 1) * 128], rhs=wv[:, kc, :],
                                                                    start=(kc == 0), stop=(kc == 7)), reads=[r_w1, r_hTs[b][kc]], writes=[rb[bk]])
            evac(vsts[b][:, bl, :, 0:64], ps[:, bk, :].rearrange("p (h d) -> p h d", d=64), [rb[bk]], [r_vsts[b]])
        dma(v_v[:, T * 4:(T + 1) * 4, :], vsts[b].rearrange("p n h c -> p n (h c)"), reads=[r_vsts[b]])
        for kc in range(8):
            P.op(PE, lambda e, kc=kc, hT=hT: e.matmul(ps[:, 4, :], lhsT=wki[:, kc, :], rhs=hT[:, kc, :], start=(kc == 0), stop=(kc == 7)),
                 reads=[r_w1, r_hTs[b][kc]], writes=[rb[4]])
        evac(kiT[:, T * 512:(T + 1) * 512], ps[:, 4, :], [rb[4]], [r_ki])
        if T + 1 < NT:
            back1a(T + 1)
        if T < 5:
            ada_piece(3 + T, bank=4, r_dst=r_mod2)
        if T == 5:
            ada_finish()
    P.barrier()
    A.release()
    A.release()

    A.mark()
    wq = A.tile([8, 512], BF16); wqi = A.tile([8, 512], BF16); wwi = A.tile([8, 8], BF16)
    wu = A.tile([8, 1024], BF16); wga = A.tile([8, 1024], BF16); wgb = A.tile([8, 1024], BF16); r_w2 = Res()
    load_w(wq, w_in_b, D, 0, 512, r_w2); load_w(wqi, w_in_b, D, 1536, 2048, r_w2); load_w(wwi, w_in_b, D, 2112, 2120, r_w2)
    load_w(wu, w_in_b, D, 2120, 3144, r_w2); load_w(wga, w_in_b, D, 3144, 4168, r_w2); load_w(wgb, w_in_b, D, 4168, 5192, r_w2)
    xt = A.tile([4, D], F32); r_xt = Res(); xn = A.tile([4, D], BF16); r_xn = Res()
    hT = A.tile([8, 512], BF16); r_hT = Res(); st = A.tile([12], F32); r_st = Res()
    xth = A.tile([1, D], F32); r_xth = Res(); xnh = A.tile([1, D], BF16); r_xnh = Res()
    hTh = A.tile([8, 32], BF16); r_hTh = Res(); sth = A.tile([12], F32); r_sth = Res()
    stg = [A.tile([512], BF16) for _ in range(3)]; r_stg = [Res() for _ in range(3)]
    sgm = [A.tile([512], F32) for _ in range(2)]; r_sgm = [Res(), Res()]
    stg_i = [0]
    qT_v = qT_s.rearrange("(m hh) d s -> (hh d) m s", hh=2)

    def stage_out(dst, src_ps, bk, func=None):
        i = stg_i[0] % 3
        stg_i[0] += 1
        evac(stg[i], src_ps, [rb[bk]], [r_stg[i]], func=func)
        dma(dst, stg[i], reads=[r_stg[i]])

    def proj_fm(w, m, hT_, r_h, bk, n):
        for kc in range(8):
            P.op(PE, lambda e, kc=kc: e.matmul(ps[:, bk, 0:n], lhsT=w[:, kc, m * 128:(m + 1) * 128], rhs=hT_[:, kc, 0:n], start=(kc == 0), stop=(kc == 7)),
                 reads=[r_w2, r_h], writes=[rb[bk]])

    for g in range(4):
        T = 4 * g + 3
        prep(xk[T * 512:(T + 1) * 512, :], 4, 128, xt, r_xt, xn, r_xn, hT, r_hT, st, r_st, [0, 1, 2, 3], a1, b1)
        cs = slice(g * 512, (g + 1) * 512)
        bkc = [0]

        def nb():
            bkc[0] += 1
            return 4 + bkc[0] % 4
        for m in range(4):
            bk = nb(); proj_fm(wq, m, hT, r_hT, bk, 512); stage_out(qT_v[:, m, cs], ps[:, bk, :], bk)
        for m in range(4):
            bk = nb(); proj_fm(wqi, m, hT, r_hT, bk, 512); stage_out(qiT_s[m, :, cs], ps[:, bk, :], bk)
        for m in range(8):
            bk = nb(); proj_fm(wga, m, hT, r_hT, bk, 512); stage_out(gaT_s[m, :, cs], ps[:, bk, :], bk, func=AF.Sigmoid)
        for m in range(8):
            bk = nb(); proj_fm(wgb, m, hT, r_hT, bk, 512); stage_out(gbT_s[m, :, cs], ps[:, bk, :], bk, func=AF.Sigmoid)
        for bl in range(4):
            bk = nb()
            for kc in range(8):
                P.op(PE, lambda e, kc=kc, bl=bl, bk=bk: e.matmul(ps[:, bk, 0:8], lhsT=hT[:, kc, bl * 128:(bl + 1) * 128], rhs=wwi[:, kc, :], start=(kc == 0), stop=(kc == 7)),
                     reads=[r_w2, r_hT], writes=[rb[bk]])
            P.op(DVE, lambda e, bl=bl, bk=bk, g=g: e.tensor_scalar(out=wiall[:, g * 4 + bl, :], in0=ps[:, bk, 0:8], scalar1=float(8 ** -0.5 * 64 ** -0.5), scalar2=None, op0=ALU.mult),
                 reads=[rb[bk]], writes=[r_wi])
        for i in range(4):
            bka = nb(); proj_fm(wu, i, hT, r_hT, bka, 512)
            bkg = nb(); proj_fm(wu, 4 + i, hT, r_hT, bkg, 512)
            si = i % 2
            P.op(ACT, lambda e, si=si, bkg=bkg: e.activation(out=sgm[si], in_=ps[:, bkg, :], func=AF.Sigmoid), reads=[rb[bkg]], writes=[r_sgm[si]])
            j = stg_i[0] % 3; stg_i[0] += 1
            P.op(DVE, lambda e, si=si, bka=bka, j=j: e.tensor_tensor(out=stg[j], in0=ps[:, bka, :], in1=sgm[si], op=ALU.mult), reads=[rb[bka], r_sgm[si]], writes=[r_stg[j]])
            dma(zT_s[i, :, g, 32:544], stg[j], reads=[r_stg[j]])
        prep(xk[T * 512 - 32:T * 512, :], 1, 32, xth, r_xth, xnh, r_xnh, hTh, r_hTh, sth, r_sth, [0, 1, 2, 3], a1, b1)
        for i in range(4):
            bka = nb(); proj_fm(wu, i, hTh, r_hTh, bka, 32)
            bkg = nb(); proj_fm(wu, 4 + i, hTh, r_hTh, bkg, 32)
            si = i % 2
            P.op(ACT, lambda e, si=si, bkg=bkg: e.activation(out=sgm[si][:, 0:32], in_=ps[:, bkg, 0:32], func=AF.Sigmoid), reads=[rb[bkg]], writes=[r_sgm[si]])
            j = stg_i[0] % 3; stg_i[0] += 1
            P.op(DVE, lambda e, si=si, bka=bka, j=j: e.tensor_tensor(out=sgm[si][:, 32:64], in0=ps[:, bka, 0:32], in1=sgm[si][:, 0:32], op=ALU.mult), reads=[rb[bka]], writes=[r_sgm[si]])
            P.op(DVE, lambda e, si=si, j=j, g=g: e.tensor_tensor(out=stg[j][:, 0:32], in0=sgm[si][:, 32:64], in1=hval[:, g * 32:(g + 1) * 32], op=ALU.mult), reads=[r_c], writes=[r_stg[j], r_sgm[si]])
            dma(zT_s[i, :, g, 0:32], stg[j][:, 0:32], reads=[r_stg[j]])
    P.barrier()
    A.release()

    A.mark()
    cbt = A.tile([4, 512], BF16); kbt = A.tile([1536], BF16); r_c2 = Res()
    dma(cbt, cb_d, writes=[r_c2]); dma(kbt[0:1, :], kbias_d[:, 0:1536], writes=[r_c2])
    onesb = A.tile([128], BF16)
    nidm = A.tile([128], BF16)
    P.op(DVE, lambda e: e.memset(onesb, 1.0), writes=[r_c2])
    P.op(DVE, lambda e: e.tensor_scalar(out=nidm, in0=ident, scalar1=-MASKB, scalar2=None, op0=ALU.mult), reads=[r_c], writes=[r_c2])
    scoress = [A.tile([S], F32) for _ in range(2)]; r_scs = [Res(), Res()]
    maskq = A.tile([S], BF16); r_mq = Res()
    junk = maskq; r_junk = r_mq
    maskTs = [A.tile([64, 256], FP8) for _ in range(2)]; r_mTs = [Res(), Res()]
    qit = A.tile([4, 256], BF16); r_qit = Res()
    qaugs = [A.tile([8, 256], BF16) for _ in range(2)]; r_qas = [Res(), Res()]
    wdgs = [A.tile([8, 128], BF16) for _ in range(2)]; r_wdgs = [Res(), Res()]
    Rt = [A.tile([2, 512], BF16) for _ in range(3)]; r_Rt = [Res() for _ in range(3)]
    bs = A.tile([16], F32); r_bs = Res()
    bsa = A.tile([4], F32); r_bsa = Res(); r_junkA = Res()
    cntk = A.tile([64], F32); r_ck = Res()
    U = A.tile([68], F32); r_U = Res()
    kt = [A.tile([8, 512], BF16) for _ in range(2)]; r_kt = [Res(), Res()]
    vt = [A.tile([4, 520], BF16) for _ in range(2)]; r_vt = [Res(), Res()]
    Pt = [A.tile([2, 256], BF16) for _ in range(3)]; r_Pt = [Res() for _ in range(3)]
    den = A.tile([16], F32); r_den = Res()
    Ob = A.tile([2, 512], BF16); r_Ob = Res()
    OTst = A.tile([4, 256], BF16); r_OT = Res()
    P.op(DVE, lambda e: e.memset(U, 0.0), writes=[r_U])
    P.op(DVE, lambda e: e.memset(U[:, 64:66], 1.0), writes=[r_U])
    for i in range(2):
        P.op(DVE, lambda e, i=i: e.memset(qaugs[i], 0.0), writes=[r_qas[i]])
    kT_hv = kT_s.rearrange("h d s -> d h s")
    kaug_v = kaug_d
    v_v2 = v_s.rearrange("(n p) c -> p n c", p=128)
    LB = 2
    SB = 7

    def idx_gen(qb):
        qp, q2 = qb // 2, qb % 2
        g = qp // 2
        E = 2048 * (g + 1)
        nch = E // 512
        c0 = g * 512 + (qp % 2) * 256
        j4 = qb % 4
        scores = scoress[qb % 2]; r_sc = r_scs[qb % 2]
        wdg = wdgs[qb % 2]; r_wdg = r_wdgs[qb % 2]
        if q2 == 0:
            dma(qit, qiT_s.rearrange("m p s -> p m s")[:, :, c0:c0 + 256], writes=[r_qit])
        for j in range(8):
            P.op(DVE, lambda e, j=j: e.tensor_scalar(out=wdg[:, j, :], in0=ident, scalar1=wiall[:, qb, j:j + 1], scalar2=None, op0=ALU.mult),
                 reads=[r_wi, r_c], writes=[r_wdg])
        yield 0.5
        units = [(c, m) for c in range(nch) for m in range(4)]

        def emit_diag(u):
            c, m = units[u]
            ks = slice(c * 512, (c + 1) * 512)
            has_kb = (c < 3)
            has_cb = (c == nch - 1)
            lastdiag = not (has_kb or has_cb)
            ri = u % 3
            for hh in range(2):
                imm = 2 * m + hh
                P.op(PE, lambda e, m=m, hh=hh, ri=ri, imm=imm, lastdiag=lastdiag: e.matmul(ps[:, SB, :], lhsT=wdg[:, 2 * m + hh, :], rhs=Rt[ri][:, hh, :], start=(imm == 0), stop=(lastdiag and imm == 7)),
                     reads=[r_wdg, r_Rt[ri]], writes=[rb[SB]])
            if m == 3:
                if has_kb:
                    P.op(PE, lambda e, ks=ks, has_cb=has_cb: e.matmul(ps[:, SB, :], lhsT=onesb[0:1, :], rhs=kbt[0:1, ks], start=False, stop=(not has_cb)),
                         reads=[r_c2], writes=[rb[SB]])
                if has_cb:
                    P.op(PE, lambda e: e.matmul(ps[:, SB, :], lhsT=ident, rhs=cbt[:, j4, :], start=False, stop=True), reads=[r_c2, r_c], writes=[rb[SB]])
                P.op(ACT, lambda e, ks=ks: e.activation(out=scores[:, ks], in_=ps[:, SB, :], func=AF.Copy), reads=[rb[SB]], writes=[r_sc])

        for u, (c, m) in enumerate(units):
            ks = slice(c * 512, (c + 1) * 512)
            for hh in range(2):
                pr = slice(hh * 64, hh * 64 + 64)
                P.op(PE, lambda e, m=m, hh=hh, pr=pr, ks=ks: e.matmul(ps[:, LB + hh, :], lhsT=qit[pr, m, q2 * 128:(q2 + 1) * 128], rhs=kiT[pr, ks],
                                                                start=True, stop=True), reads=[r_qit, r_ki], writes=[rb[LB + hh]])
            ri = u % 3
            P.op(ACT, lambda e, ri=ri: e.activation(out=Rt[ri], in_=ps[:, LB:LB + 2, :], func=AF.Relu), reads=[rb[LB], rb[LB + 1]], writes=[r_Rt[ri]])
            if u > 0:
                emit_diag(u - 1)
            yield 0.8
        emit_diag(len(units) - 1)
        if debug:
            dma(dbg_sc[qb, :, 0:E], scores[:, 0:E], reads=[r_sc])
        yield 0.5

    def bis_gen(qb):
        qp, q2 = qb // 2, qb % 2
        g = qp // 2
        E = 2048 * (g + 1)
        nkc = E // 128
        c0 = g * 512 + (qp % 2) * 256
        scores = scoress[qb % 2]; r_sc = r_scs[qb % 2]
        maskT = maskTs[qp % 2]; r_mT = r_mTs[qp % 2]
        qaug = qaugs[qp % 2]; r_qa = r_qas[qp % 2]
        if q2 == 0:
            dma(qaug[0:64, :, :], qT_s.rearrange("h d s -> d h s")[:, :, c0:c0 + 256], writes=[r_qa])
        sc = scores[:, 0:E]
        tpass = E / 960.0
        P.op(DVE, lambda e: e.tensor_reduce(out=bs[:, 0:1], in_=sc, axis=AX.X, op=ALU.max), reads=[r_sc], writes=[r_bs])
        P.op(DVE, lambda e: e.tensor_scalar(out=bs[:, 9:10], in0=bs[:, 0:1], scalar1=-1.0, scalar2=None, op0=ALU.mult), writes=[r_bs])
        P.op(DVE, lambda e: e.tensor_tensor(out=bs[:, 9:10], in0=bs[:, 9:10], in1=bs[:, 0:1], op=ALU.max), writes=[r_bs])
        P.op(DVE, lambda e: e.tensor_scalar(out=bs[:, 9:10], in0=bs[:, 9:10], scalar1=3.0, scalar2=3.0, op0=ALU.mult, op1=ALU.add), writes=[r_bs])
        P.op(DVE, lambda e: e.tensor_tensor(out=bs[:, 3:4], in0=bs[:, 0:1], in1=bs[:, 9:10], op=ALU.subtract), writes=[r_bs])
        P.op(DVE, lambda e: e.tensor_scalar(out=bs[:, 6:7], in0=bs[:, 3:4], scalar1=20000.0, scalar2=None, op0=ALU.add), writes=[r_bs])
        P.op(DVE, lambda e: e.tensor_tensor(out=bs[:, 7:8], in0=bs[:, 9:10], in1=bs[:, 6:7], op=ALU.subtract), writes=[r_bs])
        yield tpass + 1.0
        for it in range(NITER + 1):
            P.op(DVE, lambda e: e.tensor_scalar(out=junk[:, 0:E], in0=sc, scalar1=bs[:, 3:4], scalar2=None, op0=ALU.is_ge, op1=ALU.add, accum_out=bs[:, 4:5]),
                 reads=[r_sc], writes=[r_junk, r_bs])
            P.op(DVE, lambda e: e.tensor_scalar(out=bs[:, 5:6], in0=bs[:, 4:5], scalar1=255.5, scalar2=None, op0=ALU.is_ge), writes=[r_bs])
            if it == 0:
                P.op(DVE, lambda e: e.tensor_scalar(out=bs[:, 1:2], in0=bs[:, 5:6], scalar1=bs[:, 6:7], scalar2=-20000.0, op0=ALU.mult, op1=ALU.add), writes=[r_bs])
                P.op(DVE, lambda e: e.scalar_tensor_tensor(out=bs[:, 2:3], in0=bs[:, 5:6], scalar=bs[:, 7:8], in1=bs[:, 6:7], op0=ALU.mult, op1=ALU.add), writes=[r_bs])
            else:
                P.op(DVE, lambda e: e.tensor_scalar(out=bs[:, 2:3], in0=bs[:, 2:3], scalar1=0.5, scalar2=None, op0=ALU.mult), writes=[r_bs])
                P.op(DVE, lambda e: e.scalar_tensor_tensor(out=bs[:, 1:2], in0=bs[:, 5:6], scalar=bs[:, 2:3], in1=bs[:, 1:2], op0=ALU.mult, op1=ALU.add), writes=[r_bs])
            P.op(DVE, lambda e: e.scalar_tensor_tensor(out=bs[:, 3:4], in0=bs[:, 2:3], scalar=0.5, in1=bs[:, 1:2], op0=ALU.mult, op1=ALU.add), writes=[r_bs])
            yield tpass + 0.8
        if debug:
            dma(dbg_thr[:, qb * 4:qb * 4 + 4], bs[:, 0:4], reads=[r_bs])
        P.op(DVE, lambda e: e.tensor_scalar(out=maskq[:, 0:E], in0=sc, scalar1=bs[:, 1:2], scalar2=None, op0=ALU.is_ge), reads=[r_sc, r_junkA], writes=[r_mq, r_bs])
        P.op(DVE, lambda e: e.tensor_reduce(out=cntk[:, 0:nkc], in_=maskq[:, 0:E].rearrange("p (a b) -> p a b", b=128), axis=AX.X, op=ALU.add), writes=[r_ck, r_mq])
        P.op(DVE, lambda e: e.tensor_scalar(out=cntk[:, 0:nkc], in0=cntk[:, 0:nkc], scalar1=0.5, scalar2=None, op0=ALU.is_ge), writes=[r_ck])
        P.op(DVE, lambda e: e.tensor_tensor(out=cntk[:, 0:nkc], in0=cntk[:, 0:nkc], in1=kcp1[:, 0:nkc], op=ALU.mult), reads=[r_c], writes=[r_ck])
        P.op(DVE, lambda e: e.tensor_reduce(out=bs[:, 8:9], in_=cntk[:, 0:nkc], axis=AX.X, op=ALU.max), writes=[r_ck, r_bs])
        P.op(DVE, lambda e: e.tensor_scalar(out=U[:, 66:67], in0=bs[:, 8:9], scalar1=-1.0, scalar2=None, op0=ALU.add), writes=[r_U, r_bs])
        yield 2 * tpass
        P.op(PE, lambda e: e.matmul(ps[0:67, LB, 0:128], lhsT=U[:, 0:67], rhs=identf, start=True, stop=True), reads=[r_U, r_c], writes=[rb[LB]])
        for h in range(8):
            P.op(ACT, lambda e, h=h: e.activation(out=qaug[64:67, h, q2 * 128:(q2 + 1) * 128], in_=ps[64:67, LB, 0:128], func=AF.Copy), reads=[rb[LB]], writes=[r_qa])
        yield 1.0
        for k8 in range(nkc // 8):
            for kk in range(8):
                kc = k8 * 8 + kk
                P.op(PE, lambda e, kc=kc, kk=kk: e.transpose(out=psb[LB + 1][:, kk * 128:(kk + 1) * 128], in_=maskq[:, kc * 128:(kc + 1) * 128], identity=ident),
                     reads=[r_mq, r_c], writes=[rb[LB + 1]])
            mo = maskT[:, k8 * 8:(k8 + 1) * 8, q2 * 128:(q2 + 1) * 128]
            mi = psb[LB + 1].rearrange("p (a b) -> p a b", b=128)
            P.op(ACT, lambda e, mo=mo, mi=mi: e.activation(out=mo, in_=mi, func=AF.Copy, scale=-1.0, bias=1.0, saturate=False), reads=[rb[LB + 1]], writes=[r_mT])
            yield 1.0

    accb = [4, 5, 6]

    def att_gen(qp):
        g = qp // 2
        E = 2048 * (g + 1)
        nch = E // 512
        nkc = E // 128
        c0 = g * 512 + (qp % 2) * 256
        maskT = maskTs[qp % 2]; r_mT = r_mTs[qp % 2]
        qaug = qaugs[qp % 2]; r_qa = r_qas[qp % 2]
        first_in_bank = {}
        steps = [(c, kl, hp) for c in range(nch) for kl in range(4) for hp in range(4)]

        def emit_loads(c):
            kb_ = c % 2
            ks = slice(c * 512, (c + 1) * 512)
            dma(kt[kb_][0:64, :, :], kT_hv[:, :, ks], writes=[r_kt[kb_]])
            dma(kt[kb_][64:67, :, :], kaug_v[:, :, ks], writes=[r_kt[kb_]])
            dma(vt[kb_], v_v2[:, c * 4:(c + 1) * 4, :], writes=[r_vt[kb_]])

        def emit_ST(i):
            c, kl, hp = steps[i]
            kb_ = c % 2
            kc = c * 4 + kl
            sbk = i % 2
            for hh in range(2):
                h = hp * 2 + hh
                o = hh * 256
                P.op(PE, lambda e, h=h, o=o, sbk=sbk, kb_=kb_, kl=kl: e.matmul(ps[:, sbk, o:o + 256], lhsT=kt[kb_][0:67, h, kl * 128:(kl + 1) * 128],
                                                                    rhs=qaug[0:67, h, :], start=True, stop=False),
                     reads=[r_kt[kb_], r_qa], writes=[rb[sbk]])
                P.op(PE, lambda e, o=o, sbk=sbk, kc=kc: e.matmul(ps[:, sbk, o:o + 256], lhsT=nidm, rhs=maskT[:, kc, :], start=False, stop=True),
                     reads=[r_mT, r_c2], writes=[rb[sbk]])

        emit_loads(0)
        if nch > 1:
            emit_loads(1)
        emit_ST(0)
        for i, (c, kl, hp) in enumerate(steps):
            kb_ = c % 2
            kc = c * 4 + kl
            sbk = i % 2
            if i + 1 < len(steps):
                emit_ST(i + 1)
            pi = i % 3
            P.op(ACT, lambda e, pi=pi, sbk=sbk: e.activation(out=Pt[pi], in_=ps[:, sbk, :].rearrange("p (a b) -> p a b", b=256), func=AF.Exp, bias=EXPB, scale=0.125),
                 reads=[rb[sbk]], writes=[r_Pt[pi]])
            for q2 in range(2):
                for hh in range(2):
                    h = hp * 2 + hh
                    a = q2 * 8 + h
                    ab_ = accb[a // 6]
                    col = (a % 6) * 65
                    st_ = (kc == 0) and (ab_ not in first_in_bank)
                    if kc == 0:
                        first_in_bank[ab_] = True
                    P.op(PE, lambda e, pi=pi, hh=hh, q2=q2, h=h, ab_=ab_, col=col, st_=st_, kb_=kb_, kl=kl, kc=kc: e.matmul(
                        ps[:, ab_, col:col + 65], lhsT=Pt[pi][:, hh, q2 * 128:(q2 + 1) * 128], rhs=vt[kb_][:, kl, h * 65:(h + 1) * 65],
                        start=st_, stop=(kc == nkc - 1), skip_group_check=True),
                        reads=[r_Pt[pi], r_vt[kb_]], writes=[rb[ab_]])
            if kl == 3 and hp == 3 and c + 2 < nch:
                emit_loads(c + 2)
            yield 0.8
        for a in range(16):
            ab_ = accb[a // 6]; col = (a % 6) * 65
            P.op(DVE, lambda e, a=a, ab_=ab_, col=col: e.tensor_copy(out=den[:, a:a + 1], in_=ps[:, ab_, col + 64:col + 65]), reads=[rb[ab_]], writes=[r_den])
        P.op(DVE, lambda e: e.reciprocal(out=den, in_=den), writes=[r_den])
        for a in range(16):
            ab_ = accb[a // 6]; col = (a % 6) * 65
            q2 = a // 8; h = a % 8
            P.op(DVE, lambda e, a=a, ab_=ab_, col=col, q2=q2, h=h: e.tensor_scalar(out=Ob[:, q2, h * 64:(h + 1) * 64], in0=ps[:, ab_, col:col + 64], scalar1=den[:, a:a + 1], scalar2=None, op0=ALU.mult),
                 reads=[rb[ab_]], writes=[r_Ob, r_den])
        yield 3.0
        for q2 in range(2):
            for m in range(4):
                P.op(PE, lambda e, q2=q2, m=m: e.transpose(out=psb[0][:, (q2 * 4 + m) * 128:(q2 * 4 + m + 1) * 128], in_=Ob[:, q2, m * 128:(m + 1) * 128], identity=ident),
                     reads=[r_Ob, r_c], writes=[rb[0]])
        for q2 in range(2):
            P.op(ACT, lambda e, q2=q2: e.activation(out=OTst[:, :, q2 * 128:(q2 + 1) * 128], in_=psb[0][:, q2 * 512:(q2 + 1) * 512].rearrange("p (a b) -> p a b", b=128), func=AF.Copy),
                 reads=[rb[0]], writes=[r_OT])
        dma(OT_s.rearrange("m p s -> p m s")[:, :, c0:c0 + 256], OTst, reads=[r_OT])
        yield 1.0

    NQB, NPR = 16, 8
    done = {"IDX": 0, "BIS": 0, "ATT": 0}
    nitems = {"IDX": NQB, "BIS": NQB, "ATT": NPR}
    mk = {"IDX": idx_gen, "BIS": bis_gen, "ATT": att_gen}
    cur = {"IDX": None, "BIS": None, "ATT": None}
    clk = {"IDX": 0.0, "BIS": 0.0, "ATT": 0.0}
    now = [0.0]

    def can_start(s):
        i = done[s]
        if i >= nitems[s]:
            return False
        if s == "IDX":
            return done["BIS"] >= i - 1
        if s == "BIS":
            return done["IDX"] > i and done["ATT"] >= i // 2 - 1
        return done["BIS"] > 2 * i + 1

    while any(done[s] < nitems[s] for s in done):
        cands = []
        for s in ("ATT", "IDX", "BIS"):
            if cur[s] is None and can_start(s):
                cur[s] = mk[s](done[s])
                clk[s] = max(clk[s], now[0])
            if cur[s] is not None:
                cands.append(s)
        assert cands, ("scheduler deadlock", done)
        s = min(cands, key=lambda k: clk[k])
        now[0] = clk[s]
        cost = next(cur[s], None)
        if cost is None:
            cur[s] = None
            done[s] += 1
        else:
            clk[s] += cost
    P.barrier()
    A.release()
    A.release()

    A.mark()
    wap = A.tile([4, D], BF16); wcp = A.tile([4, D], BF16); wo = A.tile([8, D], BF16); r_w3 = Res()
    load_w(wap, w_ap_b, 512, 0, D, r_w3, "w_ap"); load_w(wcp, w_cp_b, 512, 0, D, r_w3, "w_cp"); load_w(wo, w_out_b, D, 0, D, r_w3, "w_out")
    wfo_t = [A.tile([22, 128], BF16) for _ in range(2)]; r_wfo = [Res(), Res()]
    wdw = A.tile([4, 31], F32); cvp = A.tile([3, 4], F32); r_cv = Res()
    dma(wdw, w_dwT, writes=[r_cv]); dma(cvp[:, 0, :], b_dwT, writes=[r_cv]); dma(cvp[:, 1, :], lngT, writes=[r_cv]); dma(cvp[:, 2, :], lnbT, writes=[r_cv])
    wdd = A.tile([31, 128], BF16); r_wdd = Res()
    xtb = [A.tile([D], F32) for _ in range(2)]; r_xtb = [Res(), Res()]
    xTs = [A.tile([8, 512], F32) for _ in range(2)]; r_xTs = [Res(), Res()]
    OT = A.tile([4, 512], BF16); zT = A.tile([4, 544], BF16); r_in3 = Res()
    gab = [A.tile([2, 512], BF16) for _ in range(2)]; r_gab = [Res(), Res()]
    cv = A.tile([4, 512], F32); r_cvo = Res()
    sq = A.tile([512], F32); r_sq = Res()
    stt = A.tile([3, 512], F32); r_stt = Res()
    zc = A.tile([4, 512], BF16); r_zc = Res()
    t1 = A.tile([512], F32); r_t1 = Res()
    mg = A.tile([8, 512], BF16); r_mg = Res()
    h2s = [A.tile([8, 512], BF16) for _ in range(2)]; r_h2s = [Res(), Res()]
    gT = A.tile([22, 512], BF16); r_gT = Res()
    wfi_t = [A.tile([8, 256], BF16) for _ in range(2)]; r_wfi = [Res() for _ in range(2)]
    sl = [A.tile([512], F32) for _ in range(2)]; r_sl = [Res(), Res()]
    sq2 = A.tile([512], F32); r_sq2 = Res()
    rs2 = A.tile([512], F32); r_rs2 = Res()
    yo = A.tile([D], F32); r_yo = Res()
    w_fo_v = w_fo_b.rearrange("(k p) n -> p k n", p=128)
    w_fi_v = w_fi_b.rearrange("(k p) n -> p k n", p=128)

    def rstd_sumsq(n, src_fn, nchunks, sqt, r_sqt, bank, dst, r_dst, rd):
        for m in range(nchunks):
            P.op(ACT, lambda e, m=m: e.activation(out=sqt, in_=src_fn(m), func=AF.Square), reads=rd, writes=[r_sqt])
            P.op(PE, lambda e, m=m: e.matmul(ps[:, bank, :], lhsT=onesf, rhs=sqt, start=(m == 0), stop=(m == nchunks - 1)), reads=[r_sqt, r_c], writes=[rb[bank]])
        P.op(ACT, lambda e: e.activation(out=dst, in_=ps[:, bank, :], func=AF.Sqrt, bias=EPS, scale=1.0 / n), reads=[rb[bank]], writes=[r_dst])
        P.op(DVE, lambda e: e.reciprocal(out=dst, in_=dst), writes=[r_dst])

    def front_gen(g):
        T = 4 * g + 3
        cs = slice(g * 512, (g + 1) * 512)
        xT = xTs[g % 2]; r_xT = r_xTs[g % 2]
        h2 = h2s[g % 2]; r_h2 = r_h2s[g % 2]
        dma(OT, OT_s.rearrange("m p s -> p m s")[:, :, cs], writes=[r_in3])
        dma(zT, zT_s[:, :, g, :].rearrange("m p s -> p m s"), writes=[r_in3])
        for bl in range(4):
            xb_ = xtb[bl % 2]; rxb = r_xtb[bl % 2]
            dma(xb_, xk[T * 512 + bl * 128:T * 512 + (bl + 1) * 128, :], writes=[rxb])
            for half in range(2):
                bk = half
                for k4 in range(4):
                    kc = half * 4 + k4
                    P.op(PE, lambda e, kc=kc, k4=k4, bk=bk, xb_=xb_: e.transpose(out=ps[:, bk, k4 * 128:(k4 + 1) * 128], in_=xb_[:, kc * 128:(kc + 1) * 128], identity=identf),
                         reads=[rxb, r_c], writes=[rb[bk]])
                evac(xT[:, half * 4:half * 4 + 4, bl * 128:(bl + 1) * 128], ps[:, bk, :].rearrange("p (a b) -> p a b", b=128), [rb[bk]], [r_xT])
            yield 3.0
        for i in range(4):
            bk = i % 2
            for k in range(31):
                P.op(DVE, lambda e, i=i, k=k: e.tensor_scalar(out=wdd[:, k, :], in0=ident, scalar1=wdw[:, i, k:k + 1], scalar2=None, op0=ALU.mult),
                     reads=[r_cv, r_c], writes=[r_wdd])
            for k in range(31):
                P.op(PE, lambda e, i=i, k=k, bk=bk: e.matmul(ps[:, bk, :], lhsT=wdd[:, k, :], rhs=zT[:, i, k + 2:k + 514], start=(k == 0), stop=(k == 30)),
                     reads=[r_wdd, r_in3], writes=[rb[bk]])
            P.op(DVE, lambda e, i=i, bk=bk: e.tensor_scalar(out=cv[:, i, :], in0=ps[:, bk, :], scalar1=cvp[:, 0, i:i + 1], scalar2=None, op0=ALU.add),
                 reads=[rb[bk], r_cv], writes=[r_cvo])
            yield 7.0
        for i in range(4):
            P.op(PE, lambda e, i=i: e.matmul(ps[:, 2, :], lhsT=onesf, rhs=cv[:, i, :], start=(i == 0), stop=(i == 3)), reads=[r_cvo, r_c], writes=[rb[2]])
        P.op(ACT, lambda e: e.activation(out=stt[:, 0, :], in_=ps[:, 2, :], func=AF.Copy, scale=1.0 / 512), reads=[rb[2]], writes=[r_stt])
        for i in range(4):
            P.op(DVE, lambda e, i=i: e.tensor_tensor(out=cv[:, i, :], in0=cv[:, i, :], in1=stt[:, 0, :], op=ALU.subtract), reads=[r_stt], writes=[r_cvo])
        yield 4.0
        rstd_sumsq(512, lambda m: cv[:, m, :], 4, sq, r_sq, 3, stt[:, 2, :], r_stt, [r_cvo])
        yield 4.0
        for i in range(4):
            P.op(DVE, lambda e, i=i: e.tensor_tensor(out=cv[:, i, :], in0=cv[:, i, :], in1=stt[:, 2, :], op=ALU.mult), reads=[r_stt], writes=[r_cvo])
            P.op(DVE, lambda e, i=i: e.tensor_scalar(out=cv[:, i, :], in0=cv[:, i, :], scalar1=cvp[:, 1, i:i + 1], scalar2=cvp[:, 2, i:i + 1], op0=ALU.mult, op1=ALU.add),
                 reads=[r_cv], writes=[r_cvo])
            P.op(ACT, lambda e, i=i: e.activation(out=zc[:, i, :], in_=cv[:, i, :], func=AF.Silu), reads=[r_cvo], writes=[r_zc])
        yield 4.0
        for m in range(8):
            b0 = (2 * m) % 4; b1_ = (2 * m + 1) % 4
            gb_ = gab[m % 2]; rg = r_gab[m % 2]
            dma(gb_[:, 0, :], gaT_s[m, :, cs], writes=[rg])
            dma(gb_[:, 1, :], gbT_s[m, :, cs], writes=[rg])
            for k in range(4):
                P.op(PE, lambda e, m=m, k=k, b0=b0: e.matmul(ps[:, b0, :], lhsT=wap[:, k, m * 128:(m + 1) * 128], rhs=OT[:, k, :], start=(k == 0), stop=(k == 3)),
                     reads=[r_w3, r_in3], writes=[rb[b0]])
            for k in range(4):
                P.op(PE, lambda e, m=m, k=k, b1_=b1_: e.matmul(ps[:, b1_, :], lhsT=wcp[:, k, m * 128:(m + 1) * 128], rhs=zc[:, k, :], start=(k == 0), stop=(k == 3)),
                     reads=[r_w3, r_zc], writes=[rb[b1_]])
            P.op(DVE, lambda e, b0=b0, gb_=gb_: e.tensor_tensor(out=t1, in0=ps[:, b0, :], in1=gb_[:, 0, :], op=ALU.mult), reads=[rb[b0], rg], writes=[r_t1])
            P.op(DVE, lambda e, b1_=b1_, gb_=gb_: e.tensor_tensor(out=sq, in0=ps[:, b1_, :], in1=gb_[:, 1, :], op=ALU.mult), reads=[rb[b1_], rg], writes=[r_sq])
            P.op(DVE, lambda e, m=m: e.tensor_tensor(out=mg[:, m, :], in0=t1, in1=sq, op=ALU.add), reads=[r_t1, r_sq], writes=[r_mg])
            yield 2.0
        for m in range(8):
            bk = m % 4
            for k in range(8):
                P.op(PE, lambda e, m=m, k=k, bk=bk: e.matmul(ps[:, bk, :], lhsT=wo[:, k, m * 128:(m + 1) * 128], rhs=mg[:, k, :], start=(k == 0), stop=(k == 7)),
                     reads=[r_w3, r_mg], writes=[rb[bk]])
            P.op(DVE, lambda e, m=m, bk=bk: e.scalar_tensor_tensor(out=xT[:, m, :], in0=ps[:, bk, :], scalar=g_m[:, m:m + 1], in1=xT[:, m, :], op0=ALU.mult, op1=ALU.add),
                 reads=[rb[bk], r_mod2], writes=[r_xT])
            yield 2.0
        rstd_sumsq(D, lambda m: xT[:, m, :], 8, sq, r_sq, 3, stt[:, 2, :], r_stt, [r_xT])
        yield 6.0
        for m in range(8):
            P.op(DVE, lambda e, m=m: e.tensor_tensor(out=t1, in0=xT[:, m, :], in1=stt[:, 2, :], op=ALU.mult), reads=[r_xT, r_stt], writes=[r_t1])
            P.op(DVE, lambda e, m=m: e.tensor_scalar(out=h2[:, m, :], in0=t1, scalar1=a2[:, m:m + 1], scalar2=b2[:, m:m + 1], op0=ALU.mult, op1=ALU.add),
                 reads=[r_mod2], writes=[r_h2, r_t1])
        yield 6.0

    def ffn_gen(g):
        xT = xTs[g % 2]; r_xT = r_xTs[g % 2]
        h2 = h2s[g % 2]; r_h2 = r_h2s[g % 2]
        for i in range(22):
            wb_ = i % 2
            dma(wfi_t[wb_][:, :, 0:128], w_fi_v[:, :, i * 128:(i + 1) * 128], reads=[r_cast["w_fi"]], writes=[r_wfi[wb_]])
            dma(wfi_t[wb_][:, :, 128:256], w_fi_v[:, :, 2816 + i * 128:2816 + (i + 1) * 128], reads=[r_cast["w_fi"]], writes=[r_wfi[wb_]])
            b0 = 4 + (2 * i) % 4; b1_ = 4 + (2 * i + 1) % 4
            for k in range(8):
                P.op(PE, lambda e, k=k, wb_=wb_, b0=b0: e.matmul(ps[:, b0, :], lhsT=wfi_t[wb_][:, k, 0:128], rhs=h2[:, k, :], start=(k == 0), stop=(k == 7)),
                     reads=[r_wfi[wb_], r_h2], writes=[rb[b0]])
            for k in range(8):
                P.op(PE, lambda e, k=k, wb_=wb_, b1_=b1_: e.matmul(ps[:, b1_, :], lhsT=wfi_t[wb_][:, k, 128:256], rhs=h2[:, k, :], start=(k == 0), stop=(k == 7)),
                     reads=[r_wfi[wb_], r_h2], writes=[rb[b1_]])
            si = i % 2
            P.op(ACT, lambda e, si=si, b0=b0: e.activation(out=sl[si], in_=ps[:, b0, :], func=AF.Silu), reads=[rb[b0]], writes=[r_sl[si]])
            P.op(DVE, lambda e, si=si, b1_=b1_, i=i: e.tensor_tensor(out=gT[:, i, :], in0=ps[:, b1_, :], in1=sl[si], op=ALU.mult), reads=[rb[b1_], r_sl[si]], writes=[r_gT])
            yield 3.5
        for m in range(8):
            bk = 4 + m % 4
            wf = wfo_t[m % 2]; rwf = r_wfo[m % 2]
            dma(wf, w_fo_v[:, :, m * 128:(m + 1) * 128], reads=[r_cast["w_fo"]], writes=[rwf])
            for i in range(22):
                P.op(PE, lambda e, i=i, bk=bk, wf=wf: e.matmul(ps[:, bk, :], lhsT=wf[:, i, :], rhs=gT[:, i, :], start=(i == 0), stop=(i == 21)),
                     reads=[rwf, r_gT], writes=[rb[bk]])
            P.op(DVE, lambda e, m=m, bk=bk: e.scalar_tensor_tensor(out=xT[:, m, :], in0=ps[:, bk, :], scalar=g_f[:, m:m + 1], in1=xT[:, m, :], op0=ALU.mult, op1=ALU.add),
                 reads=[rb[bk], r_mod2], writes=[r_xT])
            yield 4.8
        rstd_sumsq(D, lambda m: xT[:, m, :], 8, sq2, r_sq2, 7, rs2, r_rs2, [r_xT])
        yield 6.0
        for m in range(8):
            P.op(DVE, lambda e, m=m: e.scalar_tensor_tensor(out=xT[:, m, :], in0=xT[:, m, :], scalar=gfin[:, m:m + 1], in1=rs2, op0=ALU.mult, op1=ALU.mult),
                 reads=[r_rs2, r_c], writes=[r_xT])
        yield 4.0
        for bl in range(4):
            for m in range(8):
                bk = 4 + (m // 4 + 2 * bl) % 4
                P.op(PE, lambda e, m=m, bl=bl, bk=bk: e.transpose(out=ps[:, bk, (m % 4) * 128:(m % 4 + 1) * 128], in_=xT[:, m, bl * 128:(bl + 1) * 128], identity=identf),
                     reads=[r_xT, r_c], writes=[rb[bk]])
                if m % 4 == 3:
                    evac(yo[:, (m // 4) * 512:(m // 4 + 1) * 512], ps[:, bk, :], [rb[bk]], [r_yo])
            dma(out_d[g * 512 + bl * 128:g * 512 + (bl + 1) * 128, :], yo, reads=[r_yo])
            yield 2.0

    done3 = {"F": 0, "N": 0}
    mk3 = {"F": front_gen, "N": ffn_gen}
    cur3 = {"F": None, "N": None}
    clk3 = {"F": 0.0, "N": 0.0}
    now3 = [0.0]

    def can_start3(s):
        i = done3[s]
        if i >= 4:
            return False
        if s == "F":
            return done3["N"] >= i - 1
        return done3["F"] > i

    while done3["F"] < 4 or done3["N"] < 4:
        cands = []
        for s in ("N", "F"):
            if cur3[s] is None and can_start3(s):
                cur3[s] = mk3[s](done3[s])
                clk3[s] = max(clk3[s], now3[0])
            if cur3[s] is not None:
                cands.append(s)
        assert cands, ("phase-3 scheduler deadlock", done3)
        s = min(cands, key=lambda k: clk3[k])
        now3[0] = clk3[s]
        cost = next(cur3[s], None)
        if cost is None:
            cur3[s] = None
            done3[s] += 1
        else:
            clk3[s] += cost
    A.release()
    P.emit()
    return nc


_NC_CACHE = {}


def _consts():
    ident = np.eye(128, dtype=np.float32)
    slopes = np.exp2(-np.arange(1, 9, dtype=np.float64))
    s = np.arange(S)
    kaug = np.zeros((3, 8, S), np.float32)
    for h in range(8):
        kaug[0, h] = 8.0 * slopes[h] * (s % 128)
        kaug[1, h] = 8.0 * slopes[h] * 128.0 * (s // 128)
        kaug[2, h] = -8.0 * 128.0 * slopes[h]
    cb = np.zeros((128, 4, 512), np.float32)
    p = np.arange(128)[:, None]
    sp = np.arange(512)[None, :]
    for j4 in range(4):
        cb[:, j4, :] = np.where(sp <= 128 * j4 + p, 0.0, NEG)
    kcp1 = np.tile(np.arange(1, 65, dtype=np.float32)[None, :], (128, 1))
    return ident, kaug, cb, kcp1


def _in_maps(inputs):
    x = np.asarray(inputs["x"], np.float32)
    c = np.asarray(inputs["c"], np.float32)
    bf = ml_dtypes.bfloat16
    ident, kaug, cb, kcp1 = _consts()

    def fm(v, n):
        return np.ascontiguousarray(np.asarray(v, np.float32).reshape(n, 128).T)

    shared = {
        "w_ada": np.ascontiguousarray(inputs["w_ada"][0], np.float32),
        "b_adaT": fm(inputs["b_ada"][0], 48),
        "gmixT": fm(inputs["norm_mix_g"][0], 8), "gffnT": fm(inputs["norm_ffn_g"][0], 8), "gfinT": fm(inputs["norm_final_g"], 8),
        "w_in": np.ascontiguousarray(inputs["w_in"][0], np.float32),
        "w_dwT": np.ascontiguousarray(np.asarray(inputs["w_dw"][0][:, 0, :], np.float32).T.reshape(4, 128, 31).transpose(1, 0, 2)),
        "b_dwT": fm(inputs["b_dw"][0], 4), "lngT": fm(inputs["conv_ln_g"][0], 4), "lnbT": fm(inputs["conv_ln_b"][0], 4),
        "w_ap": np.ascontiguousarray(inputs["w_attn_proj"][0], np.float32), "w_cp": np.ascontiguousarray(inputs["w_conv_proj"][0], np.float32),
        "w_out": np.ascontiguousarray(inputs["w_out"][0], np.float32),
        "w_fi": np.ascontiguousarray(inputs["w_ffn_in"][0], np.float32), "w_fo": np.ascontiguousarray(inputs["w_ffn_out"][0], np.float32),
        "ident": ident.astype(bf), "identf": ident, "kaug": kaug.astype(bf), "cb": cb.astype(bf), "kcp1": kcp1,
    }
    maps = []
    for core in range(8):
        b, r = core // 4, core % 4
        pad = 1536 - 512 * r
        xk = np.zeros((S, D), np.float32)
        xk[pad:] = x[b, :S - pad]
        kbias = np.zeros((1, S), np.float32)
        kbias[0, :pad] = NEG
        hv = np.zeros((128, 128), np.float32)
        for g in range(4):
            pos = 2048 * g + 1504 + np.arange(32)
            hv[:, g * 32:(g + 1) * 32] = (pos >= pad).astype(np.float32)[None, :]
        m = dict(shared)
        m.update({"xk": xk, "cT": fm(c[b], 8), "kbias": kbias.astype(bf), "hvalid": hv})
        maps.append(m)
    return maps


def kernel(**inputs):
    if "nc" not in _NC_CACHE:
        _NC_CACHE["nc"] = build(False)
    nc = _NC_CACHE["nc"]
    maps = _in_maps(inputs)
    res = run_bass_kernel_spmd(nc, maps, core_ids=list(range(8)))
    out = np.zeros((2, S, D), np.float32)
    for core in range(8):
        b, r = core // 4, core % 4
        o = np.asarray(res.results[core]["out"], np.float32)
        for g in range(4):
            p0 = 2048 * g + 512 * r
            out[b, p0:p0 + 512] = o[g * 512:(g + 1) * 512]
    return out
```

```python
import contextlib
import numpy as np
import ml_dtypes
import concourse.bass as bass
import concourse.mybir as mybir
from concourse.bass_utils import run_bass_kernel_spmd

F32 = mybir.dt.float32
BF16 = mybir.dt.bfloat16
FP8 = mybir.dt.float8e4
AF = mybir.ActivationFunctionType
ALU = mybir.AluOpType
AX = mybir.AxisListType

PE, ACT, DVE, POOL, SP = "tensor", "scalar", "vector", "gpsimd", "sync"
ENGS = [PE, ACT, DVE, POOL, SP]
NDMASEM = 32
STQ = "gpsimd"
NPOOLSEM = 16

S = 8192
D = 1024
NT = 16
EPS = 1e-6
NEG = -30000.0
NITER = 14
EXPB = -16.0
MASKB = 240000.0
ACT_SHARE = 0.0


class Res:
    __slots__ = ("w", "rs")

    def __init__(self):
        self.w = None
        self.rs = []


class Op:
    __slots__ = ("eng", "fn", "deps", "signal", "dma", "dsem", "dval", "prev_dma")

    def __init__(self, eng, fn, dma):
        self.eng = eng; self.fn = fn; self.deps = []
        self.signal = False; self.dma = dma; self.dsem = None; self.dval = 0; self.prev_dma = None


class Prog:
    def __init__(self, nc):
        self.nc = nc
        self.ops = {e: [] for e in ENGS}
        self.ndma = 0
        self.npool = 0
        self.dma_last = [None] * (NDMASEM + NPOOLSEM)
        self.bar = {e: [] for e in ENGS}

    def barrier(self):
        lasts = []
        for e in ENGS:
            for o in reversed(self.ops[e]):
                if not o.dma:
                    lasts.append(o)
                    break
        lasts += [p for p in self.dma_last if p is not None]
        for e in ENGS:
            self.bar[e] = list(lasts)

    def op(self, eng, fn, reads=(), writes=(), dma=False):
        o = Op(eng, fn, dma)
        deps = list(self.bar[eng])
        self.bar[eng] = []
        for r in reads:
            if r.w is not None:
                deps.append(r.w)
        for w in writes:
            if w.w is not None:
                deps.append(w.w)
            deps.extend(w.rs)
        seen = set()
        for d in deps:
            if d is o or id(d) in seen:
                continue
            seen.add(id(d))
            if d.eng == eng and not d.dma and (eng == PE or eng == SP):
                continue
            o.deps.append(d)
            if not d.dma:
                d.signal = True
        if dma:
            if eng == POOL:
                slot = NDMASEM + (self.npool % NPOOLSEM)
                o.dval = 16 * (self.npool // NPOOLSEM + 1)
                self.npool += 1
            else:
                slot = self.ndma % NDMASEM
                o.dval = 16 * (self.ndma // NDMASEM + 1)
                self.ndma += 1
            o.dsem = slot
            o.prev_dma = self.dma_last[slot]
            self.dma_last[slot] = o
        self.ops[eng].append(o)
        for r in reads:
            r.rs.append(o)
        for w in writes:
            w.w = o
            w.rs = []
        return o

    def emit(self):
        nc = self.nc
        sigval = {}
        for e in ENGS:
            c = 0
            for o in self.ops[e]:
                if o.signal and not o.dma:
                    c += 1
                    sigval[id(o)] = c
        with contextlib.ExitStack() as st:
            esem = {e: st.enter_context(nc.semaphore("s_" + e)) for e in ENGS}
            dsem = [st.enter_context(nc.semaphore("d%d" % i)) for i in range(NDMASEM + NPOOLSEM)]
            block = st.enter_context(nc.Block())
            prog = self

            def run(engname, eng):
                waited = {}

                def wait(key, sem, val):
                    if waited.get(key, 0) >= val:
                        return
                    eng.wait_ge(sem, val)
                    waited[key] = val

                for o in prog.ops[engname]:
                    need = {}
                    for d in o.deps:
                        if d.dma:
                            k_, s_, v_ = ("d", d.dsem), dsem[d.dsem], d.dval
                        else:
                            k_, s_, v_ = ("e", d.eng), esem[d.eng], sigval[id(d)]
                        if k_ not in need or need[k_][1] < v_:
                            need[k_] = (s_, v_)
                    if o.dma and o.prev_dma is not None:
                        p = o.prev_dma
                        k_ = ("d", p.dsem)
                        if k_ not in need or need[k_][1] < p.dval:
                            need[k_] = (dsem[p.dsem], p.dval)
                    for k_, (s_, v_) in need.items():
                        wait(k_, s_, v_)
                    ins = o.fn(eng)
                    if o.dma:
                        ins.then_inc(dsem[o.dsem], 16)
                    elif o.signal:
                        ins.then_inc(esem[engname], 1)
                if engname == SP:
                    for p in prog.dma_last:
                        if p is not None:
                            wait(("d", p.dsem), dsem[p.dsem], p.dval)

            @block.tensor
            def _(eng):
                run(PE, eng)

            @block.scalar
            def _(eng):
                run(ACT, eng)

            @block.vector
            def _(eng):
                run(DVE, eng)

            @block.gpsimd
            def _(eng):
                run(POOL, eng)

            @block.sync
            def _(eng):
                run(SP, eng)


class Arena:
    def __init__(self, nc, kb):
        self.words = kb * 256
        self.t = nc.alloc_sbuf_tensor("arena", [128, self.words], F32)
        self.off = 0
        self.marks = []

    def mark(self):
        self.marks.append(self.off)

    def release(self):
        self.off = self.marks.pop()

    def tile(self, shape, dt):
        n = 1
        for s in shape:
            n *= s
        esz = 4 if dt == F32 else (1 if dt == FP8 else 2)
        words = (n * esz + 3) // 4
        words = (words + 7) // 8 * 8
        assert self.off + words <= self.words, ("arena overflow", self.off, words, self.words)
        ap = self.t[:, self.off:self.off + words]
        self.off += words
        if dt != F32:
            ap = ap.bitcast(dt)
        ap = ap[:, 0:n]
        if len(shape) == 2:
            ap = ap.rearrange("p (a b) -> p a b", b=shape[1])
        elif len(shape) == 3:
            ap = ap.rearrange("p (a b c) -> p a b c", b=shape[1], c=shape[2])
        return ap


def build(debug=False):
    nc = bass.Bass("TRN2", target_bir_lowering=False)

    def din(name, shape, dt=F32):
        return nc.dram_tensor(name, shape, dt, kind="ExternalInput").ap()

    def dscr(name, shape, dt=BF16):
        return nc.dram_tensor(name, shape, dt, kind="ExternalOutput" if debug else "Internal").ap()

    xk = din("xk", [S, D])
    cT = din("cT", [128, 8])
    w_ada = din("w_ada", [D, 6144])
    b_adaT = din("b_adaT", [128, 48])
    gmixT = din("gmixT", [128, 8]); gffnT = din("gffnT", [128, 8]); gfinT = din("gfinT", [128, 8])
    w_in = din("w_in", [D, 5192])
    w_dwT = din("w_dwT", [128, 4, 31])
    b_dwT = din("b_dwT", [128, 4]); lngT = din("lngT", [128, 4]); lnbT = din("lnbT", [128, 4])
    w_ap = din("w_ap", [512, D]); w_cp = din("w_cp", [512, D]); w_out = din("w_out", [D, D])
    w_fi = din("w_fi", [D, 5632]); w_fo = din("w_fo", [2816, D])
    ident_d = din("ident", [128, 128], BF16)
    identf_d = din("identf", [128, 128])
    kaug_d = din("kaug", [3, 8, S], BF16)
    kbias_d = din("kbias", [1, S], BF16)
    cb_d = din("cb", [128, 4, 512], BF16)
    hvalid_d = din("hvalid", [128, 128])
    kcp1_d = din("kcp1", [128, 64])
    out_d = nc.dram_tensor("out", [2048, D], F32, kind="ExternalOutput").ap()

    w_in_b = dscr("w_in_b", [D, 5192]); w_ap_b = dscr("w_ap_b", [512, D]); w_cp_b = dscr("w_cp_b", [512, D])
    w_out_b = dscr("w_out_b", [D, D]); w_fi_b = dscr("w_fi_b", [D, 5632]); w_fo_b = dscr("w_fo_b", [2816, D])
    kT_s = dscr("kT_s", [8, 64, S]); v_s = dscr("v_s", [S, 520])
    qT_s = dscr("qT_s", [8, 64, 2048]); qiT_s = dscr("qiT_s", [4, 128, 2048])
    zT_s = dscr("zT_s", [4, 128, 4, 544]); gaT_s = dscr("gaT_s", [8, 128, 2048]); gbT_s = dscr("gbT_s", [8, 128, 2048])
    OT_s = dscr("OT_s", [4, 128, 2048])
    if debug:
        dbg_sc = nc.dram_tensor("dbg_sc", [16, 128, S], F32, kind="ExternalOutput").ap()
        dbg_thr = nc.dram_tensor("dbg_thr", [128, 16 * 4], F32, kind="ExternalOutput").ap()

    P = Prog(nc)
    A = Arena(nc, 204)
    ps_cm = nc.psum_tensor("ps", [128, 8, 512], F32)
    ps = ps_cm.__enter__()
    psb = [ps[:, b, :].bitcast(BF16) for b in range(8)]
    rb = [Res() for _ in range(8)]

    def dma(out, in_, reads=(), writes=(), eng=SP, **kw):
        return P.op(eng, lambda e: e.dma_start(out=out, in_=in_, **kw), reads=reads, writes=writes, dma=True)

    ident = A.tile([128], BF16); r_c = Res()
    identf = A.tile([128], F32)
    onesf = A.tile([128], F32)
    modT = A.tile([48], F32); r_mod = Res()
    ab = A.tile([4, 8], F32)
    gfin = A.tile([8], F32)
    wiall = A.tile([16, 8], F32); r_wi = Res()
    hval = A.tile([128], F32)
    kcp1 = A.tile([64], F32)
    dma(ident, ident_d, writes=[r_c]); dma(identf, identf_d, writes=[r_c]); dma(hval, hvalid_d, writes=[r_c])
    dma(kcp1, kcp1_d, writes=[r_c]); dma(gfin, gfinT, writes=[r_c])
    P.op(DVE, lambda e: e.memset(onesf, 1.0), writes=[r_c])

    r_cast = {}
    for name, src, dst in [("w_in", w_in, w_in_b), ("w_ap", w_ap, w_ap_b), ("w_cp", w_cp, w_cp_b), ("w_out", w_out, w_out_b),
                           ("w_fi", w_fi, w_fi_b), ("w_fo", w_fo, w_fo_b)]:
        r_cast[name] = Res()
        dma(dst, src, writes=[r_cast[name]], eng=POOL, max_dma_last_dim=4096)

    A.mark()
    kiT = A.tile([S], BF16); r_ki = Res()
    A.mark()
    cts = A.tile([8], F32); cact = A.tile([8], F32); r_ca = Res()
    bada = A.tile([48], F32); gm = A.tile([8], F32); gf = A.tile([8], F32)
    dma(cts, cT, writes=[r_ca]); dma(bada, b_adaT, writes=[r_ca]); dma(gm, gmixT, writes=[r_ca]); dma(gf, gffnT, writes=[r_ca])
    P.op(ACT, lambda e: e.activation(out=cact, in_=cts, func=AF.Silu), reads=[r_ca], writes=[r_ca])
    wa = [A.tile([8, 768], F32) for _ in range(2)]; r_wa = [Res(), Res()]
    w_ada_v = w_ada.rearrange("(k p) n -> p k n", p=128)
    r_mod2 = Res()

    def ada_piece(j, bank=0, r_dst=None):
        b = j % 2
        dma(wa[b], w_ada_v[:, :, j * 768:(j + 1) * 768], writes=[r_wa[b]])
        for m in range(6):
            col = j * 6 + m
            for kc in range(8):
                P.op(PE, lambda e, b=b, m=m, kc=kc, col=col: e.matmul(ps[:, bank, col:col + 1], lhsT=wa[b][:, kc, m * 128:(m + 1) * 128],
                                                                  rhs=cact[:, kc:kc + 1], start=(kc == 0), stop=(kc == 7)),
                     reads=[r_wa[b], r_ca], writes=[rb[bank]])
        if r_dst is not None:
            c0_, c1_ = j * 6, j * 6 + 6
            P.op(DVE, lambda e: e.tensor_tensor(out=modT[:, c0_:c1_], in0=ps[:, bank, c0_:c1_], in1=bada[:, c0_:c1_], op=ALU.add), reads=[rb[bank], r_ca], writes=[r_dst])

    for j in range(3):
        ada_piece(j)
    P.op(DVE, lambda e: e.tensor_tensor(out=modT[:, 0:18], in0=ps[:, 0, 0:18], in1=bada[:, 0:18], op=ALU.add), reads=[rb[0], r_ca], writes=[r_mod])
    P.op(DVE, lambda e: e.tensor_scalar(out=ab[:, 1, :], in0=modT[:, 8:16], scalar1=1.0, scalar2=None, op0=ALU.add), writes=[r_mod])
    P.op(DVE, lambda e: e.tensor_tensor(out=ab[:, 0, :], in0=ab[:, 1, :], in1=gm, op=ALU.mult), reads=[r_ca], writes=[r_mod])

    def ada_finish():
        P.op(DVE, lambda e: e.tensor_scalar(out=ab[:, 3, :], in0=modT[:, 32:40], scalar1=1.0, scalar2=None, op0=ALU.add), writes=[r_mod2])
        P.op(DVE, lambda e: e.tensor_tensor(out=ab[:, 2, :], in0=ab[:, 3, :], in1=gf, op=ALU.mult), reads=[r_ca], writes=[r_mod2])
    a1 = ab[:, 0, :]; b1 = modT[:, 0:8]; a2 = ab[:, 2, :]; b2 = modT[:, 24:32]; g_m = modT[:, 16:24]; g_f = modT[:, 40:48]

    def prep_front(xrows, nblk, np_, xt, r_xt, xn, r_xn, st, r_st):
        dma(xt[0:np_, 0:nblk, :], xrows.rearrange("(b p) d -> p b d", p=np_), writes=[r_xt])
        for bl in range(nblk):
            P.op(ACT, lambda e, bl=bl: e.activation(out=xn[0:np_, bl, :], in_=xt[0:np_, bl, :], func=AF.Square, accum_out=st[0:np_, bl:bl + 1]),
                 reads=[r_xt], writes=[r_xn, r_st])
        P.op(ACT, lambda e: e.activation(out=st[0:np_, 4:4 + nblk], in_=st[0:np_, 0:nblk], func=AF.Sqrt, bias=EPS, scale=1.0 / D), writes=[r_st])
        P.op(DVE, lambda e: e.reciprocal(out=st[0:np_, 8:8 + nblk], in_=st[0:np_, 4:4 + nblk]), reads=[r_st], writes=[r_st])
        for bl in range(nblk):
            P.op(DVE, lambda e, bl=bl: e.tensor_scalar(out=xn[0:np_, bl, :], in0=xt[0:np_, bl, :], scalar1=st[0:np_, 8 + bl:9 + bl], scalar2=None, op0=ALU.mult),
                 reads=[r_xt, r_st], writes=[r_xn])

    def prep_back(nblk, np_, xn, r_xn, hT, r_hTk, banks, avec, bvec):
        ntok = nblk * np_
        for kc in range(8):
            bk = banks[kc // 2]
            for bl in range(nblk):
                o = (kc % 2) * 512 + bl * np_
                P.op(PE, lambda e, kc=kc, bl=bl, bk=bk, o=o: e.transpose(out=psb[bk][:, o:o + np_], in_=xn[0:np_, bl, kc * 128:(kc + 1) * 128],
                                                                     identity=ident[0:np_, 0:np_]),
                     reads=[r_xn, r_c], writes=[rb[bk]])
            if kc % 2 == 1:
                for k2 in (kc - 1, kc):
                    o = (k2 % 2) * 512
                    eng_ = DVE
                    if eng_ == DVE:
                        P.op(DVE, lambda e, k2=k2, bk=bk, o=o: e.tensor_scalar(out=hT[:, k2, 0:ntok], in0=psb[bk][:, o:o + ntok], scalar1=avec[:, k2:k2 + 1],
                                                                           scalar2=bvec[:, k2:k2 + 1], op0=ALU.mult, op1=ALU.add),
                             reads=[rb[bk], r_mod], writes=[r_hTk[k2]])
                    else:
                        P.op(ACT, lambda e, k2=k2, bk=bk, o=o: e.activation(out=hT[:, k2, 0:ntok], in_=psb[bk][:, o:o + ntok], func=AF.Identity, scale=avec[:, k2:k2 + 1],
                                                                        bias=bvec[:, k2:k2 + 1]),
                             reads=[rb[bk], r_mod], writes=[r_hTk[k2]])

    def prep(xrows, nblk, np_, xt, r_xt, xn, r_xn, hT, r_hT, st, r_st, banks, avec, bvec):
        prep_front(xrows, nblk, np_, xt, r_xt, xn, r_xn, st, r_st)
        prep_back(nblk, np_, xn, r_xn, hT, [r_hT] * 8, banks, avec, bvec)

    def load_w(dst, src_b, rows, c0, c1, r_w, cname="w_in"):
        dma(dst, src_b.rearrange("(k p) n -> p k n", p=128)[:, :, c0:c1], reads=[r_cast[cname]], writes=[r_w])

    evac_rr = [0]

    def evac(out, in_, reads, writes, func=None, scale=1.0):
        evac_rr[0] += 1
        if func is not None or evac_rr[0] % 2 == 0:
            f = func if func is not None else AF.Copy
            return P.op(ACT, lambda e: e.activation(out=out, in_=in_, func=f, scale=scale), reads=reads, writes=writes)
        if scale != 1.0:
            return P.op(DVE, lambda e: e.tensor_scalar(out=out, in0=in_, scalar1=scale, scalar2=None, op0=ALU.mult), reads=reads, writes=writes)
        return P.op(DVE, lambda e: e.tensor_copy(out=out, in_=in_), reads=reads, writes=writes)

    A.mark()
    wk = A.tile([8, 512], BF16); wv = A.tile([8, 512], BF16); wki = A.tile([8, 128], BF16); r_w1 = Res()
    wst = [A.tile([8, 512], F32) for _ in range(2)]; r_wst = [Res(), Res()]
    w_in_v = w_in.rearrange("(k p) n -> p k n", p=128)
    dma(wst[0], w_in_v[:, :, 512:1024], writes=[r_wst[0]])
    dma(wst[1], w_in_v[:, :, 1024:1536], writes=[r_wst[1]])
    P.op(ACT, lambda e: e.activation(out=wk, in_=wst[0], func=AF.Copy), reads=[r_wst[0]], writes=[r_w1])
    P.op(DVE, lambda e: e.tensor_copy(out=wv, in_=wst[1]), reads=[r_wst[1]], writes=[r_w1])
    dma(wst[0][:, :, 0:64], w_in_v[:, :, 2048:2112], writes=[r_wst[0]])
    P.op(ACT, lambda e: e.activation(out=wki[:, :, 0:64], in_=wst[0][:, :, 0:64], func=AF.Copy), reads=[r_wst[0]], writes=[r_w1])
    P.op(ACT, lambda e: e.activation(out=wki[:, :, 64:128], in_=wst[0][:, :, 0:64], func=AF.Copy), reads=[r_wst[0]], writes=[r_w1])
    xts = [A.tile([4, D], F32) for _ in range(2)]; r_xts = [Res(), Res()]
    xns = [A.tile([4, D], BF16) for _ in range(2)]; r_xns = [Res(), Res()]
    hTs = [A.tile([8, 512], BF16) for _ in range(2)]; r_hTs = [[Res() for _ in range(8)] for _ in range(2)]
    sts = [A.tile([12], F32) for _ in range(2)]; r_sts = [Res(), Res()]
    ksts = [A.tile([4, 512], BF16) for _ in range(2)]; r_ksts = [Res(), Res()]
    vsts = [A.tile([4, 8, 65], BF16) for _ in range(2)]; r_vsts = [Res(), Res()]
    for b in range(2):
        P.op(DVE, lambda e, b=b: e.memset(vsts[b], 1.0), writes=[r_vsts[b]])
    kT_v = kT_s.rearrange("(m hh) d s -> (hh d) m s", hh=2)
    v_v = v_s.rearrange("(n p) c -> p n c", p=128)

    def front1a(T):
        b = T % 2
        prep_front(xk[T * 512:(T + 1) * 512, :], 4, 128, xts[b], r_xts[b], xns[b], r_xns[b], sts[b], r_sts[b])

    def back1a(T):
        b = T % 2
        prep_back(4, 128, xns[b], r_xns[b], hTs[b], r_hTs[b], [0, 1, 2, 3], a1, b1)

    front1a(0)
    back1a(0)
    for T in range(NT):
        b = T % 2
        hT = hTs[b]
        if T + 1 < NT:
            front1a(T + 1)
        for m in range(4):
            bk = 4 + (m % 2)
            for kc in range(8):
                P.op(PE, lambda e, m=m, kc=kc, bk=bk, hT=hT: e.matmul(ps[:, bk, :], lhsT=wk[:, kc, m * 128:(m + 1) * 128], rhs=hT[:, kc, :],
                                                                  start=(kc == 0), stop=(kc == 7)), reads=[r_w1, r_hTs[b][kc]], writes=[rb[bk]])
            evac(ksts[b][:, m, :], ps[:, bk, :], [rb[bk]], [r_ksts[b]])
        dma(kT_v[:, :, T * 512:(T + 1) * 512], ksts[b], reads=[r_ksts[b]], eng=STQ)
        for bl in range(4):
            bk = 6 + (bl % 2)
            for kc in range(8):
                P.op(PE, lambda e, bl=bl, kc=kc, bk=bk, hT=hT: e.matmul(ps[:, bk, :], lhsT=hT[:, kc, bl * 128:(bl + 1) * 128], rhs=wv[:, kc, :],
                                                                    start=(kc == 0), stop=(kc == 7)), reads=[r_w1, r_hTs[b][kc]], writes=[rb[bk]])
            evac(vsts[b][:, bl, :, 0:64], ps[:, bk, :].rearrange("p (h d) -> p h d", d=64), [rb[bk]], [r_vsts[b]])
        dma(v_v[:, T * 4:(T + 1) * 4, :], vsts[b].rearrange("p n h c -> p n (h c)"), reads=[r_vsts[b]], eng=STQ)
        for kc in range(8):
            P.op(PE, lambda e, kc=kc, hT=hT: e.matmul(ps[:, 4, :], lhsT=wki[:, kc, :], rhs=hT[:, kc, :], start=(kc == 0), stop=(kc == 7)),
                 reads=[r_w1, r_hTs[b][kc]], writes=[rb[4]])
        evac(kiT[:, T * 512:(T + 1) * 512], ps[:, 4, :], [rb[4]], [r_ki])
        if T + 1 < NT:
            back1a(T + 1)
        if T < 5:
            ada_piece(3 + T, bank=4, r_dst=r_mod2)
        if T == 5:
            ada_finish()
    P.barrier()
    A.release()
    A.release()

    A.mark()
    wq = A.tile([8, 512], BF16); wqi = A.tile([8, 512], BF16); wwi = A.tile([8, 8], BF16)
    wu = A.tile([8, 1024], BF16); wga = A.tile([8, 1024], BF16); wgb = A.tile([8, 1024], BF16); r_w2 = Res()
    load_w(wq, w_in_b, D, 0, 512, r_w2); load_w(wqi, w_in_b, D, 1536, 2048, r_w2); load_w(wwi, w_in_b, D, 2112, 2120, r_w2)
    load_w(wu, w_in_b, D, 2120, 3144, r_w2); load_w(wga, w_in_b, D, 3144, 4168, r_w2); load_w(wgb, w_in_b, D, 4168, 5192, r_w2)
    xt = A.tile([4, D], F32); r_xt = Res(); xn = A.tile([4, D], BF16); r_xn = Res()
    hT = A.tile([8, 512], BF16); r_hT = Res(); st = A.tile([12], F32); r_st = Res()
    xth = A.tile([1, D], F32); r_xth = Res(); xnh = A.tile([1, D], BF16); r_xnh = Res()
    hTh = A.tile([8, 32], BF16); r_hTh = Res(); sth = A.tile([12], F32); r_sth = Res()
    stg = [A.tile([512], BF16) for _ in range(3)]; r_stg = [Res() for _ in range(3)]
    sgm = [A.tile([512], F32) for _ in range(2)]; r_sgm = [Res(), Res()]
    stg_i = [0]
    qT_v = qT_s.rearrange("(m hh) d s -> (hh d) m s", hh=2)

    def stage_out(dst, src_ps, bk, func=None):
        i = stg_i[0] % 3
        stg_i[0] += 1
        evac(stg[i], src_ps, [rb[bk]], [r_stg[i]], func=func)
        dma(dst, stg[i], reads=[r_stg[i]], eng=STQ)

    def proj_fm(w, m, hT_, r_h, bk, n):
        for kc in range(8):
            P.op(PE, lambda e, kc=kc: e.matmul(ps[:, bk, 0:n], lhsT=w[:, kc, m * 128:(m + 1) * 128], rhs=hT_[:, kc, 0:n], start=(kc == 0), stop=(kc == 7)),
                 reads=[r_w2, r_h], writes=[rb[bk]])

    for g in range(4):
        T = 4 * g + 3
        prep(xk[T * 512:(T + 1) * 512, :], 4, 128, xt, r_xt, xn, r_xn, hT, r_hT, st, r_st, [0, 1, 2, 3], a1, b1)
        cs = slice(g * 512, (g + 1) * 512)
        bkc = [0]

        def nb():
            bkc[0] += 1
            return 4 + bkc[0] % 4
        for m in range(4):
            bk = nb(); proj_fm(wq, m, hT, r_hT, bk, 512); stage_out(qT_v[:, m, cs], ps[:, bk, :], bk)
        for m in range(4):
            bk = nb(); proj_fm(wqi, m, hT, r_hT, bk, 512); stage_out(qiT_s[m, :, cs], ps[:, bk, :], bk)
        for m in range(8):
            bk = nb(); proj_fm(wga, m, hT, r_hT, bk, 512); stage_out(gaT_s[m, :, cs], ps[:, bk, :], bk, func=AF.Sigmoid)
        for m in range(8):
            bk = nb(); proj_fm(wgb, m, hT, r_hT, bk, 512); stage_out(gbT_s[m, :, cs], ps[:, bk, :], bk, func=AF.Sigmoid)
        for bl in range(4):
            bk = nb()
            for kc in range(8):
                P.op(PE, lambda e, kc=kc, bl=bl, bk=bk: e.matmul(ps[:, bk, 0:8], lhsT=hT[:, kc, bl * 128:(bl + 1) * 128], rhs=wwi[:, kc, :], start=(kc == 0), stop=(kc == 7)),
                     reads=[r_w2, r_hT], writes=[rb[bk]])
            P.op(DVE, lambda e, bl=bl, bk=bk, g=g: e.tensor_scalar(out=wiall[:, g * 4 + bl, :], in0=ps[:, bk, 0:8], scalar1=float(8 ** -0.5 * 64 ** -0.5), scalar2=None, op0=ALU.mult),
                 reads=[rb[bk]], writes=[r_wi])
        for i in range(4):
            bka = nb(); proj_fm(wu, i, hT, r_hT, bka, 512)
            bkg = nb(); proj_fm(wu, 4 + i, hT, r_hT, bkg, 512)
            si = i % 2
            P.op(ACT, lambda e, si=si, bkg=bkg: e.activation(out=sgm[si], in_=ps[:, bkg, :], func=AF.Sigmoid), reads=[rb[bkg]], writes=[r_sgm[si]])
            j = stg_i[0] % 3; stg_i[0] += 1
            P.op(DVE, lambda e, si=si, bka=bka, j=j: e.tensor_tensor(out=stg[j], in0=ps[:, bka, :], in1=sgm[si], op=ALU.mult), reads=[rb[bka], r_sgm[si]], writes=[r_stg[j]])
            dma(zT_s[i, :, g, 32:544], stg[j], reads=[r_stg[j]], eng=STQ)
        prep(xk[T * 512 - 32:T * 512, :], 1, 32, xth, r_xth, xnh, r_xnh, hTh, r_hTh, sth, r_sth, [0, 1, 2, 3], a1, b1)
        for i in range(4):
            bka = nb(); proj_fm(wu, i, hTh, r_hTh, bka, 32)
            bkg = nb(); proj_fm(wu, 4 + i, hTh, r_hTh, bkg, 32)
            si = i % 2
            P.op(ACT, lambda e, si=si, bkg=bkg: e.activation(out=sgm[si][:, 0:32], in_=ps[:, bkg, 0:32], func=AF.Sigmoid), reads=[rb[bkg]], writes=[r_sgm[si]])
            j = stg_i[0] % 3; stg_i[0] += 1
            P.op(DVE, lambda e, si=si, bka=bka, j=j: e.tensor_tensor(out=sgm[si][:, 32:64], in0=ps[:, bka, 0:32], in1=sgm[si][:, 0:32], op=ALU.mult), reads=[rb[bka]], writes=[r_sgm[si]])
            P.op(DVE, lambda e, si=si, j=j, g=g: e.tensor_tensor(out=stg[j][:, 0:32], in0=sgm[si][:, 32:64], in1=hval[:, g * 32:(g + 1) * 32], op=ALU.mult), reads=[r_c], writes=[r_stg[j], r_sgm[si]])
            dma(zT_s[i, :, g, 0:32], stg[j][:, 0:32], reads=[r_stg[j]], eng=STQ)
    P.barrier()
    A.release()

    A.mark()
    cbt = A.tile([4, 512], BF16); kbt = A.tile([1536], BF16); r_c2 = Res()
    dma(cbt, cb_d, writes=[r_c2]); dma(kbt[0:1, :], kbias_d[:, 0:1536], writes=[r_c2])
    onesb = A.tile([128], BF16)
    nidm = A.tile([128], BF16)
    P.op(DVE, lambda e: e.memset(onesb, 1.0), writes=[r_c2])
    P.op(DVE, lambda e: e.tensor_scalar(out=nidm, in0=ident, scalar1=-MASKB, scalar2=None, op0=ALU.mult), reads=[r_c], writes=[r_c2])
    scoress = [A.tile([S], F32) for _ in range(2)]; r_scs = [Res(), Res()]
    maskq = A.tile([S], BF16); r_mq = Res()
    junk = maskq; r_junk = r_mq
    maskTs = [A.tile([64, 256], FP8) for _ in range(2)]; r_mTs = [Res(), Res()]
    qit = A.tile([4, 256], BF16); r_qit = Res()
    qaugs = [A.tile([8, 256], BF16) for _ in range(2)]; r_qas = [Res(), Res()]
    wdgs = [A.tile([8, 128], BF16) for _ in range(2)]; r_wdgs = [Res(), Res()]
    Rt = [A.tile([2, 512], BF16) for _ in range(3)]; r_Rt = [Res() for _ in range(3)]
    bs = A.tile([16], F32); r_bs = Res()
    bsa = A.tile([4], F32); r_bsa = Res(); r_junkA = Res()
    cntk = A.tile([64], F32); r_ck = Res()
    U = A.tile([68], F32); r_U = Res()
    kt = [A.tile([8, 512], BF16) for _ in range(2)]; r_kt = [Res(), Res()]
    vt = [A.tile([4, 520], BF16) for _ in range(2)]; r_vt = [Res(), Res()]
    Pt = [A.tile([2, 256], BF16) for _ in range(3)]; r_Pt = [Res() for _ in range(3)]
    den = A.tile([16], F32); r_den = Res()
    Ob = A.tile([2, 512], BF16); r_Ob = Res()
    OTst = A.tile([4, 256], BF16); r_OT = Res()
    P.op(DVE, lambda e: e.memset(U, 0.0), writes=[r_U])
    P.op(DVE, lambda e: e.memset(U[:, 64:66], 1.0), writes=[r_U])
    for i in range(2):
        P.op(DVE, lambda e, i=i: e.memset(qaugs[i], 0.0), writes=[r_qas[i]])
    kT_hv = kT_s.rearrange("h d s -> d h s")
    kaug_v = kaug_d
    v_v2 = v_s.rearrange("(n p) c -> p n c", p=128)
    LB = 2
    SB = 7

    def idx_gen(qb):
        qp, q2 = qb // 2, qb % 2
        g = qp // 2
        E = 2048 * (g + 1)
        nch = E // 512
        c0 = g * 512 + (qp % 2) * 256
        j4 = qb % 4
        scores = scoress[qb % 2]; r_sc = r_scs[qb % 2]
        wdg = wdgs[qb % 2]; r_wdg = r_wdgs[qb % 2]
        if q2 == 0:
            dma(qit, qiT_s.rearrange("m p s -> p m s")[:, :, c0:c0 + 256], writes=[r_qit])
        for j in range(8):
            P.op(DVE, lambda e, j=j: e.tensor_scalar(out=wdg[:, j, :], in0=ident, scalar1=wiall[:, qb, j:j + 1], scalar2=None, op0=ALU.mult),
                 reads=[r_wi, r_c], writes=[r_wdg])
        yield 0.5
        units = [(c, m) for c in range(nch) for m in range(4)]

        def emit_diag(u):
            c, m = units[u]
            ks = slice(c * 512, (c + 1) * 512)
            has_kb = (c < 3)
            has_cb = (c == nch - 1)
            lastdiag = not (has_kb or has_cb)
            ri = u % 3
            for hh in range(2):
                imm = 2 * m + hh
                P.op(PE, lambda e, m=m, hh=hh, ri=ri, imm=imm, lastdiag=lastdiag: e.matmul(ps[:, SB, :], lhsT=wdg[:, 2 * m + hh, :], rhs=Rt[ri][:, hh, :], start=(imm == 0), stop=(lastdiag and imm == 7)),
                     reads=[r_wdg, r_Rt[ri]], writes=[rb[SB]])
            if m == 3:
                if has_kb:
                    P.op(PE, lambda e, ks=ks, has_cb=has_cb: e.matmul(ps[:, SB, :], lhsT=onesb[0:1, :], rhs=kbt[0:1, ks], start=False, stop=(not has_cb)),
                         reads=[r_c2], writes=[rb[SB]])
                if has_cb:
                    P.op(PE, lambda e: e.matmul(ps[:, SB, :], lhsT=ident, rhs=cbt[:, j4, :], start=False, stop=True), reads=[r_c2, r_c], writes=[rb[SB]])
                P.op(ACT, lambda e, ks=ks: e.activation(out=scores[:, ks], in_=ps[:, SB, :], func=AF.Copy), reads=[rb[SB]], writes=[r_sc])

        for u, (c, m) in enumerate(units):
            ks = slice(c * 512, (c + 1) * 512)
            for hh in range(2):
                pr = slice(hh * 64, hh * 64 + 64)
                P.op(PE, lambda e, m=m, hh=hh, pr=pr, ks=ks: e.matmul(ps[:, LB + hh, :], lhsT=qit[pr, m, q2 * 128:(q2 + 1) * 128], rhs=kiT[pr, ks],
                                                                start=True, stop=True), reads=[r_qit, r_ki], writes=[rb[LB + hh]])
            ri = u % 3
            P.op(ACT, lambda e, ri=ri: e.activation(out=Rt[ri], in_=ps[:, LB:LB + 2, :], func=AF.Relu), reads=[rb[LB], rb[LB + 1]], writes=[r_Rt[ri]])
            if u > 0:
                emit_diag(u - 1)
            yield 0.8
        emit_diag(len(units) - 1)
        if debug:
            dma(dbg_sc[qb, :, 0:E], scores[:, 0:E], reads=[r_sc])
        yield 0.5

    def bis_gen(qb):
        qp, q2 = qb // 2, qb % 2
        g = qp // 2
        E = 2048 * (g + 1)
        nkc = E // 128
        c0 = g * 512 + (qp % 2) * 256
        scores = scoress[qb % 2]; r_sc = r_scs[qb % 2]
        maskT = maskTs[qp % 2]; r_mT = r_mTs[qp % 2]
        qaug = qaugs[qp % 2]; r_qa = r_qas[qp % 2]
        if q2 == 0:
            dma(qaug[0:64, :, :], qT_s.rearrange("h d s -> d h s")[:, :, c0:c0 + 256], writes=[r_qa])
        sc = scores[:, 0:E]
        tpass = E / 960.0
        P.op(DVE, lambda e: e.tensor_reduce(out=bs[:, 0:1], in_=sc, axis=AX.X, op=ALU.max), reads=[r_sc], writes=[r_bs])
        P.op(DVE, lambda e: e.tensor_scalar(out=bs[:, 9:10], in0=bs[:, 0:1], scalar1=-1.0, scalar2=None, op0=ALU.mult), writes=[r_bs])
        P.op(DVE, lambda e: e.tensor_tensor(out=bs[:, 9:10], in0=bs[:, 9:10], in1=bs[:, 0:1], op=ALU.max), writes=[r_bs])
        P.op(DVE, lambda e: e.tensor_scalar(out=bs[:, 9:10], in0=bs[:, 9:10], scalar1=3.0, scalar2=3.0, op0=ALU.mult, op1=ALU.add), writes=[r_bs])
        P.op(DVE, lambda e: e.tensor_tensor(out=bs[:, 3:4], in0=bs[:, 0:1], in1=bs[:, 9:10], op=ALU.subtract), writes=[r_bs])
        P.op(DVE, lambda e: e.tensor_scalar(out=bs[:, 6:7], in0=bs[:, 3:4], scalar1=20000.0, scalar2=None, op0=ALU.add), writes=[r_bs])
        P.op(DVE, lambda e: e.tensor_tensor(out=bs[:, 7:8], in0=bs[:, 9:10], in1=bs[:, 6:7], op=ALU.subtract), writes=[r_bs])
        yield tpass + 1.0
        for it in range(NITER + 1):
            P.op(DVE, lambda e: e.tensor_scalar(out=junk[:, 0:E], in0=sc, scalar1=bs[:, 3:4], scalar2=None, op0=ALU.is_ge, op1=ALU.add, accum_out=bs[:, 4:5]),
                 reads=[r_sc], writes=[r_junk, r_bs])
            P.op(DVE, lambda e: e.tensor_scalar(out=bs[:, 5:6], in0=bs[:, 4:5], scalar1=255.5, scalar2=None, op0=ALU.is_ge), writes=[r_bs])
            if it == 0:
                P.op(DVE, lambda e: e.tensor_scalar(out=bs[:, 1:2], in0=bs[:, 5:6], scalar1=bs[:, 6:7], scalar2=-20000.0, op0=ALU.mult, op1=ALU.add), writes=[r_bs])
                P.op(DVE, lambda e: e.scalar_tensor_tensor(out=bs[:, 2:3], in0=bs[:, 5:6], scalar=bs[:, 7:8], in1=bs[:, 6:7], op0=ALU.mult, op1=ALU.add), writes=[r_bs])
            else:
                P.op(DVE, lambda e: e.tensor_scalar(out=bs[:, 2:3], in0=bs[:, 2:3], scalar1=0.5, scalar2=None, op0=ALU.mult), writes=[r_bs])
                P.op(DVE, lambda e: e.scalar_tensor_tensor(out=bs[:, 1:2], in0=bs[:, 5:6], scalar=bs[:, 2:3], in1=bs[:, 1:2], op0=ALU.mult, op1=ALU.add), writes=[r_bs])
            P.op(DVE, lambda e: e.scalar_tensor_tensor(out=bs[:, 3:4], in0=bs[:, 2:3], scalar=0.5, in1=bs[:, 1:2], op0=ALU.mult, op1=ALU.add), writes=[r_bs])
            yield tpass + 0.8
        if debug:
            dma(dbg_thr[:, qb * 4:qb * 4 + 4], bs[:, 0:4], reads=[r_bs])
        P.op(DVE, lambda e: e.tensor_scalar(out=maskq[:, 0:E], in0=sc, scalar1=bs[:, 1:2], scalar2=None, op0=ALU.is_ge), reads=[r_sc, r_junkA], writes=[r_mq, r_bs])
        P.op(DVE, lambda e: e.tensor_reduce(out=cntk[:, 0:nkc], in_=maskq[:, 0:E].rearrange("p (a b) -> p a b", b=128), axis=AX.X, op=ALU.add), writes=[r_ck, r_mq])
        P.op(DVE, lambda e: e.tensor_scalar(out=cntk[:, 0:nkc], in0=cntk[:, 0:nkc], scalar1=0.5, scalar2=None, op0=ALU.is_ge), writes=[r_ck])
        P.op(DVE, lambda e: e.tensor_tensor(out=cntk[:, 0:nkc], in0=cntk[:, 0:nkc], in1=kcp1[:, 0:nkc], op=ALU.mult), reads=[r_c], writes=[r_ck])
        P.op(DVE, lambda e: e.tensor_reduce(out=bs[:, 8:9], in_=cntk[:, 0:nkc], axis=AX.X, op=ALU.max), writes=[r_ck, r_bs])
        P.op(DVE, lambda e: e.tensor_scalar(out=U[:, 66:67], in0=bs[:, 8:9], scalar1=-1.0, scalar2=None, op0=ALU.add), writes=[r_U, r_bs])
        yield 2 * tpass
        P.op(PE, lambda e: e.matmul(ps[0:67, LB, 0:128], lhsT=U[:, 0:67], rhs=identf, start=True, stop=True), reads=[r_U, r_c], writes=[rb[LB]])
        for h in range(8):
            P.op(ACT, lambda e, h=h: e.activation(out=qaug[64:67, h, q2 * 128:(q2 + 1) * 128], in_=ps[64:67, LB, 0:128], func=AF.Copy), reads=[rb[LB]], writes=[r_qa])
        yield 1.0
        for k8 in range(nkc // 8):
            for kk in range(8):
                kc = k8 * 8 + kk
                P.op(PE, lambda e, kc=kc, kk=kk: e.transpose(out=psb[LB + 1][:, kk * 128:(kk + 1) * 128], in_=maskq[:, kc * 128:(kc + 1) * 128], identity=ident),
                     reads=[r_mq, r_c], writes=[rb[LB + 1]])
            mo = maskT[:, k8 * 8:(k8 + 1) * 8, q2 * 128:(q2 + 1) * 128]
            mi = psb[LB + 1].rearrange("p (a b) -> p a b", b=128)
            P.op(ACT, lambda e, mo=mo, mi=mi: e.activation(out=mo, in_=mi, func=AF.Copy, scale=-1.0, bias=1.0, saturate=False), reads=[rb[LB + 1]], writes=[r_mT])
            yield 1.0

    accb = [4, 5, 6]

    def att_gen(qp):
        g = qp // 2
        E = 2048 * (g + 1)
        nch = E // 512
        nkc = E // 128
        c0 = g * 512 + (qp % 2) * 256
        maskT = maskTs[qp % 2]; r_mT = r_mTs[qp % 2]
        qaug = qaugs[qp % 2]; r_qa = r_qas[qp % 2]
        first_in_bank = {}
        steps = [(c, kl, hp) for c in range(nch) for kl in range(4) for hp in range(4)]

        def emit_loads(c):
            kb_ = c % 2
            ks = slice(c * 512, (c + 1) * 512)
            dma(kt[kb_][0:64, :, :], kT_hv[:, :, ks], writes=[r_kt[kb_]])
            dma(kt[kb_][64:67, :, :], kaug_v[:, :, ks], writes=[r_kt[kb_]])
            dma(vt[kb_], v_v2[:, c * 4:(c + 1) * 4, :], writes=[r_vt[kb_]])

        def emit_ST(i):
            c, kl, hp = steps[i]
            kb_ = c % 2
            kc = c * 4 + kl
            sbk = i % 2
            for hh in range(2):
                h = hp * 2 + hh
                o = hh * 256
                P.op(PE, lambda e, h=h, o=o, sbk=sbk, kb_=kb_, kl=kl: e.matmul(ps[:, sbk, o:o + 256], lhsT=kt[kb_][0:67, h, kl * 128:(kl + 1) * 128],
                                                                    rhs=qaug[0:67, h, :], start=True, stop=False),
                     reads=[r_kt[kb_], r_qa], writes=[rb[sbk]])
                P.op(PE, lambda e, o=o, sbk=sbk, kc=kc: e.matmul(ps[:, sbk, o:o + 256], lhsT=nidm, rhs=maskT[:, kc, :], start=False, stop=True),
                     reads=[r_mT, r_c2], writes=[rb[sbk]])

        emit_loads(0)
        if nch > 1:
            emit_loads(1)
        emit_ST(0)
        for i, (c, kl, hp) in enumerate(steps):
            kb_ = c % 2
            kc = c * 4 + kl
            sbk = i % 2
            if i + 1 < len(steps):
                emit_ST(i + 1)
            pi = i % 3
            P.op(ACT, lambda e, pi=pi, sbk=sbk: e.activation(out=Pt[pi], in_=ps[:, sbk, :].rearrange("p (a b) -> p a b", b=256), func=AF.Exp, bias=EXPB, scale=0.125),
                 reads=[rb[sbk]], writes=[r_Pt[pi]])
            for q2 in range(2):
                for hh in range(2):
                    h = hp * 2 + hh
                    a = q2 * 8 + h
                    ab_ = accb[a // 6]
                    col = (a % 6) * 65
                    st_ = (kc == 0) and (ab_ not in first_in_bank)
                    if kc == 0:
                        first_in_bank[ab_] = True
                    P.op(PE, lambda e, pi=pi, hh=hh, q2=q2, h=h, ab_=ab_, col=col, st_=st_, kb_=kb_, kl=kl, kc=kc: e.matmul(
                        ps[:, ab_, col:col + 65], lhsT=Pt[pi][:, hh, q2 * 128:(q2 + 1) * 128], rhs=vt[kb_][:, kl, h * 65:(h + 1) * 65],
                        start=st_, stop=(kc == nkc - 1), skip_group_check=True),
                        reads=[r_Pt[pi], r_vt[kb_]], writes=[rb[ab_]])
            if kl == 3 and hp == 3 and c + 2 < nch:
                emit_loads(c + 2)
            yield 0.8
        for a in range(16):
            ab_ = accb[a // 6]; col = (a % 6) * 65
            P.op(DVE, lambda e, a=a, ab_=ab_, col=col: e.tensor_copy(out=den[:, a:a + 1], in_=ps[:, ab_, col + 64:col + 65]), reads=[rb[ab_]], writes=[r_den])
        P.op(DVE, lambda e: e.reciprocal(out=den, in_=den), writes=[r_den])
        for a in range(16):
            ab_ = accb[a // 6]; col = (a % 6) * 65
            q2 = a // 8; h = a % 8
            P.op(DVE, lambda e, a=a, ab_=ab_, col=col, q2=q2, h=h: e.tensor_scalar(out=Ob[:, q2, h * 64:(h + 1) * 64], in0=ps[:, ab_, col:col + 64], scalar1=den[:, a:a + 1], scalar2=None, op0=ALU.mult),
                 reads=[rb[ab_]], writes=[r_Ob, r_den])
        yield 3.0
        for q2 in range(2):
            for m in range(4):
                P.op(PE, lambda e, q2=q2, m=m: e.transpose(out=psb[0][:, (q2 * 4 + m) * 128:(q2 * 4 + m + 1) * 128], in_=Ob[:, q2, m * 128:(m + 1) * 128], identity=ident),
                     reads=[r_Ob, r_c], writes=[rb[0]])
        for q2 in range(2):
            P.op(ACT, lambda e, q2=q2: e.activation(out=OTst[:, :, q2 * 128:(q2 + 1) * 128], in_=psb[0][:, q2 * 512:(q2 + 1) * 512].rearrange("p (a b) -> p a b", b=128), func=AF.Copy),
                 reads=[rb[0]], writes=[r_OT])
        dma(OT_s.rearrange("m p s -> p m s")[:, :, c0:c0 + 256], OTst, reads=[r_OT], eng=STQ)
        yield 1.0

    NQB, NPR = 16, 8
    done = {"IDX": 0, "BIS": 0, "ATT": 0}
    nitems = {"IDX": NQB, "BIS": NQB, "ATT": NPR}
    mk = {"IDX": idx_gen, "BIS": bis_gen, "ATT": att_gen}
    cur = {"IDX": None, "BIS": None, "ATT": None}
    clk = {"IDX": 0.0, "BIS": 0.0, "ATT": 0.0}
    now = [0.0]

    def can_start(s):
        i = done[s]
        if i >= nitems[s]:
            return False
        if s == "IDX":
            return done["BIS"] >= i - 1
        if s == "BIS":
            return done["IDX"] > i and done["ATT"] >= i // 2 - 1
        return done["BIS"] > 2 * i + 1

    while any(done[s] < nitems[s] for s in done):
        cands = []
        for s in ("ATT", "IDX", "BIS"):
            if cur[s] is None and can_start(s):
                cur[s] = mk[s](done[s])
                clk[s] = max(clk[s], now[0])
            if cur[s] is not None:
                cands.append(s)
        assert cands, ("scheduler deadlock", done)
        s = min(cands, key=lambda k: clk[k])
        now[0] = clk[s]
        cost = next(cur[s], None)
        if cost is None:
            cur[s] = None
            done[s] += 1
        else:
            clk[s] += cost
    P.barrier()
    A.release()
    A.release()

    A.mark()
    wap = A.tile([4, D], BF16); wcp = A.tile([4, D], BF16); wo = A.tile([8, D], BF16); r_w3 = Res()
    load_w(wap, w_ap_b, 512, 0, D, r_w3, "w_ap"); load_w(wcp, w_cp_b, 512, 0, D, r_w3, "w_cp"); load_w(wo, w_out_b, D, 0, D, r_w3, "w_out")
    wfo_t = [A.tile([22, 128], BF16) for _ in range(2)]; r_wfo = [Res(), Res()]
    wdw = A.tile([4, 31], F32); cvp = A.tile([3, 4], F32); r_cv = Res()
    dma(wdw, w_dwT, writes=[r_cv]); dma(cvp[:, 0, :], b_dwT, writes=[r_cv]); dma(cvp[:, 1, :], lngT, writes=[r_cv]); dma(cvp[:, 2, :], lnbT, writes=[r_cv])
    wdd = A.tile([31, 128], BF16); r_wdd = Res()
    xtb = [A.tile([D], F32) for _ in range(2)]; r_xtb = [Res(), Res()]
    xTs = [A.tile([8, 512], F32) for _ in range(2)]; r_xTs = [Res(), Res()]
    OT = A.tile([4, 512], BF16); zT = A.tile([4, 544], BF16); r_in3 = Res()
    gab = [A.tile([2, 512], BF16) for _ in range(2)]; r_gab = [Res(), Res()]
    cv = A.tile([4, 512], F32); r_cvo = Res()
    sq = A.tile([512], F32); r_sq = Res()
    stt = A.tile([3, 512], F32); r_stt = Res()
    zc = A.tile([4, 512], BF16); r_zc = Res()
    t1 = A.tile([512], F32); r_t1 = Res()
    mg = A.tile([8, 512], BF16); r_mg = Res()
    h2s = [A.tile([8, 512], BF16) for _ in range(2)]; r_h2s = [Res(), Res()]
    gT = A.tile([22, 512], BF16); r_gT = Res()
    wfi_t = [A.tile([8, 256], BF16) for _ in range(2)]; r_wfi = [Res() for _ in range(2)]
    sl = [A.tile([512], F32) for _ in range(2)]; r_sl = [Res(), Res()]
    sq2 = A.tile([512], F32); r_sq2 = Res()
    rs2 = A.tile([512], F32); r_rs2 = Res()
    yo = A.tile([D], F32); r_yo = Res()
    w_fo_v = w_fo_b.rearrange("(k p) n -> p k n", p=128)
    w_fi_v = w_fi_b.rearrange("(k p) n -> p k n", p=128)

    def rstd_sumsq(n, src_fn, nchunks, sqt, r_sqt, bank, dst, r_dst, rd):
        for m in range(nchunks):
            P.op(ACT, lambda e, m=m: e.activation(out=sqt, in_=src_fn(m), func=AF.Square), reads=rd, writes=[r_sqt])
            P.op(PE, lambda e, m=m: e.matmul(ps[:, bank, :], lhsT=onesf, rhs=sqt, start=(m == 0), stop=(m == nchunks - 1)), reads=[r_sqt, r_c], writes=[rb[bank]])
        P.op(ACT, lambda e: e.activation(out=dst, in_=ps[:, bank, :], func=AF.Sqrt, bias=EPS, scale=1.0 / n), reads=[rb[bank]], writes=[r_dst])
        P.op(DVE, lambda e: e.reciprocal(out=dst, in_=dst), writes=[r_dst])

    def front_gen(g):
        T = 4 * g + 3
        cs = slice(g * 512, (g + 1) * 512)
        xT = xTs[g % 2]; r_xT = r_xTs[g % 2]
        h2 = h2s[g % 2]; r_h2 = r_h2s[g % 2]
        dma(OT, OT_s.rearrange("m p s -> p m s")[:, :, cs], writes=[r_in3])
        dma(zT, zT_s[:, :, g, :].rearrange("m p s -> p m s"), writes=[r_in3])
        for bl in range(4):
            xb_ = xtb[bl % 2]; rxb = r_xtb[bl % 2]
            dma(xb_, xk[T * 512 + bl * 128:T * 512 + (bl + 1) * 128, :], writes=[rxb])
            for half in range(2):
                bk = half
                for k4 in range(4):
                    kc = half * 4 + k4
                    P.op(PE, lambda e, kc=kc, k4=k4, bk=bk, xb_=xb_: e.transpose(out=ps[:, bk, k4 * 128:(k4 + 1) * 128], in_=xb_[:, kc * 128:(kc + 1) * 128], identity=identf),
                         reads=[rxb, r_c], writes=[rb[bk]])
                evac(xT[:, half * 4:half * 4 + 4, bl * 128:(bl + 1) * 128], ps[:, bk, :].rearrange("p (a b) -> p a b", b=128), [rb[bk]], [r_xT])
            yield 3.0
        for i in range(4):
            bk = i % 2
            for k in range(31):
                P.op(DVE, lambda e, i=i, k=k: e.tensor_scalar(out=wdd[:, k, :], in0=ident, scalar1=wdw[:, i, k:k + 1], scalar2=None, op0=ALU.mult),
                     reads=[r_cv, r_c], writes=[r_wdd])
            for k in range(31):
                P.op(PE, lambda e, i=i, k=k, bk=bk: e.matmul(ps[:, bk, :], lhsT=wdd[:, k, :], rhs=zT[:, i, k + 2:k + 514], start=(k == 0), stop=(k == 30)),
                     reads=[r_wdd, r_in3], writes=[rb[bk]])
            P.op(DVE, lambda e, i=i, bk=bk: e.tensor_scalar(out=cv[:, i, :], in0=ps[:, bk, :], scalar1=cvp[:, 0, i:i + 1], scalar2=None, op0=ALU.add),
                 reads=[rb[bk], r_cv], writes=[r_cvo])
            yield 7.0
        for i in range(4):
            P.op(PE, lambda e, i=i: e.matmul(ps[:, 2, :], lhsT=onesf, rhs=cv[:, i, :], start=(i == 0), stop=(i == 3)), reads=[r_cvo, r_c], writes=[rb[2]])
        P.op(ACT, lambda e: e.activation(out=stt[:, 0, :], in_=ps[:, 2, :], func=AF.Copy, scale=1.0 / 512), reads=[rb[2]], writes=[r_stt])
        for i in range(4):
            P.op(DVE, lambda e, i=i: e.tensor_tensor(out=cv[:, i, :], in0=cv[:, i, :], in1=stt[:, 0, :], op=ALU.subtract), reads=[r_stt], writes=[r_cvo])
        yield 4.0
        rstd_sumsq(512, lambda m: cv[:, m, :], 4, sq, r_sq, 3, stt[:, 2, :], r_stt, [r_cvo])
        yield 4.0
        for i in range(4):
            P.op(DVE, lambda e, i=i: e.tensor_tensor(out=cv[:, i, :], in0=cv[:, i, :], in1=stt[:, 2, :], op=ALU.mult), reads=[r_stt], writes=[r_cvo])
            P.op(DVE, lambda e, i=i: e.tensor_scalar(out=cv[:, i, :], in0=cv[:, i, :], scalar1=cvp[:, 1, i:i + 1], scalar2=cvp[:, 2, i:i + 1], op0=ALU.mult, op1=ALU.add),
                 reads=[r_cv], writes=[r_cvo])
            P.op(ACT, lambda e, i=i: e.activation(out=zc[:, i, :], in_=cv[:, i, :], func=AF.Silu), reads=[r_cvo], writes=[r_zc])
        yield 4.0
        for m in range(8):
            b0 = (2 * m) % 4; b1_ = (2 * m + 1) % 4
            gb_ = gab[m % 2]; rg = r_gab[m % 2]
            dma(gb_[:, 0, :], gaT_s[m, :, cs], writes=[rg])
            dma(gb_[:, 1, :], gbT_s[m, :, cs], writes=[rg])
            for k in range(4):
                P.op(PE, lambda e, m=m, k=k, b0=b0: e.matmul(ps[:, b0, :], lhsT=wap[:, k, m * 128:(m + 1) * 128], rhs=OT[:, k, :], start=(k == 0), stop=(k == 3)),
                     reads=[r_w3, r_in3], writes=[rb[b0]])
            for k in range(4):
                P.op(PE, lambda e, m=m, k=k, b1_=b1_: e.matmul(ps[:, b1_, :], lhsT=wcp[:, k, m * 128:(m + 1) * 128], rhs=zc[:, k, :], start=(k == 0), stop=(k == 3)),
                     reads=[r_w3, r_zc], writes=[rb[b1_]])
            P.op(DVE, lambda e, b0=b0, gb_=gb_: e.tensor_tensor(out=t1, in0=ps[:, b0, :], in1=gb_[:, 0, :], op=ALU.mult), reads=[rb[b0], rg], writes=[r_t1])
            P.op(DVE, lambda e, b1_=b1_, gb_=gb_: e.tensor_tensor(out=sq, in0=ps[:, b1_, :], in1=gb_[:, 1, :], op=ALU.mult), reads=[rb[b1_], rg], writes=[r_sq])
            P.op(DVE, lambda e, m=m: e.tensor_tensor(out=mg[:, m, :], in0=t1, in1=sq, op=ALU.add), reads=[r_t1, r_sq], writes=[r_mg])
            yield 2.0
        for m in range(8):
            bk = m % 4
            for k in range(8):
                P.op(PE, lambda e, m=m, k=k, bk=bk: e.matmul(ps[:, bk, :], lhsT=wo[:, k, m * 128:(m + 1) * 128], rhs=mg[:, k, :], start=(k == 0), stop=(k == 7)),
                     reads=[r_w3, r_mg], writes=[rb[bk]])
            P.op(DVE, lambda e, m=m, bk=bk: e.scalar_tensor_tensor(out=xT[:, m, :], in0=ps[:, bk, :], scalar=g_m[:, m:m + 1], in1=xT[:, m, :], op0=ALU.mult, op1=ALU.add),
                 reads=[rb[bk], r_mod2], writes=[r_xT])
            yield 2.0
        rstd_sumsq(D, lambda m: xT[:, m, :], 8, sq, r_sq, 3, stt[:, 2, :], r_stt, [r_xT])
        yield 6.0
        for m in range(8):
            P.op(DVE, lambda e, m=m: e.tensor_tensor(out=t1, in0=xT[:, m, :], in1=stt[:, 2, :], op=ALU.mult), reads=[r_xT, r_stt], writes=[r_t1])
            P.op(DVE, lambda e, m=m: e.tensor_scalar(out=h2[:, m, :], in0=t1, scalar1=a2[:, m:m + 1], scalar2=b2[:, m:m + 1], op0=ALU.mult, op1=ALU.add),
                 reads=[r_mod2], writes=[r_h2, r_t1])
        yield 6.0

    def ffn_gen(g):
        xT = xTs[g % 2]; r_xT = r_xTs[g % 2]
        h2 = h2s[g % 2]; r_h2 = r_h2s[g % 2]
        for i in range(22):
            wb_ = i % 2
            dma(wfi_t[wb_][:, :, 0:128], w_fi_v[:, :, i * 128:(i + 1) * 128], reads=[r_cast["w_fi"]], writes=[r_wfi[wb_]])
            dma(wfi_t[wb_][:, :, 128:256], w_fi_v[:, :, 2816 + i * 128:2816 + (i + 1) * 128], reads=[r_cast["w_fi"]], writes=[r_wfi[wb_]])
            b0 = 4 + (2 * i) % 4; b1_ = 4 + (2 * i + 1) % 4
            for k in range(8):
                P.op(PE, lambda e, k=k, wb_=wb_, b0=b0: e.matmul(ps[:, b0, :], lhsT=wfi_t[wb_][:, k, 0:128], rhs=h2[:, k, :], start=(k == 0), stop=(k == 7)),
                     reads=[r_wfi[wb_], r_h2], writes=[rb[b0]])
            for k in range(8):
                P.op(PE, lambda e, k=k, wb_=wb_, b1_=b1_: e.matmul(ps[:, b1_, :], lhsT=wfi_t[wb_][:, k, 128:256], rhs=h2[:, k, :], start=(k == 0), stop=(k == 7)),
                     reads=[r_wfi[wb_], r_h2], writes=[rb[b1_]])
            si = i % 2
            P.op(ACT, lambda e, si=si, b0=b0: e.activation(out=sl[si], in_=ps[:, b0, :], func=AF.Silu), reads=[rb[b0]], writes=[r_sl[si]])
            P.op(DVE, lambda e, si=si, b1_=b1_, i=i: e.tensor_tensor(out=gT[:, i, :], in0=ps[:, b1_, :], in1=sl[si], op=ALU.mult), reads=[rb[b1_], r_sl[si]], writes=[r_gT])
            yield 3.5
        for m in range(8):
            bk = 4 + m % 4
            wf = wfo_t[m % 2]; rwf = r_wfo[m % 2]
            dma(wf, w_fo_v[:, :, m * 128:(m + 1) * 128], reads=[r_cast["w_fo"]], writes=[rwf])
            for i in range(22):
                P.op(PE, lambda e, i=i, bk=bk, wf=wf: e.matmul(ps[:, bk, :], lhsT=wf[:, i, :], rhs=gT[:, i, :], start=(i == 0), stop=(i == 21)),
                     reads=[rwf, r_gT], writes=[rb[bk]])
            P.op(DVE, lambda e, m=m, bk=bk: e.scalar_tensor_tensor(out=xT[:, m, :], in0=ps[:, bk, :], scalar=g_f[:, m:m + 1], in1=xT[:, m, :], op0=ALU.mult, op1=ALU.add),
                 reads=[rb[bk], r_mod2], writes=[r_xT])
            yield 4.8
        rstd_sumsq(D, lambda m: xT[:, m, :], 8, sq2, r_sq2, 7, rs2, r_rs2, [r_xT])
        yield 6.0
        for m in range(8):
            P.op(DVE, lambda e, m=m: e.scalar_tensor_tensor(out=xT[:, m, :], in0=xT[:, m, :], scalar=gfin[:, m:m + 1], in1=rs2, op0=ALU.mult, op1=ALU.mult),
                 reads=[r_rs2, r_c], writes=[r_xT])
        yield 4.0
        for bl in range(4):
            for m in range(8):
                bk = 4 + (m // 4 + 2 * bl) % 4
                P.op(PE, lambda e, m=m, bl=bl, bk=bk: e.transpose(out=ps[:, bk, (m % 4) * 128:(m % 4 + 1) * 128], in_=xT[:, m, bl * 128:(bl + 1) * 128], identity=identf),
                     reads=[r_xT, r_c], writes=[rb[bk]])
                if m % 4 == 3:
                    evac(yo[:, (m // 4) * 512:(m // 4 + 1) * 512], ps[:, bk, :], [rb[bk]], [r_yo])
            dma(out_d[g * 512 + bl * 128:g * 512 + (bl + 1) * 128, :], yo, reads=[r_yo], eng=STQ)
            yield 2.0

    done3 = {"F": 0, "N": 0}
    mk3 = {"F": front_gen, "N": ffn_gen}
    cur3 = {"F": None, "N": None}
    clk3 = {"F": 0.0, "N": 0.0}
    now3 = [0.0]

    def can_start3(s):
        i = done3[s]
        if i >= 4:
            return False
        if s == "F":
            return done3["N"] >= i - 1
        return done3["F"] > i

    while done3["F"] < 4 or done3["N"] < 4:
        cands = []
        for s in ("N", "F"):
            if cur3[s] is None and can_start3(s):
                cur3[s] = mk3[s](done3[s])
                clk3[s] = max(clk3[s], now3[0])
            if cur3[s] is not None:
                cands.append(s)
        assert cands, ("phase-3 scheduler deadlock", done3)
        s = min(cands, key=lambda k: clk3[k])
        now3[0] = clk3[s]
        cost = next(cur3[s], None)
        if cost is None:
            cur3[s] = None
            done3[s] += 1
        else:
            clk3[s] += cost
    A.release()
    P.emit()
    return nc


_NC_CACHE = {}


def _consts():
    ident = np.eye(128, dtype=np.float32)
    slopes = np.exp2(-np.arange(1, 9, dtype=np.float64))
    s = np.arange(S)
    kaug = np.zeros((3, 8, S), np.float32)
    for h in range(8):
        kaug[0, h] = 8.0 * slopes[h] * (s % 128)
        kaug[1, h] = 8.0 * slopes[h] * 128.0 * (s // 128)
        kaug[2, h] = -8.0 * 128.0 * slopes[h]
    cb = np.zeros((128, 4, 512), np.float32)
    p = np.arange(128)[:, None]
    sp = np.arange(512)[None, :]
    for j4 in range(4):
        cb[:, j4, :] = np.where(sp <= 128 * j4 + p, 0.0, NEG)
    kcp1 = np.tile(np.arange(1, 65, dtype=np.float32)[None, :], (128, 1))
    return ident, kaug, cb, kcp1


def _in_maps(inputs):
    x = np.asarray(inputs["x"], np.float32)
    c = np.asarray(inputs["c"], np.float32)
    bf = ml_dtypes.bfloat16
    ident, kaug, cb, kcp1 = _consts()

    def fm(v, n):
        return np.ascontiguousarray(np.asarray(v, np.float32).reshape(n, 128).T)

    shared = {
        "w_ada": np.ascontiguousarray(inputs["w_ada"][0], np.float32),
        "b_adaT": fm(inputs["b_ada"][0], 48),
        "gmixT": fm(inputs["norm_mix_g"][0], 8), "gffnT": fm(inputs["norm_ffn_g"][0], 8), "gfinT": fm(inputs["norm_final_g"], 8),
        "w_in": np.ascontiguousarray(inputs["w_in"][0], np.float32),
        "w_dwT": np.ascontiguousarray(np.asarray(inputs["w_dw"][0][:, 0, :], np.float32).T.reshape(4, 128, 31).transpose(1, 0, 2)),
        "b_dwT": fm(inputs["b_dw"][0], 4), "lngT": fm(inputs["conv_ln_g"][0], 4), "lnbT": fm(inputs["conv_ln_b"][0], 4),
        "w_ap": np.ascontiguousarray(inputs["w_attn_proj"][0], np.float32), "w_cp": np.ascontiguousarray(inputs["w_conv_proj"][0], np.float32),
        "w_out": np.ascontiguousarray(inputs["w_out"][0], np.float32),
        "w_fi": np.ascontiguousarray(inputs["w_ffn_in"][0], np.float32), "w_fo": np.ascontiguousarray(inputs["w_ffn_out"][0], np.float32),
        "ident": ident.astype(bf), "identf": ident, "kaug": kaug.astype(bf), "cb": cb.astype(bf), "kcp1": kcp1,
    }
    maps = []
    for core in range(8):
        b, r = core // 4, core % 4
        pad = 1536 - 512 * r
        xk = np.zeros((S, D), np.float32)
        xk[pad:] = x[b, :S - pad]
        kbias = np.zeros((1, S), np.float32)
        kbias[0, :pad] = NEG
        hv = np.zeros((128, 128), np.float32)
        for g in range(4):
            pos = 2048 * g + 1504 + np.arange(32)
            hv[:, g * 32:(g + 1) * 32] = (pos >= pad).astype(np.float32)[None, :]
        m = dict(shared)
        m.update({"xk": xk, "cT": fm(c[b], 8), "kbias": kbias.astype(bf), "hvalid": hv})
        maps.append(m)
    return maps


def kernel(**inputs):
    if "nc" not in _NC_CACHE:
        _NC_CACHE["nc"] = build(False)
    nc = _NC_CACHE["nc"]
    maps = _in_maps(inputs)
    res = run_bass_kernel_spmd(nc, maps, core_ids=list(range(8)))
    out = np.zeros((2, S, D), np.float32)
    for core in range(8):
        b, r = core // 4, core % 4
        o = np.asarray(res.results[core]["out"], np.float32)
        for g in range(4):
            p0 = 2048 * g + 512 * r
            out[b, p0:p0 + 512] = o[g * 512:(g + 1) * 512]
    return out
```

```python
import contextlib
import numpy as np
import ml_dtypes
import concourse.bass as bass
import concourse.mybir as mybir
from concourse.bass_utils import run_bass_kernel_spmd

F32 = mybir.dt.float32
BF16 = mybir.dt.bfloat16
FP8 = mybir.dt.float8e4
AF = mybir.ActivationFunctionType
ALU = mybir.AluOpType
AX = mybir.AxisListType

PE, ACT, DVE, POOL, SP = "tensor", "scalar", "vector", "gpsimd", "sync"
ENGS = [PE, ACT, DVE, POOL, SP]
NDMASEM = 32
STQ = "gpsimd"
NPOOLSEM = 16

S = 8192
D = 1024
NT = 16
EPS = 1e-6
NEG = -30000.0
NITER = 14
EXPB = -16.0
MASKB = 240000.0
ACT_SHARE = 0.0


class Res:
    __slots__ = ("w", "rs")

    def __init__(self):
        self.w = None
        self.rs = []


class Op:
    __slots__ = ("eng", "fn", "deps", "signal", "dma", "dsem", "dval", "prev_dma")

    def __init__(self, eng, fn, dma):
        self.eng = eng; self.fn = fn; self.deps = []
        self.signal = False; self.dma = dma; self.dsem = None; self.dval = 0; self.prev_dma = None


class Prog:
    def __init__(self, nc):
        self.nc = nc
        self.ops = {e: [] for e in ENGS}
        self.ndma = 0
        self.npool = 0
        self.dma_last = [None] * (NDMASEM + NPOOLSEM)
        self.bar = {e: [] for e in ENGS}

    def barrier(self):
        lasts = []
        for e in ENGS:
            for o in reversed(self.ops[e]):
                if not o.dma:
                    lasts.append(o)
                    break
        lasts += [p for p in self.dma_last if p is not None]
        for e in ENGS:
            self.bar[e] = list(lasts)

    def op(self, eng, fn, reads=(), writes=(), dma=False):
        o = Op(eng, fn, dma)
        deps = list(self.bar[eng])
        self.bar[eng] = []
        for r in reads:
            if r.w is not None:
                deps.append(r.w)
        for w in writes:
            if w.w is not None:
                deps.append(w.w)
            deps.extend(w.rs)
        seen = set()
        for d in deps:
            if d is o or id(d) in seen:
                continue
            seen.add(id(d))
            if d.eng == eng and not d.dma and (eng == PE or eng == SP):
                continue
            o.deps.append(d)
            if not d.dma:
                d.signal = True
        if dma:
            if eng == POOL:
                slot = NDMASEM + (self.npool % NPOOLSEM)
                o.dval = 16 * (self.npool // NPOOLSEM + 1)
                self.npool += 1
            else:
                slot = self.ndma % NDMASEM
                o.dval = 16 * (self.ndma // NDMASEM + 1)
                self.ndma += 1
            o.dsem = slot
            o.prev_dma = self.dma_last[slot]
            self.dma_last[slot] = o
        self.ops[eng].append(o)
        for r in reads:
            r.rs.append(o)
        for w in writes:
            w.w = o
            w.rs = []
        return o

    def emit(self):
        nc = self.nc
        sigval = {}
        for e in ENGS:
            c = 0
            for o in self.ops[e]:
                if o.signal and not o.dma:
                    c += 1
                    sigval[id(o)] = c
        with contextlib.ExitStack() as st:
            esem = {e: st.enter_context(nc.semaphore("s_" + e)) for e in ENGS}
            dsem = [st.enter_context(nc.semaphore("d%d" % i)) for i in range(NDMASEM + NPOOLSEM)]
            block = st.enter_context(nc.Block())
            prog = self

            def run(engname, eng):
                waited = {}

                def wait(key, sem, val):
                    if waited.get(key, 0) >= val:
                        return
                    eng.wait_ge(sem, val)
                    waited[key] = val

                for o in prog.ops[engname]:
                    need = {}
                    for d in o.deps:
                        if d.dma:
                            k_, s_, v_ = ("d", d.dsem), dsem[d.dsem], d.dval
                        else:
                            k_, s_, v_ = ("e", d.eng), esem[d.eng], sigval[id(d)]
                        if k_ not in need or need[k_][1] < v_:
                            need[k_] = (s_, v_)
                    if o.dma and o.prev_dma is not None:
                        p = o.prev_dma
                        k_ = ("d", p.dsem)
                        if k_ not in need or need[k_][1] < p.dval:
                            need[k_] = (dsem[p.dsem], p.dval)
                    for k_, (s_, v_) in need.items():
                        wait(k_, s_, v_)
                    ins = o.fn(eng)
                    if o.dma:
                        ins.then_inc(dsem[o.dsem], 16)
                    elif o.signal:
                        ins.then_inc(esem[engname], 1)
                if engname == SP:
                    for p in prog.dma_last:
                        if p is not None:
                            wait(("d", p.dsem), dsem[p.dsem], p.dval)

            @block.tensor
            def _(eng):
                run(PE, eng)

            @block.scalar
            def _(eng):
                run(ACT, eng)

            @block.vector
            def _(eng):
                run(DVE, eng)

            @block.gpsimd
            def _(eng):
                run(POOL, eng)

            @block.sync
            def _(eng):
                run(SP, eng)


class Arena:
    def __init__(self, nc, kb):
        self.words = kb * 256
        self.t = nc.alloc_sbuf_tensor("arena", [128, self.words], F32)
        self.off = 0
        self.marks = []

    def mark(self):
        self.marks.append(self.off)

    def release(self):
        self.off = self.marks.pop()

    def tile(self, shape, dt):
        n = 1
        for s in shape:
            n *= s
        esz = 4 if dt == F32 else (1 if dt == FP8 else 2)
        words = (n * esz + 3) // 4
        words = (words + 7) // 8 * 8
        assert self.off + words <= self.words, ("arena overflow", self.off, words, self.words)
        ap = self.t[:, self.off:self.off + words]
        self.off += words
        if dt != F32:
            ap = ap.bitcast(dt)
        ap = ap[:, 0:n]
        if len(shape) == 2:
            ap = ap.rearrange("p (a b) -> p a b", b=shape[1])
        elif len(shape) == 3:
            ap = ap.rearrange("p (a b c) -> p a b c", b=shape[1], c=shape[2])
        return ap


def build(debug=False):
    nc = bass.Bass("TRN2", target_bir_lowering=False)

    def din(name, shape, dt=F32):
        return nc.dram_tensor(name, shape, dt, kind="ExternalInput").ap()

    def dscr(name, shape, dt=BF16):
        return nc.dram_tensor(name, shape, dt, kind="ExternalOutput" if debug else "Internal").ap()

    xk = din("xk", [S, D])
    cT = din("cT", [128, 8])
    w_ada = din("w_ada", [D, 6144])
    b_adaT = din("b_adaT", [128, 48])
    gmixT = din("gmixT", [128, 8]); gffnT = din("gffnT", [128, 8]); gfinT = din("gfinT", [128, 8])
    w_in = din("w_in", [D, 5192])
    w_dwT = din("w_dwT", [128, 4, 31])
    b_dwT = din("b_dwT", [128, 4]); lngT = din("lngT", [128, 4]); lnbT = din("lnbT", [128, 4])
    w_ap = din("w_ap", [512, D]); w_cp = din("w_cp", [512, D]); w_out = din("w_out", [D, D])
    w_fi = din("w_fi", [D, 5632]); w_fo = din("w_fo", [2816, D])
    ident_d = din("ident", [128, 128], BF16)
    identf_d = din("identf", [128, 128])
    kaug_d = din("kaug", [3, 8, S], BF16)
    kbias_d = din("kbias", [1, S], BF16)
    cb_d = din("cb", [128, 4, 512], BF16)
    hvalid_d = din("hvalid", [128, 128])
    kcp1_d = din("kcp1", [128, 64])
    out_d = nc.dram_tensor("out", [2048, D], F32, kind="ExternalOutput").ap()

    w_in_b = dscr("w_in_b", [D, 5192]); w_ap_b = dscr("w_ap_b", [512, D]); w_cp_b = dscr("w_cp_b", [512, D])
    w_out_b = dscr("w_out_b", [D, D]); w_fi_b = dscr("w_fi_b", [D, 5632]); w_fo_b = dscr("w_fo_b", [2816, D])
    kT_s = dscr("kT_s", [8, 64, S]); v_s = dscr("v_s", [S, 520])
    qT_s = dscr("qT_s", [8, 64, 2048]); qiT_s = dscr("qiT_s", [4, 128, 2048])
    zT_s = dscr("zT_s", [4, 128, 4, 544]); gaT_s = dscr("gaT_s", [8, 128, 2048]); gbT_s = dscr("gbT_s", [8, 128, 2048])
    OT_s = dscr("OT_s", [4, 128, 2048])
    if debug:
        dbg_sc = nc.dram_tensor("dbg_sc", [16, 128, S], F32, kind="ExternalOutput").ap()
        dbg_thr = nc.dram_tensor("dbg_thr", [128, 16 * 4], F32, kind="ExternalOutput").ap()

    P = Prog(nc)
    A = Arena(nc, 204)
    ps_cm = nc.psum_tensor("ps", [128, 8, 512], F32)
    ps = ps_cm.__enter__()
    psb = [ps[:, b, :].bitcast(BF16) for b in range(8)]
    rb = [Res() for _ in range(8)]

    def dma(out, in_, reads=(), writes=(), eng=SP, **kw):
        return P.op(eng, lambda e: e.dma_start(out=out, in_=in_, **kw), reads=reads, writes=writes, dma=True)

    ident = A.tile([128], BF16); r_c = Res()
    identf = A.tile([128], F32)
    onesf = A.tile([128], F32)
    modT = A.tile([48], F32); r_mod = Res()
    ab = A.tile([4, 8], F32)
    gfin = A.tile([8], F32)
    wiall = A.tile([16, 8], F32); r_wi = Res()
    hval = A.tile([128], F32)
    kcp1 = A.tile([64], F32)
    dma(ident, ident_d, writes=[r_c]); dma(identf, identf_d, writes=[r_c]); dma(hval, hvalid_d, writes=[r_c])
    dma(kcp1, kcp1_d, writes=[r_c]); dma(gfin, gfinT, writes=[r_c])
    P.op(DVE, lambda e: e.memset(onesf, 1.0), writes=[r_c])

    r_cast = {}
    for name, src, dst in [("w_in", w_in, w_in_b), ("w_ap", w_ap, w_ap_b), ("w_cp", w_cp, w_cp_b), ("w_out", w_out, w_out_b),
                           ("w_fi", w_fi, w_fi_b), ("w_fo", w_fo, w_fo_b)]:
        r_cast[name] = Res()
        dma(dst, src, writes=[r_cast[name]], eng=POOL, max_dma_last_dim=4096)

    A.mark()
    kiT = A.tile([S], BF16); r_ki = Res()
    A.mark()
    cts = A.tile([8], F32); cact = A.tile([8], F32); r_ca = Res()
    bada = A.tile([48], F32); gm = A.tile([8], F32); gf = A.tile([8], F32)
    dma(cts, cT, writes=[r_ca]); dma(bada, b_adaT, writes=[r_ca]); dma(gm, gmixT, writes=[r_ca]); dma(gf, gffnT, writes=[r_ca])
    P.op(ACT, lambda e: e.activation(out=cact, in_=cts, func=AF.Silu), reads=[r_ca], writes=[r_ca])
    wa = [A.tile([8, 768], F32) for _ in range(2)]; r_wa = [Res(), Res()]
    w_ada_v = w_ada.rearrange("(k p) n -> p k n", p=128)
    r_mod2 = Res()

    def ada_piece(j, bank=0, r_dst=None):
        b = j % 2
        dma(wa[b], w_ada_v[:, :, j * 768:(j + 1) * 768], writes=[r_wa[b]])
        for m in range(6):
            col = j * 6 + m
            for kc in range(8):
                P.op(PE, lambda e, b=b, m=m, kc=kc, col=col: e.matmul(ps[:, bank, col:col + 1], lhsT=wa[b][:, kc, m * 128:(m + 1) * 128],
                                                                  rhs=cact[:, kc:kc + 1], start=(kc == 0), stop=(kc == 7)),
                     reads=[r_wa[b], r_ca], writes=[rb[bank]])
        if r_dst is not None:
            c0_, c1_ = j * 6, j * 6 + 6
            P.op(DVE, lambda e: e.tensor_tensor(out=modT[:, c0_:c1_], in0=ps[:, bank, c0_:c1_], in1=bada[:, c0_:c1_], op=ALU.add), reads=[rb[bank], r_ca], writes=[r_dst])

    for j in range(3):
        ada_piece(j)
    P.op(DVE, lambda e: e.tensor_tensor(out=modT[:, 0:18], in0=ps[:, 0, 0:18], in1=bada[:, 0:18], op=ALU.add), reads=[rb[0], r_ca], writes=[r_mod])
    P.op(DVE, lambda e: e.tensor_scalar(out=ab[:, 1, :], in0=modT[:, 8:16], scalar1=1.0, scalar2=None, op0=ALU.add), writes=[r_mod])
    P.op(DVE, lambda e: e.tensor_tensor(out=ab[:, 0, :], in0=ab[:, 1, :], in1=gm, op=ALU.mult), reads=[r_ca], writes=[r_mod])

    def ada_finish():
        P.op(DVE, lambda e: e.tensor_scalar(out=ab[:, 3, :], in0=modT[:, 32:40], scalar1=1.0, scalar2=None, op0=ALU.add), writes=[r_mod2])
        P.op(DVE, lambda e: e.tensor_tensor(out=ab[:, 2, :], in0=ab[:, 3, :], in1=gf, op=ALU.mult), reads=[r_ca], writes=[r_mod2])
    a1 = ab[:, 0, :]; b1 = modT[:, 0:8]; a2 = ab[:, 2, :]; b2 = modT[:, 24:32]; g_m = modT[:, 16:24]; g_f = modT[:, 40:48]

    def prep_front(xrows, nblk, np_, xt, r_xt, xn, r_xn, st, r_st):
        if xrows is not None:
            dma(xt[0:np_, 0:nblk, :], xrows.rearrange("(b p) d -> p b d", p=np_), writes=[r_xt])
        for bl in range(nblk):
            P.op(ACT, lambda e, bl=bl: e.activation(out=xn[0:np_, bl, :], in_=xt[0:np_, bl, :], func=AF.Square, accum_out=st[0:np_, bl:bl + 1]),
                 reads=[r_xt], writes=[r_xn, r_st])
        P.op(ACT, lambda e: e.activation(out=st[0:np_, 4:4 + nblk], in_=st[0:np_, 0:nblk], func=AF.Sqrt, bias=EPS, scale=1.0 / D), writes=[r_st])
        P.op(DVE, lambda e: e.reciprocal(out=st[0:np_, 8:8 + nblk], in_=st[0:np_, 4:4 + nblk]), reads=[r_st], writes=[r_st])
        for bl in range(nblk):
            P.op(DVE, lambda e, bl=bl: e.tensor_scalar(out=xn[0:np_, bl, :], in0=xt[0:np_, bl, :], scalar1=st[0:np_, 8 + bl:9 + bl], scalar2=None, op0=ALU.mult),
                 reads=[r_xt, r_st], writes=[r_xn])

    def prep_back(nblk, np_, xn, r_xn, hT, r_hTk, banks, avec, bvec):
        ntok = nblk * np_
        for kc in range(8):
            bk = banks[kc // 2]
            for bl in range(nblk):
                o = (kc % 2) * 512 + bl * np_
                P.op(PE, lambda e, kc=kc, bl=bl, bk=bk, o=o: e.transpose(out=psb[bk][:, o:o + np_], in_=xn[0:np_, bl, kc * 128:(kc + 1) * 128],
                                                                     identity=ident[0:np_, 0:np_]),
                     reads=[r_xn, r_c], writes=[rb[bk]])
            if kc % 2 == 1:
                for k2 in (kc - 1, kc):
                    o = (k2 % 2) * 512
                    eng_ = DVE
                    if eng_ == DVE:
                        P.op(DVE, lambda e, k2=k2, bk=bk, o=o: e.tensor_scalar(out=hT[:, k2, 0:ntok], in0=psb[bk][:, o:o + ntok], scalar1=avec[:, k2:k2 + 1],
                                                                           scalar2=bvec[:, k2:k2 + 1], op0=ALU.mult, op1=ALU.add),
                             reads=[rb[bk], r_mod], writes=[r_hTk[k2]])
                    else:
                        P.op(ACT, lambda e, k2=k2, bk=bk, o=o: e.activation(out=hT[:, k2, 0:ntok], in_=psb[bk][:, o:o + ntok], func=AF.Identity, scale=avec[:, k2:k2 + 1],
                                                                        bias=bvec[:, k2:k2 + 1]),
                             reads=[rb[bk], r_mod], writes=[r_hTk[k2]])

    def prep(xrows, nblk, np_, xt, r_xt, xn, r_xn, hT, r_hT, st, r_st, banks, avec, bvec):
        prep_front(xrows, nblk, np_, xt, r_xt, xn, r_xn, st, r_st)
        prep_back(nblk, np_, xn, r_xn, hT, [r_hT] * 8, banks, avec, bvec)

    def load_w(dst, src_b, rows, c0, c1, r_w, cname="w_in"):
        dma(dst, src_b.rearrange("(k p) n -> p k n", p=128)[:, :, c0:c1], reads=[r_cast[cname]], writes=[r_w])

    evac_rr = [0]

    def evac(out, in_, reads, writes, func=None, scale=1.0):
        evac_rr[0] += 1
        if func is not None or evac_rr[0] % 2 == 0:
            f = func if func is not None else AF.Copy
            return P.op(ACT, lambda e: e.activation(out=out, in_=in_, func=f, scale=scale), reads=reads, writes=writes)
        if scale != 1.0:
            return P.op(DVE, lambda e: e.tensor_scalar(out=out, in0=in_, scalar1=scale, scalar2=None, op0=ALU.mult), reads=reads, writes=writes)
        return P.op(DVE, lambda e: e.tensor_copy(out=out, in_=in_), reads=reads, writes=writes)

    A.mark()
    wk = A.tile([8, 512], BF16); wv = A.tile([8, 512], BF16); wki = A.tile([8, 128], BF16); r_w1 = Res()
    wst0 = A.tile([8, 512], F32); wst = [wst0, wst0]; r_wst0 = Res(); r_wst = [r_wst0, r_wst0]
    w_in_v = w_in.rearrange("(k p) n -> p k n", p=128)
    dma(wst0, w_in_v[:, :, 512:1024], writes=[r_wst0])
    P.op(ACT, lambda e: e.activation(out=wk, in_=wst0, func=AF.Copy), reads=[r_wst0], writes=[r_w1])
    dma(wst0, w_in_v[:, :, 1024:1536], writes=[r_wst0])
    P.op(DVE, lambda e: e.tensor_copy(out=wv, in_=wst0), reads=[r_wst0], writes=[r_w1])
    dma(wst0[:, :, 0:64], w_in_v[:, :, 2048:2112], writes=[r_wst0])
    P.op(ACT, lambda e: e.activation(out=wki[:, :, 0:64], in_=wst0[:, :, 0:64], func=AF.Copy), reads=[r_wst0], writes=[r_w1])
    P.op(ACT, lambda e: e.activation(out=wki[:, :, 64:128], in_=wst0[:, :, 0:64], func=AF.Copy), reads=[r_wst0], writes=[r_w1])
    xts = [A.tile([4, D], F32) for _ in range(3)]; r_xts = [Res(), Res(), Res()]
    xns = [A.tile([4, D], BF16) for _ in range(2)]; r_xns = [Res(), Res()]
    hTs = [A.tile([8, 512], BF16) for _ in range(2)]; r_hTs = [[Res() for _ in range(8)] for _ in range(2)]
    sts = [A.tile([12], F32) for _ in range(2)]; r_sts = [Res(), Res()]
    ksts = [A.tile([4, 512], BF16) for _ in range(2)]; r_ksts = [Res(), Res()]
    vsts = [A.tile([4, 8, 65], BF16) for _ in range(2)]; r_vsts = [Res(), Res()]
    for b in range(2):
        P.op(DVE, lambda e, b=b: e.memset(vsts[b], 1.0), writes=[r_vsts[b]])
    kT_v = kT_s.rearrange("(m hh) d s -> (hh d) m s", hh=2)
    v_v = v_s.rearrange("(n p) c -> p n c", p=128)

    def load1a(T):
        dma(xts[T % 3][:, :, :], xk[T * 512:(T + 1) * 512, :].rearrange("(b p) d -> p b d", p=128), writes=[r_xts[T % 3]])

    def front1a(T):
        b = T % 2
        prep_front(None, 4, 128, xts[T % 3], r_xts[T % 3], xns[b], r_xns[b], sts[b], r_sts[b])

    def back1a(T):
        b = T % 2
        prep_back(4, 128, xns[b], r_xns[b], hTs[b], r_hTs[b], [0, 1, 2, 3], a1, b1)

    load1a(0)
    load1a(1)
    front1a(0)
    back1a(0)
    for T in range(NT):
        b = T % 2
        hT = hTs[b]
        if T + 2 < NT:
            load1a(T + 2)
        if T + 1 < NT:
            front1a(T + 1)
        for m in range(4):
            bk = 4 + (m % 2)
            for kc in range(8):
                P.op(PE, lambda e, m=m, kc=kc, bk=bk, hT=hT: e.matmul(ps[:, bk, :], lhsT=wk[:, kc, m * 128:(m + 1) * 128], rhs=hT[:, kc, :],
                                                                  start=(kc == 0), stop=(kc == 7)), reads=[r_w1, r_hTs[b][kc]], writes=[rb[bk]])
            evac(ksts[b][:, m, :], ps[:, bk, :], [rb[bk]], [r_ksts[b]])
        dma(kT_v[:, :, T * 512:(T + 1) * 512], ksts[b], reads=[r_ksts[b]], eng=STQ)
        if T + 1 < NT:
            back1a(T + 1)
        for bl in range(4):
            bk = 6 + (bl % 2)
            for kc in range(8):
                P.op(PE, lambda e, bl=bl, kc=kc, bk=bk, hT=hT: e.matmul(ps[:, bk, :], lhsT=hT[:, kc, bl * 128:(bl + 1) * 128], rhs=wv[:, kc, :],
                                                                    start=(kc == 0), stop=(kc == 7)), reads=[r_w1, r_hTs[b][kc]], writes=[rb[bk]])
            evac(vsts[b][:, bl, :, 0:64], ps[:, bk, :].rearrange("p (h d) -> p h d", d=64), [rb[bk]], [r_vsts[b]])
        dma(v_v[:, T * 4:(T + 1) * 4, :], vsts[b].rearrange("p n h c -> p n (h c)"), reads=[r_vsts[b]], eng=STQ)
        for kc in range(8):
            P.op(PE, lambda e, kc=kc, hT=hT: e.matmul(ps[:, 4, :], lhsT=wki[:, kc, :], rhs=hT[:, kc, :], start=(kc == 0), stop=(kc == 7)),
                 reads=[r_w1, r_hTs[b][kc]], writes=[rb[4]])
        evac(kiT[:, T * 512:(T + 1) * 512], ps[:, 4, :], [rb[4]], [r_ki])
        if T < 5:
            ada_piece(3 + T, bank=4, r_dst=r_mod2)
        if T == 5:
            ada_finish()
    P.barrier()
    A.release()
    A.release()

    A.mark()
    wq = A.tile([8, 512], BF16); wqi = A.tile([8, 512], BF16); wwi = A.tile([8, 8], BF16)
    wu = A.tile([8, 1024], BF16); wga = A.tile([8, 1024], BF16); wgb = A.tile([8, 1024], BF16); r_w2 = Res()
    load_w(wq, w_in_b, D, 0, 512, r_w2); load_w(wqi, w_in_b, D, 1536, 2048, r_w2); load_w(wwi, w_in_b, D, 2112, 2120, r_w2)
    load_w(wu, w_in_b, D, 2120, 3144, r_w2); load_w(wga, w_in_b, D, 3144, 4168, r_w2); load_w(wgb, w_in_b, D, 4168, 5192, r_w2)
    xts1 = [A.tile([4, D], F32) for _ in range(2)]; r_xts1 = [Res(), Res()]
    xns1 = [A.tile([4, D], BF16) for _ in range(2)]; r_xns1 = [Res(), Res()]
    hTs1 = [A.tile([8, 512], BF16) for _ in range(2)]; r_hTs1 = [[Res() for _ in range(8)] for _ in range(2)]
    sts1 = [A.tile([12], F32) for _ in range(2)]; r_sts1 = [Res(), Res()]
    xth = A.tile([1, D], F32); r_xth = Res(); xnh = A.tile([1, D], BF16); r_xnh = Res()
    hTh = A.tile([8, 32], BF16); r_hTh = Res(); sth = A.tile([12], F32); r_sth = Res()
    stg = [A.tile([512], BF16) for _ in range(3)]; r_stg = [Res() for _ in range(3)]
    sgm = [A.tile([512], F32) for _ in range(2)]; r_sgm = [Res(), Res()]
    stg_i = [0]
    qT_v = qT_s.rearrange("(m hh) d s -> (hh d) m s", hh=2)

    def stage_out(dst, src_ps, bk, func=None):
        i = stg_i[0] % 3
        stg_i[0] += 1
        evac(stg[i], src_ps, [rb[bk]], [r_stg[i]], func=func)
        dma(dst, stg[i], reads=[r_stg[i]], eng=STQ)

    def proj_fm(w, m, hT_, r_h, bk, n):
        for kc in range(8):
            P.op(PE, lambda e, kc=kc: e.matmul(ps[:, bk, 0:n], lhsT=w[:, kc, m * 128:(m + 1) * 128], rhs=hT_[:, kc, 0:n], start=(kc == 0), stop=(kc == 7)),
                 reads=[r_w2, r_h[kc]], writes=[rb[bk]])

    def front1b(g):
        T = 4 * g + 3
        b = g % 2
        prep_front(xk[T * 512:(T + 1) * 512, :], 4, 128, xts1[b], r_xts1[b], xns1[b], r_xns1[b], sts1[b], r_sts1[b])

    def back1b(g):
        b = g % 2
        prep_back(4, 128, xns1[b], r_xns1[b], hTs1[b], r_hTs1[b], [0, 1, 2, 3], a1, b1)

    front1b(0)
    back1b(0)

    for g in range(4):
        T = 4 * g + 3
        hT = hTs1[g % 2]; r_hT = r_hTs1[g % 2]
        if g + 1 < 4:
            front1b(g + 1)
        cs = slice(g * 512, (g + 1) * 512)
        bkc = [0]

        def nb():
            bkc[0] += 1
            return 4 + bkc[0] % 4
        for m in range(4):
            bk = nb(); proj_fm(wq, m, hT, r_hT, bk, 512); stage_out(qT_v[:, m, cs], ps[:, bk, :], bk)
        for m in range(4):
            bk = nb(); proj_fm(wqi, m, hT, r_hT, bk, 512); stage_out(qiT_s[m, :, cs], ps[:, bk, :], bk)
        if g + 1 < 4:
            back1b(g + 1)
        for m in range(8):
            bk = nb(); proj_fm(wga, m, hT, r_hT, bk, 512); stage_out(gaT_s[m, :, cs], ps[:, bk, :], bk, func=AF.Sigmoid)
        for m in range(8):
            bk = nb(); proj_fm(wgb, m, hT, r_hT, bk, 512); stage_out(gbT_s[m, :, cs], ps[:, bk, :], bk, func=AF.Sigmoid)
        for bl in range(4):
            bk = nb()
            for kc in range(8):
                P.op(PE, lambda e, kc=kc, bl=bl, bk=bk, hT=hT: e.matmul(ps[:, bk, 0:8], lhsT=hT[:, kc, bl * 128:(bl + 1) * 128], rhs=wwi[:, kc, :], start=(kc == 0), stop=(kc == 7)),
                     reads=[r_w2, r_hT[kc]], writes=[rb[bk]])
            P.op(DVE, lambda e, bl=bl, bk=bk, g=g: e.tensor_scalar(out=wiall[:, g * 4 + bl, :], in0=ps[:, bk, 0:8], scalar1=float(8 ** -0.5 * 64 ** -0.5), scalar2=None, op0=ALU.mult),
                 reads=[rb[bk]], writes=[r_wi])
        for i in range(4):
            bka = nb(); proj_fm(wu, i, hT, r_hT, bka, 512)
            bkg = nb(); proj_fm(wu, 4 + i, hT, r_hT, bkg, 512)
            si = i % 2
            P.op(ACT, lambda e, si=si, bkg=bkg: e.activation(out=sgm[si], in_=ps[:, bkg, :], func=AF.Sigmoid), reads=[rb[bkg]], writes=[r_sgm[si]])
            j = stg_i[0] % 3; stg_i[0] += 1
            P.op(DVE, lambda e, si=si, bka=bka, j=j: e.tensor_tensor(out=stg[j], in0=ps[:, bka, :], in1=sgm[si], op=ALU.mult), reads=[rb[bka], r_sgm[si]], writes=[r_stg[j]])
            dma(zT_s[i, :, g, 32:544], stg[j], reads=[r_stg[j]], eng=STQ)
        prep(xk[T * 512 - 32:T * 512, :], 1, 32, xth, r_xth, xnh, r_xnh, hTh, r_hTh, sth, r_sth, [0, 1, 2, 3], a1, b1)
        for i in range(4):
            bka = nb(); proj_fm(wu, i, hTh, [r_hTh] * 8, bka, 32)
            bkg = nb(); proj_fm(wu, 4 + i, hTh, [r_hTh] * 8, bkg, 32)
            si = i % 2
            P.op(ACT, lambda e, si=si, bkg=bkg: e.activation(out=sgm[si][:, 0:32], in_=ps[:, bkg, 0:32], func=AF.Sigmoid), reads=[rb[bkg]], writes=[r_sgm[si]])
            j = stg_i[0] % 3; stg_i[0] += 1
            P.op(DVE, lambda e, si=si, bka=bka, j=j: e.tensor_tensor(out=sgm[si][:, 32:64], in0=ps[:, bka, 0:32], in1=sgm[si][:, 0:32], op=ALU.mult), reads=[rb[bka]], writes=[r_sgm[si]])
            P.op(DVE, lambda e, si=si, j=j, g=g: e.tensor_tensor(out=stg[j][:, 0:32], in0=sgm[si][:, 32:64], in1=hval[:, g * 32:(g + 1) * 32], op=ALU.mult), reads=[r_c], writes=[r_stg[j], r_sgm[si]])
            dma(zT_s[i, :, g, 0:32], stg[j][:, 0:32], reads=[r_stg[j]], eng=STQ)
    P.barrier()
    A.release()

    A.mark()
    cbt = A.tile([4, 512], BF16); kbt = A.tile([1536], BF16); r_c2 = Res()
    dma(cbt, cb_d, writes=[r_c2]); dma(kbt[0:1, :], kbias_d[:, 0:1536], writes=[r_c2])
    onesb = A.tile([128], BF16)
    nidm = A.tile([128], BF16)
    P.op(DVE, lambda e: e.memset(onesb, 1.0), writes=[r_c2])
    P.op(DVE, lambda e: e.tensor_scalar(out=nidm, in0=ident, scalar1=-MASKB, scalar2=None, op0=ALU.mult), reads=[r_c], writes=[r_c2])
    scoress = [A.tile([S], F32) for _ in range(2)]; r_scs = [Res(), Res()]
    maskq = A.tile([S], BF16); r_mq = Res()
    junk = maskq; r_junk = r_mq
    maskTs = [A.tile([64, 256], FP8) for _ in range(2)]; r_mTs = [Res(), Res()]
    qit = A.tile([4, 256], BF16); r_qit = Res()
    qaugs = [A.tile([8, 256], BF16) for _ in range(2)]; r_qas = [Res(), Res()]
    wdgs = [A.tile([8, 128], BF16) for _ in range(2)]; r_wdgs = [Res(), Res()]
    Rt = [A.tile([2, 512], BF16) for _ in range(3)]; r_Rt = [Res() for _ in range(3)]
    bs = A.tile([16], F32); r_bs = Res()
    bsa = A.tile([4], F32); r_bsa = Res(); r_junkA = Res()
    cntk = A.tile([64], F32); r_ck = Res()
    U = A.tile([68], F32); r_U = Res()
    kt = [A.tile([8, 512], BF16) for _ in range(2)]; r_kt = [Res(), Res()]
    vt = [A.tile([4, 520], BF16) for _ in range(2)]; r_vt = [Res(), Res()]
    Pt = [A.tile([2, 256], BF16) for _ in range(3)]; r_Pt = [Res() for _ in range(3)]
    den = A.tile([16], F32); r_den = Res()
    Ob = A.tile([2, 512], BF16); r_Ob = Res()
    OTst = A.tile([4, 256], BF16); r_OT = Res()
    P.op(DVE, lambda e: e.memset(U, 0.0), writes=[r_U])
    P.op(DVE, lambda e: e.memset(U[:, 64:66], 1.0), writes=[r_U])
    for i in range(2):
        P.op(DVE, lambda e, i=i: e.memset(qaugs[i], 0.0), writes=[r_qas[i]])
    kT_hv = kT_s.rearrange("h d s -> d h s")
    kaug_v = kaug_d
    v_v2 = v_s.rearrange("(n p) c -> p n c", p=128)
    LB = 2
    SB = 7

    def idx_gen(qb):
        qp, q2 = qb // 2, qb % 2
        g = qp // 2
        E = 2048 * (g + 1)
        nch = E // 512
        c0 = g * 512 + (qp % 2) * 256
        j4 = qb % 4
        scores = scoress[qb % 2]; r_sc = r_scs[qb % 2]
        wdg = wdgs[qb % 2]; r_wdg = r_wdgs[qb % 2]
        if q2 == 0:
            dma(qit, qiT_s.rearrange("m p s -> p m s")[:, :, c0:c0 + 256], writes=[r_qit])
        for j in range(8):
            P.op(DVE, lambda e, j=j: e.tensor_scalar(out=wdg[:, j, :], in0=ident, scalar1=wiall[:, qb, j:j + 1], scalar2=None, op0=ALU.mult),
                 reads=[r_wi, r_c], writes=[r_wdg])
        yield 0.5
        units = [(c, m) for c in range(nch) for m in range(4)]

        def emit_diag(u):
            c, m = units[u]
            ks = slice(c * 512, (c + 1) * 512)
            has_kb = (c < 3)
            has_cb = (c == nch - 1)
            lastdiag = not (has_kb or has_cb)
            ri = u % 3
            for hh in range(2):
                imm = 2 * m + hh
                P.op(PE, lambda e, m=m, hh=hh, ri=ri, imm=imm, lastdiag=lastdiag: e.matmul(ps[:, SB, :], lhsT=wdg[:, 2 * m + hh, :], rhs=Rt[ri][:, hh, :], start=(imm == 0), stop=(lastdiag and imm == 7)),
                     reads=[r_wdg, r_Rt[ri]], writes=[rb[SB]])
            if m == 3:
                if has_kb:
                    P.op(PE, lambda e, ks=ks, has_cb=has_cb: e.matmul(ps[:, SB, :], lhsT=onesb[0:1, :], rhs=kbt[0:1, ks], start=False, stop=(not has_cb)),
                         reads=[r_c2], writes=[rb[SB]])
                if has_cb:
                    P.op(PE, lambda e: e.matmul(ps[:, SB, :], lhsT=ident, rhs=cbt[:, j4, :], start=False, stop=True), reads=[r_c2, r_c], writes=[rb[SB]])
                P.op(ACT, lambda e, ks=ks: e.activation(out=scores[:, ks], in_=ps[:, SB, :], func=AF.Copy), reads=[rb[SB]], writes=[r_sc])

        for u, (c, m) in enumerate(units):
            ks = slice(c * 512, (c + 1) * 512)
            for hh in range(2):
                pr = slice(hh * 64, hh * 64 + 64)
                P.op(PE, lambda e, m=m, hh=hh, pr=pr, ks=ks: e.matmul(ps[:, LB + hh, :], lhsT=qit[pr, m, q2 * 128:(q2 + 1) * 128], rhs=kiT[pr, ks],
                                                                start=True, stop=True), reads=[r_qit, r_ki], writes=[rb[LB + hh]])
            ri = u % 3
            P.op(ACT, lambda e, ri=ri: e.activation(out=Rt[ri], in_=ps[:, LB:LB + 2, :], func=AF.Relu), reads=[rb[LB], rb[LB + 1]], writes=[r_Rt[ri]])
            if u > 0:
                emit_diag(u - 1)
            yield 0.8
        emit_diag(len(units) - 1)
        if debug:
            dma(dbg_sc[qb, :, 0:E], scores[:, 0:E], reads=[r_sc])
        yield 0.5

    def bis_gen(qb):
        qp, q2 = qb // 2, qb % 2
        g = qp // 2
        E = 2048 * (g + 1)
        nkc = E // 128
        c0 = g * 512 + (qp % 2) * 256
        scores = scoress[qb % 2]; r_sc = r_scs[qb % 2]
        maskT = maskTs[qp % 2]; r_mT = r_mTs[qp % 2]
        qaug = qaugs[qp % 2]; r_qa = r_qas[qp % 2]
        if q2 == 0:
            dma(qaug[0:64, :, :], qT_s.rearrange("h d s -> d h s")[:, :, c0:c0 + 256], writes=[r_qa])
        sc = scores[:, 0:E]
        tpass = E / 960.0
        P.op(DVE, lambda e: e.tensor_reduce(out=bs[:, 0:1], in_=sc, axis=AX.X, op=ALU.max), reads=[r_sc], writes=[r_bs])
        P.op(DVE, lambda e: e.tensor_scalar(out=bs[:, 9:10], in0=bs[:, 0:1], scalar1=-1.0, scalar2=None, op0=ALU.mult), writes=[r_bs])
        P.op(DVE, lambda e: e.tensor_tensor(out=bs[:, 9:10], in0=bs[:, 9:10], in1=bs[:, 0:1], op=ALU.max), writes=[r_bs])
        P.op(DVE, lambda e: e.tensor_scalar(out=bs[:, 9:10], in0=bs[:, 9:10], scalar1=3.0, scalar2=3.0, op0=ALU.mult, op1=ALU.add), writes=[r_bs])
        P.op(DVE, lambda e: e.tensor_tensor(out=bs[:, 3:4], in0=bs[:, 0:1], in1=bs[:, 9:10], op=ALU.subtract), writes=[r_bs])
        P.op(DVE, lambda e: e.tensor_scalar(out=bs[:, 6:7], in0=bs[:, 3:4], scalar1=20000.0, scalar2=None, op0=ALU.add), writes=[r_bs])
        P.op(DVE, lambda e: e.tensor_tensor(out=bs[:, 7:8], in0=bs[:, 9:10], in1=bs[:, 6:7], op=ALU.subtract), writes=[r_bs])
        yield tpass + 1.0
        for it in range(NITER + 1):
            P.op(DVE, lambda e: e.tensor_scalar(out=junk[:, 0:E], in0=sc, scalar1=bs[:, 3:4], scalar2=None, op0=ALU.is_ge, op1=ALU.add, accum_out=bs[:, 4:5]),
                 reads=[r_sc], writes=[r_junk, r_bs])
            P.op(DVE, lambda e: e.tensor_scalar(out=bs[:, 5:6], in0=bs[:, 4:5], scalar1=255.5, scalar2=None, op0=ALU.is_ge), writes=[r_bs])
            if it == 0:
                P.op(DVE, lambda e: e.tensor_scalar(out=bs[:, 1:2], in0=bs[:, 5:6], scalar1=bs[:, 6:7], scalar2=-20000.0, op0=ALU.mult, op1=ALU.add), writes=[r_bs])
                P.op(DVE, lambda e: e.scalar_tensor_tensor(out=bs[:, 2:3], in0=bs[:, 5:6], scalar=bs[:, 7:8], in1=bs[:, 6:7], op0=ALU.mult, op1=ALU.add), writes=[r_bs])
            else:
                P.op(DVE, lambda e: e.tensor_scalar(out=bs[:, 2:3], in0=bs[:, 2:3], scalar1=0.5, scalar2=None, op0=ALU.mult), writes=[r_bs])
                P.op(DVE, lambda e: e.scalar_tensor_tensor(out=bs[:, 1:2], in0=bs[:, 5:6], scalar=bs[:, 2:3], in1=bs[:, 1:2], op0=ALU.mult, op1=ALU.add), writes=[r_bs])
            P.op(DVE, lambda e: e.scalar_tensor_tensor(out=bs[:, 3:4], in0=bs[:, 2:3], scalar=0.5, in1=bs[:, 1:2], op0=ALU.mult, op1=ALU.add), writes=[r_bs])
            yield tpass + 0.8
        if debug:
            dma(dbg_thr[:, qb * 4:qb * 4 + 4], bs[:, 0:4], reads=[r_bs])
        P.op(DVE, lambda e: e.tensor_scalar(out=maskq[:, 0:E], in0=sc, scalar1=bs[:, 1:2], scalar2=None, op0=ALU.is_ge), reads=[r_sc, r_junkA], writes=[r_mq, r_bs])
        P.op(DVE, lambda e: e.tensor_reduce(out=cntk[:, 0:nkc], in_=maskq[:, 0:E].rearrange("p (a b) -> p a b", b=128), axis=AX.X, op=ALU.add), writes=[r_ck, r_mq])
        P.op(DVE, lambda e: e.tensor_scalar(out=cntk[:, 0:nkc], in0=cntk[:, 0:nkc], scalar1=0.5, scalar2=None, op0=ALU.is_ge), writes=[r_ck])
        P.op(DVE, lambda e: e.tensor_tensor(out=cntk[:, 0:nkc], in0=cntk[:, 0:nkc], in1=kcp1[:, 0:nkc], op=ALU.mult), reads=[r_c], writes=[r_ck])
        P.op(DVE, lambda e: e.tensor_reduce(out=bs[:, 8:9], in_=cntk[:, 0:nkc], axis=AX.X, op=ALU.max), writes=[r_ck, r_bs])
        P.op(DVE, lambda e: e.tensor_scalar(out=U[:, 66:67], in0=bs[:, 8:9], scalar1=-1.0, scalar2=None, op0=ALU.add), writes=[r_U, r_bs])
        yield 2 * tpass
        P.op(PE, lambda e: e.matmul(ps[0:67, LB, 0:128], lhsT=U[:, 0:67], rhs=identf, start=True, stop=True), reads=[r_U, r_c], writes=[rb[LB]])
        for h in range(8):
            P.op(ACT, lambda e, h=h: e.activation(out=qaug[64:67, h, q2 * 128:(q2 + 1) * 128], in_=ps[64:67, LB, 0:128], func=AF.Copy), reads=[rb[LB]], writes=[r_qa])
        yield 1.0
        for k8 in range(nkc // 8):
            for kk in range(8):
                kc = k8 * 8 + kk
                P.op(PE, lambda e, kc=kc, kk=kk: e.transpose(out=psb[LB + 1][:, kk * 128:(kk + 1) * 128], in_=maskq[:, kc * 128:(kc + 1) * 128], identity=ident),
                     reads=[r_mq, r_c], writes=[rb[LB + 1]])
            mo = maskT[:, k8 * 8:(k8 + 1) * 8, q2 * 128:(q2 + 1) * 128]
            mi = psb[LB + 1].rearrange("p (a b) -> p a b", b=128)
            P.op(ACT, lambda e, mo=mo, mi=mi: e.activation(out=mo, in_=mi, func=AF.Copy, scale=-1.0, bias=1.0, saturate=False), reads=[rb[LB + 1]], writes=[r_mT])
            yield 1.0

    accb = [4, 5, 6]

    def att_gen(qp):
        g = qp // 2
        E = 2048 * (g + 1)
        nch = E // 512
        nkc = E // 128
        c0 = g * 512 + (qp % 2) * 256
        maskT = maskTs[qp % 2]; r_mT = r_mTs[qp % 2]
        qaug = qaugs[qp % 2]; r_qa = r_qas[qp % 2]
        first_in_bank = {}
        steps = [(c, kl, hp) for c in range(nch) for kl in range(4) for hp in range(4)]

        def emit_loads(c):
            kb_ = c % 2
            ks = slice(c * 512, (c + 1) * 512)
            dma(kt[kb_][0:64, :, :], kT_hv[:, :, ks], writes=[r_kt[kb_]])
            dma(kt[kb_][64:67, :, :], kaug_v[:, :, ks], writes=[r_kt[kb_]])
            dma(vt[kb_], v_v2[:, c * 4:(c + 1) * 4, :], writes=[r_vt[kb_]])

        def emit_ST(i):
            c, kl, hp = steps[i]
            kb_ = c % 2
            kc = c * 4 + kl
            sbk = i % 2
            for hh in range(2):
                h = hp * 2 + hh
                o = hh * 256
                P.op(PE, lambda e, h=h, o=o, sbk=sbk, kb_=kb_, kl=kl: e.matmul(ps[:, sbk, o:o + 256], lhsT=kt[kb_][0:67, h, kl * 128:(kl + 1) * 128],
                                                                    rhs=qaug[0:67, h, :], start=True, stop=False),
                     reads=[r_kt[kb_], r_qa], writes=[rb[sbk]])
                P.op(PE, lambda e, o=o, sbk=sbk, kc=kc: e.matmul(ps[:, sbk, o:o + 256], lhsT=nidm, rhs=maskT[:, kc, :], start=False, stop=True),
                     reads=[r_mT, r_c2], writes=[rb[sbk]])

        emit_loads(0)
        if nch > 1:
            emit_loads(1)
        emit_ST(0)
        for i, (c, kl, hp) in enumerate(steps):
            kb_ = c % 2
            kc = c * 4 + kl
            sbk = i % 2
            if i + 1 < len(steps):
                emit_ST(i + 1)
            pi = i % 3
            P.op(ACT, lambda e, pi=pi, sbk=sbk: e.activation(out=Pt[pi], in_=ps[:, sbk, :].rearrange("p (a b) -> p a b", b=256), func=AF.Exp, bias=EXPB, scale=0.125),
                 reads=[rb[sbk]], writes=[r_Pt[pi]])
            for q2 in range(2):
                for hh in range(2):
                    h = hp * 2 + hh
                    a = q2 * 8 + h
                    ab_ = accb[a // 6]
                    col = (a % 6) * 65
                    st_ = (kc == 0) and (ab_ not in first_in_bank)
                    if kc == 0:
                        first_in_bank[ab_] = True
                    P.op(PE, lambda e, pi=pi, hh=hh, q2=q2, h=h, ab_=ab_, col=col, st_=st_, kb_=kb_, kl=kl, kc=kc: e.matmul(
                        ps[:, ab_, col:col + 65], lhsT=Pt[pi][:, hh, q2 * 128:(q2 + 1) * 128], rhs=vt[kb_][:, kl, h * 65:(h + 1) * 65],
                        start=st_, stop=(kc == nkc - 1), skip_group_check=True),
                        reads=[r_Pt[pi], r_vt[kb_]], writes=[rb[ab_]])
            if kl == 3 and hp == 3 and c + 2 < nch:
                emit_loads(c + 2)
            yield 0.8
        for a in range(16):
            ab_ = accb[a // 6]; col = (a % 6) * 65
            P.op(DVE, lambda e, a=a, ab_=ab_, col=col: e.tensor_copy(out=den[:, a:a + 1], in_=ps[:, ab_, col + 64:col + 65]), reads=[rb[ab_]], writes=[r_den])
        P.op(DVE, lambda e: e.reciprocal(out=den, in_=den), writes=[r_den])
        for a in range(16):
            ab_ = accb[a // 6]; col = (a % 6) * 65
            q2 = a // 8; h = a % 8
            P.op(DVE, lambda e, a=a, ab_=ab_, col=col, q2=q2, h=h: e.tensor_scalar(out=Ob[:, q2, h * 64:(h + 1) * 64], in0=ps[:, ab_, col:col + 64], scalar1=den[:, a:a + 1], scalar2=None, op0=ALU.mult),
                 reads=[rb[ab_]], writes=[r_Ob, r_den])
        yield 3.0
        for q2 in range(2):
            for m in range(4):
                P.op(PE, lambda e, q2=q2, m=m: e.transpose(out=psb[0][:, (q2 * 4 + m) * 128:(q2 * 4 + m + 1) * 128], in_=Ob[:, q2, m * 128:(m + 1) * 128], identity=ident),
                     reads=[r_Ob, r_c], writes=[rb[0]])
        for q2 in range(2):
            P.op(ACT, lambda e, q2=q2: e.activation(out=OTst[:, :, q2 * 128:(q2 + 1) * 128], in_=psb[0][:, q2 * 512:(q2 + 1) * 512].rearrange("p (a b) -> p a b", b=128), func=AF.Copy),
                 reads=[rb[0]], writes=[r_OT])
        dma(OT_s.rearrange("m p s -> p m s")[:, :, c0:c0 + 256], OTst, reads=[r_OT], eng=STQ)
        yield 1.0

    NQB, NPR = 16, 8
    done = {"IDX": 0, "BIS": 0, "ATT": 0}
    nitems = {"IDX": NQB, "BIS": NQB, "ATT": NPR}
    mk = {"IDX": idx_gen, "BIS": bis_gen, "ATT": att_gen}
    cur = {"IDX": None, "BIS": None, "ATT": None}
    clk = {"IDX": 0.0, "BIS": 0.0, "ATT": 0.0}
    now = [0.0]

    def can_start(s):
        i = done[s]
        if i >= nitems[s]:
            return False
        if s == "IDX":
            return done["BIS"] >= i - 1
        if s == "BIS":
            return done["IDX"] > i and done["ATT"] >= i // 2 - 1
        return done["BIS"] > 2 * i + 1

    while any(done[s] < nitems[s] for s in done):
        cands = []
        for s in ("ATT", "IDX", "BIS"):
            if cur[s] is None and can_start(s):
                cur[s] = mk[s](done[s])
                clk[s] = max(clk[s], now[0])
            if cur[s] is not None:
                cands.append(s)
        assert cands, ("scheduler deadlock", done)
        s = min(cands, key=lambda k: clk[k])
        now[0] = clk[s]
        cost = next(cur[s], None)
        if cost is None:
            cur[s] = None
            done[s] += 1
        else:
            clk[s] += cost
    P.barrier()
    A.release()
    A.release()

    A.mark()
    wap = A.tile([4, D], BF16); wcp = A.tile([4, D], BF16); wo = A.tile([8, D], BF16); r_w3 = Res()
    load_w(wap, w_ap_b, 512, 0, D, r_w3, "w_ap"); load_w(wcp, w_cp_b, 512, 0, D, r_w3, "w_cp"); load_w(wo, w_out_b, D, 0, D, r_w3, "w_out")
    wfo_t = [A.tile([22, 128], BF16) for _ in range(2)]; r_wfo = [Res(), Res()]
    wdw = A.tile([4, 31], F32); cvp = A.tile([3, 4], F32); r_cv = Res()
    dma(wdw, w_dwT, writes=[r_cv]); dma(cvp[:, 0, :], b_dwT, writes=[r_cv]); dma(cvp[:, 1, :], lngT, writes=[r_cv]); dma(cvp[:, 2, :], lnbT, writes=[r_cv])
    wdd = A.tile([31, 128], BF16); r_wdd = Res()
    xtb = [A.tile([D], F32) for _ in range(2)]; r_xtb = [Res(), Res()]
    xTs = [A.tile([8, 512], F32) for _ in range(2)]; r_xTs = [Res(), Res()]
    OT = A.tile([4, 512], BF16); zT = A.tile([4, 544], BF16); r_in3 = Res()
    gab = [A.tile([2, 512], BF16) for _ in range(2)]; r_gab = [Res(), Res()]
    cv = A.tile([4, 512], F32); r_cvo = Res()
    sq = A.tile([512], F32); r_sq = Res()
    stt = A.tile([3, 512], F32); r_stt = Res()
    zc = A.tile([4, 512], BF16); r_zc = Res()
    t1 = A.tile([512], F32); r_t1 = Res()
    mg = A.tile([8, 512], BF16); r_mg = Res()
    h2s = [A.tile([8, 512], BF16) for _ in range(2)]; r_h2s = [Res(), Res()]
    gT = A.tile([22, 512], BF16); r_gT = Res()
    wfi_t = [A.tile([8, 256], BF16) for _ in range(2)]; r_wfi = [Res() for _ in range(2)]
    sl = [A.tile([512], F32) for _ in range(2)]; r_sl = [Res(), Res()]
    sq2 = A.tile([512], F32); r_sq2 = Res()
    rs2 = A.tile([512], F32); r_rs2 = Res()
    yo = A.tile([D], F32); r_yo = Res()
    w_fo_v = w_fo_b.rearrange("(k p) n -> p k n", p=128)
    w_fi_v = w_fi_b.rearrange("(k p) n -> p k n", p=128)

    def rstd_sumsq(n, src_fn, nchunks, sqt, r_sqt, bank, dst, r_dst, rd):
        for m in range(nchunks):
            P.op(ACT, lambda e, m=m: e.activation(out=sqt, in_=src_fn(m), func=AF.Square), reads=rd, writes=[r_sqt])
            P.op(PE, lambda e, m=m: e.matmul(ps[:, bank, :], lhsT=onesf, rhs=sqt, start=(m == 0), stop=(m == nchunks - 1)), reads=[r_sqt, r_c], writes=[rb[bank]])
        P.op(ACT, lambda e: e.activation(out=dst, in_=ps[:, bank, :], func=AF.Sqrt, bias=EPS, scale=1.0 / n), reads=[rb[bank]], writes=[r_dst])
        P.op(DVE, lambda e: e.reciprocal(out=dst, in_=dst), writes=[r_dst])

    def front_gen(g):
        T = 4 * g + 3
        cs = slice(g * 512, (g + 1) * 512)
        xT = xTs[g % 2]; r_xT = r_xTs[g % 2]
        h2 = h2s[g % 2]; r_h2 = r_h2s[g % 2]
        dma(OT, OT_s.rearrange("m p s -> p m s")[:, :, cs], writes=[r_in3])
        dma(zT, zT_s[:, :, g, :].rearrange("m p s -> p m s"), writes=[r_in3])
        for bl in range(4):
            xb_ = xtb[bl % 2]; rxb = r_xtb[bl % 2]
            dma(xb_, xk[T * 512 + bl * 128:T * 512 + (bl + 1) * 128, :], writes=[rxb])
            for half in range(2):
                bk = half
                for k4 in range(4):
                    kc = half * 4 + k4
                    P.op(PE, lambda e, kc=kc, k4=k4, bk=bk, xb_=xb_: e.transpose(out=ps[:, bk, k4 * 128:(k4 + 1) * 128], in_=xb_[:, kc * 128:(kc + 1) * 128], identity=identf),
                         reads=[rxb, r_c], writes=[rb[bk]])
                evac(xT[:, half * 4:half * 4 + 4, bl * 128:(bl + 1) * 128], ps[:, bk, :].rearrange("p (a b) -> p a b", b=128), [rb[bk]], [r_xT])
            yield 3.0
        for i in range(4):
            bk = i % 2
            for k in range(31):
                P.op(DVE, lambda e, i=i, k=k: e.tensor_scalar(out=wdd[:, k, :], in0=ident, scalar1=wdw[:, i, k:k + 1], scalar2=None, op0=ALU.mult),
                     reads=[r_cv, r_c], writes=[r_wdd])
            for k in range(31):
                P.op(PE, lambda e, i=i, k=k, bk=bk: e.matmul(ps[:, bk, :], lhsT=wdd[:, k, :], rhs=zT[:, i, k + 2:k + 514], start=(k == 0), stop=(k == 30)),
                     reads=[r_wdd, r_in3], writes=[rb[bk]])
            P.op(DVE, lambda e, i=i, bk=bk: e.tensor_scalar(out=cv[:, i, :], in0=ps[:, bk, :], scalar1=cvp[:, 0, i:i + 1], scalar2=None, op0=ALU.add),
                 reads=[rb[bk], r_cv], writes=[r_cvo])
            yield 7.0
        for i in range(4):
            P.op(PE, lambda e, i=i: e.matmul(ps[:, 2, :], lhsT=onesf, rhs=cv[:, i, :], start=(i == 0), stop=(i == 3)), reads=[r_cvo, r_c], writes=[rb[2]])
        P.op(ACT, lambda e: e.activation(out=stt[:, 0, :], in_=ps[:, 2, :], func=AF.Copy, scale=1.0 / 512), reads=[rb[2]], writes=[r_stt])
        for i in range(4):
            P.op(DVE, lambda e, i=i: e.tensor_tensor(out=cv[:, i, :], in0=cv[:, i, :], in1=stt[:, 0, :], op=ALU.subtract), reads=[r_stt], writes=[r_cvo])
        yield 4.0
        rstd_sumsq(512, lambda m: cv[:, m, :], 4, sq, r_sq, 3, stt[:, 2, :], r_stt, [r_cvo])
        yield 4.0
        for i in range(4):
            P.op(DVE, lambda e, i=i: e.tensor_tensor(out=cv[:, i, :], in0=cv[:, i, :], in1=stt[:, 2, :], op=ALU.mult), reads=[r_stt], writes=[r_cvo])
            P.op(DVE, lambda e, i=i: e.tensor_scalar(out=cv[:, i, :], in0=cv[:, i, :], scalar1=cvp[:, 1, i:i + 1], scalar2=cvp[:, 2, i:i + 1], op0=ALU.mult, op1=ALU.add),
                 reads=[r_cv], writes=[r_cvo])
            P.op(ACT, lambda e, i=i: e.activation(out=zc[:, i, :], in_=cv[:, i, :], func=AF.Silu), reads=[r_cvo], writes=[r_zc])
        yield 4.0
        for m in range(8):
            b0 = (2 * m) % 4; b1_ = (2 * m + 1) % 4
            gb_ = gab[m % 2]; rg = r_gab[m % 2]
            dma(gb_[:, 0, :], gaT_s[m, :, cs], writes=[rg])
            dma(gb_[:, 1, :], gbT_s[m, :, cs], writes=[rg])
            for k in range(4):
                P.op(PE, lambda e, m=m, k=k, b0=b0: e.matmul(ps[:, b0, :], lhsT=wap[:, k, m * 128:(m + 1) * 128], rhs=OT[:, k, :], start=(k == 0), stop=(k == 3)),
                     reads=[r_w3, r_in3], writes=[rb[b0]])
            for k in range(4):
                P.op(PE, lambda e, m=m, k=k, b1_=b1_: e.matmul(ps[:, b1_, :], lhsT=wcp[:, k, m * 128:(m + 1) * 128], rhs=zc[:, k, :], start=(k == 0), stop=(k == 3)),
                     reads=[r_w3, r_zc], writes=[rb[b1_]])
            P.op(DVE, lambda e, b0=b0, gb_=gb_: e.tensor_tensor(out=t1, in0=ps[:, b0, :], in1=gb_[:, 0, :], op=ALU.mult), reads=[rb[b0], rg], writes=[r_t1])
            P.op(DVE, lambda e, b1_=b1_, gb_=gb_: e.tensor_tensor(out=sq, in0=ps[:, b1_, :], in1=gb_[:, 1, :], op=ALU.mult), reads=[rb[b1_], rg], writes=[r_sq])
            P.op(DVE, lambda e, m=m: e.tensor_tensor(out=mg[:, m, :], in0=t1, in1=sq, op=ALU.add), reads=[r_t1, r_sq], writes=[r_mg])
            yield 2.0
        for m in range(8):
            bk = m % 4
            for k in range(8):
                P.op(PE, lambda e, m=m, k=k, bk=bk: e.matmul(ps[:, bk, :], lhsT=wo[:, k, m * 128:(m + 1) * 128], rhs=mg[:, k, :], start=(k == 0), stop=(k == 7)),
                     reads=[r_w3, r_mg], writes=[rb[bk]])
            P.op(DVE, lambda e, m=m, bk=bk: e.scalar_tensor_tensor(out=xT[:, m, :], in0=ps[:, bk, :], scalar=g_m[:, m:m + 1], in1=xT[:, m, :], op0=ALU.mult, op1=ALU.add),
                 reads=[rb[bk], r_mod2], writes=[r_xT])
            yield 2.0
        rstd_sumsq(D, lambda m: xT[:, m, :], 8, sq, r_sq, 3, stt[:, 2, :], r_stt, [r_xT])
        yield 6.0
        for m in range(8):
            P.op(DVE, lambda e, m=m: e.tensor_tensor(out=t1, in0=xT[:, m, :], in1=stt[:, 2, :], op=ALU.mult), reads=[r_xT, r_stt], writes=[r_t1])
            P.op(DVE, lambda e, m=m: e.tensor_scalar(out=h2[:, m, :], in0=t1, scalar1=a2[:, m:m + 1], scalar2=b2[:, m:m + 1], op0=ALU.mult, op1=ALU.add),
                 reads=[r_mod2], writes=[r_h2, r_t1])
        yield 6.0

    def ffn_gen(g):
        xT = xTs[g % 2]; r_xT = r_xTs[g % 2]
        h2 = h2s[g % 2]; r_h2 = r_h2s[g % 2]
        for i in range(22):
            wb_ = i % 2
            dma(wfi_t[wb_][:, :, 0:128], w_fi_v[:, :, i * 128:(i + 1) * 128], reads=[r_cast["w_fi"]], writes=[r_wfi[wb_]])
            dma(wfi_t[wb_][:, :, 128:256], w_fi_v[:, :, 2816 + i * 128:2816 + (i + 1) * 128], reads=[r_cast["w_fi"]], writes=[r_wfi[wb_]])
            b0 = 4 + (2 * i) % 4; b1_ = 4 + (2 * i + 1) % 4
            for k in range(8):
                P.op(PE, lambda e, k=k, wb_=wb_, b0=b0: e.matmul(ps[:, b0, :], lhsT=wfi_t[wb_][:, k, 0:128], rhs=h2[:, k, :], start=(k == 0), stop=(k == 7)),
                     reads=[r_wfi[wb_], r_h2], writes=[rb[b0]])
            for k in range(8):
                P.op(PE, lambda e, k=k, wb_=wb_, b1_=b1_: e.matmul(ps[:, b1_, :], lhsT=wfi_t[wb_][:, k, 128:256], rhs=h2[:, k, :], start=(k == 0), stop=(k == 7)),
                     reads=[r_wfi[wb_], r_h2], writes=[rb[b1_]])
            si = i % 2
            P.op(ACT, lambda e, si=si, b0=b0: e.activation(out=sl[si], in_=ps[:, b0, :], func=AF.Silu), reads=[rb[b0]], writes=[r_sl[si]])
            P.op(DVE, lambda e, si=si, b1_=b1_, i=i: e.tensor_tensor(out=gT[:, i, :], in0=ps[:, b1_, :], in1=sl[si], op=ALU.mult), reads=[rb[b1_], r_sl[si]], writes=[r_gT])
            yield 3.5
        for m in range(8):
            bk = 4 + m % 4
            wf = wfo_t[m % 2]; rwf = r_wfo[m % 2]
            dma(wf, w_fo_v[:, :, m * 128:(m + 1) * 128], reads=[r_cast["w_fo"]], writes=[rwf])
            for i in range(22):
                P.op(PE, lambda e, i=i, bk=bk, wf=wf: e.matmul(ps[:, bk, :], lhsT=wf[:, i, :], rhs=gT[:, i, :], start=(i == 0), stop=(i == 21)),
                     reads=[rwf, r_gT], writes=[rb[bk]])
            P.op(DVE, lambda e, m=m, bk=bk: e.scalar_tensor_tensor(out=xT[:, m, :], in0=ps[:, bk, :], scalar=g_f[:, m:m + 1], in1=xT[:, m, :], op0=ALU.mult, op1=ALU.add),
                 reads=[rb[bk], r_mod2], writes=[r_xT])
            yield 4.8
        rstd_sumsq(D, lambda m: xT[:, m, :], 8, sq2, r_sq2, 7, rs2, r_rs2, [r_xT])
        yield 6.0
        for m in range(8):
            P.op(DVE, lambda e, m=m: e.scalar_tensor_tensor(out=xT[:, m, :], in0=xT[:, m, :], scalar=gfin[:, m:m + 1], in1=rs2, op0=ALU.mult, op1=ALU.mult),
                 reads=[r_rs2, r_c], writes=[r_xT])
        yield 4.0
        for bl in range(4):
            for m in range(8):
                bk = 4 + (m // 4 + 2 * bl) % 4
                P.op(PE, lambda e, m=m, bl=bl, bk=bk: e.transpose(out=ps[:, bk, (m % 4) * 128:(m % 4 + 1) * 128], in_=xT[:, m, bl * 128:(bl + 1) * 128], identity=identf),
                     reads=[r_xT, r_c], writes=[rb[bk]])
                if m % 4 == 3:
                    evac(yo[:, (m // 4) * 512:(m // 4 + 1) * 512], ps[:, bk, :], [rb[bk]], [r_yo])
            dma(out_d[g * 512 + bl * 128:g * 512 + (bl + 1) * 128, :], yo, reads=[r_yo], eng=STQ)
            yield 2.0

    done3 = {"F": 0, "N": 0}
    mk3 = {"F": front_gen, "N": ffn_gen}
    cur3 = {"F": None, "N": None}
    clk3 = {"F": 0.0, "N": 0.0}
    now3 = [0.0]

    def can_start3(s):
        i = done3[s]
        if i >= 4:
            return False
        if s == "F":
            return done3["N"] >= i - 1
        return done3["F"] > i

    while done3["F"] < 4 or done3["N"] < 4:
        cands = []
        for s in ("N", "F"):
            if cur3[s] is None and can_start3(s):
                cur3[s] = mk3[s](done3[s])
                clk3[s] = max(clk3[s], now3[0])
            if cur3[s] is not None:
                cands.append(s)
        assert cands, ("phase-3 scheduler deadlock", done3)
        s = min(cands, key=lambda k: clk3[k])
        now3[0] = clk3[s]
        cost = next(cur3[s], None)
        if cost is None:
            cur3[s] = None
            done3[s] += 1
        else:
            clk3[s] += cost
    A.release()
    P.emit()
    return nc


_NC_CACHE = {}


def _consts():
    ident = np.eye(128, dtype=np.float32)
    slopes = np.exp2(-np.arange(1, 9, dtype=np.float64))
    s = np.arange(S)
    kaug = np.zeros((3, 8, S), np.float32)
    for h in range(8):
        kaug[0, h] = 8.0 * slopes[h] * (s % 128)
        kaug[1, h] = 8.0 * slopes[h] * 128.0 * (s // 128)
        kaug[2, h] = -8.0 * 128.0 * slopes[h]
    cb = np.zeros((128, 4, 512), np.float32)
    p = np.arange(128)[:, None]
    sp = np.arange(512)[None, :]
    for j4 in range(4):
        cb[:, j4, :] = np.where(sp <= 128 * j4 + p, 0.0, NEG)
    kcp1 = np.tile(np.arange(1, 65, dtype=np.float32)[None, :], (128, 1))
    return ident, kaug, cb, kcp1


def _in_maps(inputs):
    x = np.asarray(inputs["x"], np.float32)
    c = np.asarray(inputs["c"], np.float32)
    bf = ml_dtypes.bfloat16
    ident, kaug, cb, kcp1 = _consts()

    def fm(v, n):
        return np.ascontiguousarray(np.asarray(v, np.float32).reshape(n, 128).T)

    shared = {
        "w_ada": np.ascontiguousarray(inputs["w_ada"][0], np.float32),
        "b_adaT": fm(inputs["b_ada"][0], 48),
        "gmixT": fm(inputs["norm_mix_g"][0], 8), "gffnT": fm(inputs["norm_ffn_g"][0], 8), "gfinT": fm(inputs["norm_final_g"], 8),
        "w_in": np.ascontiguousarray(inputs["w_in"][0], np.float32),
        "w_dwT": np.ascontiguousarray(np.asarray(inputs["w_dw"][0][:, 0, :], np.float32).T.reshape(4, 128, 31).transpose(1, 0, 2)),
        "b_dwT": fm(inputs["b_dw"][0], 4), "lngT": fm(inputs["conv_ln_g"][0], 4), "lnbT": fm(inputs["conv_ln_b"][0], 4),
        "w_ap": np.ascontiguousarray(inputs["w_attn_proj"][0], np.float32), "w_cp": np.ascontiguousarray(inputs["w_conv_proj"][0], np.float32),
        "w_out": np.ascontiguousarray(inputs["w_out"][0], np.float32),
        "w_fi": np.ascontiguousarray(inputs["w_ffn_in"][0], np.float32), "w_fo": np.ascontiguousarray(inputs["w_ffn_out"][0], np.float32),
        "ident": ident.astype(bf), "identf": ident, "kaug": kaug.astype(bf), "cb": cb.astype(bf), "kcp1": kcp1,
    }
    maps = []
    for core in range(8):
        b, r = core // 4, core % 4
        pad = 1536 - 512 * r
        xk = np.zeros((S, D), np.float32)
        xk[pad:] = x[b, :S - pad]
        kbias = np.zeros((1, S), np.float32)
        kbias[0, :pad] = NEG
        hv = np.zeros((128, 128), np.float32)
        for g in range(4):
            pos = 2048 * g + 1504 + np.arange(32)
            hv[:, g * 32:(g + 1) * 32] = (pos >= pad).astype(np.float32)[None, :]
        m = dict(shared)
        m.update({"xk": xk, "cT": fm(c[b], 8), "kbias": kbias.astype(bf), "hvalid": hv})
        maps.append(m)
    return maps


def kernel(**inputs):
    if "nc" not in _NC_CACHE:
        _NC_CACHE["nc"] = build(False)
    nc = _NC_CACHE["nc"]
    maps = _in_maps(inputs)
    res = run_bass_kernel_spmd(nc, maps, core_ids=list(range(8)))
    out = np.zeros((2, S, D), np.float32)
    for core in range(8):
        b, r = core // 4, core % 4
        o = np.asarray(res.results[core]["out"], np.float32)
        for g in range(4):
            p0 = 2048 * g + 512 * r
            out[b, p0:p0 + 512] = o[g * 512:(g + 1) * 512]
    return out
```

```python
import contextlib
import numpy as np
import ml_dtypes
import concourse.bass as bass
import concourse.mybir as mybir
from concourse.bass_utils import run_bass_kernel_spmd

F32 = mybir.dt.float32
BF16 = mybir.dt.bfloat16
FP8 = mybir.dt.float8e4
AF = mybir.ActivationFunctionType
ALU = mybir.AluOpType
AX = mybir.AxisListType

PE, ACT, DVE, POOL, SP = "tensor", "scalar", "vector", "gpsimd", "sync"
ENGS = [PE, ACT, DVE, POOL, SP]
NDMASEM = 32
STQ = "gpsimd"
NPOOLSEM = 16

S = 8192
D = 1024
NT = 16
EPS = 1e-6
NEG = -30000.0
NITER = 14
EXPB = -16.0
MASKB = 240000.0
ACT_SHARE = 0.0


class Res:
    __slots__ = ("w", "rs")

    def __init__(self):
        self.w = None
        self.rs = []


class Op:
    __slots__ = ("eng", "fn", "deps", "signal", "dma", "dsem", "dval", "prev_dma")

    def __init__(self, eng, fn, dma):
        self.eng = eng; self.fn = fn; self.deps = []
        self.signal = False; self.dma = dma; self.dsem = None; self.dval = 0; self.prev_dma = None


class Prog:
    def __init__(self, nc):
        self.nc = nc
        self.ops = {e: [] for e in ENGS}
        self.ndma = 0
        self.npool = 0
        self.dma_last = [None] * (NDMASEM + NPOOLSEM)
        self.bar = {e: [] for e in ENGS}

    def barrier(self):
        lasts = []
        for e in ENGS:
            for o in reversed(self.ops[e]):
                if not o.dma:
                    lasts.append(o)
                    break
        lasts += [p for p in self.dma_last if p is not None]
        for e in ENGS:
            self.bar[e] = list(lasts)

    def op(self, eng, fn, reads=(), writes=(), dma=False):
        o = Op(eng, fn, dma)
        deps = list(self.bar[eng])
        self.bar[eng] = []
        for r in reads:
            if r.w is not None:
                deps.append(r.w)
        for w in writes:
            if w.w is not None:
                deps.append(w.w)
            deps.extend(w.rs)
        seen = set()
        for d in deps:
            if d is o or id(d) in seen:
                continue
            seen.add(id(d))
            if d.eng == eng and not d.dma and (eng == PE or eng == SP):
                continue
            o.deps.append(d)
            if not d.dma:
                d.signal = True
        if dma:
            if eng == POOL:
                slot = NDMASEM + (self.npool % NPOOLSEM)
                o.dval = 16 * (self.npool // NPOOLSEM + 1)
                self.npool += 1
            else:
                slot = self.ndma % NDMASEM
                o.dval = 16 * (self.ndma // NDMASEM + 1)
                self.ndma += 1
            o.dsem = slot
            o.prev_dma = self.dma_last[slot]
            self.dma_last[slot] = o
        self.ops[eng].append(o)
        for r in reads:
            r.rs.append(o)
        for w in writes:
            w.w = o
            w.rs = []
        return o

    def emit(self):
        nc = self.nc
        sigval = {}
        for e in ENGS:
            c = 0
            for o in self.ops[e]:
                if o.signal and not o.dma:
                    c += 1
                    sigval[id(o)] = c
        with contextlib.ExitStack() as st:
            esem = {e: st.enter_context(nc.semaphore("s_" + e)) for e in ENGS}
            dsem = [st.enter_context(nc.semaphore("d%d" % i)) for i in range(NDMASEM + NPOOLSEM)]
            block = st.enter_context(nc.Block())
            prog = self

            def run(engname, eng):
                waited = {}

                def wait(key, sem, val):
                    if waited.get(key, 0) >= val:
                        return
                    eng.wait_ge(sem, val)
                    waited[key] = val

                for o in prog.ops[engname]:
                    need = {}
                    for d in o.deps:
                        if d.dma:
                            k_, s_, v_ = ("d", d.dsem), dsem[d.dsem], d.dval
                        else:
                            k_, s_, v_ = ("e", d.eng), esem[d.eng], sigval[id(d)]
                        if k_ not in need or need[k_][1] < v_:
                            need[k_] = (s_, v_)
                    if o.dma and o.prev_dma is not None:
                        p = o.prev_dma
                        k_ = ("d", p.dsem)
                        if k_ not in need or need[k_][1] < p.dval:
                            need[k_] = (dsem[p.dsem], p.dval)
                    for k_, (s_, v_) in need.items():
                        wait(k_, s_, v_)
                    ins = o.fn(eng)
                    if o.dma:
                        ins.then_inc(dsem[o.dsem], 16)
                    elif o.signal:
                        ins.then_inc(esem[engname], 1)
                if engname == SP:
                    for p in prog.dma_last:
                        if p is not None:
                            wait(("d", p.dsem), dsem[p.dsem], p.dval)

            @block.tensor
            def _(eng):
                run(PE, eng)

            @block.scalar
            def _(eng):
                run(ACT, eng)

            @block.vector
            def _(eng):
                run(DVE, eng)

            @block.gpsimd
            def _(eng):
                run(POOL, eng)

            @block.sync
            def _(eng):
                run(SP, eng)


class Arena:
    def __init__(self, nc, kb):
        self.words = kb * 256
        self.t = nc.alloc_sbuf_tensor("arena", [128, self.words], F32)
        self.off = 0
        self.marks = []

    def mark(self):
        self.marks.append(self.off)

    def release(self):
        self.off = self.marks.pop()

    def tile(self, shape, dt):
        n = 1
        for s in shape:
            n *= s
        esz = 4 if dt == F32 else (1 if dt == FP8 else 2)
        words = (n * esz + 3) // 4
        words = (words + 7) // 8 * 8
        assert self.off + words <= self.words, ("arena overflow", self.off, words, self.words)
        ap = self.t[:, self.off:self.off + words]
        self.off += words
        if dt != F32:
            ap = ap.bitcast(dt)
        ap = ap[:, 0:n]
        if len(shape) == 2:
            ap = ap.rearrange("p (a b) -> p a b", b=shape[1])
        elif len(shape) == 3:
            ap = ap.rearrange("p (a b c) -> p a b c", b=shape[1], c=shape[2])
        return ap


def build(debug=False):
    nc = bass.Bass("TRN2", target_bir_lowering=False)

    def din(name, shape, dt=F32):
        return nc.dram_tensor(name, shape, dt, kind="ExternalInput").ap()

    def dscr(name, shape, dt=BF16):
        return nc.dram_tensor(name, shape, dt, kind="ExternalOutput" if debug else "Internal").ap()

    xk = din("xk", [S, D])
    cT = din("cT", [128, 8])
    w_ada = din("w_ada", [D, 6144])
    b_adaT = din("b_adaT", [128, 48])
    gmixT = din("gmixT", [128, 8]); gffnT = din("gffnT", [128, 8]); gfinT = din("gfinT", [128, 8])
    w_in = din("w_in", [D, 5192])
    w_dwT = din("w_dwT", [128, 4, 31])
    b_dwT = din("b_dwT", [128, 4]); lngT = din("lngT", [128, 4]); lnbT = din("lnbT", [128, 4])
    w_ap = din("w_ap", [512, D]); w_cp = din("w_cp", [512, D]); w_out = din("w_out", [D, D])
    w_fi = din("w_fi", [D, 5632]); w_fo = din("w_fo", [2816, D])
    ident_d = din("ident", [128, 128], BF16)
    identf_d = din("identf", [128, 128])
    kaug_d = din("kaug", [3, 8, S], BF16)
    kbias_d = din("kbias", [1, S], BF16)
    cb_d = din("cb", [128, 4, 512], BF16)
    hvalid_d = din("hvalid", [128, 128])
    kcp1_d = din("kcp1", [128, 64])
    out_d = nc.dram_tensor("out", [2048, D], F32, kind="ExternalOutput").ap()

    w_in_b = dscr("w_in_b", [D, 5192]); w_ap_b = dscr("w_ap_b", [512, D]); w_cp_b = dscr("w_cp_b", [512, D])
    w_out_b = dscr("w_out_b", [D, D]); w_fi_b = dscr("w_fi_b", [D, 5632]); w_fo_b = dscr("w_fo_b", [2816, D])
    kT_s = dscr("kT_s", [8, 64, S]); v_s = dscr("v_s", [S, 520])
    qT_s = dscr("qT_s", [8, 64, 2048]); qiT_s = dscr("qiT_s", [4, 128, 2048])
    zT_s = dscr("zT_s", [4, 128, 4, 544]); gaT_s = dscr("gaT_s", [8, 128, 2048]); gbT_s = dscr("gbT_s", [8, 128, 2048])
    OT_s = dscr("OT_s", [4, 128, 2048])
    if debug:
        dbg_sc = nc.dram_tensor("dbg_sc", [16, 128, S], F32, kind="ExternalOutput").ap()
        dbg_thr = nc.dram_tensor("dbg_thr", [128, 16 * 4], F32, kind="ExternalOutput").ap()

    P = Prog(nc)
    A = Arena(nc, 204)
    ps_cm = nc.psum_tensor("ps", [128, 8, 512], F32)
    ps = ps_cm.__enter__()
    psb = [ps[:, b, :].bitcast(BF16) for b in range(8)]
    rb = [Res() for _ in range(8)]

    def dma(out, in_, reads=(), writes=(), eng=SP, **kw):
        return P.op(eng, lambda e: e.dma_start(out=out, in_=in_, **kw), reads=reads, writes=writes, dma=True)

    ident = A.tile([128], BF16); r_c = Res()
    identf = A.tile([128], F32)
    onesf = A.tile([128], F32)
    modT = A.tile([48], F32); r_mod = Res()
    ab = A.tile([4, 8], F32)
    gfin = A.tile([8], F32)
    wiall = A.tile([16, 8], F32); r_wi = Res()
    hval = A.tile([128], F32)
    kcp1 = A.tile([64], F32)
    dma(ident, ident_d, writes=[r_c]); dma(identf, identf_d, writes=[r_c]); dma(hval, hvalid_d, writes=[r_c])
    dma(kcp1, kcp1_d, writes=[r_c]); dma(gfin, gfinT, writes=[r_c])
    P.op(DVE, lambda e: e.memset(onesf, 1.0), writes=[r_c])

    r_cast = {}
    for name, src, dst in [("w_in", w_in, w_in_b), ("w_ap", w_ap, w_ap_b), ("w_cp", w_cp, w_cp_b), ("w_out", w_out, w_out_b),
                           ("w_fi", w_fi, w_fi_b), ("w_fo", w_fo, w_fo_b)]:
        r_cast[name] = Res()
        dma(dst, src, writes=[r_cast[name]], eng=POOL, max_dma_last_dim=4096)

    A.mark()
    kiT = A.tile([S], BF16); r_ki = Res()
    A.mark()
    cts = A.tile([8], F32); cact = A.tile([8], F32); r_ca = Res()
    bada = A.tile([48], F32); gm = A.tile([8], F32); gf = A.tile([8], F32)
    dma(cts, cT, writes=[r_ca]); dma(bada, b_adaT, writes=[r_ca]); dma(gm, gmixT, writes=[r_ca]); dma(gf, gffnT, writes=[r_ca])
    P.op(ACT, lambda e: e.activation(out=cact, in_=cts, func=AF.Silu), reads=[r_ca], writes=[r_ca])
    wa = [A.tile([8, 768], F32) for _ in range(2)]; r_wa = [Res(), Res()]
    w_ada_v = w_ada.rearrange("(k p) n -> p k n", p=128)
    r_mod2 = Res()

    def ada_piece(j, bank=0, r_dst=None):
        b = j % 2
        dma(wa[b], w_ada_v[:, :, j * 768:(j + 1) * 768], writes=[r_wa[b]])
        for m in range(6):
            col = j * 6 + m
            for kc in range(8):
                P.op(PE, lambda e, b=b, m=m, kc=kc, col=col: e.matmul(ps[:, bank, col:col + 1], lhsT=wa[b][:, kc, m * 128:(m + 1) * 128],
                                                                  rhs=cact[:, kc:kc + 1], start=(kc == 0), stop=(kc == 7)),
                     reads=[r_wa[b], r_ca], writes=[rb[bank]])
        if r_dst is not None:
            c0_, c1_ = j * 6, j * 6 + 6
            P.op(DVE, lambda e: e.tensor_tensor(out=modT[:, c0_:c1_], in0=ps[:, bank, c0_:c1_], in1=bada[:, c0_:c1_], op=ALU.add), reads=[rb[bank], r_ca], writes=[r_dst])

    for j in range(3):
        ada_piece(j)
    P.op(DVE, lambda e: e.tensor_tensor(out=modT[:, 0:18], in0=ps[:, 0, 0:18], in1=bada[:, 0:18], op=ALU.add), reads=[rb[0], r_ca], writes=[r_mod])
    P.op(DVE, lambda e: e.tensor_scalar(out=ab[:, 1, :], in0=modT[:, 8:16], scalar1=1.0, scalar2=None, op0=ALU.add), writes=[r_mod])
    P.op(DVE, lambda e: e.tensor_tensor(out=ab[:, 0, :], in0=ab[:, 1, :], in1=gm, op=ALU.mult), reads=[r_ca], writes=[r_mod])

    def ada_finish():
        P.op(DVE, lambda e: e.tensor_scalar(out=ab[:, 3, :], in0=modT[:, 32:40], scalar1=1.0, scalar2=None, op0=ALU.add), writes=[r_mod2])
        P.op(DVE, lambda e: e.tensor_tensor(out=ab[:, 2, :], in0=ab[:, 3, :], in1=gf, op=ALU.mult), reads=[r_ca], writes=[r_mod2])
    a1 = ab[:, 0, :]; b1 = modT[:, 0:8]; a2 = ab[:, 2, :]; b2 = modT[:, 24:32]; g_m = modT[:, 16:24]; g_f = modT[:, 40:48]

    def prep_front(xrows, nblk, np_, xt, r_xt, xn, r_xn, st, r_st):
        if xrows is not None:
            dma(xt[0:np_, 0:nblk, :], xrows.rearrange("(b p) d -> p b d", p=np_), writes=[r_xt])
        for bl in range(nblk):
            P.op(ACT, lambda e, bl=bl: e.activation(out=xn[0:np_, bl, :], in_=xt[0:np_, bl, :], func=AF.Square, accum_out=st[0:np_, bl:bl + 1]),
                 reads=[r_xt], writes=[r_xn, r_st])
        P.op(ACT, lambda e: e.activation(out=st[0:np_, 4:4 + nblk], in_=st[0:np_, 0:nblk], func=AF.Sqrt, bias=EPS, scale=1.0 / D), writes=[r_st])
        P.op(DVE, lambda e: e.reciprocal(out=st[0:np_, 8:8 + nblk], in_=st[0:np_, 4:4 + nblk]), reads=[r_st], writes=[r_st])
        for bl in range(nblk):
            P.op(DVE, lambda e, bl=bl: e.tensor_scalar(out=xn[0:np_, bl, :], in0=xt[0:np_, bl, :], scalar1=st[0:np_, 8 + bl:9 + bl], scalar2=None, op0=ALU.mult),
                 reads=[r_xt, r_st], writes=[r_xn])

    def prep_back(nblk, np_, xn, r_xn, hT, r_hTk, banks, avec, bvec):
        ntok = nblk * np_
        for kc in range(8):
            bk = banks[kc // 2]
            for bl in range(nblk):
                o = (kc % 2) * 512 + bl * np_
                P.op(PE, lambda e, kc=kc, bl=bl, bk=bk, o=o: e.transpose(out=psb[bk][:, o:o + np_], in_=xn[0:np_, bl, kc * 128:(kc + 1) * 128],
                                                                     identity=ident[0:np_, 0:np_]),
                     reads=[r_xn, r_c], writes=[rb[bk]])
            if kc % 2 == 1:
                for k2 in (kc - 1, kc):
                    o = (k2 % 2) * 512
                    eng_ = DVE
                    if eng_ == DVE:
                        P.op(DVE, lambda e, k2=k2, bk=bk, o=o: e.tensor_scalar(out=hT[:, k2, 0:ntok], in0=psb[bk][:, o:o + ntok], scalar1=avec[:, k2:k2 + 1],
                                                                           scalar2=bvec[:, k2:k2 + 1], op0=ALU.mult, op1=ALU.add),
                             reads=[rb[bk], r_mod], writes=[r_hTk[k2]])
                    else:
                        P.op(ACT, lambda e, k2=k2, bk=bk, o=o: e.activation(out=hT[:, k2, 0:ntok], in_=psb[bk][:, o:o + ntok], func=AF.Identity, scale=avec[:, k2:k2 + 1],
                                                                        bias=bvec[:, k2:k2 + 1]),
                             reads=[rb[bk], r_mod], writes=[r_hTk[k2]])

    def prep(xrows, nblk, np_, xt, r_xt, xn, r_xn, hT, r_hT, st, r_st, banks, avec, bvec):
        prep_front(xrows, nblk, np_, xt, r_xt, xn, r_xn, st, r_st)
        prep_back(nblk, np_, xn, r_xn, hT, [r_hT] * 8, banks, avec, bvec)

    def load_w(dst, src_b, rows, c0, c1, r_w, cname="w_in"):
        dma(dst, src_b.rearrange("(k p) n -> p k n", p=128)[:, :, c0:c1], reads=[r_cast[cname]], writes=[r_w])

    evac_rr = [0]

    def evac(out, in_, reads, writes, func=None, scale=1.0):
        evac_rr[0] += 1
        if func is not None or evac_rr[0] % 2 == 0:
            f = func if func is not None else AF.Copy
            return P.op(ACT, lambda e: e.activation(out=out, in_=in_, func=f, scale=scale), reads=reads, writes=writes)
        if scale != 1.0:
            return P.op(DVE, lambda e: e.tensor_scalar(out=out, in0=in_, scalar1=scale, scalar2=None, op0=ALU.mult), reads=reads, writes=writes)
        return P.op(DVE, lambda e: e.tensor_copy(out=out, in_=in_), reads=reads, writes=writes)

    A.mark()
    wk = A.tile([8, 512], BF16); wv = A.tile([8, 512], BF16); wki = A.tile([8, 128], BF16); r_w1 = Res()
    wst0 = A.tile([8, 512], F32); wst = [wst0, wst0]; r_wst0 = Res(); r_wst = [r_wst0, r_wst0]
    w_in_v = w_in.rearrange("(k p) n -> p k n", p=128)
    dma(wst0, w_in_v[:, :, 512:1024], writes=[r_wst0])
    P.op(ACT, lambda e: e.activation(out=wk, in_=wst0, func=AF.Copy), reads=[r_wst0], writes=[r_w1])
    dma(wst0, w_in_v[:, :, 1024:1536], writes=[r_wst0])
    P.op(DVE, lambda e: e.tensor_copy(out=wv, in_=wst0), reads=[r_wst0], writes=[r_w1])
    dma(wst0[:, :, 0:64], w_in_v[:, :, 2048:2112], writes=[r_wst0])
    P.op(ACT, lambda e: e.activation(out=wki[:, :, 0:64], in_=wst0[:, :, 0:64], func=AF.Copy), reads=[r_wst0], writes=[r_w1])
    P.op(ACT, lambda e: e.activation(out=wki[:, :, 64:128], in_=wst0[:, :, 0:64], func=AF.Copy), reads=[r_wst0], writes=[r_w1])
    xts = [A.tile([4, D], F32) for _ in range(3)]; r_xts = [Res(), Res(), Res()]
    xns = [A.tile([4, D], BF16) for _ in range(2)]; r_xns = [Res(), Res()]
    hTs = [A.tile([8, 512], BF16) for _ in range(2)]; r_hTs = [[Res() for _ in range(8)] for _ in range(2)]
    sts = [A.tile([12], F32) for _ in range(2)]; r_sts = [Res(), Res()]
    ksts = [A.tile([4, 512], BF16) for _ in range(2)]; r_ksts = [Res(), Res()]
    vsts = [A.tile([4, 8, 65], BF16) for _ in range(2)]; r_vsts = [Res(), Res()]
    for b in range(2):
        P.op(DVE, lambda e, b=b: e.memset(vsts[b], 1.0), writes=[r_vsts[b]])
    kT_v = kT_s.rearrange("(m hh) d s -> (hh d) m s", hh=2)
    v_v = v_s.rearrange("(n p) c -> p n c", p=128)

    def load1a(T):
        dma(xts[T % 3][:, :, :], xk[T * 512:(T + 1) * 512, :].rearrange("(b p) d -> p b d", p=128), writes=[r_xts[T % 3]])

    def front1a(T):
        b = T % 2
        prep_front(None, 4, 128, xts[T % 3], r_xts[T % 3], xns[b], r_xns[b], sts[b], r_sts[b])

    def back1a(T):
        b = T % 2
        prep_back(4, 128, xns[b], r_xns[b], hTs[b], r_hTs[b], [0, 1, 2, 3], a1, b1)

    load1a(0)
    load1a(1)
    front1a(0)
    back1a(0)
    for T in range(NT):
        b = T % 2
        hT = hTs[b]
        if T + 2 < NT:
            load1a(T + 2)
        if T + 1 < NT:
            front1a(T + 1)
        for m in range(4):
            bk = 4 + (m % 2)
            for kc in range(8):
                P.op(PE, lambda e, m=m, kc=kc, bk=bk, hT=hT: e.matmul(ps[:, bk, :], lhsT=wk[:, kc, m * 128:(m + 1) * 128], rhs=hT[:, kc, :],
                                                                  start=(kc == 0), stop=(kc == 7)), reads=[r_w1, r_hTs[b][kc]], writes=[rb[bk]])
            evac(ksts[b][:, m, :], ps[:, bk, :], [rb[bk]], [r_ksts[b]])
        dma(kT_v[:, :, T * 512:(T + 1) * 512], ksts[b], reads=[r_ksts[b]], eng=STQ)
        if T + 1 < NT:
            back1a(T + 1)
        for bl in range(4):
            bk = 6 + (bl % 2)
            for kc in range(8):
                P.op(PE, lambda e, bl=bl, kc=kc, bk=bk, hT=hT: e.matmul(ps[:, bk, :], lhsT=hT[:, kc, bl * 128:(bl + 1) * 128], rhs=wv[:, kc, :],
                                                                    start=(kc == 0), stop=(kc == 7)), reads=[r_w1, r_hTs[b][kc]], writes=[rb[bk]])
            evac(vsts[b][:, bl, :, 0:64], ps[:, bk, :].rearrange("p (h d) -> p h d", d=64), [rb[bk]], [r_vsts[b]])
        dma(v_v[:, T * 4:(T + 1) * 4, :], vsts[b].rearrange("p n h c -> p n (h c)"), reads=[r_vsts[b]], eng=STQ)
        for kc in range(8):
            P.op(PE, lambda e, kc=kc, hT=hT: e.matmul(ps[:, 4, :], lhsT=wki[:, kc, :], rhs=hT[:, kc, :], start=(kc == 0), stop=(kc == 7)),
                 reads=[r_w1, r_hTs[b][kc]], writes=[rb[4]])
        evac(kiT[:, T * 512:(T + 1) * 512], ps[:, 4, :], [rb[4]], [r_ki])
        if T < 5:
            ada_piece(3 + T, bank=4, r_dst=r_mod2)
        if T == 5:
            ada_finish()
    P.barrier()
    A.release()
    A.release()

    A.mark()
    wq = A.tile([8, 512], BF16); wqi = A.tile([8, 512], BF16); wwi = A.tile([8, 8], BF16)
    wu = A.tile([8, 1024], BF16); wga = A.tile([8, 1024], BF16); wgb = A.tile([8, 1024], BF16); r_w2 = Res()
    load_w(wq, w_in_b, D, 0, 512, r_w2); load_w(wqi, w_in_b, D, 1536, 2048, r_w2); load_w(wwi, w_in_b, D, 2112, 2120, r_w2)
    load_w(wu, w_in_b, D, 2120, 3144, r_w2); load_w(wga, w_in_b, D, 3144, 4168, r_w2); load_w(wgb, w_in_b, D, 4168, 5192, r_w2)
    xts1 = [A.tile([4, D], F32) for _ in range(2)]; r_xts1 = [Res(), Res()]
    xns1 = [A.tile([4, D], BF16) for _ in range(2)]; r_xns1 = [Res(), Res()]
    hTs1 = [A.tile([8, 512], BF16) for _ in range(2)]; r_hTs1 = [[Res() for _ in range(8)] for _ in range(2)]
    sts1 = [A.tile([12], F32) for _ in range(2)]; r_sts1 = [Res(), Res()]
    xth = A.tile([1, D], F32); r_xth = Res(); xnh = A.tile([1, D], BF16); r_xnh = Res()
    hTh = A.tile([8, 32], BF16); r_hTh = Res(); sth = A.tile([12], F32); r_sth = Res()
    stg = [A.tile([512], BF16) for _ in range(3)]; r_stg = [Res() for _ in range(3)]
    sgm = [A.tile([512], F32) for _ in range(2)]; r_sgm = [Res(), Res()]
    stg_i = [0]
    qT_v = qT_s.rearrange("(m hh) d s -> (hh d) m s", hh=2)

    def stage_out(dst, src_ps, bk, func=None):
        i = stg_i[0] % 3
        stg_i[0] += 1
        evac(stg[i], src_ps, [rb[bk]], [r_stg[i]], func=func)
        dma(dst, stg[i], reads=[r_stg[i]], eng=STQ)

    def proj_fm(w, m, hT_, r_h, bk, n):
        for kc in range(8):
            P.op(PE, lambda e, kc=kc: e.matmul(ps[:, bk, 0:n], lhsT=w[:, kc, m * 128:(m + 1) * 128], rhs=hT_[:, kc, 0:n], start=(kc == 0), stop=(kc == 7)),
                 reads=[r_w2, r_h[kc]], writes=[rb[bk]])

    def front1b(g):
        T = 4 * g + 3
        b = g % 2
        prep_front(xk[T * 512:(T + 1) * 512, :], 4, 128, xts1[b], r_xts1[b], xns1[b], r_xns1[b], sts1[b], r_sts1[b])

    def back1b(g):
        b = g % 2
        prep_back(4, 128, xns1[b], r_xns1[b], hTs1[b], r_hTs1[b], [0, 1, 2, 3], a1, b1)

    front1b(0)
    back1b(0)

    for g in range(4):
        T = 4 * g + 3
        hT = hTs1[g % 2]; r_hT = r_hTs1[g % 2]
        if g + 1 < 4:
            front1b(g + 1)
        cs = slice(g * 512, (g + 1) * 512)
        bkc = [0]

        def nb():
            bkc[0] += 1
            return 4 + bkc[0] % 4
        for m in range(4):
            bk = nb(); proj_fm(wq, m, hT, r_hT, bk, 512); stage_out(qT_v[:, m, cs], ps[:, bk, :], bk)
        for m in range(4):
            bk = nb(); proj_fm(wqi, m, hT, r_hT, bk, 512); stage_out(qiT_s[m, :, cs], ps[:, bk, :], bk)
        if g + 1 < 4:
            back1b(g + 1)
        for m in range(8):
            bk = nb(); proj_fm(wga, m, hT, r_hT, bk, 512); stage_out(gaT_s[m, :, cs], ps[:, bk, :], bk, func=AF.Sigmoid)
        for m in range(8):
            bk = nb(); proj_fm(wgb, m, hT, r_hT, bk, 512); stage_out(gbT_s[m, :, cs], ps[:, bk, :], bk, func=AF.Sigmoid)
        for bl in range(4):
            bk = nb()
            for kc in range(8):
                P.op(PE, lambda e, kc=kc, bl=bl, bk=bk, hT=hT: e.matmul(ps[:, bk, 0:8], lhsT=hT[:, kc, bl * 128:(bl + 1) * 128], rhs=wwi[:, kc, :], start=(kc == 0), stop=(kc == 7)),
                     reads=[r_w2, r_hT[kc]], writes=[rb[bk]])
            P.op(DVE, lambda e, bl=bl, bk=bk, g=g: e.tensor_scalar(out=wiall[:, g * 4 + bl, :], in0=ps[:, bk, 0:8], scalar1=float(8 ** -0.5 * 64 ** -0.5), scalar2=None, op0=ALU.mult),
                 reads=[rb[bk]], writes=[r_wi])
        for i in range(4):
            bka = nb(); proj_fm(wu, i, hT, r_hT, bka, 512)
            bkg = nb(); proj_fm(wu, 4 + i, hT, r_hT, bkg, 512)
            si = i % 2
            P.op(ACT, lambda e, si=si, bkg=bkg: e.activation(out=sgm[si], in_=ps[:, bkg, :], func=AF.Sigmoid), reads=[rb[bkg]], writes=[r_sgm[si]])
            j = stg_i[0] % 3; stg_i[0] += 1
            P.op(DVE, lambda e, si=si, bka=bka, j=j: e.tensor_tensor(out=stg[j], in0=ps[:, bka, :], in1=sgm[si], op=ALU.mult), reads=[rb[bka], r_sgm[si]], writes=[r_stg[j]])
            dma(zT_s[i, :, g, 32:544], stg[j], reads=[r_stg[j]], eng=STQ)
        prep(xk[T * 512 - 32:T * 512, :], 1, 32, xth, r_xth, xnh, r_xnh, hTh, r_hTh, sth, r_sth, [0, 1, 2, 3], a1, b1)
        for i in range(4):
            bka = nb(); proj_fm(wu, i, hTh, [r_hTh] * 8, bka, 32)
            bkg = nb(); proj_fm(wu, 4 + i, hTh, [r_hTh] * 8, bkg, 32)
            si = i % 2
            P.op(ACT, lambda e, si=si, bkg=bkg: e.activation(out=sgm[si][:, 0:32], in_=ps[:, bkg, 0:32], func=AF.Sigmoid), reads=[rb[bkg]], writes=[r_sgm[si]])
            j = stg_i[0] % 3; stg_i[0] += 1
            P.op(DVE, lambda e, si=si, bka=bka, j=j: e.tensor_tensor(out=sgm[si][:, 32:64], in0=ps[:, bka, 0:32], in1=sgm[si][:, 0:32], op=ALU.mult), reads=[rb[bka]], writes=[r_sgm[si]])
            P.op(DVE, lambda e, si=si, j=j, g=g: e.tensor_tensor(out=stg[j][:, 0:32], in0=sgm[si][:, 32:64], in1=hval[:, g * 32:(g + 1) * 32], op=ALU.mult), reads=[r_c], writes=[r_stg[j], r_sgm[si]])
            dma(zT_s[i, :, g, 0:32], stg[j][:, 0:32], reads=[r_stg[j]], eng=STQ)
    P.barrier()
    A.release()

    A.mark()
    cbt = A.tile([4, 512], BF16); kbt = A.tile([1536], BF16); r_c2 = Res()
    dma(cbt, cb_d, writes=[r_c2]); dma(kbt[0:1, :], kbias_d[:, 0:1536], writes=[r_c2])
    onesb = A.tile([128], BF16)
    nidm = A.tile([128], BF16)
    P.op(DVE, lambda e: e.memset(onesb, 1.0), writes=[r_c2])
    P.op(DVE, lambda e: e.tensor_scalar(out=nidm, in0=ident, scalar1=-MASKB, scalar2=None, op0=ALU.mult), reads=[r_c], writes=[r_c2])
    scoress = [A.tile([S], F32) for _ in range(2)]; r_scs = [Res(), Res()]
    maskq = A.tile([S], BF16); r_mq = Res()
    junk = maskq; r_junk = r_mq
    maskTs = [A.tile([64, 256], FP8) for _ in range(2)]; r_mTs = [Res(), Res()]
    qit = A.tile([4, 256], BF16); r_qit = Res()
    qaugs = [A.tile([8, 256], BF16) for _ in range(2)]; r_qas = [Res(), Res()]
    wdgs = [A.tile([8, 128], BF16) for _ in range(2)]; r_wdgs = [Res(), Res()]
    Rt = [A.tile([2, 512], BF16) for _ in range(3)]; r_Rt = [Res() for _ in range(3)]
    bs = A.tile([16], F32); r_bs = Res()
    bsa = A.tile([4], F32); r_bsa = Res(); r_junkA = Res()
    cntk = A.tile([64], F32); r_ck = Res()
    U = A.tile([68], F32); r_U = Res()
    kt = [A.tile([8, 512], BF16) for _ in range(2)]; r_kt = [Res(), Res()]
    vt = [A.tile([4, 520], BF16) for _ in range(2)]; r_vt = [Res(), Res()]
    Pt = [A.tile([2, 256], BF16) for _ in range(3)]; r_Pt = [Res() for _ in range(3)]
    den = A.tile([16], F32); r_den = Res()
    Ob = A.tile([2, 512], BF16); r_Ob = Res()
    OTst = A.tile([4, 256], BF16); r_OT = Res()
    P.op(DVE, lambda e: e.memset(U, 0.0), writes=[r_U])
    P.op(DVE, lambda e: e.memset(U[:, 64:66], 1.0), writes=[r_U])
    for i in range(2):
        P.op(DVE, lambda e, i=i: e.memset(qaugs[i], 0.0), writes=[r_qas[i]])
    kT_hv = kT_s.rearrange("h d s -> d h s")
    kaug_v = kaug_d
    v_v2 = v_s.rearrange("(n p) c -> p n c", p=128)
    LB = 2
    SB = 7

    PORDER = [0, 2, 3, 4, 5, 6, 7, 1]

    def idx_gen(sq_):
        sp_, q2 = sq_ // 2, sq_ % 2
        qp = PORDER[sp_]
        qb = 2 * qp + q2
        g = qp // 2
        E = 2048 * (g + 1)
        nch = E // 512
        c0 = g * 512 + (qp % 2) * 256
        j4 = qb % 4
        scores = scoress[sq_ % 2]; r_sc = r_scs[sq_ % 2]
        wdg = wdgs[sq_ % 2]; r_wdg = r_wdgs[sq_ % 2]
        if q2 == 0:
            dma(qit, qiT_s.rearrange("m p s -> p m s")[:, :, c0:c0 + 256], writes=[r_qit])
        for j in range(8):
            P.op(DVE, lambda e, j=j: e.tensor_scalar(out=wdg[:, j, :], in0=ident, scalar1=wiall[:, qb, j:j + 1], scalar2=None, op0=ALU.mult),
                 reads=[r_wi, r_c], writes=[r_wdg])
        yield 0.5
        units = [(c, m) for c in range(nch) for m in range(4)]

        def emit_diag(u):
            c, m = units[u]
            ks = slice(c * 512, (c + 1) * 512)
            has_kb = (c < 3)
            has_cb = (c == nch - 1)
            lastdiag = not (has_kb or has_cb)
            ri = u % 3
            for hh in range(2):
                imm = 2 * m + hh
                P.op(PE, lambda e, m=m, hh=hh, ri=ri, imm=imm, lastdiag=lastdiag: e.matmul(ps[:, SB, :], lhsT=wdg[:, 2 * m + hh, :], rhs=Rt[ri][:, hh, :], start=(imm == 0), stop=(lastdiag and imm == 7)),
                     reads=[r_wdg, r_Rt[ri]], writes=[rb[SB]])
            if m == 3:
                if has_kb:
                    P.op(PE, lambda e, ks=ks, has_cb=has_cb: e.matmul(ps[:, SB, :], lhsT=onesb[0:1, :], rhs=kbt[0:1, ks], start=False, stop=(not has_cb)),
                         reads=[r_c2], writes=[rb[SB]])
                if has_cb:
                    P.op(PE, lambda e: e.matmul(ps[:, SB, :], lhsT=ident, rhs=cbt[:, j4, :], start=False, stop=True), reads=[r_c2, r_c], writes=[rb[SB]])
                P.op(ACT, lambda e, ks=ks: e.activation(out=scores[:, ks], in_=ps[:, SB, :], func=AF.Copy), reads=[rb[SB]], writes=[r_sc])

        for u, (c, m) in enumerate(units):
            ks = slice(c * 512, (c + 1) * 512)
            for hh in range(2):
                pr = slice(hh * 64, hh * 64 + 64)
                P.op(PE, lambda e, m=m, hh=hh, pr=pr, ks=ks: e.matmul(ps[:, LB + hh, :], lhsT=qit[pr, m, q2 * 128:(q2 + 1) * 128], rhs=kiT[pr, ks],
                                                                start=True, stop=True), reads=[r_qit, r_ki], writes=[rb[LB + hh]])
            ri = u % 3
            P.op(ACT, lambda e, ri=ri: e.activation(out=Rt[ri], in_=ps[:, LB:LB + 2, :], func=AF.Relu), reads=[rb[LB], rb[LB + 1]], writes=[r_Rt[ri]])
            if u > 0:
                emit_diag(u - 1)
            yield 0.8
        emit_diag(len(units) - 1)
        if debug:
            dma(dbg_sc[qb, :, 0:E], scores[:, 0:E], reads=[r_sc])
        yield 0.5

    def bis_gen(sq_):
        sp_, q2 = sq_ // 2, sq_ % 2
        qp = PORDER[sp_]
        qb = 2 * qp + q2
        g = qp // 2
        E = 2048 * (g + 1)
        nkc = E // 128
        c0 = g * 512 + (qp % 2) * 256
        scores = scoress[sq_ % 2]; r_sc = r_scs[sq_ % 2]
        maskT = maskTs[sp_ % 2]; r_mT = r_mTs[sp_ % 2]
        qaug = qaugs[sp_ % 2]; r_qa = r_qas[sp_ % 2]
        if q2 == 0:
            dma(qaug[0:64, :, :], qT_s.rearrange("h d s -> d h s")[:, :, c0:c0 + 256], writes=[r_qa])
        sc = scores[:, 0:E]
        tpass = E / 960.0
        P.op(DVE, lambda e: e.tensor_reduce(out=bs[:, 0:1], in_=sc, axis=AX.X, op=ALU.max), reads=[r_sc], writes=[r_bs])
        P.op(DVE, lambda e: e.tensor_scalar(out=bs[:, 9:10], in0=bs[:, 0:1], scalar1=-1.0, scalar2=None, op0=ALU.mult), writes=[r_bs])
        P.op(DVE, lambda e: e.tensor_tensor(out=bs[:, 9:10], in0=bs[:, 9:10], in1=bs[:, 0:1], op=ALU.max), writes=[r_bs])
        P.op(DVE, lambda e: e.tensor_scalar(out=bs[:, 9:10], in0=bs[:, 9:10], scalar1=3.0, scalar2=3.0, op0=ALU.mult, op1=ALU.add), writes=[r_bs])
        P.op(DVE, lambda e: e.tensor_tensor(out=bs[:, 3:4], in0=bs[:, 0:1], in1=bs[:, 9:10], op=ALU.subtract), writes=[r_bs])
        P.op(DVE, lambda e: e.tensor_scalar(out=bs[:, 6:7], in0=bs[:, 3:4], scalar1=20000.0, scalar2=None, op0=ALU.add), writes=[r_bs])
        P.op(DVE, lambda e: e.tensor_tensor(out=bs[:, 7:8], in0=bs[:, 9:10], in1=bs[:, 6:7], op=ALU.subtract), writes=[r_bs])
        yield tpass + 1.0
        for it in range(NITER + 1):
            P.op(DVE, lambda e: e.tensor_scalar(out=junk[:, 0:E], in0=sc, scalar1=bs[:, 3:4], scalar2=None, op0=ALU.is_ge, op1=ALU.add, accum_out=bs[:, 4:5]),
                 reads=[r_sc], writes=[r_junk, r_bs])
            P.op(DVE, lambda e: e.tensor_scalar(out=bs[:, 5:6], in0=bs[:, 4:5], scalar1=255.5, scalar2=None, op0=ALU.is_ge), writes=[r_bs])
            if it == 0:
                P.op(DVE, lambda e: e.tensor_scalar(out=bs[:, 1:2], in0=bs[:, 5:6], scalar1=bs[:, 6:7], scalar2=-20000.0, op0=ALU.mult, op1=ALU.add), writes=[r_bs])
                P.op(DVE, lambda e: e.scalar_tensor_tensor(out=bs[:, 2:3], in0=bs[:, 5:6], scalar=bs[:, 7:8], in1=bs[:, 6:7], op0=ALU.mult, op1=ALU.add), writes=[r_bs])
            else:
                P.op(DVE, lambda e: e.tensor_scalar(out=bs[:, 2:3], in0=bs[:, 2:3], scalar1=0.5, scalar2=None, op0=ALU.mult), writes=[r_bs])
                P.op(DVE, lambda e: e.scalar_tensor_tensor(out=bs[:, 1:2], in0=bs[:, 5:6], scalar=bs[:, 2:3], in1=bs[:, 1:2], op0=ALU.mult, op1=ALU.add), writes=[r_bs])
            P.op(DVE, lambda e: e.scalar_tensor_tensor(out=bs[:, 3:4], in0=bs[:, 2:3], scalar=0.5, in1=bs[:, 1:2], op0=ALU.mult, op1=ALU.add), writes=[r_bs])
            yield tpass + 0.8
        if debug:
            dma(dbg_thr[:, qb * 4:qb * 4 + 4], bs[:, 0:4], reads=[r_bs])
        P.op(DVE, lambda e: e.tensor_scalar(out=maskq[:, 0:E], in0=sc, scalar1=bs[:, 1:2], scalar2=None, op0=ALU.is_ge), reads=[r_sc, r_junkA], writes=[r_mq, r_bs])
        P.op(DVE, lambda e: e.tensor_reduce(out=cntk[:, 0:nkc], in_=maskq[:, 0:E].rearrange("p (a b) -> p a b", b=128), axis=AX.X, op=ALU.add), writes=[r_ck, r_mq])
        P.op(DVE, lambda e: e.tensor_scalar(out=cntk[:, 0:nkc], in0=cntk[:, 0:nkc], scalar1=0.5, scalar2=None, op0=ALU.is_ge), writes=[r_ck])
        P.op(DVE, lambda e: e.tensor_tensor(out=cntk[:, 0:nkc], in0=cntk[:, 0:nkc], in1=kcp1[:, 0:nkc], op=ALU.mult), reads=[r_c], writes=[r_ck])
        P.op(DVE, lambda e: e.tensor_reduce(out=bs[:, 8:9], in_=cntk[:, 0:nkc], axis=AX.X, op=ALU.max), writes=[r_ck, r_bs])
        P.op(DVE, lambda e: e.tensor_scalar(out=U[:, 66:67], in0=bs[:, 8:9], scalar1=-1.0, scalar2=None, op0=ALU.add), writes=[r_U, r_bs])
        yield 2 * tpass
        P.op(PE, lambda e: e.matmul(ps[0:67, LB, 0:128], lhsT=U[:, 0:67], rhs=identf, start=True, stop=True), reads=[r_U, r_c], writes=[rb[LB]])
        for h in range(8):
            P.op(ACT, lambda e, h=h: e.activation(out=qaug[64:67, h, q2 * 128:(q2 + 1) * 128], in_=ps[64:67, LB, 0:128], func=AF.Copy), reads=[rb[LB]], writes=[r_qa])
        yield 1.0
        for k8 in range(nkc // 8):
            for kk in range(8):
                kc = k8 * 8 + kk
                P.op(PE, lambda e, kc=kc, kk=kk: e.transpose(out=psb[LB + 1][:, kk * 128:(kk + 1) * 128], in_=maskq[:, kc * 128:(kc + 1) * 128], identity=ident),
                     reads=[r_mq, r_c], writes=[rb[LB + 1]])
            mo = maskT[:, k8 * 8:(k8 + 1) * 8, q2 * 128:(q2 + 1) * 128]
            mi = psb[LB + 1].rearrange("p (a b) -> p a b", b=128)
            P.op(ACT, lambda e, mo=mo, mi=mi: e.activation(out=mo, in_=mi, func=AF.Copy, scale=-1.0, bias=1.0, saturate=False), reads=[rb[LB + 1]], writes=[r_mT])
            yield 1.0

    accb = [4, 5, 6]

    def att_gen(sp_):
        qp = PORDER[sp_]
        g = qp // 2
        E = 2048 * (g + 1)
        nch = E // 512
        nkc = E // 128
        c0 = g * 512 + (qp % 2) * 256
        maskT = maskTs[sp_ % 2]; r_mT = r_mTs[sp_ % 2]
        qaug = qaugs[sp_ % 2]; r_qa = r_qas[sp_ % 2]
        first_in_bank = {}
        steps = [(c, kl, hp) for c in range(nch) for kl in range(4) for hp in range(4)]

        def emit_loads(c):
            kb_ = c % 2
            ks = slice(c * 512, (c + 1) * 512)
            dma(kt[kb_][0:64, :, :], kT_hv[:, :, ks], writes=[r_kt[kb_]])
            dma(kt[kb_][64:67, :, :], kaug_v[:, :, ks], writes=[r_kt[kb_]])
            dma(vt[kb_], v_v2[:, c * 4:(c + 1) * 4, :], writes=[r_vt[kb_]])

        def emit_ST(i):
            c, kl, hp = steps[i]
            kb_ = c % 2
            kc = c * 4 + kl
            sbk = i % 2
            for hh in range(2):
                h = hp * 2 + hh
                o = hh * 256
                P.op(PE, lambda e, h=h, o=o, sbk=sbk, kb_=kb_, kl=kl: e.matmul(ps[:, sbk, o:o + 256], lhsT=kt[kb_][0:67, h, kl * 128:(kl + 1) * 128],
                                                                    rhs=qaug[0:67, h, :], start=True, stop=False),
                     reads=[r_kt[kb_], r_qa], writes=[rb[sbk]])
                P.op(PE, lambda e, o=o, sbk=sbk, kc=kc: e.matmul(ps[:, sbk, o:o + 256], lhsT=nidm, rhs=maskT[:, kc, :], start=False, stop=True),
                     reads=[r_mT, r_c2], writes=[rb[sbk]])

        emit_loads(0)
        if nch > 1:
            emit_loads(1)
        emit_ST(0)
        for i, (c, kl, hp) in enumerate(steps):
            kb_ = c % 2
            kc = c * 4 + kl
            sbk = i % 2
            if i + 1 < len(steps):
                emit_ST(i + 1)
            pi = i % 3
            P.op(ACT, lambda e, pi=pi, sbk=sbk: e.activation(out=Pt[pi], in_=ps[:, sbk, :].rearrange("p (a b) -> p a b", b=256), func=AF.Exp, bias=EXPB, scale=0.125),
                 reads=[rb[sbk]], writes=[r_Pt[pi]])
            for q2 in range(2):
                for hh in range(2):
                    h = hp * 2 + hh
                    a = q2 * 8 + h
                    ab_ = accb[a // 6]
                    col = (a % 6) * 65
                    st_ = (kc == 0) and (ab_ not in first_in_bank)
                    if kc == 0:
                        first_in_bank[ab_] = True
                    P.op(PE, lambda e, pi=pi, hh=hh, q2=q2, h=h, ab_=ab_, col=col, st_=st_, kb_=kb_, kl=kl, kc=kc: e.matmul(
                        ps[:, ab_, col:col + 65], lhsT=Pt[pi][:, hh, q2 * 128:(q2 + 1) * 128], rhs=vt[kb_][:, kl, h * 65:(h + 1) * 65],
                        start=st_, stop=(kc == nkc - 1), skip_group_check=True),
                        reads=[r_Pt[pi], r_vt[kb_]], writes=[rb[ab_]])
            if kl == 3 and hp == 3 and c + 2 < nch:
                emit_loads(c + 2)
            yield 0.8
        for a in range(16):
            ab_ = accb[a // 6]; col = (a % 6) * 65
            P.op(DVE, lambda e, a=a, ab_=ab_, col=col: e.tensor_copy(out=den[:, a:a + 1], in_=ps[:, ab_, col + 64:col + 65]), reads=[rb[ab_]], writes=[r_den])
        P.op(DVE, lambda e: e.reciprocal(out=den, in_=den), writes=[r_den])
        for a in range(16):
            ab_ = accb[a // 6]; col = (a % 6) * 65
            q2 = a // 8; h = a % 8
            P.op(DVE, lambda e, a=a, ab_=ab_, col=col, q2=q2, h=h: e.tensor_scalar(out=Ob[:, q2, h * 64:(h + 1) * 64], in0=ps[:, ab_, col:col + 64], scalar1=den[:, a:a + 1], scalar2=None, op0=ALU.mult),
                 reads=[rb[ab_]], writes=[r_Ob, r_den])
        yield 3.0
        for q2 in range(2):
            for m in range(4):
                P.op(PE, lambda e, q2=q2, m=m: e.transpose(out=psb[0][:, (q2 * 4 + m) * 128:(q2 * 4 + m + 1) * 128], in_=Ob[:, q2, m * 128:(m + 1) * 128], identity=ident),
                     reads=[r_Ob, r_c], writes=[rb[0]])
        for q2 in range(2):
            P.op(ACT, lambda e, q2=q2: e.activation(out=OTst[:, :, q2 * 128:(q2 + 1) * 128], in_=psb[0][:, q2 * 512:(q2 + 1) * 512].rearrange("p (a b) -> p a b", b=128), func=AF.Copy),
                 reads=[rb[0]], writes=[r_OT])
        dma(OT_s.rearrange("m p s -> p m s")[:, :, c0:c0 + 256], OTst, reads=[r_OT], eng=STQ)
        yield 1.0

    NQB, NPR = 16, 8
    done = {"IDX": 0, "BIS": 0, "ATT": 0}
    nitems = {"IDX": NQB, "BIS": NQB, "ATT": NPR}
    mk = {"IDX": idx_gen, "BIS": bis_gen, "ATT": att_gen}
    cur = {"IDX": None, "BIS": None, "ATT": None}
    clk = {"IDX": 0.0, "BIS": 0.0, "ATT": 0.0}
    now = [0.0]

    def can_start(s):
        i = done[s]
        if i >= nitems[s]:
            return False
        if s == "IDX":
            return done["BIS"] >= i - 1
        if s == "BIS":
            return done["IDX"] > i and done["ATT"] >= i // 2 - 1
        return done["BIS"] > 2 * i + 1

    while any(done[s] < nitems[s] for s in done):
        cands = []
        for s in ("ATT", "IDX", "BIS"):
            if cur[s] is None and can_start(s):
                cur[s] = mk[s](done[s])
                clk[s] = max(clk[s], now[0])
            if cur[s] is not None:
                cands.append(s)
        assert cands, ("scheduler deadlock", done)
        s = min(cands, key=lambda k: clk[k])
        now[0] = clk[s]
        cost = next(cur[s], None)
        if cost is None:
            cur[s] = None
            done[s] += 1
        else:
            clk[s] += cost
    P.barrier()
    A.release()
    A.release()

    A.mark()
    wap = A.tile([4, D], BF16); wcp = A.tile([4, D], BF16); wo = A.tile([8, D], BF16); r_w3 = Res()
    load_w(wap, w_ap_b, 512, 0, D, r_w3, "w_ap"); load_w(wcp, w_cp_b, 512, 0, D, r_w3, "w_cp"); load_w(wo, w_out_b, D, 0, D, r_w3, "w_out")
    wfo_t = [A.tile([22, 128], BF16) for _ in range(2)]; r_wfo = [Res(), Res()]
    wdw = A.tile([4, 31], F32); cvp = A.tile([3, 4], F32); r_cv = Res()
    dma(wdw, w_dwT, writes=[r_cv]); dma(cvp[:, 0, :], b_dwT, writes=[r_cv]); dma(cvp[:, 1, :], lngT, writes=[r_cv]); dma(cvp[:, 2, :], lnbT, writes=[r_cv])
    wdd = A.tile([31, 128], BF16); r_wdd = Res()
    xtb = [A.tile([D], F32) for _ in range(2)]; r_xtb = [Res(), Res()]
    xTs = [A.tile([8, 512], F32) for _ in range(2)]; r_xTs = [Res(), Res()]
    OT = A.tile([4, 512], BF16); zT = A.tile([4, 544], BF16); r_in3 = Res()
    gab = [A.tile([2, 512], BF16) for _ in range(2)]; r_gab = [Res(), Res()]
    cv = A.tile([4, 512], F32); r_cvo = Res()
    sq = A.tile([512], F32); r_sq = Res()
    stt = A.tile([3, 512], F32); r_stt = Res()
    zc = A.tile([4, 512], BF16); r_zc = Res()
    t1 = A.tile([512], F32); r_t1 = Res()
    mg = A.tile([8, 512], BF16); r_mg = Res()
    h2s = [A.tile([8, 512], BF16) for _ in range(2)]; r_h2s = [Res(), Res()]
    gT = A.tile([22, 512], BF16); r_gT = Res()
    wfi_t = [A.tile([8, 256], BF16) for _ in range(2)]; r_wfi = [Res() for _ in range(2)]
    sl = [A.tile([512], F32) for _ in range(2)]; r_sl = [Res(), Res()]
    sq2 = A.tile([512], F32); r_sq2 = Res()
    rs2 = A.tile([512], F32); r_rs2 = Res()
    yo = A.tile([D], F32); r_yo = Res()
    w_fo_v = w_fo_b.rearrange("(k p) n -> p k n", p=128)
    w_fi_v = w_fi_b.rearrange("(k p) n -> p k n", p=128)

    def rstd_sumsq(n, src_fn, nchunks, sqt, r_sqt, bank, dst, r_dst, rd):
        for m in range(nchunks):
            P.op(ACT, lambda e, m=m: e.activation(out=sqt, in_=src_fn(m), func=AF.Square), reads=rd, writes=[r_sqt])
            P.op(PE, lambda e, m=m: e.matmul(ps[:, bank, :], lhsT=onesf, rhs=sqt, start=(m == 0), stop=(m == nchunks - 1)), reads=[r_sqt, r_c], writes=[rb[bank]])
        P.op(ACT, lambda e: e.activation(out=dst, in_=ps[:, bank, :], func=AF.Sqrt, bias=EPS, scale=1.0 / n), reads=[rb[bank]], writes=[r_dst])
        P.op(DVE, lambda e: e.reciprocal(out=dst, in_=dst), writes=[r_dst])

    def front_gen(g):
        T = 4 * g + 3
        cs = slice(g * 512, (g + 1) * 512)
        xT = xTs[g % 2]; r_xT = r_xTs[g % 2]
        h2 = h2s[g % 2]; r_h2 = r_h2s[g % 2]
        dma(OT, OT_s.rearrange("m p s -> p m s")[:, :, cs], writes=[r_in3])
        dma(zT, zT_s[:, :, g, :].rearrange("m p s -> p m s"), writes=[r_in3])
        for bl in range(4):
            xb_ = xtb[bl % 2]; rxb = r_xtb[bl % 2]
            dma(xb_, xk[T * 512 + bl * 128:T * 512 + (bl + 1) * 128, :], writes=[rxb])
            for half in range(2):
                bk = half
                for k4 in range(4):
                    kc = half * 4 + k4
                    P.op(PE, lambda e, kc=kc, k4=k4, bk=bk, xb_=xb_: e.transpose(out=ps[:, bk, k4 * 128:(k4 + 1) * 128], in_=xb_[:, kc * 128:(kc + 1) * 128], identity=identf),
                         reads=[rxb, r_c], writes=[rb[bk]])
                evac(xT[:, half * 4:half * 4 + 4, bl * 128:(bl + 1) * 128], ps[:, bk, :].rearrange("p (a b) -> p a b", b=128), [rb[bk]], [r_xT])
            yield 3.0
        for i in range(4):
            bk = i % 2
            for k in range(31):
                P.op(DVE, lambda e, i=i, k=k: e.tensor_scalar(out=wdd[:, k, :], in0=ident, scalar1=wdw[:, i, k:k + 1], scalar2=None, op0=ALU.mult),
                     reads=[r_cv, r_c], writes=[r_wdd])
            for k in range(31):
                P.op(PE, lambda e, i=i, k=k, bk=bk: e.matmul(ps[:, bk, :], lhsT=wdd[:, k, :], rhs=zT[:, i, k + 2:k + 514], start=(k == 0), stop=(k == 30)),
                     reads=[r_wdd, r_in3], writes=[rb[bk]])
            P.op(DVE, lambda e, i=i, bk=bk: e.tensor_scalar(out=cv[:, i, :], in0=ps[:, bk, :], scalar1=cvp[:, 0, i:i + 1], scalar2=None, op0=ALU.add),
                 reads=[rb[bk], r_cv], writes=[r_cvo])
            yield 7.0
        for i in range(4):
            P.op(PE, lambda e, i=i: e.matmul(ps[:, 2, :], lhsT=onesf, rhs=cv[:, i, :], start=(i == 0), stop=(i == 3)), reads=[r_cvo, r_c], writes=[rb[2]])
        P.op(ACT, lambda e: e.activation(out=stt[:, 0, :], in_=ps[:, 2, :], func=AF.Copy, scale=1.0 / 512), reads=[rb[2]], writes=[r_stt])
        for i in range(4):
            P.op(DVE, lambda e, i=i: e.tensor_tensor(out=cv[:, i, :], in0=cv[:, i, :], in1=stt[:, 0, :], op=ALU.subtract), reads=[r_stt], writes=[r_cvo])
        yield 4.0
        rstd_sumsq(512, lambda m: cv[:, m, :], 4, sq, r_sq, 3, stt[:, 2, :], r_stt, [r_cvo])
        yield 4.0
        for i in range(4):
            P.op(DVE, lambda e, i=i: e.tensor_tensor(out=cv[:, i, :], in0=cv[:, i, :], in1=stt[:, 2, :], op=ALU.mult), reads=[r_stt], writes=[r_cvo])
            P.op(DVE, lambda e, i=i: e.tensor_scalar(out=cv[:, i, :], in0=cv[:, i, :], scalar1=cvp[:, 1, i:i + 1], scalar2=cvp[:, 2, i:i + 1], op0=ALU.mult, op1=ALU.add),
                 reads=[r_cv], writes=[r_cvo])
            P.op(ACT, lambda e, i=i: e.activation(out=zc[:, i, :], in_=cv[:, i, :], func=AF.Silu), reads=[r_cvo], writes=[r_zc])
        yield 4.0
        for m in range(8):
            b0 = (2 * m) % 4; b1_ = (2 * m + 1) % 4
            gb_ = gab[m % 2]; rg = r_gab[m % 2]
            dma(gb_[:, 0, :], gaT_s[m, :, cs], writes=[rg])
            dma(gb_[:, 1, :], gbT_s[m, :, cs], writes=[rg])
            for k in range(4):
                P.op(PE, lambda e, m=m, k=k, b0=b0: e.matmul(ps[:, b0, :], lhsT=wap[:, k, m * 128:(m + 1) * 128], rhs=OT[:, k, :], start=(k == 0), stop=(k == 3)),
                     reads=[r_w3, r_in3], writes=[rb[b0]])
            for k in range(4):
                P.op(PE, lambda e, m=m, k=k, b1_=b1_: e.matmul(ps[:, b1_, :], lhsT=wcp[:, k, m * 128:(m + 1) * 128], rhs=zc[:, k, :], start=(k == 0), stop=(k == 3)),
                     reads=[r_w3, r_zc], writes=[rb[b1_]])
            P.op(DVE, lambda e, b0=b0, gb_=gb_: e.tensor_tensor(out=t1, in0=ps[:, b0, :], in1=gb_[:, 0, :], op=ALU.mult), reads=[rb[b0], rg], writes=[r_t1])
            P.op(DVE, lambda e, b1_=b1_, gb_=gb_: e.tensor_tensor(out=sq, in0=ps[:, b1_, :], in1=gb_[:, 1, :], op=ALU.mult), reads=[rb[b1_], rg], writes=[r_sq])
            P.op(DVE, lambda e, m=m: e.tensor_tensor(out=mg[:, m, :], in0=t1, in1=sq, op=ALU.add), reads=[r_t1, r_sq], writes=[r_mg])
            yield 2.0
        for m in range(8):
            bk = m % 4
            for k in range(8):
                P.op(PE, lambda e, m=m, k=k, bk=bk: e.matmul(ps[:, bk, :], lhsT=wo[:, k, m * 128:(m + 1) * 128], rhs=mg[:, k, :], start=(k == 0), stop=(k == 7)),
                     reads=[r_w3, r_mg], writes=[rb[bk]])
            P.op(DVE, lambda e, m=m, bk=bk: e.scalar_tensor_tensor(out=xT[:, m, :], in0=ps[:, bk, :], scalar=g_m[:, m:m + 1], in1=xT[:, m, :], op0=ALU.mult, op1=ALU.add),
                 reads=[rb[bk], r_mod2], writes=[r_xT])
            yield 2.0
        rstd_sumsq(D, lambda m: xT[:, m, :], 8, sq, r_sq, 3, stt[:, 2, :], r_stt, [r_xT])
        yield 6.0
        for m in range(8):
            P.op(DVE, lambda e, m=m: e.tensor_tensor(out=t1, in0=xT[:, m, :], in1=stt[:, 2, :], op=ALU.mult), reads=[r_xT, r_stt], writes=[r_t1])
            P.op(DVE, lambda e, m=m: e.tensor_scalar(out=h2[:, m, :], in0=t1, scalar1=a2[:, m:m + 1], scalar2=b2[:, m:m + 1], op0=ALU.mult, op1=ALU.add),
                 reads=[r_mod2], writes=[r_h2, r_t1])
        yield 6.0

    def ffn_gen(g):
        xT = xTs[g % 2]; r_xT = r_xTs[g % 2]
        h2 = h2s[g % 2]; r_h2 = r_h2s[g % 2]
        for i in range(22):
            wb_ = i % 2
            dma(wfi_t[wb_][:, :, 0:128], w_fi_v[:, :, i * 128:(i + 1) * 128], reads=[r_cast["w_fi"]], writes=[r_wfi[wb_]])
            dma(wfi_t[wb_][:, :, 128:256], w_fi_v[:, :, 2816 + i * 128:2816 + (i + 1) * 128], reads=[r_cast["w_fi"]], writes=[r_wfi[wb_]])
            b0 = 4 + (2 * i) % 4; b1_ = 4 + (2 * i + 1) % 4
            for k in range(8):
                P.op(PE, lambda e, k=k, wb_=wb_, b0=b0: e.matmul(ps[:, b0, :], lhsT=wfi_t[wb_][:, k, 0:128], rhs=h2[:, k, :], start=(k == 0), stop=(k == 7)),
                     reads=[r_wfi[wb_], r_h2], writes=[rb[b0]])
            for k in range(8):
                P.op(PE, lambda e, k=k, wb_=wb_, b1_=b1_: e.matmul(ps[:, b1_, :], lhsT=wfi_t[wb_][:, k, 128:256], rhs=h2[:, k, :], start=(k == 0), stop=(k == 7)),
                     reads=[r_wfi[wb_], r_h2], writes=[rb[b1_]])
            si = i % 2
            P.op(ACT, lambda e, si=si, b0=b0: e.activation(out=sl[si], in_=ps[:, b0, :], func=AF.Silu), reads=[rb[b0]], writes=[r_sl[si]])
            P.op(DVE, lambda e, si=si, b1_=b1_, i=i: e.tensor_tensor(out=gT[:, i, :], in0=ps[:, b1_, :], in1=sl[si], op=ALU.mult), reads=[rb[b1_], r_sl[si]], writes=[r_gT])
            yield 3.5
        for m in range(8):
            bk = 4 + m % 4
            wf = wfo_t[m % 2]; rwf = r_wfo[m % 2]
            dma(wf, w_fo_v[:, :, m * 128:(m + 1) * 128], reads=[r_cast["w_fo"]], writes=[rwf])
            for i in range(22):
                P.op(PE, lambda e, i=i, bk=bk, wf=wf: e.matmul(ps[:, bk, :], lhsT=wf[:, i, :], rhs=gT[:, i, :], start=(i == 0), stop=(i == 21)),
                     reads=[rwf, r_gT], writes=[rb[bk]])
            P.op(DVE, lambda e, m=m, bk=bk: e.scalar_tensor_tensor(out=xT[:, m, :], in0=ps[:, bk, :], scalar=g_f[:, m:m + 1], in1=xT[:, m, :], op0=ALU.mult, op1=ALU.add),
                 reads=[rb[bk], r_mod2], writes=[r_xT])
            yield 4.8
        rstd_sumsq(D, lambda m: xT[:, m, :], 8, sq2, r_sq2, 7, rs2, r_rs2, [r_xT])
        yield 6.0
        for m in range(8):
            P.op(DVE, lambda e, m=m: e.scalar_tensor_tensor(out=xT[:, m, :], in0=xT[:, m, :], scalar=gfin[:, m:m + 1], in1=rs2, op0=ALU.mult, op1=ALU.mult),
                 reads=[r_rs2, r_c], writes=[r_xT])
        yield 4.0
        for bl in range(4):
            for m in range(8):
                bk = 4 + (m // 4 + 2 * bl) % 4
                P.op(PE, lambda e, m=m, bl=bl, bk=bk: e.transpose(out=ps[:, bk, (m % 4) * 128:(m % 4 + 1) * 128], in_=xT[:, m, bl * 128:(bl + 1) * 128], identity=identf),
                     reads=[r_xT, r_c], writes=[rb[bk]])
                if m % 4 == 3:
                    evac(yo[:, (m // 4) * 512:(m // 4 + 1) * 512], ps[:, bk, :], [rb[bk]], [r_yo])
            dma(out_d[g * 512 + bl * 128:g * 512 + (bl + 1) * 128, :], yo, reads=[r_yo], eng=STQ)
            yield 2.0

    done3 = {"F": 0, "N": 0}
    mk3 = {"F": front_gen, "N": ffn_gen}
    cur3 = {"F": None, "N": None}
    clk3 = {"F": 0.0, "N": 0.0}
    now3 = [0.0]

    def can_start3(s):
        i = done3[s]
        if i >= 4:
            return False
        if s == "F":
            return done3["N"] >= i - 1
        return done3["F"] > i

    while done3["F"] < 4 or done3["N"] < 4:
        cands = []
        for s in ("N", "F"):
            if cur3[s] is None and can_start3(s):
                cur3[s] = mk3[s](done3[s])
                clk3[s] = max(clk3[s], now3[0])
            if cur3[s] is not None:
                cands.append(s)
        assert cands, ("phase-3 scheduler deadlock", done3)
        s = min(cands, key=lambda k: clk3[k])
        now3[0] = clk3[s]
        cost = next(cur3[s], None)
        if cost is None:
            cur3[s] = None
            done3[s] += 1
        else:
            clk3[s] += cost
    A.release()
    P.emit()
    return nc


_NC_CACHE = {}


def _consts():
    ident = np.eye(128, dtype=np.float32)
    slopes = np.exp2(-np.arange(1, 9, dtype=np.float64))
    s = np.arange(S)
    kaug = np.zeros((3, 8, S), np.float32)
    for h in range(8):
        kaug[0, h] = 8.0 * slopes[h] * (s % 128)
        kaug[1, h] = 8.0 * slopes[h] * 128.0 * (s // 128)
        kaug[2, h] = -8.0 * 128.0 * slopes[h]
    cb = np.zeros((128, 4, 512), np.float32)
    p = np.arange(128)[:, None]
    sp = np.arange(512)[None, :]
    for j4 in range(4):
        cb[:, j4, :] = np.where(sp <= 128 * j4 + p, 0.0, NEG)
    kcp1 = np.tile(np.arange(1, 65, dtype=np.float32)[None, :], (128, 1))
    return ident, kaug, cb, kcp1


def _in_maps(inputs):
    x = np.asarray(inputs["x"], np.float32)
    c = np.asarray(inputs["c"], np.float32)
    bf = ml_dtypes.bfloat16
    ident, kaug, cb, kcp1 = _consts()

    def fm(v, n):
        return np.ascontiguousarray(np.asarray(v, np.float32).reshape(n, 128).T)

    shared = {
        "w_ada": np.ascontiguousarray(inputs["w_ada"][0], np.float32),
        "b_adaT": fm(inputs["b_ada"][0], 48),
        "gmixT": fm(inputs["norm_mix_g"][0], 8), "gffnT": fm(inputs["norm_ffn_g"][0], 8), "gfinT": fm(inputs["norm_final_g"], 8),
        "w_in": np.ascontiguousarray(inputs["w_in"][0], np.float32),
        "w_dwT": np.ascontiguousarray(np.asarray(inputs["w_dw"][0][:, 0, :], np.float32).T.reshape(4, 128, 31).transpose(1, 0, 2)),
        "b_dwT": fm(inputs["b_dw"][0], 4), "lngT": fm(inputs["conv_ln_g"][0], 4), "lnbT": fm(inputs["conv_ln_b"][0], 4),
        "w_ap": np.ascontiguousarray(inputs["w_attn_proj"][0], np.float32), "w_cp": np.ascontiguousarray(inputs["w_conv_proj"][0], np.float32),
        "w_out": np.ascontiguousarray(inputs["w_out"][0], np.float32),
        "w_fi": np.ascontiguousarray(inputs["w_ffn_in"][0], np.float32), "w_fo": np.ascontiguousarray(inputs["w_ffn_out"][0], np.float32),
        "ident": ident.astype(bf), "identf": ident, "kaug": kaug.astype(bf), "cb": cb.astype(bf), "kcp1": kcp1,
    }
    maps = []
    for core in range(8):
        b, r = core // 4, core % 4
        pad = 1536 - 512 * r
        xk = np.zeros((S, D), np.float32)
        xk[pad:] = x[b, :S - pad]
        kbias = np.zeros((1, S), np.float32)
        kbias[0, :pad] = NEG
        hv = np.zeros((128, 128), np.float32)
        for g in range(4):
            pos = 2048 * g + 1504 + np.arange(32)
            hv[:, g * 32:(g + 1) * 32] = (pos >= pad).astype(np.float32)[None, :]
        m = dict(shared)
        m.update({"xk": xk, "cT": fm(c[b], 8), "kbias": kbias.astype(bf), "hvalid": hv})
        maps.append(m)
    return maps


def kernel(**inputs):
    if "nc" not in _NC_CACHE:
        _NC_CACHE["nc"] = build(False)
    nc = _NC_CACHE["nc"]
    maps = _in_maps(inputs)
    res = run_bass_kernel_spmd(nc, maps, core_ids=list(range(8)))
    out = np.zeros((2, S, D), np.float32)
    for core in range(8):
        b, r = core // 4, core % 4
        o = np.asarray(res.results[core]["out"], np.float32)
        for g in range(4):
            p0 = 2048 * g + 512 * r
            out[b, p0:p0 + 512] = o[g * 512:(g + 1) * 512]
    return out
```

```python
import contextlib
import numpy as np
import ml_dtypes
import concourse.bass as bass
import concourse.mybir as mybir
from concourse.bass_utils import run_bass_kernel_spmd

F32 = mybir.dt.float32
BF16 = mybir.dt.bfloat16
FP8 = mybir.dt.float8e4
AF = mybir.ActivationFunctionType
ALU = mybir.AluOpType
AX = mybir.AxisListType

PE, ACT, DVE, POOL, SP = "tensor", "scalar", "vector", "gpsimd", "sync"
ENGS = [PE, ACT, DVE, POOL, SP]
NDMASEM = 32
STQ = "gpsimd"
NPOOLSEM = 8

S = 8192
D = 1024
NT = 16
EPS = 1e-6
NEG = -30000.0
NITER = 14
EXPB = -16.0
MASKB = 240000.0
ACT_SHARE = 0.0


class Res:
    __slots__ = ("w", "rs")

    def __init__(self):
        self.w = None
        self.rs = []


class Op:
    __slots__ = ("eng", "fn", "deps", "signal", "dma", "dsem", "dval", "prev_dma")

    def __init__(self, eng, fn, dma):
        self.eng = eng; self.fn = fn; self.deps = []
        self.signal = False; self.dma = dma; self.dsem = None; self.dval = 0; self.prev_dma = None


class Prog:
    def __init__(self, nc):
        self.nc = nc
        self.ops = {e: [] for e in ENGS}
        self.ndma = 0
        self.npool = 0
        self.dma_last = [None] * (NDMASEM + NPOOLSEM)
        self.bar = {e: [] for e in ENGS}

    def barrier(self):
        lasts = []
        for e in ENGS:
            for o in reversed(self.ops[e]):
                if not o.dma:
                    lasts.append(o)
                    break
        lasts += [p for p in self.dma_last if p is not None]
        for e in ENGS:
            self.bar[e] = list(lasts)

    def op(self, eng, fn, reads=(), writes=(), dma=False):
        o = Op(eng, fn, dma)
        deps = list(self.bar[eng])
        self.bar[eng] = []
        for r in reads:
            if r.w is not None:
                deps.append(r.w)
        for w in writes:
            if w.w is not None:
                deps.append(w.w)
            deps.extend(w.rs)
        seen = set()
        for d in deps:
            if d is o or id(d) in seen:
                continue
            seen.add(id(d))
            if d.eng == eng and not d.dma and (eng == PE or eng == SP):
                continue
            o.deps.append(d)
            if not d.dma:
                d.signal = True
        if dma:
            if eng == POOL:
                slot = NDMASEM + (self.npool % NPOOLSEM)
                o.dval = 16 * (self.npool // NPOOLSEM + 1)
                self.npool += 1
            else:
                slot = self.ndma % NDMASEM
                o.dval = 16 * (self.ndma // NDMASEM + 1)
                self.ndma += 1
            o.dsem = slot
            o.prev_dma = self.dma_last[slot]
            self.dma_last[slot] = o
        self.ops[eng].append(o)
        for r in reads:
            r.rs.append(o)
        for w in writes:
            w.w = o
            w.rs = []
        return o

    def emit(self):
        nc = self.nc
        sigval = {}
        for e in ENGS:
            c = 0
            for o in self.ops[e]:
                if o.signal and not o.dma:
                    c += 1
                    sigval[id(o)] = c
        with contextlib.ExitStack() as st:
            esem = {e: st.enter_context(nc.semaphore("s_" + e)) for e in ENGS}
            dsem = [st.enter_context(nc.semaphore("d%d" % i)) for i in range(NDMASEM + NPOOLSEM)]
            block = st.enter_context(nc.Block())
            prog = self

            def run(engname, eng):
                waited = {}

                def wait(key, sem, val):
                    if waited.get(key, 0) >= val:
                        return
                    eng.wait_ge(sem, val)
                    waited[key] = val

                for o in prog.ops[engname]:
                    need = {}
                    for d in o.deps:
                        if d.dma:
                            k_, s_, v_ = ("d", d.dsem), dsem[d.dsem], d.dval
                        else:
                            k_, s_, v_ = ("e", d.eng), esem[d.eng], sigval[id(d)]
                        if k_ not in need or need[k_][1] < v_:
                            need[k_] = (s_, v_)
                    if o.dma and o.prev_dma is not None:
                        p = o.prev_dma
                        k_ = ("d", p.dsem)
                        if k_ not in need or need[k_][1] < p.dval:
                            need[k_] = (dsem[p.dsem], p.dval)
                    for k_, (s_, v_) in need.items():
                        wait(k_, s_, v_)
                    ins = o.fn(eng)
                    if o.dma:
                        ins.then_inc(dsem[o.dsem], 16)
                    elif o.signal:
                        ins.then_inc(esem[engname], 1)
                if engname == SP:
                    for p in prog.dma_last:
                        if p is not None:
                            wait(("d", p.dsem), dsem[p.dsem], p.dval)

            @block.tensor
            def _(eng):
                run(PE, eng)

            @block.scalar
            def _(eng):
                run(ACT, eng)

            @block.vector
            def _(eng):
                run(DVE, eng)

            @block.gpsimd
            def _(eng):
                run(POOL, eng)

            @block.sync
            def _(eng):
                run(SP, eng)


class Arena:
    def __init__(self, nc, kb):
        self.words = kb * 256
        self.t = nc.alloc_sbuf_tensor("arena", [128, self.words], F32)
        self.off = 0
        self.marks = []

    def mark(self):
        self.marks.append(self.off)

    def release(self):
        self.off = self.marks.pop()

    def tile(self, shape, dt):
        n = 1
        for s in shape:
            n *= s
        esz = 4 if dt == F32 else (1 if dt == FP8 else 2)
        words = (n * esz + 3) // 4
        words = (words + 7) // 8 * 8
        assert self.off + words <= self.words, ("arena overflow", self.off, words, self.words)
        ap = self.t[:, self.off:self.off + words]
        self.off += words
        if dt != F32:
            ap = ap.bitcast(dt)
        ap = ap[:, 0:n]
        if len(shape) == 2:
            ap = ap.rearrange("p (a b) -> p a b", b=shape[1])
        elif len(shape) == 3:
            ap = ap.rearrange("p (a b c) -> p a b c", b=shape[1], c=shape[2])
        return ap


def build(debug=False):
    nc = bass.Bass("TRN2", target_bir_lowering=False)

    def din(name, shape, dt=F32):
        return nc.dram_tensor(name, shape, dt, kind="ExternalInput").ap()

    def dscr(name, shape, dt=BF16):
        return nc.dram_tensor(name, shape, dt, kind="ExternalOutput" if debug else "Internal").ap()

    xk = din("xk", [S, D])
    cT = din("cT", [128, 8])
    w_ada = din("w_ada", [D, 6144])
    b_adaT = din("b_adaT", [128, 48])
    gmixT = din("gmixT", [128, 8]); gffnT = din("gffnT", [128, 8]); gfinT = din("gfinT", [128, 8])
    w_in = din("w_in", [D, 5192])
    w_dwT = din("w_dwT", [128, 4, 31])
    b_dwT = din("b_dwT", [128, 4]); lngT = din("lngT", [128, 4]); lnbT = din("lnbT", [128, 4])
    w_ap = din("w_ap", [512, D]); w_cp = din("w_cp", [512, D]); w_out = din("w_out", [D, D])
    w_fi = din("w_fi", [D, 5632]); w_fo = din("w_fo", [2816, D])
    ident_d = din("ident", [128, 128], BF16)
    identf_d = din("identf", [128, 128])
    kaug_d = din("kaug", [3, 8, S], BF16)
    kbias_d = din("kbias", [1, S], BF16)
    cb_d = din("cb", [128, 4, 512], BF16)
    hvalid_d = din("hvalid", [128, 128])
    kcp1_d = din("kcp1", [128, 64])
    out_d = nc.dram_tensor("out", [2048, D], F32, kind="ExternalOutput").ap()

    w_in_b = dscr("w_in_b", [D, 5192]); w_ap_b = dscr("w_ap_b", [512, D]); w_cp_b = dscr("w_cp_b", [512, D])
    w_out_b = dscr("w_out_b", [D, D]); w_fi_b = dscr("w_fi_b", [D, 5632]); w_fo_b = dscr("w_fo_b", [2816, D])
    kT_s = dscr("kT_s", [8, 64, S]); v_s = dscr("v_s", [S, 520])
    qT_s = dscr("qT_s", [8, 64, 2048]); qiT_s = dscr("qiT_s", [4, 128, 2048])
    zT_s = dscr("zT_s", [4, 128, 4, 544]); gaT_s = dscr("gaT_s", [8, 128, 2048]); gbT_s = dscr("gbT_s", [8, 128, 2048])
    OT_s = dscr("OT_s", [4, 128, 2048])
    if debug:
        dbg_sc = nc.dram_tensor("dbg_sc", [16, 128, S], F32, kind="ExternalOutput").ap()
        dbg_thr = nc.dram_tensor("dbg_thr", [128, 16 * 4], F32, kind="ExternalOutput").ap()

    P = Prog(nc)
    A = Arena(nc, 204)
    ps_cm = nc.psum_tensor("ps", [128, 8, 512], F32)
    ps = ps_cm.__enter__()
    psb = [ps[:, b, :].bitcast(BF16) for b in range(8)]
    rb = [Res() for _ in range(8)]

    def dma(out, in_, reads=(), writes=(), eng=SP, **kw):
        return P.op(eng, lambda e: e.dma_start(out=out, in_=in_, **kw), reads=reads, writes=writes, dma=True)

    ident = A.tile([128], BF16); r_c = Res()
    identf = A.tile([128], F32)
    onesf = A.tile([128], F32)
    modT = A.tile([48], F32); r_mod = Res()
    ab = A.tile([4, 8], F32)
    gfin = A.tile([8], F32)
    wiall = A.tile([16, 8], F32); r_wi = Res()
    hval = A.tile([128], F32)
    kcp1 = A.tile([64], F32)
    dma(ident, ident_d, writes=[r_c]); dma(identf, identf_d, writes=[r_c]); dma(hval, hvalid_d, writes=[r_c])
    dma(kcp1, kcp1_d, writes=[r_c]); dma(gfin, gfinT, writes=[r_c])
    P.op(DVE, lambda e: e.memset(onesf, 1.0), writes=[r_c])

    r_cast = {}
    prev_cast = None
    for name, src, dst in [("w_in", w_in, w_in_b), ("w_ap", w_ap, w_ap_b), ("w_cp", w_cp, w_cp_b), ("w_out", w_out, w_out_b),
                           ("w_fi", w_fi, w_fi_b), ("w_fo", w_fo, w_fo_b)]:
        r_cast[name] = Res()
        dma(dst, src, reads=([prev_cast] if prev_cast is not None else []), writes=[r_cast[name]], eng=POOL, max_dma_last_dim=4096)
        prev_cast = r_cast[name]

    A.mark()
    kiT = A.tile([S], BF16); r_ki = Res()
    A.mark()
    cts = A.tile([8], F32); cact = A.tile([8], F32); r_ca = Res()
    bada = A.tile([48], F32); gm = A.tile([8], F32); gf = A.tile([8], F32)
    dma(cts, cT, writes=[r_ca]); dma(bada, b_adaT, writes=[r_ca]); dma(gm, gmixT, writes=[r_ca]); dma(gf, gffnT, writes=[r_ca])
    P.op(ACT, lambda e: e.activation(out=cact, in_=cts, func=AF.Silu), reads=[r_ca], writes=[r_ca])
    wa = [A.tile([8, 768], F32) for _ in range(2)]; r_wa = [Res(), Res()]
    w_ada_v = w_ada.rearrange("(k p) n -> p k n", p=128)
    r_mod2 = Res()

    def ada_piece(j, bank=0, r_dst=None):
        b = j % 2
        dma(wa[b], w_ada_v[:, :, j * 768:(j + 1) * 768], writes=[r_wa[b]])
        for m in range(6):
            col = j * 6 + m
            for kc in range(8):
                P.op(PE, lambda e, b=b, m=m, kc=kc, col=col: e.matmul(ps[:, bank, col:col + 1], lhsT=wa[b][:, kc, m * 128:(m + 1) * 128],
                                                                  rhs=cact[:, kc:kc + 1], start=(kc == 0), stop=(kc == 7)),
                     reads=[r_wa[b], r_ca], writes=[rb[bank]])
        if r_dst is not None:
            c0_, c1_ = j * 6, j * 6 + 6
            P.op(DVE, lambda e: e.tensor_tensor(out=modT[:, c0_:c1_], in0=ps[:, bank, c0_:c1_], in1=bada[:, c0_:c1_], op=ALU.add), reads=[rb[bank], r_ca], writes=[r_dst])

    for j in range(3):
        ada_piece(j)
    P.op(DVE, lambda e: e.tensor_tensor(out=modT[:, 0:18], in0=ps[:, 0, 0:18], in1=bada[:, 0:18], op=ALU.add), reads=[rb[0], r_ca], writes=[r_mod])
    P.op(DVE, lambda e: e.tensor_scalar(out=ab[:, 1, :], in0=modT[:, 8:16], scalar1=1.0, scalar2=None, op0=ALU.add), writes=[r_mod])
    P.op(DVE, lambda e: e.tensor_tensor(out=ab[:, 0, :], in0=ab[:, 1, :], in1=gm, op=ALU.mult), reads=[r_ca], writes=[r_mod])

    def ada_finish():
        P.op(DVE, lambda e: e.tensor_scalar(out=ab[:, 3, :], in0=modT[:, 32:40], scalar1=1.0, scalar2=None, op0=ALU.add), writes=[r_mod2])
        P.op(DVE, lambda e: e.tensor_tensor(out=ab[:, 2, :], in0=ab[:, 3, :], in1=gf, op=ALU.mult), reads=[r_ca], writes=[r_mod2])
    a1 = ab[:, 0, :]; b1 = modT[:, 0:8]; a2 = ab[:, 2, :]; b2 = modT[:, 24:32]; g_m = modT[:, 16:24]; g_f = modT[:, 40:48]

    def prep_front(xrows, nblk, np_, xt, r_xt, xn, r_xn, st, r_st):
        if xrows is not None:
            dma(xt[0:np_, 0:nblk, :], xrows.rearrange("(b p) d -> p b d", p=np_), writes=[r_xt])
        for bl in range(nblk):
            P.op(ACT, lambda e, bl=bl: e.activation(out=xn[0:np_, bl, :], in_=xt[0:np_, bl, :], func=AF.Square, accum_out=st[0:np_, bl:bl + 1]),
                 reads=[r_xt], writes=[r_xn, r_st])
        P.op(ACT, lambda e: e.activation(out=st[0:np_, 4:4 + nblk], in_=st[0:np_, 0:nblk], func=AF.Sqrt, bias=EPS, scale=1.0 / D), writes=[r_st])
        P.op(DVE, lambda e: e.reciprocal(out=st[0:np_, 8:8 + nblk], in_=st[0:np_, 4:4 + nblk]), reads=[r_st], writes=[r_st])
        for bl in range(nblk):
            P.op(DVE, lambda e, bl=bl: e.tensor_scalar(out=xn[0:np_, bl, :], in0=xt[0:np_, bl, :], scalar1=st[0:np_, 8 + bl:9 + bl], scalar2=None, op0=ALU.mult),
                 reads=[r_xt, r_st], writes=[r_xn])

    def prep_back(nblk, np_, xn, r_xn, hT, r_hTk, banks, avec, bvec):
        ntok = nblk * np_
        for kc in range(8):
            bk = banks[kc // 2]
            for bl in range(nblk):
                o = (kc % 2) * 512 + bl * np_
                P.op(PE, lambda e, kc=kc, bl=bl, bk=bk, o=o: e.transpose(out=psb[bk][:, o:o + np_], in_=xn[0:np_, bl, kc * 128:(kc + 1) * 128],
                                                                     identity=ident[0:np_, 0:np_]),
                     reads=[r_xn, r_c], writes=[rb[bk]])
            if kc % 2 == 1:
                for k2 in (kc - 1, kc):
                    o = (k2 % 2) * 512
                    eng_ = DVE
                    if eng_ == DVE:
                        P.op(DVE, lambda e, k2=k2, bk=bk, o=o: e.tensor_scalar(out=hT[:, k2, 0:ntok], in0=psb[bk][:, o:o + ntok], scalar1=avec[:, k2:k2 + 1],
                                                                           scalar2=bvec[:, k2:k2 + 1], op0=ALU.mult, op1=ALU.add),
                             reads=[rb[bk], r_mod], writes=[r_hTk[k2]])
                    else:
                        P.op(ACT, lambda e, k2=k2, bk=bk, o=o: e.activation(out=hT[:, k2, 0:ntok], in_=psb[bk][:, o:o + ntok], func=AF.Identity, scale=avec[:, k2:k2 + 1],
                                                                        bias=bvec[:, k2:k2 + 1]),
                             reads=[rb[bk], r_mod], writes=[r_hTk[k2]])

    def prep(xrows, nblk, np_, xt, r_xt, xn, r_xn, hT, r_hT, st, r_st, banks, avec, bvec):
        prep_front(xrows, nblk, np_, xt, r_xt, xn, r_xn, st, r_st)
        prep_back(nblk, np_, xn, r_xn, hT, [r_hT] * 8, banks, avec, bvec)

    def load_w(dst, src_b, rows, c0, c1, r_w, cname="w_in"):
        dma(dst, src_b.rearrange("(k p) n -> p k n", p=128)[:, :, c0:c1], reads=[r_cast[cname]], writes=[r_w])

    evac_rr = [0]

    def evac(out, in_, reads, writes, func=None, scale=1.0):
        evac_rr[0] += 1
        if func is not None or evac_rr[0] % 2 == 0:
            f = func if func is not None else AF.Copy
            return P.op(ACT, lambda e: e.activation(out=out, in_=in_, func=f, scale=scale), reads=reads, writes=writes)
        if scale != 1.0:
            return P.op(DVE, lambda e: e.tensor_scalar(out=out, in0=in_, scalar1=scale, scalar2=None, op0=ALU.mult), reads=reads, writes=writes)
        return P.op(DVE, lambda e: e.tensor_copy(out=out, in_=in_), reads=reads, writes=writes)

    A.mark()
    wk = A.tile([8, 512], BF16); wv = A.tile([8, 512], BF16); wki = A.tile([8, 128], BF16); r_w1 = Res()
    wst0 = A.tile([8, 512], F32); wst = [wst0, wst0]; r_wst0 = Res(); r_wst = [r_wst0, r_wst0]
    w_in_v = w_in.rearrange("(k p) n -> p k n", p=128)
    dma(wst0, w_in_v[:, :, 512:1024], writes=[r_wst0])
    P.op(ACT, lambda e: e.activation(out=wk, in_=wst0, func=AF.Copy), reads=[r_wst0], writes=[r_w1])
    dma(wst0, w_in_v[:, :, 1024:1536], writes=[r_wst0])
    P.op(DVE, lambda e: e.tensor_copy(out=wv, in_=wst0), reads=[r_wst0], writes=[r_w1])
    dma(wst0[:, :, 0:64], w_in_v[:, :, 2048:2112], writes=[r_wst0])
    P.op(ACT, lambda e: e.activation(out=wki[:, :, 0:64], in_=wst0[:, :, 0:64], func=AF.Copy), reads=[r_wst0], writes=[r_w1])
    P.op(ACT, lambda e: e.activation(out=wki[:, :, 64:128], in_=wst0[:, :, 0:64], func=AF.Copy), reads=[r_wst0], writes=[r_w1])
    xts = [A.tile([4, D], F32) for _ in range(3)]; r_xts = [Res(), Res(), Res()]
    xns = [A.tile([4, D], BF16) for _ in range(2)]; r_xns = [Res(), Res()]
    hTs = [A.tile([8, 512], BF16) for _ in range(2)]; r_hTs = [[Res() for _ in range(8)] for _ in range(2)]
    sts = [A.tile([12], F32) for _ in range(2)]; r_sts = [Res(), Res()]
    ksts = [A.tile([4, 512], BF16) for _ in range(2)]; r_ksts = [Res(), Res()]
    vsts = [A.tile([4, 8, 65], BF16) for _ in range(2)]; r_vsts = [Res(), Res()]
    for b in range(2):
        P.op(DVE, lambda e, b=b: e.memset(vsts[b], 1.0), writes=[r_vsts[b]])
    kT_v = kT_s.rearrange("(m hh) d s -> (hh d) m s", hh=2)
    v_v = v_s.rearrange("(n p) c -> p n c", p=128)

    def load1a(T):
        dma(xts[T % 3][:, :, :], xk[T * 512:(T + 1) * 512, :].rearrange("(b p) d -> p b d", p=128), writes=[r_xts[T % 3]])

    def front1a(T):
        b = T % 2
        prep_front(None, 4, 128, xts[T % 3], r_xts[T % 3], xns[b], r_xns[b], sts[b], r_sts[b])

    def back1a(T):
        b = T % 2
        prep_back(4, 128, xns[b], r_xns[b], hTs[b], r_hTs[b], [0, 1, 2, 3], a1, b1)

    load1a(0)
    load1a(1)
    front1a(0)
    back1a(0)
    for T in range(NT):
        b = T % 2
        hT = hTs[b]
        if T + 2 < NT:
            load1a(T + 2)
        if T + 1 < NT:
            front1a(T + 1)
        for m in range(4):
            bk = 4 + (m % 2)
            for kc in range(8):
                P.op(PE, lambda e, m=m, kc=kc, bk=bk, hT=hT: e.matmul(ps[:, bk, :], lhsT=wk[:, kc, m * 128:(m + 1) * 128], rhs=hT[:, kc, :],
                                                                  start=(kc == 0), stop=(kc == 7)), reads=[r_w1, r_hTs[b][kc]], writes=[rb[bk]])
            evac(ksts[b][:, m, :], ps[:, bk, :], [rb[bk]], [r_ksts[b]])
        dma(kT_v[:, :, T * 512:(T + 1) * 512], ksts[b], reads=[r_ksts[b]], eng=STQ)
        if T + 1 < NT:
            back1a(T + 1)
        for bl in range(4):
            bk = 6 + (bl % 2)
            for kc in range(8):
                P.op(PE, lambda e, bl=bl, kc=kc, bk=bk, hT=hT: e.matmul(ps[:, bk, :], lhsT=hT[:, kc, bl * 128:(bl + 1) * 128], rhs=wv[:, kc, :],
                                                                    start=(kc == 0), stop=(kc == 7)), reads=[r_w1, r_hTs[b][kc]], writes=[rb[bk]])
            evac(vsts[b][:, bl, :, 0:64], ps[:, bk, :].rearrange("p (h d) -> p h d", d=64), [rb[bk]], [r_vsts[b]])
        dma(v_v[:, T * 4:(T + 1) * 4, :], vsts[b].rearrange("p n h c -> p n (h c)"), reads=[r_vsts[b]], eng=STQ)
        for kc in range(8):
            P.op(PE, lambda e, kc=kc, hT=hT: e.matmul(ps[:, 4, :], lhsT=wki[:, kc, :], rhs=hT[:, kc, :], start=(kc == 0), stop=(kc == 7)),
                 reads=[r_w1, r_hTs[b][kc]], writes=[rb[4]])
        evac(kiT[:, T * 512:(T + 1) * 512], ps[:, 4, :], [rb[4]], [r_ki])
        if T < 5:
            ada_piece(3 + T, bank=4, r_dst=r_mod2)
        if T == 5:
            ada_finish()
    P.barrier()
    A.release()
    A.release()

    A.mark()
    wq = A.tile([8, 512], BF16); wqi = A.tile([8, 512], BF16); wwi = A.tile([8, 8], BF16)
    wu = A.tile([8, 1024], BF16); wga = A.tile([8, 1024], BF16); wgb = A.tile([8, 1024], BF16); r_w2 = Res()
    load_w(wq, w_in_b, D, 0, 512, r_w2); load_w(wqi, w_in_b, D, 1536, 2048, r_w2); load_w(wwi, w_in_b, D, 2112, 2120, r_w2)
    load_w(wu, w_in_b, D, 2120, 3144, r_w2); load_w(wga, w_in_b, D, 3144, 4168, r_w2); load_w(wgb, w_in_b, D, 4168, 5192, r_w2)
    xts1 = [A.tile([4, D], F32) for _ in range(2)]; r_xts1 = [Res(), Res()]
    xns1 = [A.tile([4, D], BF16) for _ in range(2)]; r_xns1 = [Res(), Res()]
    hTs1 = [A.tile([8, 512], BF16) for _ in range(2)]; r_hTs1 = [[Res() for _ in range(8)] for _ in range(2)]
    sts1 = [A.tile([12], F32) for _ in range(2)]; r_sts1 = [Res(), Res()]
    xth = A.tile([1, D], F32); r_xth = Res(); xnh = A.tile([1, D], BF16); r_xnh = Res()
    hTh = A.tile([8, 32], BF16); r_hTh = Res(); sth = A.tile([12], F32); r_sth = Res()
    stg = [A.tile([512], BF16) for _ in range(3)]; r_stg = [Res() for _ in range(3)]
    sgm = [A.tile([512], F32) for _ in range(2)]; r_sgm = [Res(), Res()]
    stg_i = [0]
    qT_v = qT_s.rearrange("(m hh) d s -> (hh d) m s", hh=2)

    def stage_out(dst, src_ps, bk, func=None):
        i = stg_i[0] % 3
        stg_i[0] += 1
        evac(stg[i], src_ps, [rb[bk]], [r_stg[i]], func=func)
        dma(dst, stg[i], reads=[r_stg[i]], eng=STQ)

    def proj_fm(w, m, hT_, r_h, bk, n):
        for kc in range(8):
            P.op(PE, lambda e, kc=kc: e.matmul(ps[:, bk, 0:n], lhsT=w[:, kc, m * 128:(m + 1) * 128], rhs=hT_[:, kc, 0:n], start=(kc == 0), stop=(kc == 7)),
                 reads=[r_w2, r_h[kc]], writes=[rb[bk]])

    def front1b(g):
        T = 4 * g + 3
        b = g % 2
        prep_front(xk[T * 512:(T + 1) * 512, :], 4, 128, xts1[b], r_xts1[b], xns1[b], r_xns1[b], sts1[b], r_sts1[b])

    def back1b(g):
        b = g % 2
        prep_back(4, 128, xns1[b], r_xns1[b], hTs1[b], r_hTs1[b], [0, 1, 2, 3], a1, b1)

    front1b(0)
    back1b(0)

    for g in range(4):
        T = 4 * g + 3
        hT = hTs1[g % 2]; r_hT = r_hTs1[g % 2]
        if g + 1 < 4:
            front1b(g + 1)
        cs = slice(g * 512, (g + 1) * 512)
        bkc = [0]

        def nb():
            bkc[0] += 1
            return 4 + bkc[0] % 4
        for m in range(4):
            bk = nb(); proj_fm(wq, m, hT, r_hT, bk, 512); stage_out(qT_v[:, m, cs], ps[:, bk, :], bk)
        for m in range(4):
            bk = nb(); proj_fm(wqi, m, hT, r_hT, bk, 512); stage_out(qiT_s[m, :, cs], ps[:, bk, :], bk)
        if g + 1 < 4:
            back1b(g + 1)
        for m in range(8):
            bk = nb(); proj_fm(wga, m, hT, r_hT, bk, 512); stage_out(gaT_s[m, :, cs], ps[:, bk, :], bk, func=AF.Sigmoid)
        for m in range(8):
            bk = nb(); proj_fm(wgb, m, hT, r_hT, bk, 512); stage_out(gbT_s[m, :, cs], ps[:, bk, :], bk, func=AF.Sigmoid)
        for bl in range(4):
            bk = nb()
            for kc in range(8):
                P.op(PE, lambda e, kc=kc, bl=bl, bk=bk, hT=hT: e.matmul(ps[:, bk, 0:8], lhsT=hT[:, kc, bl * 128:(bl + 1) * 128], rhs=wwi[:, kc, :], start=(kc == 0), stop=(kc == 7)),
                     reads=[r_w2, r_hT[kc]], writes=[rb[bk]])
            P.op(DVE, lambda e, bl=bl, bk=bk, g=g: e.tensor_scalar(out=wiall[:, g * 4 + bl, :], in0=ps[:, bk, 0:8], scalar1=float(8 ** -0.5 * 64 ** -0.5), scalar2=None, op0=ALU.mult),
                 reads=[rb[bk]], writes=[r_wi])
        for i in range(4):
            bka = nb(); proj_fm(wu, i, hT, r_hT, bka, 512)
            bkg = nb(); proj_fm(wu, 4 + i, hT, r_hT, bkg, 512)
            si = i % 2
            P.op(ACT, lambda e, si=si, bkg=bkg: e.activation(out=sgm[si], in_=ps[:, bkg, :], func=AF.Sigmoid), reads=[rb[bkg]], writes=[r_sgm[si]])
            j = stg_i[0] % 3; stg_i[0] += 1
            P.op(DVE, lambda e, si=si, bka=bka, j=j: e.tensor_tensor(out=stg[j], in0=ps[:, bka, :], in1=sgm[si], op=ALU.mult), reads=[rb[bka], r_sgm[si]], writes=[r_stg[j]])
            dma(zT_s[i, :, g, 32:544], stg[j], reads=[r_stg[j]], eng=STQ)
        prep(xk[T * 512 - 32:T * 512, :], 1, 32, xth, r_xth, xnh, r_xnh, hTh, r_hTh, sth, r_sth, [0, 1, 2, 3], a1, b1)
        for i in range(4):
            bka = nb(); proj_fm(wu, i, hTh, [r_hTh] * 8, bka, 32)
            bkg = nb(); proj_fm(wu, 4 + i, hTh, [r_hTh] * 8, bkg, 32)
            si = i % 2
            P.op(ACT, lambda e, si=si, bkg=bkg: e.activation(out=sgm[si][:, 0:32], in_=ps[:, bkg, 0:32], func=AF.Sigmoid), reads=[rb[bkg]], writes=[r_sgm[si]])
            j = stg_i[0] % 3; stg_i[0] += 1
            P.op(DVE, lambda e, si=si, bka=bka, j=j: e.tensor_tensor(out=sgm[si][:, 32:64], in0=ps[:, bka, 0:32], in1=sgm[si][:, 0:32], op=ALU.mult), reads=[rb[bka]], writes=[r_sgm[si]])
            P.op(DVE, lambda e, si=si, j=j, g=g: e.tensor_tensor(out=stg[j][:, 0:32], in0=sgm[si][:, 32:64], in1=hval[:, g * 32:(g + 1) * 32], op=ALU.mult), reads=[r_c], writes=[r_stg[j], r_sgm[si]])
            dma(zT_s[i, :, g, 0:32], stg[j][:, 0:32], reads=[r_stg[j]], eng=STQ)
    P.barrier()
    A.release()

    A.mark()
    cbt = A.tile([4, 512], BF16); kbt = A.tile([1536], BF16); r_c2 = Res()
    dma(cbt, cb_d, writes=[r_c2]); dma(kbt[0:1, :], kbias_d[:, 0:1536], writes=[r_c2])
    onesb = A.tile([128], BF16)
    nidm = A.tile([128], BF16)
    P.op(DVE, lambda e: e.memset(onesb, 1.0), writes=[r_c2])
    P.op(DVE, lambda e: e.tensor_scalar(out=nidm, in0=ident, scalar1=-MASKB, scalar2=None, op0=ALU.mult), reads=[r_c], writes=[r_c2])
    scoress = [A.tile([S], F32) for _ in range(2)]; r_scs = [Res(), Res()]
    maskq = A.tile([S], BF16); r_mq = Res()
    junk = maskq; r_junk = r_mq
    maskTs = [A.tile([64, 256], FP8) for _ in range(2)]; r_mTs = [Res(), Res()]
    qit = A.tile([4, 256], BF16); r_qit = Res()
    qaugs = [A.tile([8, 256], BF16) for _ in range(2)]; r_qas = [Res(), Res()]
    wdgs = [A.tile([8, 128], BF16) for _ in range(2)]; r_wdgs = [Res(), Res()]
    Rt = [A.tile([2, 512], BF16) for _ in range(3)]; r_Rt = [Res() for _ in range(3)]
    bs = A.tile([16], F32); r_bs = Res()
    bsa = A.tile([4], F32); r_bsa = Res(); r_junkA = Res()
    cntk = A.tile([64], F32); r_ck = Res()
    U = A.tile([68], F32); r_U = Res()
    kt = [A.tile([8, 512], BF16) for _ in range(2)]; r_kt = [Res(), Res()]
    vt = [A.tile([4, 520], BF16) for _ in range(2)]; r_vt = [Res(), Res()]
    Pt = [A.tile([2, 256], BF16) for _ in range(3)]; r_Pt = [Res() for _ in range(3)]
    den = A.tile([16], F32); r_den = Res()
    Ob = A.tile([2, 512], BF16); r_Ob = Res()
    OTst = A.tile([4, 256], BF16); r_OT = Res()
    P.op(DVE, lambda e: e.memset(U, 0.0), writes=[r_U])
    P.op(DVE, lambda e: e.memset(U[:, 64:66], 1.0), writes=[r_U])
    for i in range(2):
        P.op(DVE, lambda e, i=i: e.memset(qaugs[i], 0.0), writes=[r_qas[i]])
    kT_hv = kT_s.rearrange("h d s -> d h s")
    kaug_v = kaug_d
    v_v2 = v_s.rearrange("(n p) c -> p n c", p=128)
    LB = 2
    SB = 7

    PORDER = [0, 2, 3, 4, 5, 6, 7, 1]

    def idx_gen(sq_):
        sp_, q2 = sq_ // 2, sq_ % 2
        qp = PORDER[sp_]
        qb = 2 * qp + q2
        g = qp // 2
        E = 2048 * (g + 1)
        nch = E // 512
        c0 = g * 512 + (qp % 2) * 256
        j4 = qb % 4
        scores = scoress[sq_ % 2]; r_sc = r_scs[sq_ % 2]
        wdg = wdgs[sq_ % 2]; r_wdg = r_wdgs[sq_ % 2]
        if q2 == 0:
            dma(qit, qiT_s.rearrange("m p s -> p m s")[:, :, c0:c0 + 256], writes=[r_qit])
        for j in range(8):
            P.op(DVE, lambda e, j=j: e.tensor_scalar(out=wdg[:, j, :], in0=ident, scalar1=wiall[:, qb, j:j + 1], scalar2=None, op0=ALU.mult),
                 reads=[r_wi, r_c], writes=[r_wdg])
        yield 0.5
        units = [(c, m) for c in range(nch) for m in range(4)]

        def emit_diag(u):
            c, m = units[u]
            ks = slice(c * 512, (c + 1) * 512)
            has_kb = (c < 3)
            has_cb = (c == nch - 1)
            lastdiag = not (has_kb or has_cb)
            ri = u % 3
            for hh in range(2):
                imm = 2 * m + hh
                P.op(PE, lambda e, m=m, hh=hh, ri=ri, imm=imm, lastdiag=lastdiag: e.matmul(ps[:, SB, :], lhsT=wdg[:, 2 * m + hh, :], rhs=Rt[ri][:, hh, :], start=(imm == 0), stop=(lastdiag and imm == 7)),
                     reads=[r_wdg, r_Rt[ri]], writes=[rb[SB]])
            if m == 3:
                if has_kb:
                    P.op(PE, lambda e, ks=ks, has_cb=has_cb: e.matmul(ps[:, SB, :], lhsT=onesb[0:1, :], rhs=kbt[0:1, ks], start=False, stop=(not has_cb)),
                         reads=[r_c2], writes=[rb[SB]])
                if has_cb:
                    P.op(PE, lambda e: e.matmul(ps[:, SB, :], lhsT=ident, rhs=cbt[:, j4, :], start=False, stop=True), reads=[r_c2, r_c], writes=[rb[SB]])
                P.op(ACT, lambda e, ks=ks: e.activation(out=scores[:, ks], in_=ps[:, SB, :], func=AF.Copy), reads=[rb[SB]], writes=[r_sc])

        for u, (c, m) in enumerate(units):
            ks = slice(c * 512, (c + 1) * 512)
            for hh in range(2):
                pr = slice(hh * 64, hh * 64 + 64)
                P.op(PE, lambda e, m=m, hh=hh, pr=pr, ks=ks: e.matmul(ps[:, LB + hh, :], lhsT=qit[pr, m, q2 * 128:(q2 + 1) * 128], rhs=kiT[pr, ks],
                                                                start=True, stop=True), reads=[r_qit, r_ki], writes=[rb[LB + hh]])
            ri = u % 3
            P.op(ACT, lambda e, ri=ri: e.activation(out=Rt[ri], in_=ps[:, LB:LB + 2, :], func=AF.Relu), reads=[rb[LB], rb[LB + 1]], writes=[r_Rt[ri]])
            if u > 0:
                emit_diag(u - 1)
            yield 0.8
        emit_diag(len(units) - 1)
        if debug:
            dma(dbg_sc[qb, :, 0:E], scores[:, 0:E], reads=[r_sc])
        yield 0.5

    def bis_gen(sq_):
        sp_, q2 = sq_ // 2, sq_ % 2
        qp = PORDER[sp_]
        qb = 2 * qp + q2
        g = qp // 2
        E = 2048 * (g + 1)
        nkc = E // 128
        c0 = g * 512 + (qp % 2) * 256
        scores = scoress[sq_ % 2]; r_sc = r_scs[sq_ % 2]
        maskT = maskTs[sp_ % 2]; r_mT = r_mTs[sp_ % 2]
        qaug = qaugs[sp_ % 2]; r_qa = r_qas[sp_ % 2]
        if q2 == 0:
            dma(qaug[0:64, :, :], qT_s.rearrange("h d s -> d h s")[:, :, c0:c0 + 256], writes=[r_qa])
        sc = scores[:, 0:E]
        tpass = E / 960.0
        P.op(DVE, lambda e: e.tensor_reduce(out=bs[:, 0:1], in_=sc, axis=AX.X, op=ALU.max), reads=[r_sc], writes=[r_bs])
        P.op(DVE, lambda e: e.tensor_scalar(out=bs[:, 9:10], in0=bs[:, 0:1], scalar1=-1.0, scalar2=None, op0=ALU.mult), writes=[r_bs])
        P.op(DVE, lambda e: e.tensor_tensor(out=bs[:, 9:10], in0=bs[:, 9:10], in1=bs[:, 0:1], op=ALU.max), writes=[r_bs])
        P.op(DVE, lambda e: e.tensor_scalar(out=bs[:, 9:10], in0=bs[:, 9:10], scalar1=3.0, scalar2=3.0, op0=ALU.mult, op1=ALU.add), writes=[r_bs])
        P.op(DVE, lambda e: e.tensor_tensor(out=bs[:, 3:4], in0=bs[:, 0:1], in1=bs[:, 9:10], op=ALU.subtract), writes=[r_bs])
        P.op(DVE, lambda e: e.tensor_scalar(out=bs[:, 6:7], in0=bs[:, 3:4], scalar1=20000.0, scalar2=None, op0=ALU.add), writes=[r_bs])
        P.op(DVE, lambda e: e.tensor_tensor(out=bs[:, 7:8], in0=bs[:, 9:10], in1=bs[:, 6:7], op=ALU.subtract), writes=[r_bs])
        yield tpass + 1.0
        for it in range(NITER + 1):
            P.op(DVE, lambda e: e.tensor_scalar(out=junk[:, 0:E], in0=sc, scalar1=bs[:, 3:4], scalar2=None, op0=ALU.is_ge, op1=ALU.add, accum_out=bs[:, 4:5]),
                 reads=[r_sc], writes=[r_junk, r_bs])
            P.op(DVE, lambda e: e.tensor_scalar(out=bs[:, 5:6], in0=bs[:, 4:5], scalar1=255.5, scalar2=None, op0=ALU.is_ge), writes=[r_bs])
            if it == 0:
                P.op(DVE, lambda e: e.tensor_scalar(out=bs[:, 1:2], in0=bs[:, 5:6], scalar1=bs[:, 6:7], scalar2=-20000.0, op0=ALU.mult, op1=ALU.add), writes=[r_bs])
                P.op(DVE, lambda e: e.scalar_tensor_tensor(out=bs[:, 2:3], in0=bs[:, 5:6], scalar=bs[:, 7:8], in1=bs[:, 6:7], op0=ALU.mult, op1=ALU.add), writes=[r_bs])
            else:
                P.op(DVE, lambda e: e.tensor_scalar(out=bs[:, 2:3], in0=bs[:, 2:3], scalar1=0.5, scalar2=None, op0=ALU.mult), writes=[r_bs])
                P.op(DVE, lambda e: e.scalar_tensor_tensor(out=bs[:, 1:2], in0=bs[:, 5:6], scalar=bs[:, 2:3], in1=bs[:, 1:2], op0=ALU.mult, op1=ALU.add), writes=[r_bs])
            P.op(DVE, lambda e: e.scalar_tensor_tensor(out=bs[:, 3:4], in0=bs[:, 2:3], scalar=0.5, in1=bs[:, 1:2], op0=ALU.mult, op1=ALU.add), writes=[r_bs])
            yield tpass + 0.8
        if debug:
            dma(dbg_thr[:, qb * 4:qb * 4 + 4], bs[:, 0:4], reads=[r_bs])
        P.op(DVE, lambda e: e.tensor_scalar(out=maskq[:, 0:E], in0=sc, scalar1=bs[:, 1:2], scalar2=None, op0=ALU.is_ge), reads=[r_sc, r_junkA], writes=[r_mq, r_bs])
        P.op(DVE, lambda e: e.tensor_reduce(out=cntk[:, 0:nkc], in_=maskq[:, 0:E].rearrange("p (a b) -> p a b", b=128), axis=AX.X, op=ALU.add), writes=[r_ck, r_mq])
        P.op(DVE, lambda e: e.tensor_scalar(out=cntk[:, 0:nkc], in0=cntk[:, 0:nkc], scalar1=0.5, scalar2=None, op0=ALU.is_ge), writes=[r_ck])
        P.op(DVE, lambda e: e.tensor_tensor(out=cntk[:, 0:nkc], in0=cntk[:, 0:nkc], in1=kcp1[:, 0:nkc], op=ALU.mult), reads=[r_c], writes=[r_ck])
        P.op(DVE, lambda e: e.tensor_reduce(out=bs[:, 8:9], in_=cntk[:, 0:nkc], axis=AX.X, op=ALU.max), writes=[r_ck, r_bs])
        P.op(DVE, lambda e: e.tensor_scalar(out=U[:, 66:67], in0=bs[:, 8:9], scalar1=-1.0, scalar2=None, op0=ALU.add), writes=[r_U, r_bs])
        yield 2 * tpass
        P.op(PE, lambda e: e.matmul(ps[0:67, LB, 0:128], lhsT=U[:, 0:67], rhs=identf, start=True, stop=True), reads=[r_U, r_c], writes=[rb[LB]])
        for h in range(8):
            P.op(ACT, lambda e, h=h: e.activation(out=qaug[64:67, h, q2 * 128:(q2 + 1) * 128], in_=ps[64:67, LB, 0:128], func=AF.Copy), reads=[rb[LB]], writes=[r_qa])
        yield 1.0
        for k8 in range(nkc // 8):
            for kk in range(8):
                kc = k8 * 8 + kk
                P.op(PE, lambda e, kc=kc, kk=kk: e.transpose(out=psb[LB + 1][:, kk * 128:(kk + 1) * 128], in_=maskq[:, kc * 128:(kc + 1) * 128], identity=ident),
                     reads=[r_mq, r_c], writes=[rb[LB + 1]])
            mo = maskT[:, k8 * 8:(k8 + 1) * 8, q2 * 128:(q2 + 1) * 128]
            mi = psb[LB + 1].rearrange("p (a b) -> p a b", b=128)
            P.op(ACT, lambda e, mo=mo, mi=mi: e.activation(out=mo, in_=mi, func=AF.Copy, scale=-1.0, bias=1.0, saturate=False), reads=[rb[LB + 1]], writes=[r_mT])
            yield 1.0

    accb = [4, 5, 6]

    def att_gen(sp_):
        qp = PORDER[sp_]
        g = qp // 2
        E = 2048 * (g + 1)
        nch = E // 512
        nkc = E // 128
        c0 = g * 512 + (qp % 2) * 256
        maskT = maskTs[sp_ % 2]; r_mT = r_mTs[sp_ % 2]
        qaug = qaugs[sp_ % 2]; r_qa = r_qas[sp_ % 2]
        first_in_bank = {}
        steps = [(c, kl, hp) for c in range(nch) for kl in range(4) for hp in range(4)]

        def emit_loads(c):
            kb_ = c % 2
            ks = slice(c * 512, (c + 1) * 512)
            dma(kt[kb_][0:64, :, :], kT_hv[:, :, ks], writes=[r_kt[kb_]])
            dma(kt[kb_][64:67, :, :], kaug_v[:, :, ks], writes=[r_kt[kb_]])
            dma(vt[kb_], v_v2[:, c * 4:(c + 1) * 4, :], writes=[r_vt[kb_]])

        def emit_ST(i):
            c, kl, hp = steps[i]
            kb_ = c % 2
            kc = c * 4 + kl
            sbk = i % 2
            for hh in range(2):
                h = hp * 2 + hh
                o = hh * 256
                P.op(PE, lambda e, h=h, o=o, sbk=sbk, kb_=kb_, kl=kl: e.matmul(ps[:, sbk, o:o + 256], lhsT=kt[kb_][0:67, h, kl * 128:(kl + 1) * 128],
                                                                    rhs=qaug[0:67, h, :], start=True, stop=False),
                     reads=[r_kt[kb_], r_qa], writes=[rb[sbk]])
                P.op(PE, lambda e, o=o, sbk=sbk, kc=kc: e.matmul(ps[:, sbk, o:o + 256], lhsT=nidm, rhs=maskT[:, kc, :], start=False, stop=True),
                     reads=[r_mT, r_c2], writes=[rb[sbk]])

        emit_loads(0)
        if nch > 1:
            emit_loads(1)
        emit_ST(0)
        for i, (c, kl, hp) in enumerate(steps):
            kb_ = c % 2
            kc = c * 4 + kl
            sbk = i % 2
            if i + 1 < len(steps):
                emit_ST(i + 1)
            pi = i % 3
            P.op(ACT, lambda e, pi=pi, sbk=sbk: e.activation(out=Pt[pi], in_=ps[:, sbk, :].rearrange("p (a b) -> p a b", b=256), func=AF.Exp, bias=EXPB, scale=0.125),
                 reads=[rb[sbk]], writes=[r_Pt[pi]])
            for q2 in range(2):
                for hh in range(2):
                    h = hp * 2 + hh
                    a = q2 * 8 + h
                    ab_ = accb[a // 6]
                    col = (a % 6) * 65
                    st_ = (kc == 0) and (ab_ not in first_in_bank)
                    if kc == 0:
                        first_in_bank[ab_] = True
                    P.op(PE, lambda e, pi=pi, hh=hh, q2=q2, h=h, ab_=ab_, col=col, st_=st_, kb_=kb_, kl=kl, kc=kc: e.matmul(
                        ps[:, ab_, col:col + 65], lhsT=Pt[pi][:, hh, q2 * 128:(q2 + 1) * 128], rhs=vt[kb_][:, kl, h * 65:(h + 1) * 65],
                        start=st_, stop=(kc == nkc - 1), skip_group_check=True),
                        reads=[r_Pt[pi], r_vt[kb_]], writes=[rb[ab_]])
            if kl == 3 and hp == 3 and c + 2 < nch:
                emit_loads(c + 2)
            yield 0.8
        for a in range(16):
            ab_ = accb[a // 6]; col = (a % 6) * 65
            P.op(DVE, lambda e, a=a, ab_=ab_, col=col: e.tensor_copy(out=den[:, a:a + 1], in_=ps[:, ab_, col + 64:col + 65]), reads=[rb[ab_]], writes=[r_den])
        P.op(DVE, lambda e: e.reciprocal(out=den, in_=den), writes=[r_den])
        for a in range(16):
            ab_ = accb[a // 6]; col = (a % 6) * 65
            q2 = a // 8; h = a % 8
            P.op(DVE, lambda e, a=a, ab_=ab_, col=col, q2=q2, h=h: e.tensor_scalar(out=Ob[:, q2, h * 64:(h + 1) * 64], in0=ps[:, ab_, col:col + 64], scalar1=den[:, a:a + 1], scalar2=None, op0=ALU.mult),
                 reads=[rb[ab_]], writes=[r_Ob, r_den])
        yield 3.0
        for q2 in range(2):
            for m in range(4):
                P.op(PE, lambda e, q2=q2, m=m: e.transpose(out=psb[0][:, (q2 * 4 + m) * 128:(q2 * 4 + m + 1) * 128], in_=Ob[:, q2, m * 128:(m + 1) * 128], identity=ident),
                     reads=[r_Ob, r_c], writes=[rb[0]])
        for q2 in range(2):
            P.op(ACT, lambda e, q2=q2: e.activation(out=OTst[:, :, q2 * 128:(q2 + 1) * 128], in_=psb[0][:, q2 * 512:(q2 + 1) * 512].rearrange("p (a b) -> p a b", b=128), func=AF.Copy),
                 reads=[rb[0]], writes=[r_OT])
        dma(OT_s.rearrange("m p s -> p m s")[:, :, c0:c0 + 256], OTst, reads=[r_OT], eng=STQ)
        yield 1.0

    NQB, NPR = 16, 8
    done = {"IDX": 0, "BIS": 0, "ATT": 0}
    nitems = {"IDX": NQB, "BIS": NQB, "ATT": NPR}
    mk = {"IDX": idx_gen, "BIS": bis_gen, "ATT": att_gen}
    cur = {"IDX": None, "BIS": None, "ATT": None}
    clk = {"IDX": 0.0, "BIS": 0.0, "ATT": 0.0}
    now = [0.0]

    def can_start(s):
        i = done[s]
        if i >= nitems[s]:
            return False
        if s == "IDX":
            return done["BIS"] >= i - 1
        if s == "BIS":
            return done["IDX"] > i and done["ATT"] >= i // 2 - 1
        return done["BIS"] > 2 * i + 1

    while any(done[s] < nitems[s] for s in done):
        cands = []
        for s in ("ATT", "IDX", "BIS"):
            if cur[s] is None and can_start(s):
                cur[s] = mk[s](done[s])
                clk[s] = max(clk[s], now[0])
            if cur[s] is not None:
                cands.append(s)
        assert cands, ("scheduler deadlock", done)
        s = min(cands, key=lambda k: clk[k])
        now[0] = clk[s]
        cost = next(cur[s], None)
        if cost is None:
            cur[s] = None
            done[s] += 1
        else:
            clk[s] += cost
    P.barrier()
    A.release()
    A.release()

    A.mark()
    wap = A.tile([4, D], BF16); wcp = A.tile([4, D], BF16); wo = A.tile([8, D], BF16); r_w3 = Res()
    load_w(wap, w_ap_b, 512, 0, D, r_w3, "w_ap"); load_w(wcp, w_cp_b, 512, 0, D, r_w3, "w_cp"); load_w(wo, w_out_b, D, 0, D, r_w3, "w_out")
    wfo_t = [A.tile([22, 128], BF16) for _ in range(2)]; r_wfo = [Res(), Res()]
    wdw = A.tile([4, 31], F32); cvp = A.tile([3, 4], F32); r_cv = Res()
    dma(wdw, w_dwT, writes=[r_cv]); dma(cvp[:, 0, :], b_dwT, writes=[r_cv]); dma(cvp[:, 1, :], lngT, writes=[r_cv]); dma(cvp[:, 2, :], lnbT, writes=[r_cv])
    wdd = A.tile([31, 128], BF16); r_wdd = Res()
    xtb = [A.tile([D], F32) for _ in range(2)]; r_xtb = [Res(), Res()]
    xTs = [A.tile([8, 512], F32) for _ in range(2)]; r_xTs = [Res(), Res()]
    OT = A.tile([4, 512], BF16); zT = A.tile([4, 544], BF16); r_in3 = Res()
    gab = [A.tile([2, 512], BF16) for _ in range(2)]; r_gab = [Res(), Res()]
    cv = A.tile([4, 512], F32); r_cvo = Res()
    sq = A.tile([512], F32); r_sq = Res()
    stt = A.tile([3, 512], F32); r_stt = Res()
    zc = A.tile([4, 512], BF16); r_zc = Res()
    t1 = A.tile([512], F32); r_t1 = Res()
    mg = A.tile([8, 512], BF16); r_mg = Res()
    h2s = [A.tile([8, 512], BF16) for _ in range(2)]; r_h2s = [Res(), Res()]
    gT = A.tile([22, 512], BF16); r_gT = Res()
    wfi_t = [A.tile([8, 256], BF16) for _ in range(2)]; r_wfi = [Res() for _ in range(2)]
    sl = [A.tile([512], F32) for _ in range(2)]; r_sl = [Res(), Res()]
    sq2 = A.tile([512], F32); r_sq2 = Res()
    rs2 = A.tile([512], F32); r_rs2 = Res()
    yo = A.tile([D], F32); r_yo = Res()
    w_fo_v = w_fo_b.rearrange("(k p) n -> p k n", p=128)
    w_fi_v = w_fi_b.rearrange("(k p) n -> p k n", p=128)

    def rstd_sumsq(n, src_fn, nchunks, sqt, r_sqt, bank, dst, r_dst, rd):
        for m in range(nchunks):
            P.op(ACT, lambda e, m=m: e.activation(out=sqt, in_=src_fn(m), func=AF.Square), reads=rd, writes=[r_sqt])
            P.op(PE, lambda e, m=m: e.matmul(ps[:, bank, :], lhsT=onesf, rhs=sqt, start=(m == 0), stop=(m == nchunks - 1)), reads=[r_sqt, r_c], writes=[rb[bank]])
        P.op(ACT, lambda e: e.activation(out=dst, in_=ps[:, bank, :], func=AF.Sqrt, bias=EPS, scale=1.0 / n), reads=[rb[bank]], writes=[r_dst])
        P.op(DVE, lambda e: e.reciprocal(out=dst, in_=dst), writes=[r_dst])

    def front_gen(g):
        T = 4 * g + 3
        cs = slice(g * 512, (g + 1) * 512)
        xT = xTs[g % 2]; r_xT = r_xTs[g % 2]
        h2 = h2s[g % 2]; r_h2 = r_h2s[g % 2]
        dma(OT, OT_s.rearrange("m p s -> p m s")[:, :, cs], writes=[r_in3])
        dma(zT, zT_s[:, :, g, :].rearrange("m p s -> p m s"), writes=[r_in3])
        for bl in range(4):
            xb_ = xtb[bl % 2]; rxb = r_xtb[bl % 2]
            dma(xb_, xk[T * 512 + bl * 128:T * 512 + (bl + 1) * 128, :], writes=[rxb])
            for half in range(2):
                bk = half
                for k4 in range(4):
                    kc = half * 4 + k4
                    P.op(PE, lambda e, kc=kc, k4=k4, bk=bk, xb_=xb_: e.transpose(out=ps[:, bk, k4 * 128:(k4 + 1) * 128], in_=xb_[:, kc * 128:(kc + 1) * 128], identity=identf),
                         reads=[rxb, r_c], writes=[rb[bk]])
                evac(xT[:, half * 4:half * 4 + 4, bl * 128:(bl + 1) * 128], ps[:, bk, :].rearrange("p (a b) -> p a b", b=128), [rb[bk]], [r_xT])
            yield 3.0
        for i in range(4):
            bk = i % 2
            for k in range(31):
                P.op(DVE, lambda e, i=i, k=k: e.tensor_scalar(out=wdd[:, k, :], in0=ident, scalar1=wdw[:, i, k:k + 1], scalar2=None, op0=ALU.mult),
                     reads=[r_cv, r_c], writes=[r_wdd])
            for k in range(31):
                P.op(PE, lambda e, i=i, k=k, bk=bk: e.matmul(ps[:, bk, :], lhsT=wdd[:, k, :], rhs=zT[:, i, k + 2:k + 514], start=(k == 0), stop=(k == 30)),
                     reads=[r_wdd, r_in3], writes=[rb[bk]])
            P.op(DVE, lambda e, i=i, bk=bk: e.tensor_scalar(out=cv[:, i, :], in0=ps[:, bk, :], scalar1=cvp[:, 0, i:i + 1], scalar2=None, op0=ALU.add),
                 reads=[rb[bk], r_cv], writes=[r_cvo])
            yield 7.0
        for i in range(4):
            P.op(PE, lambda e, i=i: e.matmul(ps[:, 2, :], lhsT=onesf, rhs=cv[:, i, :], start=(i == 0), stop=(i == 3)), reads=[r_cvo, r_c], writes=[rb[2]])
        P.op(ACT, lambda e: e.activation(out=stt[:, 0, :], in_=ps[:, 2, :], func=AF.Copy, scale=1.0 / 512), reads=[rb[2]], writes=[r_stt])
        for i in range(4):
            P.op(DVE, lambda e, i=i: e.tensor_tensor(out=cv[:, i, :], in0=cv[:, i, :], in1=stt[:, 0, :], op=ALU.subtract), reads=[r_stt], writes=[r_cvo])
        yield 4.0
        rstd_sumsq(512, lambda m: cv[:, m, :], 4, sq, r_sq, 3, stt[:, 2, :], r_stt, [r_cvo])
        yield 4.0
        for i in range(4):
            P.op(DVE, lambda e, i=i: e.tensor_tensor(out=cv[:, i, :], in0=cv[:, i, :], in1=stt[:, 2, :], op=ALU.mult), reads=[r_stt], writes=[r_cvo])
            P.op(DVE, lambda e, i=i: e.tensor_scalar(out=cv[:, i, :], in0=cv[:, i, :], scalar1=cvp[:, 1, i:i + 1], scalar2=cvp[:, 2, i:i + 1], op0=ALU.mult, op1=ALU.add),
                 reads=[r_cv], writes=[r_cvo])
            P.op(ACT, lambda e, i=i: e.activation(out=zc[:, i, :], in_=cv[:, i, :], func=AF.Silu), reads=[r_cvo], writes=[r_zc])
        yield 4.0
        for m in range(8):
            b0 = (2 * m) % 4; b1_ = (2 * m + 1) % 4
            gb_ = gab[m % 2]; rg = r_gab[m % 2]
            dma(gb_[:, 0, :], gaT_s[m, :, cs], writes=[rg])
            dma(gb_[:, 1, :], gbT_s[m, :, cs], writes=[rg])
            for k in range(4):
                P.op(PE, lambda e, m=m, k=k, b0=b0: e.matmul(ps[:, b0, :], lhsT=wap[:, k, m * 128:(m + 1) * 128], rhs=OT[:, k, :], start=(k == 0), stop=(k == 3)),
                     reads=[r_w3, r_in3], writes=[rb[b0]])
            for k in range(4):
                P.op(PE, lambda e, m=m, k=k, b1_=b1_: e.matmul(ps[:, b1_, :], lhsT=wcp[:, k, m * 128:(m + 1) * 128], rhs=zc[:, k, :], start=(k == 0), stop=(k == 3)),
                     reads=[r_w3, r_zc], writes=[rb[b1_]])
            P.op(DVE, lambda e, b0=b0, gb_=gb_: e.tensor_tensor(out=t1, in0=ps[:, b0, :], in1=gb_[:, 0, :], op=ALU.mult), reads=[rb[b0], rg], writes=[r_t1])
            P.op(DVE, lambda e, b1_=b1_, gb_=gb_: e.tensor_tensor(out=sq, in0=ps[:, b1_, :], in1=gb_[:, 1, :], op=ALU.mult), reads=[rb[b1_], rg], writes=[r_sq])
            P.op(DVE, lambda e, m=m: e.tensor_tensor(out=mg[:, m, :], in0=t1, in1=sq, op=ALU.add), reads=[r_t1, r_sq], writes=[r_mg])
            yield 2.0
        for m in range(8):
            bk = m % 4
            for k in range(8):
                P.op(PE, lambda e, m=m, k=k, bk=bk: e.matmul(ps[:, bk, :], lhsT=wo[:, k, m * 128:(m + 1) * 128], rhs=mg[:, k, :], start=(k == 0), stop=(k == 7)),
                     reads=[r_w3, r_mg], writes=[rb[bk]])
            P.op(DVE, lambda e, m=m, bk=bk: e.scalar_tensor_tensor(out=xT[:, m, :], in0=ps[:, bk, :], scalar=g_m[:, m:m + 1], in1=xT[:, m, :], op0=ALU.mult, op1=ALU.add),
                 reads=[rb[bk], r_mod2], writes=[r_xT])
            yield 2.0
        rstd_sumsq(D, lambda m: xT[:, m, :], 8, sq, r_sq, 3, stt[:, 2, :], r_stt, [r_xT])
        yield 6.0
        for m in range(8):
            P.op(DVE, lambda e, m=m: e.tensor_tensor(out=t1, in0=xT[:, m, :], in1=stt[:, 2, :], op=ALU.mult), reads=[r_xT, r_stt], writes=[r_t1])
            P.op(DVE, lambda e, m=m: e.tensor_scalar(out=h2[:, m, :], in0=t1, scalar1=a2[:, m:m + 1], scalar2=b2[:, m:m + 1], op0=ALU.mult, op1=ALU.add),
                 reads=[r_mod2], writes=[r_h2, r_t1])
        yield 6.0

    def ffn_gen(g):
        xT = xTs[g % 2]; r_xT = r_xTs[g % 2]
        h2 = h2s[g % 2]; r_h2 = r_h2s[g % 2]
        for i in range(22):
            wb_ = i % 2
            dma(wfi_t[wb_][:, :, 0:128], w_fi_v[:, :, i * 128:(i + 1) * 128], reads=[r_cast["w_fi"]], writes=[r_wfi[wb_]])
            dma(wfi_t[wb_][:, :, 128:256], w_fi_v[:, :, 2816 + i * 128:2816 + (i + 1) * 128], reads=[r_cast["w_fi"]], writes=[r_wfi[wb_]])
            b0 = 4 + (2 * i) % 4; b1_ = 4 + (2 * i + 1) % 4
            for k in range(8):
                P.op(PE, lambda e, k=k, wb_=wb_, b0=b0: e.matmul(ps[:, b0, :], lhsT=wfi_t[wb_][:, k, 0:128], rhs=h2[:, k, :], start=(k == 0), stop=(k == 7)),
                     reads=[r_wfi[wb_], r_h2], writes=[rb[b0]])
            for k in range(8):
                P.op(PE, lambda e, k=k, wb_=wb_, b1_=b1_: e.matmul(ps[:, b1_, :], lhsT=wfi_t[wb_][:, k, 128:256], rhs=h2[:, k, :], start=(k == 0), stop=(k == 7)),
                     reads=[r_wfi[wb_], r_h2], writes=[rb[b1_]])
            si = i % 2
            P.op(ACT, lambda e, si=si, b0=b0: e.activation(out=sl[si], in_=ps[:, b0, :], func=AF.Silu), reads=[rb[b0]], writes=[r_sl[si]])
            P.op(DVE, lambda e, si=si, b1_=b1_, i=i: e.tensor_tensor(out=gT[:, i, :], in0=ps[:, b1_, :], in1=sl[si], op=ALU.mult), reads=[rb[b1_], r_sl[si]], writes=[r_gT])
            yield 3.5
        for m in range(8):
            bk = 4 + m % 4
            wf = wfo_t[m % 2]; rwf = r_wfo[m % 2]
            dma(wf, w_fo_v[:, :, m * 128:(m + 1) * 128], reads=[r_cast["w_fo"]], writes=[rwf])
            for i in range(22):
                P.op(PE, lambda e, i=i, bk=bk, wf=wf: e.matmul(ps[:, bk, :], lhsT=wf[:, i, :], rhs=gT[:, i, :], start=(i == 0), stop=(i == 21)),
                     reads=[rwf, r_gT], writes=[rb[bk]])
            P.op(DVE, lambda e, m=m, bk=bk: e.scalar_tensor_tensor(out=xT[:, m, :], in0=ps[:, bk, :], scalar=g_f[:, m:m + 1], in1=xT[:, m, :], op0=ALU.mult, op1=ALU.add),
                 reads=[rb[bk], r_mod2], writes=[r_xT])
            yield 4.8
        rstd_sumsq(D, lambda m: xT[:, m, :], 8, sq2, r_sq2, 7, rs2, r_rs2, [r_xT])
        yield 6.0
        for m in range(8):
            P.op(DVE, lambda e, m=m: e.scalar_tensor_tensor(out=xT[:, m, :], in0=xT[:, m, :], scalar=gfin[:, m:m + 1], in1=rs2, op0=ALU.mult, op1=ALU.mult),
                 reads=[r_rs2, r_c], writes=[r_xT])
        yield 4.0
        for bl in range(4):
            for m in range(8):
                bk = 4 + (m // 4 + 2 * bl) % 4
                P.op(PE, lambda e, m=m, bl=bl, bk=bk: e.transpose(out=ps[:, bk, (m % 4) * 128:(m % 4 + 1) * 128], in_=xT[:, m, bl * 128:(bl + 1) * 128], identity=identf),
                     reads=[r_xT, r_c], writes=[rb[bk]])
                if m % 4 == 3:
                    evac(yo[:, (m // 4) * 512:(m // 4 + 1) * 512], ps[:, bk, :], [rb[bk]], [r_yo])
            dma(out_d[g * 512 + bl * 128:g * 512 + (bl + 1) * 128, :], yo, reads=[r_yo], eng=STQ)
            yield 2.0

    done3 = {"F": 0, "N": 0}
    mk3 = {"F": front_gen, "N": ffn_gen}
    cur3 = {"F": None, "N": None}
    clk3 = {"F": 0.0, "N": 0.0}
    now3 = [0.0]

    def can_start3(s):
        i = done3[s]
        if i >= 4:
            return False
        if s == "F":
            return done3["N"] >= i - 1
        return done3["F"] > i

    while done3["F"] < 4 or done3["N"] < 4:
        cands = []
        for s in ("N", "F"):
            if cur3[s] is None and can_start3(s):
                cur3[s] = mk3[s](done3[s])
                clk3[s] = max(clk3[s], now3[0])
            if cur3[s] is not None:
                cands.append(s)
        assert cands, ("phase-3 scheduler deadlock", done3)
        s = min(cands, key=lambda k: clk3[k])
        now3[0] = clk3[s]
        cost = next(cur3[s], None)
        if cost is None:
            cur3[s] = None
            done3[s] += 1
        else:
            clk3[s] += cost
    A.release()
    P.emit()
    return nc


_NC_CACHE = {}


def _consts():
    ident = np.eye(128, dtype=np.float32)
    slopes = np.exp2(-np.arange(1, 9, dtype=np.float64))
    s = np.arange(S)
    kaug = np.zeros((3, 8, S), np.float32)
    for h in range(8):
        kaug[0, h] = 8.0 * slopes[h] * (s % 128)
        kaug[1, h] = 8.0 * slopes[h] * 128.0 * (s // 128)
        kaug[2, h] = -8.0 * 128.0 * slopes[h]
    cb = np.zeros((128, 4, 512), np.float32)
    p = np.arange(128)[:, None]
    sp = np.arange(512)[None, :]
    for j4 in range(4):
        cb[:, j4, :] = np.where(sp <= 128 * j4 + p, 0.0, NEG)
    kcp1 = np.tile(np.arange(1, 65, dtype=np.float32)[None, :], (128, 1))
    return ident, kaug, cb, kcp1


def _in_maps(inputs):
    x = np.asarray(inputs["x"], np.float32)
    c = np.asarray(inputs["c"], np.float32)
    bf = ml_dtypes.bfloat16
    ident, kaug, cb, kcp1 = _consts()

    def fm(v, n):
        return np.ascontiguousarray(np.asarray(v, np.float32).reshape(n, 128).T)

    shared = {
        "w_ada": np.ascontiguousarray(inputs["w_ada"][0], np.float32),
        "b_adaT": fm(inputs["b_ada"][0], 48),
        "gmixT": fm(inputs["norm_mix_g"][0], 8), "gffnT": fm(inputs["norm_ffn_g"][0], 8), "gfinT": fm(inputs["norm_final_g"], 8),
        "w_in": np.ascontiguousarray(inputs["w_in"][0], np.float32),
        "w_dwT": np.ascontiguousarray(np.asarray(inputs["w_dw"][0][:, 0, :], np.float32).T.reshape(4, 128, 31).transpose(1, 0, 2)),
        "b_dwT": fm(inputs["b_dw"][0], 4), "lngT": fm(inputs["conv_ln_g"][0], 4), "lnbT": fm(inputs["conv_ln_b"][0], 4),
        "w_ap": np.ascontiguousarray(inputs["w_attn_proj"][0], np.float32), "w_cp": np.ascontiguousarray(inputs["w_conv_proj"][0], np.float32),
        "w_out": np.ascontiguousarray(inputs["w_out"][0], np.float32),
        "w_fi": np.ascontiguousarray(inputs["w_ffn_in"][0], np.float32), "w_fo": np.ascontiguousarray(inputs["w_ffn_out"][0], np.float32),
        "ident": ident.astype(bf), "identf": ident, "kaug": kaug.astype(bf), "cb": cb.astype(bf), "kcp1": kcp1,
    }
    maps = []
    for core in range(8):
        b, r = core // 4, core % 4
        pad = 1536 - 512 * r
        xk = np.zeros((S, D), np.float32)
        xk[pad:] = x[b, :S - pad]
        kbias = np.zeros((1, S), np.float32)
        kbias[0, :pad] = NEG
        hv = np.zeros((128, 128), np.float32)
        for g in range(4):
            pos = 2048 * g + 1504 + np.arange(32)
            hv[:, g * 32:(g + 1) * 32] = (pos >= pad).astype(np.float32)[None, :]
        m = dict(shared)
        m.update({"xk": xk, "cT": fm(c[b], 8), "kbias": kbias.astype(bf), "hvalid": hv})
        maps.append(m)
    return maps


def kernel(**inputs):
    if "nc" not in _NC_CACHE:
        _NC_CACHE["nc"] = build(False)
    nc = _NC_CACHE["nc"]
    maps = _in_maps(inputs)
    res = run_bass_kernel_spmd(nc, maps, core_ids=list(range(8)))
    out = np.zeros((2, S, D), np.float32)
    for core in range(8):
        b, r = core // 4, core % 4
        o = np.asarray(res.results[core]["out"], np.float32)
        for g in range(4):
            p0 = 2048 * g + 512 * r
            out[b, p0:p0 + 512] = o[g * 512:(g + 1) * 512]
    return out
```
